# Optimizing a Trainium2 kernel written in Bass

```python
import math
import jax
import jax.numpy as jnp
from jax import lax
import numpy as np

D_MODEL = 1024
BATCH = 2
SEQ = 8192
DEPTH = 1
DEC_BATCH = 128
DEC_SEQ = 1
PAST_LEN = 2048
PAGE_SIZE = 128

SSD_D_INNER = 2 * D_MODEL
SSD_HEAD_DIM = 64
SSD_HEADS = SSD_D_INNER // SSD_HEAD_DIM
SSD_GROUPS = 4
SSD_HPG = SSD_HEADS // SSD_GROUPS
SSD_STATE = 128
SSD_CONV = 4
SSD_CHUNK = 128
SSD_CONV_CH = SSD_D_INNER + 2 * SSD_GROUPS * SSD_STATE
NSA_HEAD_DIM = 64
NSA_HEADS = D_MODEL // NSA_HEAD_DIM
NSA_KV_HEADS = 4
NSA_HPG = NSA_HEADS // NSA_KV_HEADS
CMP_BLOCK = 32
CMP_STRIDE = 16
CMP_HIDDEN = 2 * NSA_HEAD_DIM
SLC_BLOCK = 64
SLC_TOPK = 16
WINDOW = 512
Q_BLOCK = 128
N_EXPERTS = 64
TOP_K = 8
N_ROUTE_GROUPS = 8
TOPK_ROUTE_GROUPS = 4
EXPERT_FF = 256
SHARED_FF = 256
ROUTE_SCALE = 2.5
MOE_BLOCK = 128
ALPHA = (2.0 * DEPTH) ** 0.25
BETA = (8.0 * DEPTH) ** -0.25
NORM_EPS = 1e-5
IN_SPLITS = (SSD_D_INNER, SSD_CONV_CH, SSD_HEADS, NSA_HEADS * NSA_HEAD_DIM,
             6 * NSA_KV_HEADS * NSA_HEAD_DIM, 3 * NSA_HEADS, 2 * D_MODEL)
IN_OFFSETS = tuple(int(v) for v in np.cumsum(IN_SPLITS)[:-1])
N_IN = int(sum(IN_SPLITS))

kernel_name = 'hybrid_ssd_nsa_moe_decode_step'


def _alibi_slopes():
    h = np.arange(1, NSA_HEADS + 1, dtype=np.float32)
    return (2.0 ** (-8.0 * h / NSA_HEADS)).astype(np.float32)


def _layer_norm(x, g, b):
    xf = x.astype(jnp.float32)
    mu = jnp.mean(xf, -1, keepdims=True)
    var = jnp.mean(jnp.square(xf - mu), -1, keepdims=True)
    return ((xf - mu) * lax.rsqrt(var + NORM_EPS) * g + b).astype(x.dtype)


def _last_rows(x, n):
    L = x.shape[1]
    if L >= n:
        return x[:, L - n:]
    pad = [(0, 0)] * x.ndim
    pad[1] = (n - L, 0)
    return jnp.pad(x, pad)


def _masked_softmax(s, mask):
    s = jnp.where(mask, s, -jnp.inf)
    mx = jnp.max(s, axis=-1, keepdims=True)
    mx = jnp.where(jnp.isfinite(mx), mx, 0.0)
    p = jnp.exp(s - mx)
    return p / jnp.maximum(jnp.sum(p, -1, keepdims=True), 1e-30)


def _project_in(x, w_in, b_in):
    b, L, _ = x.shape
    h = x @ w_in + b_in
    z, xbc, dt_raw, q, kv, g_nsa, g_merge = jnp.split(h, list(IN_OFFSETS), axis=-1)
    q = q.reshape(b, L, NSA_KV_HEADS, NSA_HPG, NSA_HEAD_DIM)
    kv = kv.reshape(b, L, 6, NSA_KV_HEADS, NSA_HEAD_DIM)
    g_nsa = jax.nn.sigmoid(g_nsa).reshape(b, L, NSA_KV_HEADS, NSA_HPG, 3)
    return z, xbc, dt_raw, q, kv, g_nsa, g_merge


def _causal_conv(xh, w, bias):
    out = lax.conv_general_dilated(xh, w[:, None, :].astype(xh.dtype), window_strides=(1,), padding='VALID',
                                   dimension_numbers=('NWC', 'WIO', 'NWC'), feature_group_count=xh.shape[-1])
    return jax.nn.silu(out + bias.astype(xh.dtype))


def _ssd_scan(x, dt, a, bm, cm, init, chunk):
    f32 = jnp.float32
    b, L = x.shape[:2]
    nc = L // chunk
    xr = x.reshape(b, nc, chunk, SSD_GROUPS, SSD_HPG, SSD_HEAD_DIM).astype(f32)
    dtr = dt.reshape(b, nc, chunk, SSD_GROUPS, SSD_HPG)
    br = bm.reshape(b, nc, chunk, SSD_GROUPS, SSD_STATE).astype(f32)
    cr = cm.reshape(b, nc, chunk, SSD_GROUPS, SSD_STATE).astype(f32)
    a_cs = jnp.cumsum(dtr * a.reshape(SSD_GROUPS, SSD_HPG), axis=2)
    xdt = xr * dtr[..., None]
    tri = np.tril(np.ones((chunk, chunk), dtype=bool))[None, None, :, :, None, None]
    lmat = jnp.exp(jnp.where(tri, a_cs[:, :, :, None] - a_cs[:, :, None, :], -jnp.inf))
    cb = jnp.einsum('bclgn,bcsgn->bclsg', cr, br)
    y_diag = jnp.einsum('bclsgj,bcsgjp->bclgjp', cb[..., None] * lmat, xdt)
    chunk_states = jnp.einsum('bclgn,bclgjp->bcgjpn', br, xdt * jnp.exp(a_cs[:, :, -1:] - a_cs)[..., None])
    chunk_decay = jnp.exp(a_cs[:, :, -1])

    def step(h, inp):
        st, dec = inp
        return h * dec[..., None, None] + st, h

    h0 = init.astype(f32).reshape(b, SSD_GROUPS, SSD_HPG, SSD_HEAD_DIM, SSD_STATE)
    h_fin, h_in = lax.scan(step, h0, (jnp.moveaxis(chunk_states, 1, 0), jnp.moveaxis(chunk_decay, 1, 0)))
    h_in = jnp.moveaxis(h_in, 0, 1)
    y_off = jnp.einsum('bclgn,bcgjpn->bclgjp', cr, h_in) * jnp.exp(a_cs)[..., None]
    y = (y_diag + y_off).reshape(b, L, SSD_HEADS, SSD_HEAD_DIM).astype(x.dtype)
    return y, h_fin.reshape(b, SSD_HEADS, SSD_HEAD_DIM, SSD_STATE).astype(init.dtype)


def _ssd_branch(xbc_hist, z, dt_raw, init, chunk, ssd_w):
    conv_w, conv_b, dt_bias, a_log, d_skip, norm_w = ssd_w
    b, L, _ = z.shape
    xbc = _causal_conv(xbc_hist, conv_w, conv_b)
    xs, bs, cs = jnp.split(xbc, [SSD_D_INNER, SSD_D_INNER + SSD_GROUPS * SSD_STATE], axis=-1)
    xs = xs.reshape(b, L, SSD_HEADS, SSD_HEAD_DIM)
    bs = bs.reshape(b, L, SSD_GROUPS, SSD_STATE)
    cs = cs.reshape(b, L, SSD_GROUPS, SSD_STATE)
    dt = jax.nn.softplus(dt_raw.astype(jnp.float32) + dt_bias.astype(jnp.float32))
    a = -jnp.exp(a_log.astype(jnp.float32))
    y, st = _ssd_scan(xs, dt, a, bs, cs, init, chunk)
    y = y + d_skip[:, None].astype(y.dtype) * xs
    y = y.reshape(b, L, SSD_D_INNER) * jax.nn.silu(z)
    yg = y.reshape(b, L, SSD_GROUPS, SSD_D_INNER // SSD_GROUPS).astype(jnp.float32)
    yg = yg * lax.rsqrt(jnp.mean(jnp.square(yg), -1, keepdims=True) + NORM_EPS)
    return (yg.reshape(b, L, SSD_D_INNER) * norm_w).astype(z.dtype), st


def _compress(rows, cmp, which):
    w1, b1, w2, b2, pe = (p[which] for p in cmp)
    T = rows.shape[1]
    nc = (T - CMP_BLOCK) // CMP_STRIDE + 1
    idx = np.arange(nc, dtype=np.int32)[:, None] * CMP_STRIDE + np.arange(CMP_BLOCK, dtype=np.int32)[None, :]
    blk = rows[:, idx] + pe[None, None, :, None, :]
    hid = jax.nn.silu(jnp.einsum('bnlgd,lde->bnge', blk, w1) + b1)
    return hid @ w2 + b2


def _to_sel_blocks(rows):
    b, T = rows.shape[:2]
    ns = -(-T // SLC_BLOCK)
    rows = jnp.pad(rows, ((0, 0), (0, ns * SLC_BLOCK - T), (0, 0), (0, 0)))
    return rows.reshape(b, ns, SLC_BLOCK, NSA_KV_HEADS, NSA_HEAD_DIM).transpose(0, 3, 1, 2, 4)


def _overlap(nc, ns):
    i = np.arange(nc)[:, None] * CMP_STRIDE
    m = np.arange(ns)[None, :] * SLC_BLOCK
    return ((i < m + SLC_BLOCK) & (i + CMP_BLOCK > m)).astype(np.float32)


def _nsa_core(q, qpos, gates, kc, vc, ksb, vsb, kw, vw, wpos):
    f32 = jnp.float32
    scale = NSA_HEAD_DIM ** -0.5
    slopes = jnp.asarray(_alibi_slopes().reshape(NSA_KV_HEADS, NSA_HPG))[None, :, :, None, None]
    bsz, lq = q.shape[0], q.shape[1]
    nc, ns = kc.shape[1], ksb.shape[2]
    ends = np.arange(nc, dtype=np.int32) * CMP_STRIDE + (CMP_BLOCK - 1)
    d_c = qpos[:, None] - ends[None, :]
    s_c = jnp.einsum('bqgjd,bngd->bgjqn', q, kc).astype(f32) * scale - slopes * d_c.astype(f32)
    p_c = _masked_softmax(s_c, d_c >= 0)
    o_c = jnp.einsum('bgjqn,bngd->bqgjd', p_c.astype(vc.dtype), vc)
    imp = jnp.einsum('bgjqn,nm->bgqm', p_c, jnp.asarray(_overlap(nc, ns)))
    m = np.arange(ns, dtype=np.int32)
    valid = (m * SLC_BLOCK)[None, :] <= qpos[:, None]
    cur = (qpos // SLC_BLOCK)[:, None]
    forced = (m[None, :] == 0) | (m[None, :] == cur) | (m[None, :] == cur - 1)
    imp = jnp.where(forced & valid, jnp.inf, imp)
    imp = jnp.where(valid, imp, -jnp.inf)
    top_v, top_i = lax.top_k(imp, min(SLC_TOPK, ns))
    gather = jax.vmap(jax.vmap(lambda blk, i: blk[i]))
    ks = gather(ksb, top_i)
    vs = gather(vsb, top_i)
    kpos = top_i[..., None] * SLC_BLOCK + np.arange(SLC_BLOCK, dtype=np.int32)
    d_s = qpos[None, None, :, None, None] - kpos
    mask_s = (d_s >= 0) & (top_v > -jnp.inf)[..., None]
    s_s = jnp.einsum('bqgjd,bgqkrd->bgjqkr', q, ks).astype(f32) * scale - slopes[..., None] * d_s[:, :, None].astype(f32)
    kk = top_i.shape[-1] * SLC_BLOCK
    p_s = _masked_softmax(s_s.reshape(bsz, NSA_KV_HEADS, NSA_HPG, lq, kk), mask_s.reshape(bsz, NSA_KV_HEADS, 1, lq, kk))
    o_s = jnp.einsum('bgjqt,bgqtd->bqgjd', p_s.astype(vs.dtype), vs.reshape(bsz, NSA_KV_HEADS, lq, kk, NSA_HEAD_DIM))
    d_w = qpos[:, None] - wpos[None, :]
    mask_w = (d_w >= 0) & (d_w < WINDOW) & (wpos >= 0)[None, :]
    s_w = jnp.einsum('bqgjd,bwgd->bgjqw', q, kw).astype(f32) * scale - slopes * d_w.astype(f32)
    p_w = _masked_softmax(s_w, mask_w)
    o_w = jnp.einsum('bgjqw,bwgd->bqgjd', p_w.astype(vw.dtype), vw)
    return gates[..., 0:1] * o_c + gates[..., 1:2] * o_s + gates[..., 2:3] * o_w


def _nsa_prompt(q, kv, gates, cmp):
    b, S = q.shape[:2]
    kc = _compress(kv[:, :, 0], cmp, 0)
    vc = _compress(kv[:, :, 1], cmp, 1)
    ksb = _to_sel_blocks(kv[:, :, 2])
    vsb = _to_sel_blocks(kv[:, :, 3])
    kw = jnp.pad(kv[:, :, 4], ((0, 0), (WINDOW, 0), (0, 0), (0, 0)))
    vw = jnp.pad(kv[:, :, 5], ((0, 0), (WINDOW, 0), (0, 0), (0, 0)))
    nqb = S // Q_BLOCK
    qb = q.reshape(b, nqb, Q_BLOCK, NSA_KV_HEADS, NSA_HPG, NSA_HEAD_DIM).swapaxes(0, 1)
    gb = gates.reshape(b, nqb, Q_BLOCK, NSA_KV_HEADS, NSA_HPG, 3).swapaxes(0, 1)

    def one_block(args):
        q_i, g_i, i = args
        start = i * Q_BLOCK
        qpos = start + jnp.arange(Q_BLOCK, dtype=jnp.int32)
        wpos = start - WINDOW + jnp.arange(Q_BLOCK + WINDOW, dtype=jnp.int32)
        kw_i = lax.dynamic_slice_in_dim(kw, start, Q_BLOCK + WINDOW, axis=1)
        vw_i = lax.dynamic_slice_in_dim(vw, start, Q_BLOCK + WINDOW, axis=1)
        return _nsa_core(q_i, qpos, g_i, kc, vc, ksb, vsb, kw_i, vw_i, wpos)

    o = lax.map(one_block, (qb, gb, jnp.arange(nqb, dtype=jnp.int32)))
    return o.swapaxes(0, 1).reshape(b, S, NSA_HEADS * NSA_HEAD_DIM)


def _nsa_sample(q, kv, gates, cache_kv_paged, page_table, cache_kv_win, cmp):
    db, ds = q.shape[:2]
    past = page_table.shape[1] * PAGE_SIZE
    hist = cache_kv_paged[page_table].reshape(db, past, 4, NSA_KV_HEADS, NSA_HEAD_DIM)
    rows = jnp.concatenate([hist, kv[:, :, :4].astype(hist.dtype)], axis=1)
    kc = _compress(rows[:, :, 0], cmp, 0)
    vc = _compress(rows[:, :, 1], cmp, 1)
    ksb = _to_sel_blocks(rows[:, :, 2])
    vsb = _to_sel_blocks(rows[:, :, 3])
    wbuf = cache_kv_win.shape[1]
    win = jnp.concatenate([cache_kv_win, kv[:, :, 4:].astype(cache_kv_win.dtype)], axis=1)
    qpos = past + jnp.arange(ds, dtype=jnp.int32)
    wpos = past - wbuf + jnp.arange(wbuf + ds, dtype=jnp.int32)
    o = _nsa_core(q, qpos, gates, kc, vc, ksb, vsb, win[:, :, 0], win[:, :, 1], wpos)
    return o.reshape(db, ds, NSA_HEADS * NSA_HEAD_DIM), win[:, -wbuf:]


def _moe(x, w_router, b_router, w_e1, w_e3, w_e2, w_s1, w_s3, w_s2):
    t, d = x.shape
    scores = jax.nn.sigmoid((x @ w_router).astype(jnp.float32))
    biased = scores + b_router.astype(jnp.float32)
    grp = biased.reshape(t, N_ROUTE_GROUPS, N_EXPERTS // N_ROUTE_GROUPS)
    grp_score = jnp.sum(lax.top_k(grp, 2)[0], -1)
    _, grp_idx = lax.top_k(grp_score, TOPK_ROUTE_GROUPS)
    grp_keep = jnp.sum(jax.nn.one_hot(grp_idx, N_ROUTE_GROUPS, dtype=jnp.float32), 1) > 0
    masked = jnp.where(grp_keep[:, :, None], grp, -jnp.inf).reshape(t, N_EXPERTS)
    _, e_idx = lax.top_k(masked, TOP_K)
    gate = jnp.take_along_axis(scores, e_idx, axis=1)
    gate = gate / jnp.sum(gate, -1, keepdims=True) * ROUTE_SCALE
    n_asg = t * TOP_K
    e_flat = e_idx.reshape(-1)
    tok_flat = jnp.repeat(jnp.arange(t, dtype=jnp.int32), TOP_K)
    order = jnp.argsort(e_flat)
    e_s, tok_s, g_s = e_flat[order], tok_flat[order], gate.reshape(-1)[order]
    counts = jnp.bincount(e_flat, length=N_EXPERTS)
    starts = jnp.cumsum(counts) - counts
    padded = (counts + MOE_BLOCK - 1) // MOE_BLOCK * MOE_BLOCK
    pad_ends = jnp.cumsum(padded)
    dest = (pad_ends - padded)[e_s] + jnp.arange(n_asg, dtype=jnp.int32) - starts[e_s]
    n_blk = -(-(n_asg + N_EXPERTS * (MOE_BLOCK - 1)) // MOE_BLOCK)
    n_rows = n_blk * MOE_BLOCK
    row_tok = jnp.full((n_rows,), t, jnp.int32).at[dest].set(tok_s)
    row_gate = jnp.zeros((n_rows,), jnp.float32).at[dest].set(g_s)
    blk_exp = jnp.minimum(jnp.searchsorted(pad_ends, jnp.arange(n_blk, dtype=jnp.int32) * MOE_BLOCK, side='right'), N_EXPERTS - 1)
    x_rows = jnp.concatenate([x, jnp.zeros((1, d), x.dtype)], 0)[row_tok].reshape(n_blk, MOE_BLOCK, d)

    def expert_block(args):
        xb, e = args
        return (jax.nn.silu(xb @ w_e1[e]) * (xb @ w_e3[e])) @ w_e2[e]

    out = lax.map(expert_block, (x_rows, blk_exp)).reshape(n_rows, d)
    routed = jnp.zeros((t + 1, d), x.dtype).at[row_tok].add((out * row_gate[:, None]).astype(x.dtype))[:t]
    shared = (jax.nn.silu(x @ w_s1) * (x @ w_s3)) @ w_s2
    return routed + shared


def _finish(x, ssd_y, nsa_y, g_merge, out_w):
    (w_ssd_down, w_nsa_down, w_out, b_out, ln1_g, ln1_b, w_router, b_router,
     w_e1, w_e3, w_e2, w_s1, w_s3, w_s2, ln2_g, ln2_b) = out_w
    g_ssd, g_nsa = jnp.split(jax.nn.sigmoid(g_merge), 2, axis=-1)
    mixed = g_ssd * (ssd_y @ w_ssd_down) + g_nsa * (nsa_y @ w_nsa_down)
    h = _layer_norm(ALPHA * x + (mixed @ w_out + b_out), ln1_g, ln1_b)
    b, L, d = h.shape
    f = _moe(h.reshape(b * L, d), w_router, b_router, w_e1, w_e3, w_e2, w_s1, w_s3, w_s2).reshape(b, L, d)
    return _layer_norm(ALPHA * h + f, ln2_g, ln2_b)


def setup_inputs(seed: int = 0) -> dict:
    key = jax.random.key(seed)
    ks = jax.random.split(key, 48)
    f32 = jnp.float32

    def nrm(k, shape, scale):
        return jax.random.normal(k, shape, f32) * scale

    n_pages = PAST_LEN // PAGE_SIZE
    n_phys = (DEC_BATCH * n_pages * 5 + 3) // 4
    wbuf = min(WINDOW, PAST_LEN)
    page_table = jax.random.permutation(ks[0], n_phys)[: DEC_BATCH * n_pages].reshape(DEC_BATCH, n_pages).astype(jnp.int32)
    dt0 = jnp.exp(jax.random.uniform(ks[1], (SSD_HEADS,), f32, math.log(1e-3), math.log(1e-1)))
    dt_bias = dt0 + jnp.log(-jnp.expm1(-dt0))
    a_log = jnp.log(jax.random.uniform(ks[2], (SSD_HEADS,), f32, 1.0, 16.0))
    kvh, dh = NSA_KV_HEADS, NSA_HEAD_DIM
    return {
        'x_prompt': nrm(ks[3], (BATCH, SEQ, D_MODEL), 1.0),
        'x_sample': nrm(ks[4], (DEC_BATCH, DEC_SEQ, D_MODEL), 1.0),
        'cache_kv_paged': nrm(ks[5], (n_phys, PAGE_SIZE, 4, kvh, dh), 1.0),
        'cache_kv_win': nrm(ks[6], (DEC_BATCH, wbuf, 2, kvh, dh), 1.0),
        'state_ssm': nrm(ks[7], (DEC_BATCH, SSD_HEADS, SSD_HEAD_DIM, SSD_STATE), 0.1),
        'state_conv': nrm(ks[8], (DEC_BATCH, SSD_CONV - 1, SSD_CONV_CH), 1.0),
        'page_table': page_table,
        'w_in': nrm(ks[9], (D_MODEL, N_IN), D_MODEL ** -0.5),
        'b_in': nrm(ks[10], (N_IN,), 0.02),
        'conv_w': nrm(ks[11], (SSD_CONV, SSD_CONV_CH), SSD_CONV ** -0.5),
        'conv_b': nrm(ks[12], (SSD_CONV_CH,), 0.02),
        'dt_bias': dt_bias,
        'a_log': a_log,
        'd_skip': 1.0 + nrm(ks[13], (SSD_HEADS,), 0.1),
        'ssd_norm_w': 1.0 + nrm(ks[14], (SSD_D_INNER,), 0.02),
        'cmp_w1': nrm(ks[15], (2, CMP_BLOCK, dh, CMP_HIDDEN), (CMP_BLOCK * dh) ** -0.5),
        'cmp_b1': nrm(ks[16], (2, CMP_HIDDEN), 0.02),
        'cmp_w2': nrm(ks[17], (2, CMP_HIDDEN, dh), CMP_HIDDEN ** -0.5),
        'cmp_b2': nrm(ks[18], (2, dh), 0.02),
        'cmp_pe': nrm(ks[19], (2, CMP_BLOCK, dh), 0.1),
        'w_ssd_down': nrm(ks[20], (SSD_D_INNER, D_MODEL), SSD_D_INNER ** -0.5),
        'w_nsa_down': nrm(ks[21], (NSA_HEADS * dh, D_MODEL), (NSA_HEADS * dh) ** -0.5),
        'w_out': nrm(ks[22], (D_MODEL, D_MODEL), BETA * D_MODEL ** -0.5),
        'b_out': nrm(ks[23], (D_MODEL,), 0.02),
        'ln1_g': 1.0 + nrm(ks[24], (D_MODEL,), 0.02),
        'ln1_b': nrm(ks[25], (D_MODEL,), 0.02),
        'w_router': nrm(ks[26], (D_MODEL, N_EXPERTS), D_MODEL ** -0.5),
        'b_router': nrm(ks[27], (N_EXPERTS,), 0.01),
        'w_e1': nrm(ks[28], (N_EXPERTS, D_MODEL, EXPERT_FF), D_MODEL ** -0.5),
        'w_e3': nrm(ks[29], (N_EXPERTS, D_MODEL, EXPERT_FF), D_MODEL ** -0.5),
        'w_e2': nrm(ks[30], (N_EXPERTS, EXPERT_FF, D_MODEL), BETA * EXPERT_FF ** -0.5),
        'w_s1': nrm(ks[31], (D_MODEL, SHARED_FF), D_MODEL ** -0.5),
        'w_s3': nrm(ks[32], (D_MODEL, SHARED_FF), D_MODEL ** -0.5),
        'w_s2': nrm(ks[33], (SHARED_FF, D_MODEL), BETA * SHARED_FF ** -0.5),
        'ln2_g': 1.0 + nrm(ks[34], (D_MODEL,), 0.02),
        'ln2_b': nrm(ks[35], (D_MODEL,), 0.02),
    }


def reference(x_prompt, x_sample, cache_kv_paged, cache_kv_win, state_ssm, state_conv, page_table,
              w_in, b_in, conv_w, conv_b, dt_bias, a_log, d_skip, ssd_norm_w,
              cmp_w1, cmp_b1, cmp_w2, cmp_b2, cmp_pe,
              w_ssd_down, w_nsa_down, w_out, b_out, ln1_g, ln1_b,
              w_router, b_router, w_e1, w_e3, w_e2, w_s1, w_s3, w_s2, ln2_g, ln2_b):
    cmp = (cmp_w1, cmp_b1, cmp_w2, cmp_b2, cmp_pe)
    ssd_w = (conv_w, conv_b, dt_bias, a_log, d_skip, ssd_norm_w)
    out_w = (w_ssd_down, w_nsa_down, w_out, b_out, ln1_g, ln1_b, w_router, b_router,
             w_e1, w_e3, w_e2, w_s1, w_s3, w_s2, ln2_g, ln2_b)
    wbuf = cache_kv_win.shape[1]
    yp, ys = x_prompt, x_sample
    for _ in range(DEPTH):
        z, xbc, dt_raw, q, kv, g_nsa, g_merge = _project_in(yp, w_in, b_in)
        xbc_hist = jnp.pad(xbc, ((0, 0), (SSD_CONV - 1, 0), (0, 0)))
        init = jnp.zeros((yp.shape[0], SSD_HEADS, SSD_HEAD_DIM, SSD_STATE), state_ssm.dtype)
        ssd_y, ssm_p = _ssd_branch(xbc_hist, z, dt_raw, init, SSD_CHUNK, ssd_w)
        nsa_y = _nsa_prompt(q, kv, g_nsa, cmp)
        conv_p = _last_rows(xbc, SSD_CONV - 1)
        kv_rows_p = kv[:, :, :4]
        win_p = _last_rows(kv[:, :, 4:], wbuf)
        yp = _finish(yp, ssd_y, nsa_y, g_merge, out_w)
        z, xbc, dt_raw, q, kv, g_nsa, g_merge = _project_in(ys, w_in, b_in)
        xbc_hist = jnp.concatenate([state_conv.astype(xbc.dtype), xbc], axis=1)
        ssd_y, ssm_s = _ssd_branch(xbc_hist, z, dt_raw, state_ssm, ys.shape[1], ssd_w)
        nsa_y, win_s = _nsa_sample(q, kv, g_nsa, cache_kv_paged, page_table, cache_kv_win, cmp)
        conv_s = xbc_hist[:, -(SSD_CONV - 1):]
        kv_rows_s = kv[:, :, :4]
        ys = _finish(ys, ssd_y, nsa_y, g_merge, out_w)
    return (yp, ys, kv_rows_p, kv_rows_s, win_p, win_s, ssm_p, ssm_s, conv_p, conv_s)
```

```python
from contextlib import ExitStack
import numpy as np
import concourse.bass as bass
import concourse.mybir as mybir
from concourse.bass_utils import run_bass_kernel_spmd

F32 = mybir.dt.float32
BF16 = mybir.dt.bfloat16
I32 = mybir.dt.int32
U32 = mybir.dt.uint32
AF = mybir.ActivationFunctionType
ALU = mybir.AluOpType
AX = mybir.AxisListType

D_MODEL = 1024
ALPHA = 2.0 ** 0.25
NORM_EPS = 1e-5
N_EXPERTS = 64
ROUTE_SCALE = 2.5
BIG = 1.0e9


class Buf:
    __slots__ = ("name", "w", "r")

    def __init__(self, name):
        self.name = name
        self.w = None
        self.r = {}


class Prog:
    def __init__(self, nc, es):
        self.nc = nc
        self.es = es
        self.sem_es = es
        self.eng = {"pe": nc.tensor, "dve": nc.vector, "act": nc.scalar, "pool": nc.gpsimd, "sp": nc.sync}
        self.sems = {}
        self.cnt = {}
        self.seen = {e: {} for e in self.eng}
        self.nbuf = 0
        self.store_keys = set()
        for e in self.eng:
            self._sem(e)

    def _sem(self, key):
        if key not in self.sems:
            self.sems[key] = self.sem_es.enter_context(self.nc.semaphore("s_" + key))
            self.cnt[key] = 0
        return self.sems[key]

    def buf(self, name=None):
        self.nbuf += 1
        return Buf(name or ("b%d" % self.nbuf))

    def sb(self, name, shape, dt):
        t = self.es.enter_context(self.nc.sbuf_tensor("sb_" + name, list(shape), dt))
        return t, Buf(name)

    def ps(self, name, shape, dt=F32):
        t = self.es.enter_context(self.nc.psum_tensor("pp_" + name, list(shape), dt))
        return t, Buf(name)

    def _wait(self, e, key, val):
        if e == "pe" and key == "pe":
            return
        if self.seen[e].get(key, 0) >= val:
            return
        self.eng[e].wait_ge(self.sems[key], val)
        self.seen[e][key] = val

    def _deps(self, e, reads, writes):
        for b in reads:
            if b.w is not None:
                self._wait(e, b.w[0], b.w[1])
        for b in writes:
            if b.w is not None:
                self._wait(e, b.w[0], b.w[1])
            for k, v in b.r.items():
                self._wait(e, k, v)

    def op(self, e, fn, reads=(), writes=()):
        self._deps(e, reads, writes)
        inst = fn(self.eng[e])
        self.cnt[e] += 1
        inst.then_inc(self.sems[e], 1)
        c = self.cnt[e]
        for b in reads:
            b.r[e] = c
        for b in writes:
            b.w = (e, c)
            b.r = {}
        self.seen[e][e] = max(self.seen[e].get(e, 0), 0)

    def dma(self, q, out, in_, reads=(), writes=(), key=None, **kw):
        if key is None:
            key = ("dl_" + writes[0].name) if writes else ("ds_" + reads[0].name)
        self._sem(key)
        if not writes:
            self.store_keys.add(key)
        self._deps(q, reads, writes)
        inst = self.eng[q].dma_start(out=out, in_=in_, **kw)
        self.cnt[key] += 16
        inst.then_inc(self.sems[key], 16)
        c = self.cnt[key]
        for b in reads:
            b.r[key] = c
        for b in writes:
            b.w = (key, c)
            b.r = {}

    def barrier(self):
        for e in self.eng:
            for key, v in self.cnt.items():
                if v > 0 and key != e:
                    self._wait(e, key, v)

    def finish(self):
        for key, v in self.cnt.items():
            if key.startswith("ds_") or key.startswith("dl_"):
                if v > 0:
                    self._wait("sp", key, v)
        for e in ("pe", "dve", "act", "pool"):
            if self.cnt[e] > 0:
                self._wait("sp", e, self.cnt[e])


class _Stop(Exception):
    pass


def build_l2(NT_TILES, n_exp=N_EXPERTS, stop=None):
    try:
        return _build_l2(NT_TILES, n_exp, stop)
    except _Stop as ex:
        return ex.args[0]


def _build_l2(NT_TILES, n_exp, stop):
    NT = NT_TILES * 128
    nc = bass.Bass("TRN2", target_bir_lowering=False)
    dr = lambda name, shape, dt=F32, kind="ExternalInput": nc.dram_tensor(name, list(shape), dt, kind=kind).ap()
    xT = dr("xT", [1024, NT])
    x_tok = dr("x_tok", [NT, 1024])
    yT = dr("yT", [3072, NT])
    w_gm = dr("w_gm", [1024, 2048])
    b_gm = dr("b_gm", [128, 16])
    w_sd = dr("w_sd", [2048, 1024])
    w_nd = dr("w_nd", [1024, 1024])
    w_out = dr("w_out", [1024, 1024])
    bc = dr("bcast", [128, 6, 1024])
    w_r = dr("w_r", [1024, 64])
    w_e1 = dr("w_e1", [n_exp + 1, 1024, 256])
    w_e3 = dr("w_e3", [n_exp + 1, 1024, 256])
    w_e2 = dr("w_e2", [n_exp + 1, 256, 1024])
    ident = dr("ident", [128, 128])
    y = dr("y", [NT, 1024], kind="ExternalOutput")

    blocks = []
    t = 0
    while t < NT_TILES:
        n = min(4, NT_TILES - t)
        blocks.append((t, n))
        t += n

    with ExitStack() as es:
        P = Prog(nc, es)
        mixT, mixT_b = P.sb("mixT", [128, 8, NT], BF16)
        mix_bufs = [P.buf("mixb%d" % i) for i in range(NT_TILES)]
        idf, idf_b = P.sb("idf", [128, 128], F32)
        P.dma("sp", idf[:], ident, writes=[idf_b])
        pst = [P.ps("ps%d" % i, [128, 1024], F32) for i in range(4)]
        psh = []
        for i in range(4):
            psh.append((pst[i][0], 0, P.buf("psb%da" % i)))
            psh.append((pst[i][0], 512, P.buf("psb%db" % i)))
        rr = [0]

        def next_ps():
            r = psh[rr[0] % 8]
            rr[0] += 1
            return r

        with ExitStack() as esA:
            old_es = P.es
            P.es = esA
            wgm, wgm_b = P.sb("wgm", [128, 8, 2048], BF16)
            wsd, wsd_b = P.sb("wsd", [128, 16, 1024], BF16)
            wnd, wnd_b = P.sb("wnd", [128, 8, 1024], BF16)
            bgm, bgm_b = P.sb("bgm", [128, 16], F32)
            for k in range(8):
                P.dma("pool", wgm[:, k, :], w_gm[k * 128:(k + 1) * 128, :], writes=[wgm_b])
            for k in range(16):
                P.dma("pool", wsd[:, k, :], w_sd[k * 128:(k + 1) * 128, :], writes=[wsd_b])
            for k in range(8):
                P.dma("pool", wnd[:, k, :], w_nd[k * 128:(k + 1) * 128, :], writes=[wnd_b])
            P.dma("sp", bgm[:], b_gm, writes=[bgm_b])
            xTb = [P.sb("xTb%d" % i, [128, 8, 512], BF16) for i in range(1)]
            yTb = [P.sb("yTb%d" % i, [128, 24, 512], BF16) for i in range(1)]
            gT = [P.sb("gT%d" % i, [128, 16, 512], F32) for i in range(1)]
            t1 = [P.sb("t1_%d" % i, [128, 512], F32) for i in range(2)]
            t2 = [P.sb("t2_%d" % i, [128, 512], F32) for i in range(2)]
            for bi, (t0, ntl) in enumerate(blocks):
                W = ntl * 128
                c0 = t0 * 128
                xb_t, xb_b = xTb[0]
                yb_t, yb_b = yTb[0]
                for k in range(8):
                    P.dma("pool", xb_t[:, k, :W], xT[k * 128:(k + 1) * 128, c0:c0 + W], writes=[xb_b])
                for k in range(24):
                    P.dma("pool", yb_t[:, k, :W], yT[k * 128:(k + 1) * 128, c0:c0 + W], writes=[yb_b])
                g_t, g_b = gT[0]
                mb = [mix_bufs[t0 + i] for i in range(ntl)]
                for ct in range(16):
                    pt, po, pb = next_ps()
                    for k in range(8):
                        P.op("pe", lambda e: e.matmul(pt[:, po:po + W], lhsT=wgm[:, k, ct * 128:(ct + 1) * 128],
                                                      rhs=xb_t[:, k, :W], start=(k == 0), stop=(k == 7)),
                             reads=[wgm_b, xb_b], writes=[pb])
                    P.op("act", lambda e: e.activation(out=g_t[:, ct, :W], in_=pt[:, po:po + W], func=AF.Sigmoid,
                                                       bias=bgm[:, ct:ct + 1], scale=1.0),
                         reads=[pb, bgm_b], writes=[g_b])
                for ct in range(8):
                    pa_t, pa_o, pa_b = next_ps()
                    for k in range(16):
                        P.op("pe", lambda e: e.matmul(pa_t[:, pa_o:pa_o + W], lhsT=wsd[:, k, ct * 128:(ct + 1) * 128],
                                                      rhs=yb_t[:, k, :W], start=(k == 0), stop=(k == 15)),
                             reads=[wsd_b, yb_b], writes=[pa_b])
                    pn_t, pn_o, pn_b = next_ps()
                    for k in range(8):
                        P.op("pe", lambda e: e.matmul(pn_t[:, pn_o:pn_o + W], lhsT=wnd[:, k, ct * 128:(ct + 1) * 128],
                                                      rhs=yb_t[:, 16 + k, :W], start=(k == 0), stop=(k == 7)),
                             reads=[wnd_b, yb_b], writes=[pn_b])
                    a_t, a_b = t1[ct % 2]
                    b_t, b_b = t2[ct % 2]
                    P.op("dve", lambda e: e.tensor_tensor(out=a_t[:, :W], in0=pa_t[:, pa_o:pa_o + W], in1=g_t[:, ct, :W], op=ALU.mult),
                         reads=[pa_b, g_b], writes=[a_b])
                    P.op("dve", lambda e: e.tensor_tensor(out=b_t[:, :W], in0=pn_t[:, pn_o:pn_o + W], in1=g_t[:, 8 + ct, :W], op=ALU.mult),
                         reads=[pn_b, g_b], writes=[b_b])
                    P.op("pool", lambda e: e.tensor_tensor(out=mixT[:, ct, c0:c0 + W], in0=a_t[:, :W], in1=b_t[:, :W], op=ALU.add),
                         reads=[a_b, b_b], writes=mb)
            P.barrier()
            if stop == "A1":
                P.finish()
                raise _Stop(nc)
            P.es = old_es
        acc = [P.sb("acc%d" % i, [128, 1024], F32) for i in range(NT_TILES)]
        hT, hT_b = P.sb("hT", [128, 8, NT], BF16)
        hT_bufs = [P.buf("hTb%d" % i) for i in range(NT_TILES)]
        gates = [P.sb("gate%d" % i, [128, n_exp + 1], F32) for i in range(NT_TILES)]
        bct, bct_b = P.sb("bct", [128, 6, 1024], F32)
        P.dma("sp", bct[:], bc, writes=[bct_b])
        with ExitStack() as esA:
            old_es = P.es
            P.es = esA
            wo, wo_b = P.sb("wo", [128, 8, 1024], BF16)
            wr, wr_b = P.sb("wr", [128, 8, 64], F32)
            for k in range(8):
                P.dma("pool", wo[:, k, :], w_out[k * 128:(k + 1) * 128, :], writes=[wo_b])
            P.dma("sp", wr[:], w_r.rearrange("(k p) e -> p k e", p=128), writes=[wr_b])
            xt = [P.sb("xt%d" % i, [128, 1024], F32) for i in range(2)]
            hTf = [P.sb("hTf%d" % i, [128, 8, 128], F32) for i in range(2)]
            st6 = [P.sb("st6_%d" % i, [128, 2, 6], F32) for i in range(2)]
            mv = [P.sb("mv%d" % i, [128, 2], F32) for i in range(2)]
            rs = [P.sb("rs%d" % i, [128, 2], F32) for i in range(2)]
            rt = [P.sb("rt%d" % i, [128, 8, 64], F32) for i in range(2)]
            r8 = [P.sb("r8_%d" % i, [128, 8, 8], F32) for i in range(2)]
            for ti in range(NT_TILES):
                s = ti % 2
                x_t, x_b = xt[s]
                P.dma("sp", x_t[:], x_tok[ti * 128:(ti + 1) * 128, :], writes=[x_b])
                halves = []
                for hh in range(2):
                    pt, po, pb = next_ps()
                    for k in range(8):
                        P.op("pe", lambda e: e.matmul(pt[:, po:po + 512], lhsT=mixT[:, k, ti * 128:(ti + 1) * 128],
                                                      rhs=wo[:, k, hh * 512:(hh + 1) * 512], start=(k == 0), stop=(k == 7)),
                             reads=[mix_bufs[ti], wo_b], writes=[pb])
                    halves.append((pt, po, pb))
                for hh in range(2):
                    pt, po, pb = halves[hh]
                    P.op("dve", lambda e: e.scalar_tensor_tensor(out=x_t[:, hh * 512:(hh + 1) * 512], in0=x_t[:, hh * 512:(hh + 1) * 512],
                                                                 scalar=ALPHA, in1=pt[:, po:po + 512], op0=ALU.mult, op1=ALU.add),
                         reads=[x_b, pb], writes=[x_b])
                P.op("pool", lambda e: e.tensor_tensor(out=x_t[:], in0=x_t[:], in1=bct[:, 0, :], op=ALU.add),
                     reads=[x_b, bct_b], writes=[x_b])
                a_t, a_b = acc[ti]
                if stop == "A2pre":
                    P.finish()
                    raise _Stop(nc)
                _layer_norm(P, x_t, x_b, a_t, a_b, bct, bct_b, 1, 2, st6[s], mv[s], rs[s])
                if stop == "A2a":
                    P.finish()
                    raise _Stop(nc)
                f_t, f_b = hTf[s]
                for hh in range(2):
                    pt, po, pb = next_ps()
                    for j in range(4):
                        k = hh * 4 + j
                        P.op("pe", lambda e: e.transpose(out=pt[:, po + j * 128:po + (j + 1) * 128], in_=a_t[:, k * 128:(k + 1) * 128],
                                                         identity=idf[:]),
                             reads=[a_b, idf_b], writes=[pb])
                    P.op("dve", lambda e: e.tensor_copy(out=f_t[:, hh * 4:(hh + 1) * 4, :], in_=pt[:, po:po + 512].rearrange("p (j t) -> p j t", j=4)),
                         reads=[pb], writes=[f_b])
                    P.op("dve", lambda e: e.tensor_copy(out=hT[:, hh * 4:(hh + 1) * 4, ti * 128:(ti + 1) * 128],
                                                        in_=pt[:, po:po + 512].rearrange("p (j t) -> p j t", j=4)),
                         reads=[pb], writes=[hT_bufs[ti]])
                if stop == "A2b":
                    P.finish()
                    raise _Stop(nc)
                P.op("pool", lambda e: e.tensor_scalar(out=a_t[:], in0=a_t[:], scalar1=ALPHA, scalar2=None, op0=ALU.mult),
                     reads=[a_b], writes=[a_b])
                pt, po, pb = next_ps()
                for k in range(8):
                    P.op("pe", lambda e: e.matmul(pt[:, po:po + 64], lhsT=f_t[:, k, :], rhs=wr[:, k, :], start=(k == 0), stop=(k == 7)),
                         reads=[f_b, wr_b], writes=[pb])
                if stop == "A2c":
                    P.finish()
                    raise _Stop(nc)
                _routing(P, pt, po, pb, gates[ti], bct, bct_b, rt[s], r8[s], n_exp)
            P.barrier()
            if stop == "A2":
                P.finish()
                raise _Stop(nc)
            P.es = old_es
        with ExitStack() as esB:
            old_es = P.es
            P.es = esB
            w1 = [P.sb("w1_%d" % i, [128, 8, 256], BF16) for i in range(2)]
            w3 = [P.sb("w3_%d" % i, [128, 8, 256], BF16) for i in range(2)]
            w2 = [P.sb("w2_%d" % i, [128, 2, 1024], BF16) for i in range(2)]
            sil = [P.sb("sil%d" % i, [128, 2, 512], F32) for i in range(2)]
            aT = [P.sb("aT%d" % i, [128, 2, 512], BF16) for i in range(2)]
            cnt = 0
            for e_i in range(n_exp + 1):
                s = e_i % 2
                w1_t, w1_b = w1[s]
                w3_t, w3_b = w3[s]
                w2_t, w2_b = w2[s]
                P.dma("pool", w1_t[:], w_e1[e_i].rearrange("(k p) f -> p k f", p=128), writes=[w1_b])
                P.dma("pool", w3_t[:], w_e3[e_i].rearrange("(k p) f -> p k f", p=128), writes=[w3_b])
                P.dma("pool", w2_t[:], w_e2[e_i].rearrange("(k p) f -> p k f", p=128), writes=[w2_b])
                for bi, (t0, ntl) in enumerate(blocks):
                    W = ntl * 128
                    c0 = t0 * 128
                    hb = [hT_bufs[t0 + i] for i in range(ntl)]
                    h1 = [next_ps() for _ in range(2)]
                    h3 = [next_ps() for _ in range(2)]
                    for f in range(2):
                        pt, po, pb = h1[f]
                        for k in range(8):
                            P.op("pe", lambda e, k=k, f=f: e.matmul(pt[:, po:po + W], lhsT=w1_t[:, k, f * 128:(f + 1) * 128], rhs=hT[:, k, c0:c0 + W],
                                                                 start=(k == 0), stop=(k == 7)),
                                 reads=[w1_b] + hb, writes=[pb])
                        pt3, po3, pb3 = h3[f]
                        for k in range(8):
                            P.op("pe", lambda e, k=k, f=f: e.matmul(pt3[:, po3:po3 + W], lhsT=w3_t[:, k, f * 128:(f + 1) * 128], rhs=hT[:, k, c0:c0 + W],
                                                                 start=(k == 0), stop=(k == 7)),
                                 reads=[w3_b] + hb, writes=[pb3])
                    sl_t, sl_b = sil[cnt % 2]
                    at_t, at_b = aT[cnt % 2]
                    cnt += 1
                    for f in range(2):
                        pt, po, pb = h1[f]
                        P.op("act", lambda e, f=f: e.activation(out=sl_t[:, f, :W], in_=pt[:, po:po + W], func=AF.Silu),
                             reads=[pb], writes=[sl_b])
                    for f in range(2):
                        pt3, po3, pb3 = h3[f]
                        P.op("dve", lambda e, f=f: e.tensor_tensor(out=at_t[:, f, :W], in0=pt3[:, po3:po3 + W], in1=sl_t[:, f, :W], op=ALU.mult),
                             reads=[pb3, sl_b], writes=[at_b])
                    for tl in range(ntl):
                        ti = t0 + tl
                        a_t, a_b = acc[ti]
                        g_t, g_b = gates[ti]
                        for hh in range(2):
                            pt, po, pb = next_ps()
                            for f in range(2):
                                P.op("pe", lambda e, f=f, hh=hh: e.matmul(pt[:, po:po + 512], lhsT=at_t[:, f, tl * 128:(tl + 1) * 128],
                                                                       rhs=w2_t[:, f, hh * 512:(hh + 1) * 512], start=(f == 0), stop=(f == 1)),
                                     reads=[at_b, w2_b], writes=[pb])
                            P.op("dve", lambda e, hh=hh: e.scalar_tensor_tensor(out=a_t[:, hh * 512:(hh + 1) * 512], in0=pt[:, po:po + 512],
                                                                             scalar=g_t[:, e_i:e_i + 1], in1=a_t[:, hh * 512:(hh + 1) * 512],
                                                                             op0=ALU.mult, op1=ALU.add),
                                 reads=[pb, g_b, a_b], writes=[a_b])
            P.barrier()
            if stop == "B":
                P.finish()
                raise _Stop(nc)
            P.es = old_es
        with ExitStack() as esC:
            old_es = P.es
            P.es = esC
            ot = [P.sb("ot%d" % i, [128, 1024], F32) for i in range(2)]
            st6 = [P.sb("cst6_%d" % i, [128, 2, 6], F32) for i in range(2)]
            mv = [P.sb("cmv%d" % i, [128, 2], F32) for i in range(2)]
            rs = [P.sb("crs%d" % i, [128, 2], F32) for i in range(2)]
            for ti in range(NT_TILES):
                s = ti % 2
                a_t, a_b = acc[ti]
                o_t, o_b = ot[s]
                _layer_norm(P, a_t, a_b, o_t, o_b, bct, bct_b, 3, 4, st6[s], mv[s], rs[s])
                P.dma("sp", y[ti * 128:(ti + 1) * 128, :], o_t[:], reads=[o_b])
            P.finish()
            P.es = old_es
    return nc


def _layer_norm(P, x_t, x_b, o_t, o_b, bct, bct_b, gi, bi, st6, mv, rs):
    s_t, s_b = st6
    m_t, m_b = mv
    r_t, r_b = rs
    for hh in range(2):
        P.op("dve", lambda e, hh=hh: e.bn_stats(out=s_t[:, hh, :], in_=x_t[:, hh * 512:(hh + 1) * 512]), reads=[x_b], writes=[s_b])
    P.op("dve", lambda e: e.bn_aggr(out=m_t[:], in_=s_t[:].rearrange("p a b -> p (a b)")), reads=[s_b], writes=[m_b])
    P.op("dve", lambda e: e.tensor_scalar(out=r_t[:, 0:1], in0=m_t[:, 1:2], scalar1=NORM_EPS, scalar2=None, op0=ALU.add), reads=[m_b], writes=[r_b])
    P.op("act", lambda e: e.activation(out=r_t[:, 1:2], in_=r_t[:, 0:1], func=AF.Sqrt), reads=[r_b], writes=[r_b])
    P.op("dve", lambda e: e.reciprocal(out=r_t[:, 0:1], in_=r_t[:, 1:2]), reads=[r_b], writes=[r_b])
    P.op("dve", lambda e: e.tensor_scalar(out=o_t[:], in0=x_t[:], scalar1=m_t[:, 0:1], scalar2=r_t[:, 0:1], op0=ALU.subtract, op1=ALU.mult),
         reads=[x_b, m_b, r_b], writes=[o_b])
    P.op("pool", lambda e: e.tensor_tensor(out=o_t[:], in0=o_t[:], in1=bct[:, gi, :], op=ALU.mult), reads=[o_b, bct_b], writes=[o_b])
    P.op("pool", lambda e: e.tensor_tensor(out=o_t[:], in0=o_t[:], in1=bct[:, bi, :], op=ALU.add), reads=[o_b, bct_b], writes=[o_b])


def _routing(P, pt, po, pb, gate, bct, bct_b, rt, r8, n_exp):
    g_t, g_b = gate
    t_t, t_b = rt
    q_t, q_b = r8
    sc = t_t[:, 0, :]
    bi = t_t[:, 1, :]
    tmp = t_t[:, 2, :]
    msk = t_t[:, 3, :]
    sel = t_t[:, 4, :]
    v3 = lambda ap: ap.rearrange("p (a b) -> p a b", a=8)
    P.op("act", lambda e: e.activation(out=sc, in_=pt[:, po:po + 64], func=AF.Sigmoid), reads=[pb], writes=[t_b])
    P.op("dve", lambda e: e.tensor_tensor(out=bi, in0=sc, in1=bct[:, 5, 0:64], op=ALU.add), reads=[t_b, bct_b], writes=[t_b])
    P.op("dve", lambda e: e.tensor_reduce(out=q_t[:, 0, :], in_=v3(bi), axis=AX.X, op=ALU.max), reads=[t_b], writes=[q_b])
    P.op("dve", lambda e: e.tensor_tensor(out=v3(tmp), in0=v3(bi), in1=q_t[:, 0, :].unsqueeze(2).to_broadcast([128, 8, 8]), op=ALU.is_equal),
         reads=[t_b, q_b], writes=[t_b])
    P.op("dve", lambda e: e.scalar_tensor_tensor(out=tmp, in0=tmp, scalar=-BIG, in1=bi, op0=ALU.mult, op1=ALU.add), reads=[t_b], writes=[t_b])
    P.op("dve", lambda e: e.tensor_reduce(out=q_t[:, 1, :], in_=v3(tmp), axis=AX.X, op=ALU.max), reads=[t_b], writes=[q_b])
    P.op("dve", lambda e: e.tensor_tensor(out=q_t[:, 2, :], in0=q_t[:, 0, :], in1=q_t[:, 1, :], op=ALU.add), reads=[q_b], writes=[q_b])
    P.op("dve", lambda e: e.max(out=q_t[:, 3, :], in_=q_t[:, 2, :]), reads=[q_b], writes=[q_b])
    P.op("dve", lambda e: e.tensor_scalar(out=q_t[:, 4, :], in0=q_t[:, 2, :], scalar1=q_t[:, 3, 3:4], scalar2=None, op0=ALU.is_ge), reads=[q_b], writes=[q_b])
    P.op("dve", lambda e: e.tensor_scalar(out=q_t[:, 4, :], in0=q_t[:, 4, :], scalar1=1.0, scalar2=BIG, op0=ALU.subtract, op1=ALU.mult), reads=[q_b], writes=[q_b])
    P.op("dve", lambda e: e.tensor_tensor(out=v3(msk), in0=v3(bi), in1=q_t[:, 4, :].unsqueeze(2).to_broadcast([128, 8, 8]), op=ALU.add),
         reads=[t_b, q_b], writes=[t_b])
    P.op("dve", lambda e: e.max(out=q_t[:, 5, :], in_=msk), reads=[t_b], writes=[q_b])
    P.op("dve", lambda e: e.tensor_scalar(out=sel, in0=msk, scalar1=q_t[:, 5, 7:8], scalar2=None, op0=ALU.is_ge), reads=[t_b, q_b], writes=[t_b])
    P.op("dve", lambda e: e.tensor_tensor(out=sel, in0=sel, in1=sc, op=ALU.mult), reads=[t_b], writes=[t_b])
    P.op("dve", lambda e: e.tensor_reduce(out=q_t[:, 6, 0:1], in_=sel, axis=AX.X, op=ALU.add), reads=[t_b], writes=[q_b])
    P.op("dve", lambda e: e.reciprocal(out=q_t[:, 6, 1:2], in_=q_t[:, 6, 0:1]), reads=[q_b], writes=[q_b])
    P.op("dve", lambda e: e.tensor_scalar(out=g_t[:, 0:n_exp], in0=sel, scalar1=q_t[:, 6, 1:2], scalar2=ROUTE_SCALE, op0=ALU.mult, op1=ALU.mult),
         reads=[t_b, q_b], writes=[g_b])
    P.op("dve", lambda e: e.memset(g_t[:, n_exp:n_exp + 1], 1.0), reads=[], writes=[g_b])


NEG = -240000.0
COLS_A = [128] * 10 + [64] * 4 + [128] * 3
OFF_A = [int(v) for v in np.cumsum([0] + COLS_A)]


def _nsa_tables(S):
    h = np.arange(1, 17, dtype=np.float64)
    slopes = 2.0 ** (-8.0 * h / 16.0)
    import ml_dtypes
    bf = lambda a: np.asarray(a, np.float32).astype(ml_dtypes.bfloat16).astype(np.float32)
    t = {}
    pos = np.arange(S)
    ka = np.zeros((7, S), np.float32)
    ka[0] = ka[1] = (pos // 128) * 128
    ka[2] = ka[3] = pos % 128
    ka[4:] = 1.0
    t["kaug"] = ka
    nn = np.arange(512)
    ends = nn * 16 + 31
    kc = np.zeros((7, 512), np.float32)
    kc[0] = kc[1] = (ends // 128) * 128
    kc[2] = kc[3] = ends % 128
    kc[4:] = 1.0
    t["kaug_c"] = kc
    qa = np.zeros((4, 7, 4, S), np.float32)
    for g in range(4):
        for j in range(4):
            sl = np.float32(slopes[g * 4 + j] * 8.0)
            sh = bf(sl)
            slo = bf(sl - sh)
            Q = (-(np.float64(sh) + np.float64(slo)) * pos).astype(np.float32)
            qh = bf(Q)
            ql = bf(Q - qh)
            qll = bf(Q - qh - ql)
            qa[g, 0, j] = sh
            qa[g, 1, j] = slo
            qa[g, 2, j] = sh
            qa[g, 3, j] = slo
            qa[g, 4, j] = qh
            qa[g, 5, j] = ql
            qa[g, 6, j] = qll
    t["qaug"] = qa
    i = np.arange(128)[:, None]
    rq = np.arange(128)[None, :]
    t["caus4"] = np.tile(np.where(i <= rq, 0.0, NEG).astype(np.float32), (1, 4))
    t["low4"] = np.tile(np.where(i > rq, 0.0, NEG).astype(np.float32), (1, 4))
    y = np.arange(S)[None, :]
    t["EW"] = (y // 64 == np.arange(128)[:, None]).astype(np.float32)
    yy = np.arange(2432)[None, :]
    t["Zm"] = np.where(yy >= 16 * i + 128, 0.0, NEG).astype(np.float32)
    yf = np.arange(256)[None, :]
    rel = yf - 126
    c = (np.arange(128)[:, None] >= 64).astype(np.int64)
    F = np.zeros((128, 256), np.float32)
    F[(rel == c) | (rel == c - 1)] = 1e30
    F[rel > c] = -1e30
    t["F"] = F
    n = np.arange(512)[:, None] * 16
    m = np.arange(128)[None, :] * 64
    t["ov"] = ((n < m + 64) & (n + 32 > m)).astype(np.float32).reshape(4, 128, 128).transpose(1, 0, 2).copy()
    t["U"] = (i <= rq).astype(np.float32)
    t["ones"] = np.ones((128, 128), np.float32)
    t["ident"] = np.eye(128, dtype=np.float32)
    return t


def build_l1(S, NS, NPG=16, stop=None, nphys=None):
    try:
        return _build_l1(S, NS, NPG, stop, nphys)
    except _Stop as ex:
        return ex.args[0]


def _build_l1(S, NS, NPG, stop, nphys):
    BW = 256
    nc = bass.Bass("TRN2", target_bir_lowering=False)
    dr = lambda name, shape, dt=F32, kind="ExternalInput": nc.dram_tensor(name, list(shape), dt, kind=kind).ap()
    NTL = S // 128
    SS = NPG * 128 + 128
    NTS = SS // 128
    NPHYS = nphys or (128 * NPG * 5 + 3) // 4
    xT = dr("xT", [1024, S])
    wA = dr("wA", [1024, OFF_A[-1]])
    bA = dr("bA", [128, 17])
    wT = dr("wT", [1024, 652])
    bT = dr("bT", [128, 652])
    dtb = dr("dtb", [128, 512])
    alog = dr("alog", [128, 512])
    cw = dr("cw", [128, 6, 4])
    cb = dr("cb", [128, 6])
    dsk = dr("dsk", [128, 4])
    nw = dr("nw", [128, 4])
    w1kv = dr("w1kv", [128, 32, 128])
    pekv = dr("pekv", [128, 32])
    b1 = dr("b1", [128, 2])
    w2kv = dr("w2kv", [128, 128])
    b2k = dr("b2k", [64, 1])
    b2v = dr("b2v", [128, 64])
    kaug = dr("kaug", [7, max(S, SS)])
    kaug_c = dr("kaug_c", [7, 512])
    qaug = dr("qaug", [7, 4, max(S, SS)])
    caus4 = dr("caus4", [128, 512])
    low4 = dr("low4", [128, 512])
    EW = dr("EW", [128, max(S, SS)])
    Zm = dr("Zm", [128, 2432])
    Ftab = dr("F", [128, 256])
    ov = dr("ov", [128, 4, 128])
    Ut = dr("U", [128, 128])
    ones_d = dr("ones", [128, 128])
    ident = dr("ident", [128, 128])
    xTs = dr("xTs", [1024, NS])
    cache = dr("cache", [NPHYS * 128, 256])
    pt_rep = dr("pt_rep", [128, NS * NPG], I32)
    kvwin_s = dr("kvwin_s", [NS, 512, 128])
    ssm_s = dr("ssm_s", [NS, 512, 128])
    convT_s = dr("convT_s", [3, 768, NS])
    colp = dr("colp", [128, 3, 4])
    yT = dr("yT", [768, S], kind="ExternalOutput")
    kvT = dr("kvT", [256, S], kind="ExternalOutput")
    winT = dr("winT", [128, 512], kind="ExternalOutput")
    ssm_p = dr("ssm_p", [512, 128], kind="ExternalOutput")
    convT_p = dr("convT_p", [768, 3], kind="ExternalOutput")
    ysT = dr("ysT", [512, NS], kind="ExternalOutput")
    ynS = dr("ynS", [NS, 256], kind="ExternalOutput")
    kvS = dr("kvS", [256, NS], kind="ExternalOutput")
    winS = dr("winS", [NS, 511, 128], kind="ExternalOutput")
    ssm_o = dr("ssm_o", [NS, 512, 128], kind="ExternalOutput")
    convS = dr("convS", [3, 768, NS], kind="ExternalOutput")
    winnewT = dr("winnewT", [128, NS], kind="ExternalOutput")

    with ExitStack() as es:
        P = Prog(nc, es)
        op = P.op
        pst = [P.ps("ps%d" % i, [128, 1024], F32) for i in range(4)]
        psh = []
        for i in range(4):
            psh.append((pst[i][0], 0, P.buf("psb%da" % i)))
            psh.append((pst[i][0], 512, P.buf("psb%db" % i)))
        rr = [0]

        def next_ps():
            r = psh[6 + rr[0] % 2]
            rr[0] += 1
            return r

        def const(name, src, shape, dt=F32, q="sp"):
            t_, b_ = P.sb(name, shape, dt)
            P.dma("pool" if dt != F32 else q, t_[:], src, writes=[b_])
            return t_, b_

        U_t, U_b = const("U", Ut, [128, 128])
        one_t, one_b = const("ones", ones_d, [128, 128])
        id_t, id_b = const("identf", ident, [128, 128])
        idb_t, idb_b = const("identb", ident, [128, 128], BF16)
        caus_t, caus_b = const("caus4", caus4, [128, 512], BF16)
        low_t, low_b = const("low4", low4, [128, 512], BF16)
        EW_t, EW_b = const("EW", EW, [128, max(S, SS)], BF16)
        Zm_t, Zm_b = const("Zm", Zm, [128, 2432], BF16)
        F_t, F_b = const("Ft", Ftab, [128, 256])
        wA_t, wA_b = P.sb("wA", [128, 8, OFF_A[-1]], BF16)
        for k in range(8):
            P.dma("pool", wA_t[:, k, :], wA[k * 128:(k + 1) * 128, :], writes=[wA_b])
        wT_t, wT_b = P.sb("wT", [128, 8, 652], BF16)
        for k in range(8):
            P.dma("pool", wT_t[:, k, :], wT[k * 128:(k + 1) * 128, :], writes=[wT_b])
        bA_t, bA_b = const("bA", bA, [128, 17])
        bT_t, bT_b = const("bT", bT, [128, 652])
        dtb_t, dtb_b = const("dtb", dtb, [128, 512])
        A_t, A_b = const("Arep", alog, [128, 512])
        cw_t, cw_b = const("cw", cw, [128, 6, 4])
        cb_t, cb_b = const("cb", cb, [128, 6])
        dsk_t, dsk_b = const("dsk", dsk, [128, 4])
        nw_t, nw_b = const("nw", nw, [128, 4])
        w1_t, w1_b = const("w1kv", w1kv, [128, 32, 128], BF16)
        pe_t, pe_b = const("pekv", pekv, [128, 32])
        b1_t, b1_b = const("b1", b1, [128, 2])
        w2_t, w2_b = const("w2kv", w2kv, [128, 128], BF16)
        b2k_t, b2k_b = const("b2k", b2k, [64, 1])
        b2v_t, b2v_b = const("b2v", b2v, [128, 64])
        op("act", lambda e: e.activation(out=A_t[:], in_=A_t[:], func=AF.Exp), reads=[A_b], writes=[A_b])
        op("dve", lambda e: e.tensor_scalar(out=A_t[:], in0=A_t[:], scalar1=-1.0, scalar2=None, op0=ALU.mult), reads=[A_b], writes=[A_b])
        op("dve", lambda e: e.tensor_tensor(out=dtb_t[:], in0=dtb_t[:], in1=bT_t[:, 0:512], op=ALU.add), reads=[dtb_b, bT_b], writes=[dtb_b])
        b1p_t, b1p_b = P.sb("b1p", [128, 2], F32)
        es_tmp = ExitStack()
        P.es = es_tmp
        w1f_t, w1f_b = const("w1kvf", w1kv, [128, 32, 128])
        P.es = es
        for tkv in range(2):
            pt, po, pb = next_ps()
            for l in range(32):
                op("pe", lambda e: e.matmul(pt[:, po:po + 1], lhsT=w1f_t[tkv * 64:(tkv + 1) * 64, l, :], rhs=pe_t[tkv * 64:(tkv + 1) * 64, l:l + 1],
                                            start=(l == 0), stop=(l == 31)), reads=[w1f_b, pe_b], writes=[pb])
            op("dve", lambda e: e.tensor_tensor(out=b1p_t[:, tkv:tkv + 1], in0=pt[:, po:po + 1], in1=b1_t[:, tkv:tkv + 1], op=ALU.add),
               reads=[pb, b1_b], writes=[b1p_b])
        P.barrier()
        es_tmp.close()

        def alloc_kv(pref, Sx, NCx):
            d = {}
            d["Sx"] = Sx
            d["kvc"] = P.sb(pref + "kvc", [128, Sx], BF16)
            d["ksl"] = P.sb(pref + "ksl", [71, Sx], BF16)
            d["kwn"] = P.sb(pref + "kwn", [71, 1024], BF16)
            d["vsl"] = P.sb(pref + "vsl", [128, Sx // 128, 65], BF16)
            d["vwn"] = P.sb(pref + "vwn", [128, 8, 65], BF16)
            d["kc"] = P.sb(pref + "kc", [71, NCx * 128], BF16)
            d["vc"] = P.sb(pref + "vc", [128, NCx, 193], BF16)
            d["qa"] = P.sb(pref + "qa", [71, 4, 128], BF16)
            d["NCx"] = NCx
            P.dma("pool", d["ksl"][0][64:71, :], kaug[:, 0:Sx], writes=[d["ksl"][1]])
            P.dma("pool", d["kc"][0][64:71, :], kaug_c[:, 0:NCx * 128], writes=[d["kc"][1]])
            P.dma("pool", d["vc"][0][:, :, 65:193], ov[:, 0:NCx, :], writes=[d["vc"][1]])
            op("dve", lambda e: e.memset(d["vsl"][0][:, :, 64:65], 1.0), writes=[d["vsl"][1]])
            op("dve", lambda e: e.memset(d["vwn"][0][:, :, 64:65], 1.0), writes=[d["vwn"][1]])
            op("dve", lambda e: e.memset(d["vc"][0][:, :, 64:65], 1.0), writes=[d["vc"][1]])
            return d

        scr = {}
        scr["hid"] = P.sb("hid", [128, 512], BF16)
        scr["pT"] = [P.sb("pT%d" % i, [128, 512], BF16) for i in range(3)]
        scr["imp"] = P.sb("imp", [128, 4, 128], F32)
        scr["sadd"] = P.sb("sadd", [128, 512], F32)
        scr["m8"] = P.sb("m8", [128, 16], F32)
        scr["nst4"] = P.sb("nst4", [128, 4, 128], BF16)
        scr["rd"] = P.sb("rd", [128, 16], F32)
        scr["osb"] = P.sb("osb", [128, 3, 4, 64], F32)
        scr["ny"] = P.sb("ny", [128, 256], F32)
        scr["nyT"] = P.sb("nyT", [128, 2, 128], F32)
        pcnt = [0]

        def compress(kv, n0, nn):
            kvc_t, kvc_b = kv["kvc"]
            kc_t, kc_b = kv["kc"]
            vc_t, vc_b = kv["vc"]
            h_t, h_b = scr["hid"]
            for tkv in range(2):
                pt, po, pb = next_ps()
                lo = tkv * 64
                for l in range(32):
                    rhs = kvc_t[lo:lo + 64, n0 * 16 + l:n0 * 16 + l + (nn - 1) * 16 + 1:16]
                    op("pe", lambda e: e.matmul(pt[:, po:po + nn], lhsT=w1_t[lo:lo + 64, l, :], rhs=rhs, start=(l == 0), stop=(l == 31)),
                       reads=[w1_b, kvc_b], writes=[pb])
                op("act", lambda e: e.activation(out=h_t[:, :nn], in_=pt[:, po:po + nn], func=AF.Silu, bias=b1p_t[:, tkv:tkv + 1], scale=1.0),
                   reads=[pb, b1p_b], writes=[h_b])
                if tkv == 0:
                    p2, o2, b2 = next_ps()
                    op("pe", lambda e: e.matmul(p2[0:64, o2:o2 + nn], lhsT=w2_t[:, 0:64], rhs=h_t[:, :nn], start=True, stop=True),
                       reads=[w2_b, h_b], writes=[b2])
                    op("act", lambda e: e.activation(out=kc_t[0:64, n0:n0 + nn], in_=p2[0:64, o2:o2 + nn], func=AF.Identity, bias=b2k_t[:, 0:1], scale=1.0),
                       reads=[b2, b2k_b], writes=[kc_b])
                else:
                    for c0 in range(0, nn, 128):
                        w = min(128, nn - c0)
                        assert (n0 + c0) % 128 == 0
                        p2, o2, b2 = next_ps()
                        op("pe", lambda e: e.matmul(p2[0:w, o2:o2 + 64], lhsT=h_t[:, c0:c0 + w], rhs=w2_t[:, 64:128], start=True, stop=True),
                           reads=[w2_b, h_b], writes=[b2])
                        op("dve", lambda e: e.tensor_tensor(out=vc_t[0:w, (n0 + c0) // 128, 0:64], in0=p2[0:w, o2:o2 + 64], in1=b2v_t[0:w, :], op=ALU.add),
                           reads=[b2, b2v_b], writes=[vc_b])

        def attn_branch(kv, t, kname, vname, ktiles, mask_fn, acc, ncols, vcols):
            k_t, k_b = kv[kname]
            v_t, v_b = kv[vname]
            qa_t, qa_b = kv["qa"]
            n = len(ktiles)
            for idx, kt in enumerate(ktiles):
                kk = kt % 8 if kname == "kwn" else kt
                pt, po, pb = next_ps()
                extra = mask_fn(kt)
                mm_extra = [x for x in extra if x[0] == "mm"]
                post = [x for x in extra if x[0] == "mul"]
                pre_add = [x for x in extra if x[0] == "add"]
                op("pe", lambda e: e.matmul(pt[:, po:po + 512], lhsT=k_t[0:71, kk * 128:(kk + 1) * 128], rhs=qa_t[0:71, :, :],
                                            start=True, stop=(len(mm_extra) == 0)), reads=[k_b, qa_b], writes=[pb])
                for i_, (_, l_, r_, rd_) in enumerate(mm_extra):
                    op("pe", lambda e: e.matmul(pt[:, po:po + 512], lhsT=l_, rhs=r_, start=False, stop=(i_ == len(mm_extra) - 1)),
                       reads=rd_, writes=[pb])
                p_t, p_b = scr["pT"][pcnt[0] % 3]
                pcnt[0] += 1
                if pre_add:
                    sc_t, sc_b = scr["sadd"]
                    (_, m_ap, rd_) = pre_add[0]
                    op("dve", lambda e: e.tensor_tensor(out=sc_t[:].rearrange("p (h q) -> p h q", h=4), in0=pt[:, po:po + 512].rearrange("p (h q) -> p h q", h=4),
                                                        in1=m_ap, op=ALU.add), reads=[pb] + rd_, writes=[sc_b])
                    op("act", lambda e: e.activation(out=p_t[:], in_=sc_t[:], func=AF.Exp, scale=0.125), reads=[sc_b], writes=[p_b])
                else:
                    op("act", lambda e: e.activation(out=p_t[:], in_=pt[:, po:po + 512], func=AF.Exp, scale=0.125), reads=[pb], writes=[p_b])
                for (_, m_ap, rd_) in post:
                    op("dve", lambda e: e.tensor_tensor(out=p_t[:].rearrange("p (h q) -> p h q", h=4), in0=p_t[:].rearrange("p (h q) -> p h q", h=4),
                                                        in1=m_ap, op=ALU.mult), reads=[p_b] + rd_, writes=[p_b])
                for h in range(4):
                    at, ao, ab = acc[h // 2]
                    co = ao + (h % 2) * ncols
                    op("pe", lambda e: e.matmul(at[:, co:co + vcols], lhsT=p_t[:, h * 128:(h + 1) * 128], rhs=v_t[:, kk, 0:vcols],
                                                start=(idx == 0 and h % 2 == 0), stop=(idx == n - 1 and h % 2 == 1)), reads=[p_b, v_b], writes=[ab])

        def nsa_qtile(kv, t, gate_ap, gate_b, out_fn):
            i_t, i_b = scr["imp"]
            m_t, m_b = scr["m8"]
            n_t, n_b = scr["nst4"]
            r_t, r_b = scr["rd"]
            o_t, o_b = scr["osb"]
            nvalid = min(8 * t + 7, kv["NCx"] * 128)
            cchunks = list(range((nvalid + 127) // 128))
            accc = [psh[0], psh[1]]

            def cmask(c):
                K = 2048 * c - 128 * t + 31
                if 16 * 127 + K <= 0:
                    return []
                o = 128 - K
                assert 0 <= o <= 2432 - 128
                return [("add", Zm_t[:, o:o + 128].unsqueeze(1).to_broadcast([128, 4, 128]), [Zm_b])]
            if cchunks:
                attn_branch(kv, t, "kc", "vc", cchunks, cmask, accc, 193, 193)
            else:
                for (at, ao, ab) in accc:
                    op("dve", lambda e: e.memset(at[:, ao:ao + 386], 0.0), writes=[ab])
            for h in range(4):
                at, ao, ab = accc[h // 2]
                co = ao + (h % 2) * 193
                op("dve", lambda e: e.tensor_scalar(out=r_t[:, h:h + 1], in0=at[:, co + 64:co + 65], scalar1=1e-30, scalar2=None, op0=ALU.max),
                   reads=[ab], writes=[r_b])
            op("dve", lambda e: e.reciprocal(out=r_t[:, 0:4], in_=r_t[:, 0:4]), reads=[r_b], writes=[r_b])
            for h in range(4):
                at, ao, ab = accc[h // 2]
                co = ao + (h % 2) * 193
                if h == 0:
                    op("dve", lambda e: e.tensor_scalar(out=i_t[:, 0, :], in0=at[:, co + 65:co + 193], scalar1=r_t[:, 0:1], scalar2=None, op0=ALU.mult),
                       reads=[ab, r_b], writes=[i_b])
                else:
                    op("dve", lambda e: e.scalar_tensor_tensor(out=i_t[:, 0, :], in0=at[:, co + 65:co + 193], scalar=r_t[:, h:h + 1], in1=i_t[:, 0, :],
                                                               op0=ALU.mult, op1=ALU.add), reads=[ab, r_b, i_b], writes=[i_b])
                op("dve", lambda e: e.tensor_copy(out=o_t[:, 0, h, :], in_=at[:, co:co + 64]), reads=[ab], writes=[o_b])
            o2 = 126 - 2 * t
            op("dve", lambda e: e.tensor_tensor(out=i_t[:, 0, :], in0=i_t[:, 0, :], in1=F_t[:, o2:o2 + 128], op=ALU.add), reads=[i_b, F_b], writes=[i_b])
            op("dve", lambda e: e.memset(i_t[:, 0, 0:1], 1e30), writes=[i_b])
            op("dve", lambda e: e.max(out=m_t[:, 0:8], in_=i_t[:, 0, :]), reads=[i_b], writes=[m_b])
            op("dve", lambda e: e.match_replace(out=i_t[:, 1, :], in_to_replace=m_t[:, 0:8], in_values=i_t[:, 0, :], imm_value=-3.0e38),
               reads=[i_b, m_b], writes=[i_b])
            op("dve", lambda e: e.max(out=m_t[:, 8:16], in_=i_t[:, 1, :]), reads=[i_b], writes=[m_b])
            op("dve", lambda e: e.tensor_scalar(out=m_t[:, 15:16], in0=m_t[:, 15:16], scalar1=-1e29, scalar2=None, op0=ALU.max), reads=[m_b], writes=[m_b])
            op("dve", lambda e: e.tensor_scalar(out=i_t[:, 2, :], in0=i_t[:, 0, :], scalar1=m_t[:, 15:16], scalar2=None, op0=ALU.is_ge),
               reads=[i_b, m_b], writes=[i_b])
            op("dve", lambda e: e.tensor_scalar(out=i_t[:, 2, :], in0=i_t[:, 2, :], scalar1=1.0, scalar2=-NEG, op0=ALU.subtract, op1=ALU.mult),
               reads=[i_b], writes=[i_b])
            pt, po, pb = next_ps()
            op("pe", lambda e: e.transpose(out=pt[:, po:po + 128], in_=i_t[:, 2, :], identity=id_t[:]), reads=[i_b, id_b], writes=[pb])
            op("dve", lambda e: e.tensor_copy(out=n_t[:], in_=pt[:, po:po + 128].unsqueeze(1).to_broadcast([128, 4, 128])), reads=[pb], writes=[n_b])
            accs = [psh[2], psh[3]]

            def smask(kt):
                r = [("mm", EW_t[:, kt * 128:(kt + 1) * 128], n_t[:].rearrange("p h q -> p (h q)"), [EW_b, n_b])]
                if kt == t:
                    r.append(("mm", idb_t[:], caus_t[:], [idb_b, caus_b]))
                return r
            attn_branch(kv, t, "ksl", "vsl", list(range(t + 1)), smask, accs, 65, 65)
            accw = [psh[4], psh[5]]

            def wmask(kt):
                if kt == t:
                    return [("mm", idb_t[:], caus_t[:], [idb_b, caus_b])]
                if kt == t - 4:
                    return [("mm", idb_t[:], low_t[:], [idb_b, low_b])]
                return []
            attn_branch(kv, t, "kwn", "vwn", [k_ for k_ in range(t - 4, t + 1) if k_ >= 0], wmask, accw, 65, 65)
            for bi_, acc_ in ((1, accs), (2, accw)):
                for h in range(4):
                    at, ao, ab = acc_[h // 2]
                    co = ao + (h % 2) * 65
                    op("dve", lambda e: e.tensor_copy(out=r_t[:, bi_ * 4 + h:bi_ * 4 + h + 1], in_=at[:, co + 64:co + 65]), reads=[ab], writes=[r_b])
                    op("dve", lambda e: e.tensor_copy(out=o_t[:, bi_, h, :], in_=at[:, co:co + 64]), reads=[ab], writes=[o_b])
            op("dve", lambda e: e.reciprocal(out=r_t[:, 4:12], in_=r_t[:, 4:12]), reads=[r_b], writes=[r_b])
            op("dve", lambda e: e.tensor_tensor(out=r_t[:, 0:12].rearrange("p (b h) -> p b h", b=3), in0=r_t[:, 0:12].rearrange("p (b h) -> p b h", b=3),
                                                in1=gate_ap.rearrange("p (h b) -> p b h", b=3), op=ALU.mult), reads=[r_b, gate_b], writes=[r_b])
            y_t, y_b = scr["ny"]
            for h in range(4):
                op("dve", lambda e: e.tensor_scalar(out=y_t[:, h * 64:(h + 1) * 64], in0=o_t[:, 0, h, :], scalar1=r_t[:, h:h + 1], scalar2=None, op0=ALU.mult),
                   reads=[o_b, r_b], writes=[y_b])
                for bi_ in (1, 2):
                    op("dve", lambda e: e.scalar_tensor_tensor(out=y_t[:, h * 64:(h + 1) * 64], in0=o_t[:, bi_, h, :], scalar=r_t[:, bi_ * 4 + h:bi_ * 4 + h + 1],
                                                               in1=y_t[:, h * 64:(h + 1) * 64], op0=ALU.mult, op1=ALU.add), reads=[o_b, r_b, y_b], writes=[y_b])
            out_fn(y_t, y_b)

        ssd = {}
        for nm, shp, dt_ in (("sq", [128, 4, 128], F32), ("rstd", [128, 128], F32), ("yo", [128, 4, 128], F32)):
            ssd[nm] = P.sb("ssd_" + nm, shp, dt_)
        def ssd_chunk(c, blk, col0):
            cs = slice(col0, col0 + 128)
            xTb_t, xTb_b = blk["xTb"]
            T = lambda nm: ssd[nm][0]
            Bf = lambda nm: ssd[nm][1]
            pt, po, pb = psh[0]
            for k in range(8):
                op("pe", lambda e: e.matmul(pt[:, po:po + 512], lhsT=xTb_t[:, k, cs], rhs=wT_t[:, k, 0:512], start=(k == 0), stop=(k == 7)),
                   reads=[xTb_b, wT_b], writes=[pb])
            op("dve", lambda e: e.tensor_tensor(out=T("dt")[:], in0=pt[:, po:po + 512], in1=dtb_t[:], op=ALU.add), reads=[pb, dtb_b], writes=[Bf("dt")])
            op("act", lambda e: e.activation(out=T("dt")[:], in_=T("dt")[:], func=AF.Exp), reads=[Bf("dt")], writes=[Bf("dt")])
            op("act", lambda e: e.activation(out=T("dt")[:], in_=T("dt")[:], func=AF.Ln, bias=1.0, scale=1.0), reads=[Bf("dt")], writes=[Bf("dt")])
            op("dve", lambda e: e.tensor_tensor(out=T("dtA")[:], in0=T("dt")[:], in1=A_t[:], op=ALU.mult), reads=[Bf("dt"), A_b], writes=[Bf("dtA")])
            p1, o1, b1_ = psh[1]
            xs_t, xs_b = blk["xsT"]
            for k in range(4):
                op("pe", lambda e: e.transpose(out=p1[:, o1 + k * 128:o1 + (k + 1) * 128], in_=xs_t[:, k, cs], identity=id_t[:]),
                   reads=[xs_b, id_b], writes=[b1_])
            op("dve", lambda e: e.tensor_copy(out=T("xs")[:], in_=p1[:, o1:o1 + 512]), reads=[b1_], writes=[Bf("xs")])
            p2, o2, b2_ = psh[2]
            BT_t, BT_b = blk["BT"]
            op("pe", lambda e: e.transpose(out=p2[:, o2:o2 + 128], in_=BT_t[:, cs], identity=id_t[:]), reads=[BT_b, id_b], writes=[b2_])
            op("dve", lambda e: e.tensor_copy(out=T("Btb")[:], in_=p2[:, o2:o2 + 128]), reads=[b2_], writes=[Bf("Btb")])
            p3, o3, b3_ = psh[3]
            op("pe", lambda e: e.matmul(p3[:, o3:o3 + 512], lhsT=U_t[:], rhs=T("dtA")[:], start=True, stop=True), reads=[U_b, Bf("dtA")], writes=[b3_])
            p4, o4, b4_ = psh[4]
            op("pe", lambda e: e.matmul(p4[:, o4:o4 + 512], lhsT=one_t[:], rhs=T("dtA")[:], start=True, stop=True), reads=[one_b, Bf("dtA")], writes=[b4_])
            op("dve", lambda e: e.tensor_copy(out=T("acs")[:], in_=p3[:, o3:o3 + 512]), reads=[b3_], writes=[Bf("acs")])
            op("dve", lambda e: e.tensor_tensor(out=T("w")[:], in0=p4[:, o4:o4 + 512], in1=T("acs")[:], op=ALU.subtract), reads=[b4_, Bf("acs")], writes=[Bf("w")])
            op("act", lambda e: e.activation(out=T("w")[:], in_=T("w")[:], func=AF.Exp), reads=[Bf("w")], writes=[Bf("w")])
            op("act", lambda e: e.activation(out=T("dec")[:], in_=p4[:, o4:o4 + 512], func=AF.Exp), reads=[b4_], writes=[Bf("dec")])
            op("dve", lambda e: e.tensor_tensor(out=T("xdt")[:], in0=T("xs")[:], in1=T("dt")[:], op=ALU.mult), reads=[Bf("xs"), Bf("dt")], writes=[Bf("xdt")])
            op("pool", lambda e: e.tensor_copy(out=T("xdtb")[:], in_=T("xdt")[:]), reads=[Bf("xdt")], writes=[Bf("xdtb")])
            op("dve", lambda e: e.tensor_tensor(out=T("xdtw")[:], in0=T("xdt")[:], in1=T("w")[:], op=ALU.mult), reads=[Bf("xdt"), Bf("w")], writes=[Bf("xdtw")])
            p5, o5, b5_ = psh[5]
            op("pe", lambda e: e.matmul(p5[:, o5:o5 + 512], lhsT=T("Btb")[:], rhs=T("xdtw")[:], start=True, stop=True), reads=[Bf("Btb"), Bf("xdtw")], writes=[b5_])
            p6, o6, b6_ = psh[6]
            CTb_t, CTb_b = blk["CTb"]
            BTb_t, BTb_b = blk["BTb"]
            for k in range(4):
                op("pe", lambda e: e.matmul(p6[:, o6 + k * 128:o6 + (k + 1) * 128], lhsT=T("hTb")[:, k * 128:(k + 1) * 128], rhs=CTb_t[:, cs], start=True, stop=True),
                   reads=[Bf("hTb"), CTb_b], writes=[b6_])
            p7, o7, b7_ = psh[7]
            for k in range(4):
                op("pe", lambda e: e.matmul(p7[:, o7 + k * 128:o7 + (k + 1) * 128], lhsT=T("dtA")[:, k * 128:(k + 1) * 128], rhs=U_t[:], start=True, stop=True),
                   reads=[Bf("dtA"), U_b], writes=[b7_])
            op("act", lambda e: e.activation(out=T("ea")[:].rearrange("p k l -> p (k l)"), in_=p7[:, o7:o7 + 512], func=AF.Exp), reads=[b7_], writes=[Bf("ea")])
            op("pe", lambda e: e.matmul(pt[:, po:po + 128], lhsT=BTb_t[:, cs], rhs=CTb_t[:, cs], start=True, stop=True), reads=[BTb_b, CTb_b], writes=[pb])
            op("dve", lambda e: e.tensor_tensor(out=T("cbm")[:], in0=pt[:, po:po + 128], in1=U_t[:], op=ALU.mult), reads=[pb, U_b], writes=[Bf("cbm")])
            dcol = T("dtA")[:].rearrange("p (j q) -> p j q", q=64)[:, :, 0:1]
            op("dve", lambda e: e.tensor_copy(out=T("L1")[:], in_=dcol.to_broadcast([128, 8, 128])), reads=[Bf("dtA")], writes=[Bf("L1")])
            op("dve", lambda e: e.scalar_tensor_tensor(out=T("L2")[:], in0=T("L1")[:], scalar=-1.0, in1=U_t[:].unsqueeze(1).to_broadcast([128, 8, 128]),
                                                       op0=ALU.mult, op1=ALU.mult), reads=[Bf("L1"), U_b], writes=[Bf("L2")])
            for half, (pd, od, bd) in enumerate((psh[1], psh[2])):
                for jj in range(4):
                    j = half * 4 + jj
                    op("pe", lambda e: e.matmul(pd[:, od + jj * 128:od + (jj + 1) * 128], lhsT=T("L1")[:, j, :], rhs=U_t[:], start=True, stop=False),
                       reads=[Bf("L1"), U_b], writes=[bd])
                    op("pe", lambda e: e.matmul(pd[:, od + jj * 128:od + (jj + 1) * 128], lhsT=T("L2")[:, j, :], rhs=one_t[:], start=False, stop=True),
                       reads=[Bf("L2"), one_b], writes=[bd])
                Ef = T("E")[:].rearrange("p k l -> p (k l)")
                op("dve", lambda e: e.tensor_scalar(out=Ef, in0=pd[:, od:od + 512], scalar1=0.0, scalar2=None, op0=ALU.min), reads=[bd], writes=[Bf("E")])
                op("act", lambda e: e.activation(out=Ef, in_=Ef, func=AF.Exp), reads=[Bf("E")], writes=[Bf("E")])
                op("dve", lambda e: e.tensor_tensor(out=T("Mj")[:, half * 4:(half + 1) * 4, :], in0=T("E")[:], in1=T("cbm")[:].unsqueeze(1).to_broadcast([128, 4, 128]),
                                                    op=ALU.mult), reads=[Bf("E"), Bf("cbm")], writes=[Bf("Mj")])
            for j in range(8):
                k, hf = j // 2, j % 2
                op("pe", lambda e: e.matmul(p3[hf * 64:(hf + 1) * 64, o3 + k * 128:o3 + (k + 1) * 128], lhsT=T("xdtb")[:, j * 64:(j + 1) * 64], rhs=T("Mj")[:, j, :],
                                            start=True, stop=True), reads=[Bf("xdtb"), Bf("Mj")], writes=[b3_])
            yf = T("y")[:].rearrange("p k l -> p (k l)")
            op("dve", lambda e: e.tensor_tensor(out=yf, in0=p6[:, o6:o6 + 512], in1=T("ea")[:].rearrange("p k l -> p (k l)"), op=ALU.mult),
               reads=[b6_, Bf("ea")], writes=[Bf("y")])
            op("dve", lambda e: e.tensor_tensor(out=yf, in0=p3[:, o3:o3 + 512], in1=yf, op=ALU.add), reads=[b3_, Bf("y")], writes=[Bf("y")])
            sz_t, sz_b = blk["szT"]
            for k in range(4):
                op("dve", lambda e: e.scalar_tensor_tensor(out=T("y")[:, k, :], in0=xs_t[:, k, cs], scalar=dsk_t[:, k:k + 1], in1=T("y")[:, k, :],
                                                           op0=ALU.mult, op1=ALU.add), reads=[xs_b, dsk_b, Bf("y")], writes=[Bf("y")])
            op("dve", lambda e: e.tensor_tensor(out=T("y")[:], in0=T("y")[:], in1=sz_t[:, :, cs], op=ALU.mult), reads=[Bf("y"), sz_b], writes=[Bf("y")])
            _rms_out(T("y"), Bf("y"), 128, lambda k: yT[k * 128:(k + 1) * 128, c * 128:(c + 1) * 128], p4, o4, b4_)
            op("dve", lambda e: e.tensor_tensor(out=T("tmp")[:], in0=T("hT")[:], in1=T("dec")[:], op=ALU.mult), reads=[Bf("hT"), Bf("dec")], writes=[Bf("tmp")])
            op("dve", lambda e: e.tensor_tensor(out=T("hT")[:], in0=p5[:, o5:o5 + 512], in1=T("tmp")[:], op=ALU.add), reads=[b5_, Bf("tmp")], writes=[Bf("hT")])
            op("pool", lambda e: e.tensor_copy(out=T("hTb")[:], in_=T("hT")[:]), reads=[Bf("hT")], writes=[Bf("hTb")])

        def _rms_out(y_t, y_b, W, dst_fn, pq, oq, bq):
            sq_t, sq_b = ssd["sq"]
            r_t, r_b = ssd["rstd"]
            o_t, o_b = ssd["yo"]
            op("pool", lambda e: e.tensor_tensor(out=sq_t[:, :, :W], in0=y_t[:, :, :W], in1=y_t[:, :, :W], op=ALU.mult), reads=[y_b], writes=[sq_b])
            for k in range(4):
                op("pe", lambda e: e.matmul(pq[:, oq:oq + W], lhsT=one_t[:], rhs=sq_t[:, k, :W], start=(k == 0), stop=(k == 3)), reads=[one_b, sq_b], writes=[bq])
            op("dve", lambda e: e.tensor_scalar(out=r_t[:, :W], in0=pq[:, oq:oq + W], scalar1=1.0 / 512.0, scalar2=NORM_EPS, op0=ALU.mult, op1=ALU.add),
               reads=[bq], writes=[r_b])
            op("act", lambda e: e.activation(out=r_t[:, :W], in_=r_t[:, :W], func=AF.Sqrt), reads=[r_b], writes=[r_b])
            op("dve", lambda e: e.reciprocal(out=r_t[:, :W], in_=r_t[:, :W]), reads=[r_b], writes=[r_b])
            for k in range(4):
                op("dve", lambda e: e.scalar_tensor_tensor(out=o_t[:, k, :W], in0=y_t[:, k, :W], scalar=nw_t[:, k:k + 1], in1=r_t[:, :W], op0=ALU.mult, op1=ALU.mult),
                   reads=[y_b, nw_b, r_b], writes=[o_b])
            for k in range(4):
                P.dma("sp", dst_fn(k), o_t[:, k, :W], reads=[o_b])

        blkb = {}
        blkb["xTb"] = P.sb("xTb", [128, 8, BW], BF16)
        blkb["pre"] = P.sb("pre", [128, 6, BW + 3], F32)
        blkb["xsT"] = P.sb("xsT", [128, 4, BW], F32)
        blkb["BT"] = P.sb("BT", [128, BW], F32)
        blkb["BTb"] = P.sb("BTb", [128, BW], BF16)
        blkb["CTb"] = P.sb("CTb", [128, BW], BF16)
        blkb["szT"] = P.sb("szT", [128, 4, BW], F32)
        blkb["qb"] = P.sb("qb", [64, 4, BW], BF16)
        blkb["stg"] = [P.sb("stg0", [128, BW], F32)] * 3
        blkb["cacc"] = P.sb("cacc", [128, BW], F32)
        blkb["gate"] = P.sb("gateq", [128, 12], F32)
        blkb["vtok"] = P.sb("vtok", [128, 192], F32)
        op("dve", lambda e: e.memset(blkb["pre"][0][:, :, 0:3], 0.0), writes=[blkb["pre"][1]])

        def inproj_block(xsrc, c0, W, kv, kvcol0, outs, wpos=None):
            xTb_t, xTb_b = blkb["xTb"]
            for k in range(8):
                P.dma("pool", xTb_t[:, k, :W], xsrc[k * 128:(k + 1) * 128, c0:c0 + W], writes=[xTb_b])
            pre_t, pre_b = blkb["pre"]
            sz_t, sz_b = blkb["szT"]
            q_t, q_b = blkb["qb"]
            for ct in range(17):
                M = COLS_A[ct]
                pt, po, pb = next_ps()
                for k in range(8):
                    op("pe", lambda e: e.matmul(pt[0:M, po:po + W], lhsT=wA_t[:, k, OFF_A[ct]:OFF_A[ct] + M], rhs=xTb_t[:, k, :W], start=(k == 0), stop=(k == 7)),
                       reads=[wA_b, xTb_b], writes=[pb])
                bias = bA_t[0:M, ct:ct + 1]
                if ct < 4:
                    op("act", lambda e: e.activation(out=sz_t[:, ct, :W], in_=pt[:, po:po + W], func=AF.Silu, bias=bias, scale=1.0), reads=[pb, bA_b], writes=[sz_b])
                elif ct < 10:
                    op("act", lambda e: e.activation(out=pre_t[:, ct - 4, 3:3 + W], in_=pt[:, po:po + W], func=AF.Identity, bias=bias, scale=1.0),
                       reads=[pb, bA_b], writes=[pre_b])
                elif ct < 14:
                    op("act", lambda e: e.activation(out=q_t[:, ct - 10, :W], in_=pt[0:64, po:po + W], func=AF.Identity, bias=bias, scale=1.0),
                       reads=[pb, bA_b], writes=[q_b])
                else:
                    st_t, st_b = blkb["stg"][ct - 14]
                    op("act", lambda e: e.activation(out=st_t[:, :W], in_=pt[:, po:po + W], func=AF.Identity, bias=bias, scale=1.0), reads=[pb, bA_b], writes=[st_b])
                    if ct == 14:
                        op("pool", lambda e: e.tensor_copy(out=kv["kvc"][0][:, kvcol0:kvcol0 + W], in_=st_t[:, :W]), reads=[st_b], writes=[kv["kvc"][1]])
                    elif ct == 15:
                        op("pool", lambda e: e.tensor_copy(out=kv["ksl"][0][0:64, kvcol0:kvcol0 + W], in_=st_t[0:64, :W]), reads=[st_b], writes=[kv["ksl"][1]])
                    else:
                        rc = kvcol0 % 1024 if wpos is not None else kvcol0
                        op("pool", lambda e: e.tensor_copy(out=kv["kwn"][0][0:64, rc:rc + W], in_=st_t[0:64, :W]), reads=[st_b], writes=[kv["kwn"][1]])
                        if wpos is not None:
                            P.dma("pool", kv["kwn"][0][64:71, rc:rc + W], kaug[:, wpos:wpos + W], writes=[kv["kwn"][1]])
                    outs(ct - 14, st_t, st_b)

        def conv_block(W, hist_fn=None):
            pre_t, pre_b = blkb["pre"]
            a_t, a_b = blkb["cacc"]
            xs_t, xs_b = blkb["xsT"]
            for ci in range(6):
                op("dve", lambda e: e.tensor_scalar(out=a_t[:, :W], in0=pre_t[:, ci, 0:W], scalar1=cw_t[:, ci, 0:1], scalar2=None, op0=ALU.mult),
                   reads=[pre_b, cw_b], writes=[a_b])
                for k in range(1, 4):
                    op("dve", lambda e: e.scalar_tensor_tensor(out=a_t[:, :W], in0=pre_t[:, ci, k:k + W], scalar=cw_t[:, ci, k:k + 1], in1=a_t[:, :W],
                                                               op0=ALU.mult, op1=ALU.add), reads=[pre_b, cw_b, a_b], writes=[a_b])
                if ci < 4:
                    op("act", lambda e: e.activation(out=xs_t[:, ci, :W], in_=a_t[:, :W], func=AF.Silu, bias=cb_t[:, ci:ci + 1], scale=1.0),
                       reads=[a_b, cb_b], writes=[xs_b])
                elif ci == 4:
                    op("act", lambda e: e.activation(out=blkb["BT"][0][:, :W], in_=a_t[:, :W], func=AF.Silu, bias=cb_t[:, ci:ci + 1], scale=1.0),
                       reads=[a_b, cb_b], writes=[blkb["BT"][1]])
                    op("pool", lambda e: e.tensor_copy(out=blkb["BTb"][0][:, :W], in_=blkb["BT"][0][:, :W]), reads=[blkb["BT"][1]], writes=[blkb["BTb"][1]])
                else:
                    op("act", lambda e: e.activation(out=blkb["CTb"][0][:, :W], in_=a_t[:, :W], func=AF.Silu, bias=cb_t[:, ci:ci + 1], scale=1.0),
                       reads=[a_b, cb_b], writes=[blkb["CTb"][1]])

        def tok_proj(c_ap_fn, W, kv, tile_idx, want_dt=False):
            xTb_t, xTb_b = blkb["xTb"]
            pt, po, pb = next_ps()
            for k in range(8):
                op("pe", lambda e: e.matmul(pt[0:W, po:po + 140], lhsT=c_ap_fn(k), rhs=wT_t[:, k, 512:652], start=(k == 0), stop=(k == 7)),
                   reads=[xTb_b, wT_b], writes=[pb])
            g_t, g_b = blkb["gate"]
            v_t, v_b = blkb["vtok"]
            op("dve", lambda e: e.tensor_tensor(out=v_t[0:W, 0:140], in0=pt[0:W, po:po + 140], in1=bT_t[0:W, 512:652], op=ALU.add), reads=[pb, bT_b], writes=[v_b])
            op("act", lambda e: e.activation(out=g_t[0:W, :], in_=v_t[0:W, 0:12], func=AF.Sigmoid), reads=[v_b], writes=[g_b])
            if kv is not None:
                op("pool", lambda e: e.tensor_copy(out=kv["vsl"][0][0:W, tile_idx, 0:64], in_=v_t[0:W, 12:76]), reads=[v_b], writes=[kv["vsl"][1]])
                op("pool", lambda e: e.tensor_copy(out=kv["vwn"][0][0:W, tile_idx % 8, 0:64], in_=v_t[0:W, 76:140]), reads=[v_b], writes=[kv["vwn"][1]])

        es_p = ExitStack()
        P.es = es_p
        ssd["hT"] = P.sb("hT", [128, 512], F32)
        ssd["hTb"] = P.sb("hTb", [128, 512], BF16)
        op("dve", lambda e: e.memset(ssd["hT"][0][:], 0.0), writes=[ssd["hT"][1]])
        op("dve", lambda e: e.memset(ssd["hTb"][0][:], 0.0), writes=[ssd["hTb"][1]])
        for nm, shp, dt_ in (("dt", [128, 512], F32), ("dtA", [128, 512], F32), ("xs", [128, 512], F32), ("Btb", [128, 128], BF16),
                             ("acs", [128, 512], F32), ("w", [128, 512], F32), ("dec", [128, 512], F32), ("xdt", [128, 512], F32),
                             ("xdtb", [128, 512], BF16), ("xdtw", [128, 512], BF16), ("ea", [128, 4, 128], F32), ("cbm", [128, 128], F32),
                             ("L1", [128, 8, 128], F32), ("L2", [128, 8, 128], F32), ("E", [128, 4, 128], F32), ("Mj", [128, 8, 128], BF16),
                             ):
            ssd[nm] = P.sb("ssd_" + nm, shp, dt_)
        ssd["tmp"] = ssd["acs"]
        ssd["y"] = P.sb("ssd_y", [128, 4, 128], F32)

        pkv = alloc_kv("p_", S, 4)
        P.es = es
        for nm in ("kc",):
            op("dve", lambda e: e.memset(pkv[nm][0][0:64, :], 0.0), writes=[pkv[nm][1]])
        op("dve", lambda e: e.memset(pkv["vc"][0][:, :, 0:64], 0.0), writes=[pkv["vc"][1]])
        done_n = 0
        NB = S // BW
        TPB = BW // 128
        for tb in range(NB):
            c0 = tb * BW

            def outs(i, st_t, st_b, c0=c0, tb=tb):
                if i < 2:
                    P.dma("sp", kvT[i * 128:(i + 1) * 128, c0:c0 + BW], st_t[:, :BW], reads=[st_b])
                elif c0 >= S - 512:
                    P.dma("sp", winT[:, c0 - (S - 512):c0 - (S - 512) + BW], st_t[:, :BW], reads=[st_b])
            inproj_block(xT, c0, BW, pkv, c0, outs, wpos=c0)
            if tb == NB - 1:
                P.dma("sp", convT_p.rearrange("(c p) k -> p c k", p=128), blkb["pre"][0][:, :, BW:BW + 3], reads=[blkb["pre"][1]])
            conv_block(BW)
            nmax = min((BW // 16) * (tb + 1) - 1, 511)
            n = done_n
            while n < nmax:
                cch = n // 128
                hi = min(nmax, cch * 128 + 128)
                compress(pkv, cch * 128, hi - cch * 128)
                n = hi
            done_n = nmax
            for tl in range(TPB):
                t = tb * TPB + tl
                cs = slice(tl * 128, (tl + 1) * 128)
                ssd_chunk(t, blkb, tl * 128)
                tok_proj(lambda k: blkb["xTb"][0][:, k, cs], 128, pkv, t)
                qa_t, qa_b = pkv["qa"]
                P.dma("pool", qa_t[64:71, :, :], qaug[:, :, t * 128:(t + 1) * 128], writes=[qa_b])
                op("pool", lambda e: e.tensor_copy(out=qa_t[0:64, :, :], in_=blkb["qb"][0][:, :, cs]), reads=[blkb["qb"][1]], writes=[qa_b])

                def out_fn(y_t, y_b, t=t):
                    n_t, n_b = scr["nyT"]
                    pt, po, pb = next_ps()
                    for k in range(2):
                        op("pe", lambda e: e.transpose(out=pt[:, po + k * 128:po + (k + 1) * 128], in_=y_t[:, k * 128:(k + 1) * 128], identity=id_t[:]),
                           reads=[y_b, id_b], writes=[pb])
                    op("dve", lambda e: e.tensor_copy(out=n_t[:].rearrange("p k q -> p (k q)"), in_=pt[:, po:po + 256]), reads=[pb], writes=[n_b])
                    P.dma("sp", yT[512:768, t * 128:(t + 1) * 128].rearrange("(k p) q -> p k q", p=128), n_t[:], reads=[n_b])
                nsa_qtile(pkv, t, blkb["gate"][0][:, :], blkb["gate"][1], out_fn)
            op("dve", lambda e: e.tensor_copy(out=blkb["pre"][0][:, :, 0:3], in_=blkb["pre"][0][:, :, BW:BW + 3]), reads=[blkb["pre"][1]], writes=[blkb["pre"][1]])
        pt, po, pb = next_ps()
        for k in range(4):
            op("pe", lambda e: e.transpose(out=pt[:, po + k * 128:po + (k + 1) * 128], in_=ssd["hT"][0][:, k * 128:(k + 1) * 128], identity=id_t[:]),
               reads=[ssd["hT"][1], id_b], writes=[pb])
        op("dve", lambda e: e.tensor_copy(out=ssd["tmp"][0][:], in_=pt[:, po:po + 512]), reads=[pb], writes=[ssd["tmp"][1]])
        P.dma("sp", ssm_p.rearrange("(k p) n -> p k n", p=128), ssd["tmp"][0][:].rearrange("p (k n) -> p k n", k=4), reads=[ssd["tmp"][1]])
        if stop == "prompt":
            P.finish()
            raise _Stop(nc)

        P.barrier()
        es_p.close()
        skv = alloc_kv("s_", SS, 2)
        P.dma("pool", skv["kwn"][0][64:71, 512:1024], kaug[:, (NPG - 4) * 128:NPG * 128], writes=[skv["kwn"][1]], key="dl_s_kwn_aug")
        P.dma("pool", skv["kwn"][0][64:71, 0:128], kaug[:, NPG * 128:NPG * 128 + 128], writes=[skv["kwn"][1]], key="dl_s_kwn_aug")
        for nm in ("kvc", "vsl", "vwn", "vc"):
            pass
        for nm, rows in (("kvc", 128), ("ksl", 64), ("kwn", 64), ("kc", 64)):
            op("dve", lambda e: e.memset(skv[nm][0][0:rows, :], 0.0), writes=[skv[nm][1]])
        for nm in ("vsl", "vwn", "vc"):
            op("dve", lambda e: e.memset(skv[nm][0][:, :, 0:64], 0.0), writes=[skv[nm][1]])
        qa_t, qa_b = skv["qa"]
        TQ = NTS - 1
        op("dve", lambda e: e.memset(qa_t[0:64, :, :], 0.0), writes=[qa_b])
        P.dma("pool", qa_t[64:71, :, :], qaug[:, :, TQ * 128:(TQ + 1) * 128], writes=[qa_b])
        newkv = {"kvc": P.sb("n_kvc", [128, NS], BF16), "ksl": P.sb("n_ksl", [64, NS], BF16), "kwn": P.sb("n_kwn", [64, NS], BF16)}
        colp_t, colp_b = const("colp", colp, [128, 3, 4])
        hs_t, hs_b = P.sb("hs", [128, 3, 6, NS], F32)
        for k in range(3):
            P.dma("sp", hs_t[:, k, :, :], convT_s[k].rearrange("(c p) s -> p c s", p=128), writes=[hs_b])

        def outs_s(i, st_t, st_b):
            if i < 2:
                P.dma("sp", kvS[i * 128:(i + 1) * 128, :], st_t[:, :NS], reads=[st_b])
            else:
                P.dma("sp", winnewT[:, :], st_t[:, :NS], reads=[st_b])
        inproj_block(xTs, 0, NS, newkv, 0, outs_s)
        pre_t, pre_b = blkb["pre"]
        P.dma("sp", convS[0].rearrange("(c p) s -> p c s", p=128), hs_t[:, 1, :, :], reads=[hs_b])
        P.dma("sp", convS[1].rearrange("(c p) s -> p c s", p=128), hs_t[:, 2, :, :], reads=[hs_b])
        P.dma("sp", convS[2].rearrange("(c p) s -> p c s", p=128), pre_t[:, :, 3:3 + NS], reads=[pre_b])
        a_t, a_b = blkb["cacc"]
        xs_t, xs_b = blkb["xsT"]
        sBT, sBT_b = blkb["BT"]
        sCT, sCT_b = P.sb("sCT", [128, NS], F32)
        for ci in range(6):
            op("dve", lambda e: e.tensor_scalar(out=a_t[:, :NS], in0=hs_t[:, 0, ci, :], scalar1=cw_t[:, ci, 0:1], scalar2=None, op0=ALU.mult),
               reads=[hs_b, cw_b], writes=[a_b])
            for k in range(1, 4):
                src = hs_t[:, k, ci, :] if k < 3 else pre_t[:, ci, 3:3 + NS]
                op("dve", lambda e: e.scalar_tensor_tensor(out=a_t[:, :NS], in0=src, scalar=cw_t[:, ci, k:k + 1], in1=a_t[:, :NS], op0=ALU.mult, op1=ALU.add),
                   reads=[hs_b, pre_b, cw_b, a_b], writes=[a_b])
            dst, dst_b = (xs_t[:, ci, :NS], xs_b) if ci < 4 else ((sBT[:, :NS], sBT_b) if ci == 4 else (sCT[:, :NS], sCT_b))
            op("act", lambda e: e.activation(out=dst, in_=a_t[:, :NS], func=AF.Silu, bias=cb_t[:, ci:ci + 1], scale=1.0), reads=[a_b, cb_b], writes=[dst_b])
        tok_proj(lambda k: blkb["xTb"][0][:, k, :NS], NS, None, 0)
        gtok_t, gtok_b = P.sb("gtok", [128, 12], F32)
        vtokb_t, vtokb_b = P.sb("vtokb", [128, 128], BF16)
        op("dve", lambda e: e.tensor_copy(out=gtok_t[0:NS, :], in_=blkb["gate"][0][0:NS, :]), reads=[blkb["gate"][1]], writes=[gtok_b])
        op("dve", lambda e: e.tensor_copy(out=vtokb_t[0:NS, :], in_=blkb["vtok"][0][0:NS, 12:140]), reads=[blkb["vtok"][1]], writes=[vtokb_b])
        dtT_t, dtT_b = P.sb("dtT", [128, 4, NS], F32)
        dec_t, dec_b = P.sb("decT", [128, 4, NS], F32)
        xdtT_t, xdtT_b = P.sb("xdtT", [128, 4, NS], F32)
        ysm_t, ysm_b = P.sb("ysm", [128, 4, NS], F32)
        bsum_t, bsum_b = P.sb("bsum", [128, 4], F32)
        Acol_t, Acol_b = P.sb("Acol", [128, 4], F32)
        op("dve", lambda e: e.tensor_tensor(out=bsum_t[:], in0=colp_t[:, 0, :], in1=colp_t[:, 1, :], op=ALU.add), reads=[colp_b], writes=[bsum_b])
        op("act", lambda e: e.activation(out=Acol_t[:], in_=colp_t[:, 2, :], func=AF.Exp), reads=[colp_b], writes=[Acol_b])
        op("dve", lambda e: e.tensor_scalar(out=Acol_t[:], in0=Acol_t[:], scalar1=-1.0, scalar2=None, op0=ALU.mult), reads=[Acol_b], writes=[Acol_b])
        for k4 in range(4):
            pt, po, pb = next_ps()
            for k in range(8):
                op("pe", lambda e: e.matmul(pt[:, po:po + NS], lhsT=wT_t[:, k, k4 * 128:(k4 + 1) * 128], rhs=blkb["xTb"][0][:, k, :NS], start=(k == 0), stop=(k == 7)),
                   reads=[wT_b, blkb["xTb"][1]], writes=[pb])
            op("act", lambda e: e.activation(out=dtT_t[:, k4, :], in_=pt[:, po:po + NS], func=AF.Exp, bias=bsum_t[:, k4:k4 + 1], scale=1.0),
               reads=[pb, bsum_b], writes=[dtT_b])
        op("act", lambda e: e.activation(out=dtT_t[:], in_=dtT_t[:], func=AF.Ln, bias=1.0, scale=1.0), reads=[dtT_b], writes=[dtT_b])
        for k4 in range(4):
            op("act", lambda e: e.activation(out=dec_t[:, k4, :], in_=dtT_t[:, k4, :], func=AF.Exp, scale=Acol_t[:, k4:k4 + 1]), reads=[dtT_b, Acol_b], writes=[dec_b])
        op("dve", lambda e: e.tensor_tensor(out=xdtT_t[:], in0=xs_t[:, :, :NS], in1=dtT_t[:], op=ALU.mult), reads=[xs_b, dtT_b], writes=[xdtT_b])
        hst = [P.sb("hst%d" % i, [128, 4, 128], F32) for i in range(2)]
        dgb, dgb_b = P.sb("dgb", [128, 256], F32)
        st1, st1_b = P.sb("sst1", [128, 4, 128], F32)
        st2, st2_b = P.sb("sst2", [128, 4, 128], F32)
        for s_i in range(NS):
            h_t, h_b = hst[s_i % 2]
            P.dma("sp", h_t[:], ssm_s[s_i].rearrange("(k p) n -> p k n", p=128), writes=[h_b])
            op("dve", lambda e: e.tensor_scalar(out=dgb[:, 0:128], in0=id_t[:], scalar1=sBT[:, s_i:s_i + 1], scalar2=None, op0=ALU.mult),
               reads=[id_b, sBT_b], writes=[dgb_b])
            op("dve", lambda e: e.tensor_scalar(out=dgb[:, 128:256], in0=id_t[:], scalar1=sCT[:, s_i:s_i + 1], scalar2=None, op0=ALU.mult),
               reads=[id_b, sCT_b], writes=[dgb_b])
            pt, po, pb = next_ps()
            op("pe", lambda e: e.matmul(pt[:, po:po + 256], lhsT=one_t[:], rhs=dgb[:], start=True, stop=True), reads=[one_b, dgb_b], writes=[pb])
            op("dve", lambda e: e.tensor_tensor(out=st1[:], in0=h_t[:], in1=dec_t[:, :, s_i:s_i + 1].to_broadcast([128, 4, 128]), op=ALU.mult),
               reads=[h_b, dec_b], writes=[st1_b])
            op("dve", lambda e: e.tensor_tensor(out=st2[:], in0=pt[:, po:po + 128].unsqueeze(1).to_broadcast([128, 4, 128]),
                                                in1=xdtT_t[:, :, s_i:s_i + 1].to_broadcast([128, 4, 128]), op=ALU.mult), reads=[pb, xdtT_b], writes=[st2_b])
            op("dve", lambda e: e.tensor_tensor(out=h_t[:], in0=st1[:], in1=st2[:], op=ALU.add), reads=[st1_b, st2_b], writes=[h_b])
            P.dma("sp", ssm_o[s_i].rearrange("(k p) n -> p k n", p=128), h_t[:], reads=[h_b])
            op("dve", lambda e: e.tensor_tensor(out=st1[:], in0=h_t[:], in1=pt[:, po + 128:po + 256].unsqueeze(1).to_broadcast([128, 4, 128]), op=ALU.mult),
               reads=[h_b, pb], writes=[st1_b])
            op("dve", lambda e: e.tensor_reduce(out=ysm_t[:, :, s_i], in_=st1[:], axis=AX.X, op=ALU.add), reads=[st1_b], writes=[ysm_b])
        for k in range(4):
            op("dve", lambda e: e.scalar_tensor_tensor(out=ysm_t[:, k, :], in0=xs_t[:, k, :NS], scalar=dsk_t[:, k:k + 1], in1=ysm_t[:, k, :], op0=ALU.mult, op1=ALU.add),
               reads=[xs_b, dsk_b, ysm_b], writes=[ysm_b])
        op("dve", lambda e: e.tensor_tensor(out=ysm_t[:], in0=ysm_t[:], in1=blkb["szT"][0][:, :, :NS], op=ALU.mult), reads=[ysm_b, blkb["szT"][1]], writes=[ysm_b])
        pq, oq, bq = next_ps()
        _rms_out(ysm_t, ysm_b, NS, lambda k: ysT[k * 128:(k + 1) * 128, :], pq, oq, bq)
        idx_t, idx_b = P.sb("idx", [128, NS * NPG], I32)
        idxf_t, idxf_b = P.sb("idxf", [128, NS * NPG], F32)
        iot_t, iot_b = P.sb("iot", [128, 1], I32)
        iotf_t, iotf_b = P.sb("iotf", [128, 1], F32)
        P.dma("sp", idx_t[:], pt_rep, writes=[idx_b])
        op("pool", lambda e: e.iota(iot_t[:], pattern=[[0, 1]], base=0, channel_multiplier=1), writes=[iot_b])
        op("dve", lambda e: e.tensor_copy(out=iotf_t[:], in_=iot_t[:]), reads=[iot_b], writes=[iotf_b])
        op("dve", lambda e: e.tensor_copy(out=idxf_t[:], in_=idx_t[:]), reads=[idx_b], writes=[idxf_b])
        op("dve", lambda e: e.tensor_scalar(out=idxf_t[:], in0=idxf_t[:], scalar1=128.0, scalar2=iotf_t[:, 0:1], op0=ALU.mult, op1=ALU.add),
           reads=[idxf_b, iotf_b], writes=[idxf_b])
        op("dve", lambda e: e.tensor_copy(out=idx_t[:], in_=idxf_t[:]), reads=[idxf_b], writes=[idx_b])
        G = [P.sb("G0", [128, NPG, 256], F32)] * 2
        Wt = [P.sb("Wt%d" % i, [128, 4, 128], F32) for i in range(2)]
        gq_t, gq_b = P.sb("gq", [128, 12], F32)
        op("dve", lambda e: e.memset(gq_t[:], 0.0), writes=[gq_b])
        for s_i in range(NS):
            g_t, g_b = G[s_i % 2]
            w_t, w_b = Wt[s_i % 2]
            P._deps("pool", [idx_b], [g_b])
            for j in range(NPG):
                inst = nc.gpsimd.indirect_dma_start(out=g_t[:, j, :], out_offset=None, in_=cache,
                                                    in_offset=bass.IndirectOffsetOnAxis(ap=idx_t[:, s_i * NPG + j:s_i * NPG + j + 1], axis=0))
                key = "dl_" + g_b.name
                P._sem(key)
                P.cnt[key] += 16
                inst.then_inc(P.sems[key], 16)
            g_b.w = ("dl_" + g_b.name, P.cnt["dl_" + g_b.name])
            g_b.r = {}
            idx_b.r["dl_" + g_b.name] = P.cnt["dl_" + g_b.name]
            P.dma("sp", w_t[:], kvwin_s[s_i].rearrange("(c p) f -> p c f", p=128), writes=[w_b])
            P.dma("sp", winS[s_i], kvwin_s[s_i, 1:512, :], key="ds_winS")
            for j0 in range(0, NPG, 4):
                pt, po, pb = next_ps()
                for jj in range(4):
                    op("pe", lambda e: e.transpose(out=pt[:, po + jj * 128:po + (jj + 1) * 128], in_=g_t[:, j0 + jj, 0:128], identity=id_t[:]),
                       reads=[g_b, id_b], writes=[pb])
                op("dve", lambda e: e.tensor_copy(out=skv["kvc"][0][:, j0 * 128:(j0 + 4) * 128], in_=pt[:, po:po + 512]), reads=[pb], writes=[skv["kvc"][1]])
                pt, po, pb = next_ps()
                for jj in range(4):
                    op("pe", lambda e: e.transpose(out=pt[0:64, po + jj * 128:po + (jj + 1) * 128], in_=g_t[:, j0 + jj, 128:192], identity=id_t[:]),
                       reads=[g_b, id_b], writes=[pb])
                op("dve", lambda e: e.tensor_copy(out=skv["ksl"][0][0:64, j0 * 128:(j0 + 4) * 128], in_=pt[0:64, po:po + 512]), reads=[pb], writes=[skv["ksl"][1]])
            op("pool", lambda e: e.tensor_copy(out=skv["vsl"][0][:, 0:NPG, 0:64], in_=g_t[:, :, 192:256]), reads=[g_b], writes=[skv["vsl"][1]])
            pt, po, pb = next_ps()
            for cc in range(4):
                op("pe", lambda e: e.transpose(out=pt[0:64, po + cc * 128:po + (cc + 1) * 128], in_=w_t[:, cc, 0:64], identity=id_t[:]),
                   reads=[w_b, id_b], writes=[pb])
            op("dve", lambda e: e.tensor_copy(out=skv["kwn"][0][0:64, 512:1024], in_=pt[0:64, po:po + 512]), reads=[pb], writes=[skv["kwn"][1]])
            op("pool", lambda e: e.tensor_copy(out=skv["vwn"][0][:, 4:8, 0:64], in_=w_t[:, :, 64:128]), reads=[w_b], writes=[skv["vwn"][1]])
            NP0 = NPG * 128
            op("pool", lambda e: e.tensor_copy(out=skv["ksl"][0][0:64, NP0:NP0 + 1], in_=newkv["ksl"][0][:, s_i:s_i + 1]), reads=[newkv["ksl"][1]], writes=[skv["ksl"][1]])
            op("pool", lambda e: e.tensor_copy(out=skv["kwn"][0][0:64, 0:1], in_=newkv["kwn"][0][:, s_i:s_i + 1]), reads=[newkv["kwn"][1]], writes=[skv["kwn"][1]])
            P.dma("sp", skv["vsl"][0][0:1, NPG, 0:64], vtokb_t[s_i:s_i + 1, 0:64], reads=[vtokb_b], writes=[skv["vsl"][1]], key="dl_svsl")
            P.dma("sp", skv["vwn"][0][0:1, 0, 0:64], vtokb_t[s_i:s_i + 1, 64:128], reads=[vtokb_b], writes=[skv["vwn"][1]], key="dl_svwn")
            P.dma("sp", gq_t[0:1, :], gtok_t[s_i:s_i + 1, :], reads=[gtok_b], writes=[gq_b], key="dl_gq")
            op("pool", lambda e: e.tensor_copy(out=qa_t[0:64, :, 0:1], in_=blkb["qb"][0][:, :, s_i:s_i + 1]), reads=[blkb["qb"][1]], writes=[qa_b])
            compress(skv, 0, 128)

            def out_s(y_t, y_b, s_i=s_i):
                P.dma("sp", ynS[s_i:s_i + 1, :], y_t[0:1, :], reads=[y_b])
            nsa_qtile(skv, TQ, gq_t[:, :], gq_b, out_s)
        P.finish()
    return nc


def _chan(g):
    return np.concatenate([np.arange(g * 512, (g + 1) * 512), 2048 + np.arange(g * 128, (g + 1) * 128), 2560 + np.arange(g * 128, (g + 1) * 128)])


def prep_l1_weights(inp, g, Smax):
    w_in, b_in = inp["w_in"], inp["b_in"]
    colsA = np.concatenate([
        np.arange(g * 512, (g + 1) * 512), 2048 + _chan(g),
        5152 + g * 256 + np.arange(256),
        np.concatenate([6176 + tt * 256 + g * 64 + np.arange(64) for tt in range(6)])])
    wA = np.ascontiguousarray(w_in[:, colsA])
    bfull = b_in[colsA]
    bA = np.zeros((128, 17), np.float32)
    off = 0
    for ct, m in enumerate(COLS_A):
        bA[:m, ct] = bfull[off:off + m]
        off += m
    hd = np.repeat(np.arange(8), 64)
    colsT = np.concatenate([5120 + g * 8 + hd, 7712 + g * 12 + np.arange(12), 6176 + 3 * 256 + g * 64 + np.arange(64), 6176 + 5 * 256 + g * 64 + np.arange(64)])
    wT = np.ascontiguousarray(w_in[:, colsT])
    bT = np.ascontiguousarray(np.broadcast_to(b_in[colsT][None, :], (128, 652)))
    heads = g * 8 + hd
    dtb = np.ascontiguousarray(np.broadcast_to(inp["dt_bias"][heads][None, :], (128, 512)))
    alog = np.ascontiguousarray(np.broadcast_to(inp["a_log"][heads][None, :], (128, 512)))
    ch = _chan(g)
    cw = np.ascontiguousarray(inp["conv_w"][:, ch].reshape(4, 6, 128).transpose(2, 1, 0))
    cb = np.ascontiguousarray(inp["conv_b"][ch].reshape(6, 128).T)
    hp = heads.reshape(4, 128).T
    dsk = np.ascontiguousarray(inp["d_skip"][hp])
    nw = np.ascontiguousarray(inp["ssd_norm_w"][g * 512:(g + 1) * 512].reshape(4, 128).T)
    colp = np.stack([b_in[5120 + hp], inp["dt_bias"][hp], inp["a_log"][hp]], 1).astype(np.float32)
    w1 = inp["cmp_w1"]
    w1kv = np.ascontiguousarray(w1.transpose(0, 2, 1, 3).reshape(128, 32, 128))
    pekv = np.ascontiguousarray(inp["cmp_pe"].transpose(0, 2, 1).reshape(128, 32))
    b1 = np.ascontiguousarray(inp["cmp_b1"].T)
    w2kv = np.ascontiguousarray(np.concatenate([inp["cmp_w2"][0], inp["cmp_w2"][1]], 1))
    b2k = np.ascontiguousarray(inp["cmp_b2"][0][:, None])
    b2v = np.ascontiguousarray(np.broadcast_to(inp["cmp_b2"][1][None, :], (128, 64)))
    t = _nsa_tables(Smax)
    d = dict(wA=wA, bA=bA, wT=wT, bT=bT, dtb=dtb, alog=alog, cw=cw, cb=cb, dsk=dsk, nw=nw, colp=np.ascontiguousarray(colp),
             w1kv=w1kv, pekv=pekv, b1=b1, w2kv=w2kv, b2k=b2k, b2v=b2v,
             kaug=t["kaug"], kaug_c=t["kaug_c"], qaug=np.ascontiguousarray(t["qaug"][g]), caus4=t["caus4"], low4=t["low4"], EW=t["EW"], Zm=t["Zm"],
             F=t["F"], ov=t["ov"], U=t["U"], ones=t["ones"], ident=t["ident"])
    return {k: np.ascontiguousarray(v, dtype=np.float32) for k, v in d.items()}


def prep_l1_data(inp, b, g, S, samples, NPG):
    NS = len(samples)
    ch = _chan(g)
    d = {}
    d["xT"] = np.ascontiguousarray(inp["x_prompt"][b, :S].T)
    d["xTs"] = np.ascontiguousarray(inp["x_sample"][samples, 0].T)
    d["pt_rep"] = np.ascontiguousarray(np.broadcast_to(inp["page_table"][samples].reshape(1, -1), (128, NS * NPG))).astype(np.int32)
    d["kvwin_s"] = np.ascontiguousarray(inp["cache_kv_win"][samples][:, :, :, g, :].reshape(NS, 512, 128))
    d["ssm_s"] = np.ascontiguousarray(inp["state_ssm"][samples, g * 8:(g + 1) * 8].reshape(NS, 512, 128))
    d["convT_s"] = np.ascontiguousarray(inp["state_conv"][samples][:, :, ch].transpose(1, 2, 0))
    return d


def cache_group(inp, g):
    c = inp["cache_kv_paged"]
    return np.ascontiguousarray(c[:, :, :, g, :].reshape(c.shape[0] * 128, 256))


def l2_weights(w):
    bc = np.zeros((128, 6, 1024), np.float32)
    for i, k in enumerate(["b_out", "ln1_g", "ln1_b", "ln2_g", "ln2_b"]):
        bc[:, i, :] = w[k][None, :]
    bc[:, 5, :64] = w["b_router"][None, :]
    return dict(
        w_gm=np.ascontiguousarray(w["w_in"][:, 7760:]), b_gm=np.ascontiguousarray(w["b_in"][7760:].reshape(16, 128).T),
        w_sd=w["w_ssd_down"], w_nd=w["w_nsa_down"], w_out=w["w_out"], bcast=bc, w_r=w["w_router"],
        w_e1=np.concatenate([w["w_e1"], w["w_s1"][None]], 0), w_e3=np.concatenate([w["w_e3"], w["w_s3"][None]], 0),
        w_e2=np.concatenate([w["w_e2"], w["w_s2"][None]], 0), ident=np.eye(128, dtype=np.float32))


def kernel(**inp):
    inp = {k: np.asarray(v) for k, v in inp.items()}
    B, S = inp["x_prompt"].shape[:2]
    DB = inp["x_sample"].shape[0]
    NPG = inp["page_table"].shape[1]
    NS = DB // 2
    SS = NPG * 128 + 128
    nc1 = build_l1(S, NS, NPG, nphys=inp["cache_kv_paged"].shape[0])
    caches = [cache_group(inp, g) for g in range(4)]
    wts = [prep_l1_weights(inp, g, max(S, SS)) for g in range(4)]
    maps = []
    for c in range(8):
        b, g = c // 4, c % 4
        samples = np.arange(b * NS, (b + 1) * NS)
        m = dict(wts[g])
        m.update(prep_l1_data(inp, b, g, S, samples, NPG))
        m["cache"] = caches[g]
        maps.append(m)
    r1 = run_bass_kernel_spmd(nc1, maps, core_ids=list(range(8))).results
    del maps, caches
    f32 = np.float32
    ssd_y = np.zeros((B, S, 2048), f32); nsa_y = np.zeros((B, S, 1024), f32)
    kv_p = np.zeros((B, S, 4, 4, 64), f32); win_p = np.zeros((B, 512, 2, 4, 64), f32)
    ssm_pr = np.zeros((B, 32, 64, 128), f32); conv_p = np.zeros((B, 3, 3072), f32)
    ssd_ys = np.zeros((DB, 2048), f32); nsa_ys = np.zeros((DB, 1024), f32)
    kv_s = np.zeros((DB, 1, 4, 4, 64), f32); win_s = np.zeros((DB, 512, 2, 4, 64), f32)
    ssm_sm = np.zeros((DB, 32, 64, 128), f32); conv_s = np.zeros((DB, 3, 3072), f32)
    for c in range(8):
        b, g = c // 4, c % 4
        r = r1[c]
        sm = slice(b * NS, (b + 1) * NS)
        ch = _chan(g)
        ssd_y[b, :, g * 512:(g + 1) * 512] = r["yT"][0:512].T
        nsa_y[b, :, g * 256:(g + 1) * 256] = r["yT"][512:768].T
        for tt in range(4):
            kv_p[b, :, tt, g, :] = r["kvT"][tt * 64:(tt + 1) * 64].T
            kv_s[sm, 0, tt, g, :] = r["kvS"][tt * 64:(tt + 1) * 64].T
        for t2 in range(2):
            win_p[b, :, t2, g, :] = r["winT"][t2 * 64:(t2 + 1) * 64].T
            win_s[sm, 0:511, t2, g, :] = r["winS"][:, :, t2 * 64:(t2 + 1) * 64]
            win_s[sm, 511, t2, g, :] = r["winnewT"][t2 * 64:(t2 + 1) * 64].T
        ssm_pr[b, g * 8:(g + 1) * 8] = r["ssm_p"].reshape(8, 64, 128)
        conv_p[b][:, ch] = r["convT_p"].T
        ssd_ys[sm, g * 512:(g + 1) * 512] = r["ysT"].T
        nsa_ys[sm, g * 256:(g + 1) * 256] = r["ynS"]
        ssm_sm[sm, g * 8:(g + 1) * 8] = r["ssm_o"].reshape(NS, 8, 64, 128)
        conv_s[sm][:, :, ch] = 0
        cs_ = conv_s[sm]
        cs_[:, :, ch] = r["convS"].transpose(2, 0, 1)
        conv_s[sm] = cs_
    TQ = S // 4
    SQ = DB // 8
    NT_TILES = (TQ + SQ + 127) // 128
    NT = NT_TILES * 128
    nc2 = build_l2(NT_TILES)
    w2 = l2_weights(inp)
    maps = []
    for c in range(8):
        b, k = c // 4, c % 4
        srows = np.arange(b * NS + k * SQ, b * NS + (k + 1) * SQ)
        x = np.zeros((NT, 1024), f32)
        x[:TQ] = inp["x_prompt"][b, k * TQ:(k + 1) * TQ]
        x[TQ:TQ + SQ] = inp["x_sample"][srows, 0]
        ym = np.zeros((NT, 3072), f32)
        ym[:TQ, :2048] = ssd_y[b, k * TQ:(k + 1) * TQ]
        ym[:TQ, 2048:] = nsa_y[b, k * TQ:(k + 1) * TQ]
        ym[TQ:TQ + SQ, :2048] = ssd_ys[srows]
        ym[TQ:TQ + SQ, 2048:] = nsa_ys[srows]
        m = dict(w2)
        m.update(xT=np.ascontiguousarray(x.T), x_tok=x, yT=np.ascontiguousarray(ym.T))
        maps.append(m)
    r2 = run_bass_kernel_spmd(nc2, maps, core_ids=list(range(8))).results
    y_p = np.zeros((B, S, 1024), f32)
    y_s = np.zeros((DB, 1, 1024), f32)
    for c in range(8):
        b, k = c // 4, c % 4
        srows = np.arange(b * NS + k * SQ, b * NS + (k + 1) * SQ)
        y_p[b, k * TQ:(k + 1) * TQ] = r2[c]["y"][:TQ]
        y_s[srows, 0] = r2[c]["y"][TQ:TQ + SQ]
    return (y_p, y_s, kv_p, kv_s, win_p, win_s, ssm_pr, ssm_sm, conv_p, conv_s)
```

```python
from contextlib import ExitStack
import numpy as np
import concourse.bass as bass
import concourse.mybir as mybir
from concourse.bass_utils import run_bass_kernel_spmd

F32 = mybir.dt.float32
BF16 = mybir.dt.bfloat16
I32 = mybir.dt.int32
U32 = mybir.dt.uint32
AF = mybir.ActivationFunctionType
ALU = mybir.AluOpType
AX = mybir.AxisListType

D_MODEL = 1024
ALPHA = 2.0 ** 0.25
NORM_EPS = 1e-5
N_EXPERTS = 64
ROUTE_SCALE = 2.5
BIG = 1.0e9


class Buf:
    __slots__ = ("name", "w", "r")

    def __init__(self, name):
        self.name = name
        self.w = None
        self.r = {}


class Prog:
    def __init__(self, nc, es):
        self.nc = nc
        self.es = es
        self.sem_es = es
        self.eng = {"pe": nc.tensor, "dve": nc.vector, "act": nc.scalar, "pool": nc.gpsimd, "sp": nc.sync}
        self.sems = {}
        self.cnt = {}
        self.seen = {e: {} for e in self.eng}
        self.nbuf = 0
        self.store_keys = set()
        for e in self.eng:
            self._sem(e)

    def _sem(self, key):
        if key not in self.sems:
            self.sems[key] = self.sem_es.enter_context(self.nc.semaphore("s_" + key))
            self.cnt[key] = 0
        return self.sems[key]

    def buf(self, name=None):
        self.nbuf += 1
        return Buf(name or ("b%d" % self.nbuf))

    def sb(self, name, shape, dt):
        t = self.es.enter_context(self.nc.sbuf_tensor("sb_" + name, list(shape), dt))
        return t, Buf(name)

    def ps(self, name, shape, dt=F32):
        t = self.es.enter_context(self.nc.psum_tensor("pp_" + name, list(shape), dt))
        return t, Buf(name)

    def _wait(self, e, key, val):
        if e == "pe" and key == "pe":
            return
        if self.seen[e].get(key, 0) >= val:
            return
        self.eng[e].wait_ge(self.sems[key], val)
        self.seen[e][key] = val

    def _deps(self, e, reads, writes):
        for b in reads:
            if b.w is not None:
                self._wait(e, b.w[0], b.w[1])
        for b in writes:
            if b.w is not None:
                self._wait(e, b.w[0], b.w[1])
            for k, v in b.r.items():
                self._wait(e, k, v)

    def op(self, e, fn, reads=(), writes=()):
        self._deps(e, reads, writes)
        inst = fn(self.eng[e])
        self.cnt[e] += 1
        inst.then_inc(self.sems[e], 1)
        c = self.cnt[e]
        for b in reads:
            b.r[e] = c
        for b in writes:
            b.w = (e, c)
            b.r = {}
        self.seen[e][e] = max(self.seen[e].get(e, 0), 0)

    def dma(self, q, out, in_, reads=(), writes=(), key=None, **kw):
        if key is None:
            key = ("dl_" + writes[0].name) if writes else ("ds_" + reads[0].name)
        self._sem(key)
        if not writes:
            self.store_keys.add(key)
        self._deps(q, reads, writes)
        inst = self.eng[q].dma_start(out=out, in_=in_, **kw)
        self.cnt[key] += 16
        inst.then_inc(self.sems[key], 16)
        c = self.cnt[key]
        for b in reads:
            b.r[key] = c
        for b in writes:
            b.w = (key, c)
            b.r = {}

    def barrier(self):
        for e in self.eng:
            for key, v in self.cnt.items():
                if v > 0 and key != e:
                    self._wait(e, key, v)

    def finish(self):
        for key, v in self.cnt.items():
            if key.startswith("ds_") or key.startswith("dl_"):
                if v > 0:
                    self._wait("sp", key, v)
        for e in ("pe", "dve", "act", "pool"):
            if self.cnt[e] > 0:
                self._wait("sp", e, self.cnt[e])


class _Stop(Exception):
    pass


def build_l2(NT_TILES, n_exp=N_EXPERTS, stop=None):
    try:
        return _build_l2(NT_TILES, n_exp, stop)
    except _Stop as ex:
        return ex.args[0]


def _build_l2(NT_TILES, n_exp, stop):
    NT = NT_TILES * 128
    nc = bass.Bass("TRN2", target_bir_lowering=False)
    dr = lambda name, shape, dt=F32, kind="ExternalInput": nc.dram_tensor(name, list(shape), dt, kind=kind).ap()
    xT = dr("xT", [1024, NT])
    x_tok = dr("x_tok", [NT, 1024])
    yT = dr("yT", [3072, NT])
    w_gm = dr("w_gm", [1024, 2048])
    b_gm = dr("b_gm", [128, 16])
    w_sd = dr("w_sd", [2048, 1024])
    w_nd = dr("w_nd", [1024, 1024])
    w_out = dr("w_out", [1024, 1024])
    bc = dr("bcast", [128, 6, 1024])
    w_r = dr("w_r", [1024, 64])
    w_e1 = dr("w_e1", [n_exp + 1, 1024, 256])
    w_e3 = dr("w_e3", [n_exp + 1, 1024, 256])
    w_e2 = dr("w_e2", [n_exp + 1, 256, 1024])
    ident = dr("ident", [128, 128])
    y = dr("y", [NT, 1024], kind="ExternalOutput")

    blocks = []
    t = 0
    while t < NT_TILES:
        n = min(4, NT_TILES - t)
        blocks.append((t, n))
        t += n

    with ExitStack() as es:
        P = Prog(nc, es)
        mixT, mixT_b = P.sb("mixT", [128, 8, NT], BF16)
        mix_bufs = [P.buf("mixb%d" % i) for i in range(NT_TILES)]
        idf, idf_b = P.sb("idf", [128, 128], F32)
        P.dma("sp", idf[:], ident, writes=[idf_b])
        pst = [P.ps("ps%d" % i, [128, 1024], F32) for i in range(4)]
        psh = []
        for i in range(4):
            psh.append((pst[i][0], 0, P.buf("psb%da" % i)))
            psh.append((pst[i][0], 512, P.buf("psb%db" % i)))
        rr = [0]

        def next_ps():
            r = psh[rr[0] % 8]
            rr[0] += 1
            return r

        with ExitStack() as esA:
            old_es = P.es
            P.es = esA
            wgm, wgm_b = P.sb("wgm", [128, 8, 2048], BF16)
            wsd, wsd_b = P.sb("wsd", [128, 16, 1024], BF16)
            wnd, wnd_b = P.sb("wnd", [128, 8, 1024], BF16)
            bgm, bgm_b = P.sb("bgm", [128, 16], F32)
            for k in range(8):
                P.dma("pool", wgm[:, k, :], w_gm[k * 128:(k + 1) * 128, :], writes=[wgm_b])
            for k in range(16):
                P.dma("pool", wsd[:, k, :], w_sd[k * 128:(k + 1) * 128, :], writes=[wsd_b])
            for k in range(8):
                P.dma("pool", wnd[:, k, :], w_nd[k * 128:(k + 1) * 128, :], writes=[wnd_b])
            P.dma("sp", bgm[:], b_gm, writes=[bgm_b])
            xTb = [P.sb("xTb%d" % i, [128, 8, 512], BF16) for i in range(1)]
            yTb = [P.sb("yTb%d" % i, [128, 24, 512], BF16) for i in range(1)]
            gT = [P.sb("gT%d" % i, [128, 16, 512], F32) for i in range(1)]
            t1 = [P.sb("t1_%d" % i, [128, 512], F32) for i in range(2)]
            t2 = [P.sb("t2_%d" % i, [128, 512], F32) for i in range(2)]
            for bi, (t0, ntl) in enumerate(blocks):
                W = ntl * 128
                c0 = t0 * 128
                xb_t, xb_b = xTb[0]
                yb_t, yb_b = yTb[0]
                for k in range(8):
                    P.dma("pool", xb_t[:, k, :W], xT[k * 128:(k + 1) * 128, c0:c0 + W], writes=[xb_b])
                for k in range(24):
                    P.dma("pool", yb_t[:, k, :W], yT[k * 128:(k + 1) * 128, c0:c0 + W], writes=[yb_b])
                g_t, g_b = gT[0]
                mb = [mix_bufs[t0 + i] for i in range(ntl)]
                for ct in range(16):
                    pt, po, pb = next_ps()
                    for k in range(8):
                        P.op("pe", lambda e: e.matmul(pt[:, po:po + W], lhsT=wgm[:, k, ct * 128:(ct + 1) * 128],
                                                      rhs=xb_t[:, k, :W], start=(k == 0), stop=(k == 7)),
                             reads=[wgm_b, xb_b], writes=[pb])
                    P.op("act", lambda e: e.activation(out=g_t[:, ct, :W], in_=pt[:, po:po + W], func=AF.Sigmoid,
                                                       bias=bgm[:, ct:ct + 1], scale=1.0),
                         reads=[pb, bgm_b], writes=[g_b])
                for ct in range(8):
                    pa_t, pa_o, pa_b = next_ps()
                    for k in range(16):
                        P.op("pe", lambda e: e.matmul(pa_t[:, pa_o:pa_o + W], lhsT=wsd[:, k, ct * 128:(ct + 1) * 128],
                                                      rhs=yb_t[:, k, :W], start=(k == 0), stop=(k == 15)),
                             reads=[wsd_b, yb_b], writes=[pa_b])
                    pn_t, pn_o, pn_b = next_ps()
                    for k in range(8):
                        P.op("pe", lambda e: e.matmul(pn_t[:, pn_o:pn_o + W], lhsT=wnd[:, k, ct * 128:(ct + 1) * 128],
                                                      rhs=yb_t[:, 16 + k, :W], start=(k == 0), stop=(k == 7)),
                             reads=[wnd_b, yb_b], writes=[pn_b])
                    a_t, a_b = t1[ct % 2]
                    b_t, b_b = t2[ct % 2]
                    P.op("dve", lambda e: e.tensor_tensor(out=a_t[:, :W], in0=pa_t[:, pa_o:pa_o + W], in1=g_t[:, ct, :W], op=ALU.mult),
                         reads=[pa_b, g_b], writes=[a_b])
                    P.op("dve", lambda e: e.tensor_tensor(out=b_t[:, :W], in0=pn_t[:, pn_o:pn_o + W], in1=g_t[:, 8 + ct, :W], op=ALU.mult),
                         reads=[pn_b, g_b], writes=[b_b])
                    P.op("pool", lambda e: e.tensor_tensor(out=mixT[:, ct, c0:c0 + W], in0=a_t[:, :W], in1=b_t[:, :W], op=ALU.add),
                         reads=[a_b, b_b], writes=mb)
            P.barrier()
            if stop == "A1":
                P.finish()
                raise _Stop(nc)
            P.es = old_es
        acc = [P.sb("acc%d" % i, [128, 1024], F32) for i in range(NT_TILES)]
        hT, hT_b = P.sb("hT", [128, 8, NT], BF16)
        hT_bufs = [P.buf("hTb%d" % i) for i in range(NT_TILES)]
        gates = [P.sb("gate%d" % i, [128, n_exp + 1], F32) for i in range(NT_TILES)]
        bct, bct_b = P.sb("bct", [128, 6, 1024], F32)
        P.dma("sp", bct[:], bc, writes=[bct_b])
        with ExitStack() as esA:
            old_es = P.es
            P.es = esA
            wo, wo_b = P.sb("wo", [128, 8, 1024], BF16)
            wr, wr_b = P.sb("wr", [128, 8, 64], F32)
            for k in range(8):
                P.dma("pool", wo[:, k, :], w_out[k * 128:(k + 1) * 128, :], writes=[wo_b])
            P.dma("sp", wr[:], w_r.rearrange("(k p) e -> p k e", p=128), writes=[wr_b])
            xt = [P.sb("xt%d" % i, [128, 1024], F32) for i in range(2)]
            hTf = [P.sb("hTf%d" % i, [128, 8, 128], F32) for i in range(2)]
            st6 = [P.sb("st6_%d" % i, [128, 2, 6], F32) for i in range(2)]
            mv = [P.sb("mv%d" % i, [128, 2], F32) for i in range(2)]
            rs = [P.sb("rs%d" % i, [128, 2], F32) for i in range(2)]
            rt = [P.sb("rt%d" % i, [128, 8, 64], F32) for i in range(2)]
            r8 = [P.sb("r8_%d" % i, [128, 8, 8], F32) for i in range(2)]
            for ti in range(NT_TILES):
                s = ti % 2
                x_t, x_b = xt[s]
                P.dma("sp", x_t[:], x_tok[ti * 128:(ti + 1) * 128, :], writes=[x_b])
                halves = []
                for hh in range(2):
                    pt, po, pb = next_ps()
                    for k in range(8):
                        P.op("pe", lambda e: e.matmul(pt[:, po:po + 512], lhsT=mixT[:, k, ti * 128:(ti + 1) * 128],
                                                      rhs=wo[:, k, hh * 512:(hh + 1) * 512], start=(k == 0), stop=(k == 7)),
                             reads=[mix_bufs[ti], wo_b], writes=[pb])
                    halves.append((pt, po, pb))
                for hh in range(2):
                    pt, po, pb = halves[hh]
                    P.op("dve", lambda e: e.scalar_tensor_tensor(out=x_t[:, hh * 512:(hh + 1) * 512], in0=x_t[:, hh * 512:(hh + 1) * 512],
                                                                 scalar=ALPHA, in1=pt[:, po:po + 512], op0=ALU.mult, op1=ALU.add),
                         reads=[x_b, pb], writes=[x_b])
                P.op("pool", lambda e: e.tensor_tensor(out=x_t[:], in0=x_t[:], in1=bct[:, 0, :], op=ALU.add),
                     reads=[x_b, bct_b], writes=[x_b])
                a_t, a_b = acc[ti]
                if stop == "A2pre":
                    P.finish()
                    raise _Stop(nc)
                _layer_norm(P, x_t, x_b, a_t, a_b, bct, bct_b, 1, 2, st6[s], mv[s], rs[s])
                if stop == "A2a":
                    P.finish()
                    raise _Stop(nc)
                f_t, f_b = hTf[s]
                for hh in range(2):
                    pt, po, pb = next_ps()
                    for j in range(4):
                        k = hh * 4 + j
                        P.op("pe", lambda e: e.transpose(out=pt[:, po + j * 128:po + (j + 1) * 128], in_=a_t[:, k * 128:(k + 1) * 128],
                                                         identity=idf[:]),
                             reads=[a_b, idf_b], writes=[pb])
                    P.op("dve", lambda e: e.tensor_copy(out=f_t[:, hh * 4:(hh + 1) * 4, :], in_=pt[:, po:po + 512].rearrange("p (j t) -> p j t", j=4)),
                         reads=[pb], writes=[f_b])
                    P.op("dve", lambda e: e.tensor_copy(out=hT[:, hh * 4:(hh + 1) * 4, ti * 128:(ti + 1) * 128],
                                                        in_=pt[:, po:po + 512].rearrange("p (j t) -> p j t", j=4)),
                         reads=[pb], writes=[hT_bufs[ti]])
                if stop == "A2b":
                    P.finish()
                    raise _Stop(nc)
                P.op("pool", lambda e: e.tensor_scalar(out=a_t[:], in0=a_t[:], scalar1=ALPHA, scalar2=None, op0=ALU.mult),
                     reads=[a_b], writes=[a_b])
                pt, po, pb = next_ps()
                for k in range(8):
                    P.op("pe", lambda e: e.matmul(pt[:, po:po + 64], lhsT=f_t[:, k, :], rhs=wr[:, k, :], start=(k == 0), stop=(k == 7)),
                         reads=[f_b, wr_b], writes=[pb])
                if stop == "A2c":
                    P.finish()
                    raise _Stop(nc)
                _routing(P, pt, po, pb, gates[ti], bct, bct_b, rt[s], r8[s], n_exp)
            P.barrier()
            if stop == "A2":
                P.finish()
                raise _Stop(nc)
            P.es = old_es
        with ExitStack() as esB:
            old_es = P.es
            P.es = esB
            w1 = [P.sb("w1_%d" % i, [128, 8, 256], BF16) for i in range(2)]
            w3 = [P.sb("w3_%d" % i, [128, 8, 256], BF16) for i in range(2)]
            w2 = [P.sb("w2_%d" % i, [128, 2, 1024], BF16) for i in range(2)]
            sil = [P.sb("sil%d" % i, [128, 2, 512], F32) for i in range(2)]
            aT = [P.sb("aT%d" % i, [128, 2, 512], BF16) for i in range(2)]
            cnt = 0
            items = []
            for e_i in range(n_exp + 1):
                for bi, (t0, ntl) in enumerate(blocks):
                    items.append((e_i, t0, ntl, bi == 0))

            def stage1(it, cnt):
                e_i, t0, ntl, first = it
                s = e_i % 2
                w1_t, w1_b = w1[s]
                w3_t, w3_b = w3[s]
                w2_t, w2_b = w2[s]
                if first:
                    P.dma("pool", w1_t[:], w_e1[e_i].rearrange("(k p) f -> p k f", p=128), writes=[w1_b])
                    P.dma("pool", w3_t[:], w_e3[e_i].rearrange("(k p) f -> p k f", p=128), writes=[w3_b])
                    P.dma("pool", w2_t[:], w_e2[e_i].rearrange("(k p) f -> p k f", p=128), writes=[w2_b])
                W = ntl * 128
                c0 = t0 * 128
                hb = [hT_bufs[t0 + i] for i in range(ntl)]
                h1 = [next_ps() for _ in range(2)]
                h3 = [next_ps() for _ in range(2)]
                for f in range(2):
                    pt, po, pb = h1[f]
                    for k in range(8):
                        P.op("pe", lambda e: e.matmul(pt[:, po:po + W], lhsT=w1_t[:, k, f * 128:(f + 1) * 128], rhs=hT[:, k, c0:c0 + W],
                                                      start=(k == 0), stop=(k == 7)), reads=[w1_b] + hb, writes=[pb])
                    pt3, po3, pb3 = h3[f]
                    for k in range(8):
                        P.op("pe", lambda e: e.matmul(pt3[:, po3:po3 + W], lhsT=w3_t[:, k, f * 128:(f + 1) * 128], rhs=hT[:, k, c0:c0 + W],
                                                      start=(k == 0), stop=(k == 7)), reads=[w3_b] + hb, writes=[pb3])
                sl_t, sl_b = sil[cnt % 2]
                at_t, at_b = aT[cnt % 2]
                for f in range(2):
                    pt, po, pb = h1[f]
                    P.op("act", lambda e: e.activation(out=sl_t[:, f, :W], in_=pt[:, po:po + W], func=AF.Silu), reads=[pb], writes=[sl_b])
                for f in range(2):
                    pt3, po3, pb3 = h3[f]
                    P.op("dve", lambda e: e.tensor_tensor(out=at_t[:, f, :W], in0=pt3[:, po3:po3 + W], in1=sl_t[:, f, :W], op=ALU.mult),
                         reads=[pb3, sl_b], writes=[at_b])
                return (at_t, at_b, w2_t, w2_b)

            def stage2(it, st):
                e_i, t0, ntl, first = it
                at_t, at_b, w2_t, w2_b = st
                for tl in range(ntl):
                    ti = t0 + tl
                    a_t, a_b = acc[ti]
                    g_t, g_b = gates[ti]
                    for hh in range(2):
                        pt, po, pb = next_ps()
                        for f in range(2):
                            P.op("pe", lambda e: e.matmul(pt[:, po:po + 512], lhsT=at_t[:, f, tl * 128:(tl + 1) * 128],
                                                          rhs=w2_t[:, f, hh * 512:(hh + 1) * 512], start=(f == 0), stop=(f == 1)),
                                 reads=[at_b, w2_b], writes=[pb])
                        P.op("dve", lambda e: e.scalar_tensor_tensor(out=a_t[:, hh * 512:(hh + 1) * 512], in0=pt[:, po:po + 512],
                                                                     scalar=g_t[:, e_i:e_i + 1], in1=a_t[:, hh * 512:(hh + 1) * 512],
                                                                     op0=ALU.mult, op1=ALU.add), reads=[pb, g_b, a_b], writes=[a_b])
            st = stage1(items[0], 0)
            for i in range(len(items)):
                nxt = stage1(items[i + 1], i + 1) if i + 1 < len(items) else None
                stage2(items[i], st)
                st = nxt
            P.barrier()
            if stop == "B":
                P.finish()
                raise _Stop(nc)
            P.es = old_es
        with ExitStack() as esC:
            old_es = P.es
            P.es = esC
            ot = [P.sb("ot%d" % i, [128, 1024], F32) for i in range(2)]
            st6 = [P.sb("cst6_%d" % i, [128, 2, 6], F32) for i in range(2)]
            mv = [P.sb("cmv%d" % i, [128, 2], F32) for i in range(2)]
            rs = [P.sb("crs%d" % i, [128, 2], F32) for i in range(2)]
            for ti in range(NT_TILES):
                s = ti % 2
                a_t, a_b = acc[ti]
                o_t, o_b = ot[s]
                _layer_norm(P, a_t, a_b, o_t, o_b, bct, bct_b, 3, 4, st6[s], mv[s], rs[s])
                P.dma("sp", y[ti * 128:(ti + 1) * 128, :], o_t[:], reads=[o_b])
            P.finish()
            P.es = old_es
    return nc


def _layer_norm(P, x_t, x_b, o_t, o_b, bct, bct_b, gi, bi, st6, mv, rs):
    s_t, s_b = st6
    m_t, m_b = mv
    r_t, r_b = rs
    for hh in range(2):
        P.op("dve", lambda e, hh=hh: e.bn_stats(out=s_t[:, hh, :], in_=x_t[:, hh * 512:(hh + 1) * 512]), reads=[x_b], writes=[s_b])
    P.op("dve", lambda e: e.bn_aggr(out=m_t[:], in_=s_t[:].rearrange("p a b -> p (a b)")), reads=[s_b], writes=[m_b])
    P.op("dve", lambda e: e.tensor_scalar(out=r_t[:, 0:1], in0=m_t[:, 1:2], scalar1=NORM_EPS, scalar2=None, op0=ALU.add), reads=[m_b], writes=[r_b])
    P.op("act", lambda e: e.activation(out=r_t[:, 1:2], in_=r_t[:, 0:1], func=AF.Sqrt), reads=[r_b], writes=[r_b])
    P.op("dve", lambda e: e.reciprocal(out=r_t[:, 0:1], in_=r_t[:, 1:2]), reads=[r_b], writes=[r_b])
    P.op("dve", lambda e: e.tensor_scalar(out=o_t[:], in0=x_t[:], scalar1=m_t[:, 0:1], scalar2=r_t[:, 0:1], op0=ALU.subtract, op1=ALU.mult),
         reads=[x_b, m_b, r_b], writes=[o_b])
    P.op("pool", lambda e: e.tensor_tensor(out=o_t[:], in0=o_t[:], in1=bct[:, gi, :], op=ALU.mult), reads=[o_b, bct_b], writes=[o_b])
    P.op("pool", lambda e: e.tensor_tensor(out=o_t[:], in0=o_t[:], in1=bct[:, bi, :], op=ALU.add), reads=[o_b, bct_b], writes=[o_b])


def _routing(P, pt, po, pb, gate, bct, bct_b, rt, r8, n_exp):
    g_t, g_b = gate
    t_t, t_b = rt
    q_t, q_b = r8
    sc = t_t[:, 0, :]
    bi = t_t[:, 1, :]
    tmp = t_t[:, 2, :]
    msk = t_t[:, 3, :]
    sel = t_t[:, 4, :]
    v3 = lambda ap: ap.rearrange("p (a b) -> p a b", a=8)
    P.op("act", lambda e: e.activation(out=sc, in_=pt[:, po:po + 64], func=AF.Sigmoid), reads=[pb], writes=[t_b])
    P.op("dve", lambda e: e.tensor_tensor(out=bi, in0=sc, in1=bct[:, 5, 0:64], op=ALU.add), reads=[t_b, bct_b], writes=[t_b])
    P.op("dve", lambda e: e.tensor_reduce(out=q_t[:, 0, :], in_=v3(bi), axis=AX.X, op=ALU.max), reads=[t_b], writes=[q_b])
    P.op("dve", lambda e: e.tensor_tensor(out=v3(tmp), in0=v3(bi), in1=q_t[:, 0, :].unsqueeze(2).to_broadcast([128, 8, 8]), op=ALU.is_equal),
         reads=[t_b, q_b], writes=[t_b])
    P.op("dve", lambda e: e.scalar_tensor_tensor(out=tmp, in0=tmp, scalar=-BIG, in1=bi, op0=ALU.mult, op1=ALU.add), reads=[t_b], writes=[t_b])
    P.op("dve", lambda e: e.tensor_reduce(out=q_t[:, 1, :], in_=v3(tmp), axis=AX.X, op=ALU.max), reads=[t_b], writes=[q_b])
    P.op("dve", lambda e: e.tensor_tensor(out=q_t[:, 2, :], in0=q_t[:, 0, :], in1=q_t[:, 1, :], op=ALU.add), reads=[q_b], writes=[q_b])
    P.op("dve", lambda e: e.max(out=q_t[:, 3, :], in_=q_t[:, 2, :]), reads=[q_b], writes=[q_b])
    P.op("dve", lambda e: e.tensor_scalar(out=q_t[:, 4, :], in0=q_t[:, 2, :], scalar1=q_t[:, 3, 3:4], scalar2=None, op0=ALU.is_ge), reads=[q_b], writes=[q_b])
    P.op("dve", lambda e: e.tensor_scalar(out=q_t[:, 4, :], in0=q_t[:, 4, :], scalar1=1.0, scalar2=BIG, op0=ALU.subtract, op1=ALU.mult), reads=[q_b], writes=[q_b])
    P.op("dve", lambda e: e.tensor_tensor(out=v3(msk), in0=v3(bi), in1=q_t[:, 4, :].unsqueeze(2).to_broadcast([128, 8, 8]), op=ALU.add),
         reads=[t_b, q_b], writes=[t_b])
    P.op("dve", lambda e: e.max(out=q_t[:, 5, :], in_=msk), reads=[t_b], writes=[q_b])
    P.op("dve", lambda e: e.tensor_scalar(out=sel, in0=msk, scalar1=q_t[:, 5, 7:8], scalar2=None, op0=ALU.is_ge), reads=[t_b, q_b], writes=[t_b])
    P.op("dve", lambda e: e.tensor_tensor(out=sel, in0=sel, in1=sc, op=ALU.mult), reads=[t_b], writes=[t_b])
    P.op("dve", lambda e: e.tensor_reduce(out=q_t[:, 6, 0:1], in_=sel, axis=AX.X, op=ALU.add), reads=[t_b], writes=[q_b])
    P.op("dve", lambda e: e.reciprocal(out=q_t[:, 6, 1:2], in_=q_t[:, 6, 0:1]), reads=[q_b], writes=[q_b])
    P.op("dve", lambda e: e.tensor_scalar(out=g_t[:, 0:n_exp], in0=sel, scalar1=q_t[:, 6, 1:2], scalar2=ROUTE_SCALE, op0=ALU.mult, op1=ALU.mult),
         reads=[t_b, q_b], writes=[g_b])
    P.op("dve", lambda e: e.memset(g_t[:, n_exp:n_exp + 1], 1.0), reads=[], writes=[g_b])


NEG = -240000.0
COLS_A = [128] * 10 + [64] * 4 + [128] * 3
OFF_A = [int(v) for v in np.cumsum([0] + COLS_A)]


def _nsa_tables(S):
    h = np.arange(1, 17, dtype=np.float64)
    slopes = 2.0 ** (-8.0 * h / 16.0)
    import ml_dtypes
    bf = lambda a: np.asarray(a, np.float32).astype(ml_dtypes.bfloat16).astype(np.float32)
    t = {}
    pos = np.arange(S)
    ka = np.zeros((7, S), np.float32)
    ka[0] = ka[1] = (pos // 128) * 128
    ka[2] = ka[3] = pos % 128
    ka[4:] = 1.0
    t["kaug"] = ka
    nn = np.arange(512)
    ends = nn * 16 + 31
    kc = np.zeros((7, 512), np.float32)
    kc[0] = kc[1] = (ends // 128) * 128
    kc[2] = kc[3] = ends % 128
    kc[4:] = 1.0
    t["kaug_c"] = kc
    qa = np.zeros((4, 7, 4, S), np.float32)
    for g in range(4):
        for j in range(4):
            sl = np.float32(slopes[g * 4 + j] * 8.0)
            sh = bf(sl)
            slo = bf(sl - sh)
            Q = (-(np.float64(sh) + np.float64(slo)) * pos).astype(np.float32)
            qh = bf(Q)
            ql = bf(Q - qh)
            qll = bf(Q - qh - ql)
            qa[g, 0, j] = sh
            qa[g, 1, j] = slo
            qa[g, 2, j] = sh
            qa[g, 3, j] = slo
            qa[g, 4, j] = qh
            qa[g, 5, j] = ql
            qa[g, 6, j] = qll
    t["qaug"] = qa
    i = np.arange(128)[:, None]
    rq = np.arange(128)[None, :]
    t["caus4"] = np.tile(np.where(i <= rq, 0.0, NEG).astype(np.float32), (1, 4))
    t["low4"] = np.tile(np.where(i > rq, 0.0, NEG).astype(np.float32), (1, 4))
    y = np.arange(S)[None, :]
    t["EW"] = (y // 64 == np.arange(128)[:, None]).astype(np.float32)
    yy = np.arange(2432)[None, :]
    t["Zm"] = np.where(yy >= 16 * i + 128, 0.0, NEG).astype(np.float32)
    yf = np.arange(256)[None, :]
    rel = yf - 126
    c = (np.arange(128)[:, None] >= 64).astype(np.int64)
    F = np.zeros((128, 256), np.float32)
    F[(rel == c) | (rel == c - 1)] = 1e30
    F[rel > c] = -1e30
    t["F"] = F
    n = np.arange(512)[:, None] * 16
    m = np.arange(128)[None, :] * 64
    t["ov"] = ((n < m + 64) & (n + 32 > m)).astype(np.float32).reshape(4, 128, 128).transpose(1, 0, 2).copy()
    t["U"] = (i <= rq).astype(np.float32)
    t["ones"] = np.ones((128, 128), np.float32)
    t["ident"] = np.eye(128, dtype=np.float32)
    return t


def build_l1(S, NS, NPG=16, stop=None, nphys=None):
    try:
        return _build_l1(S, NS, NPG, stop, nphys)
    except _Stop as ex:
        return ex.args[0]


def _build_l1(S, NS, NPG, stop, nphys):
    BW = 256
    nc = bass.Bass("TRN2", target_bir_lowering=False)
    dr = lambda name, shape, dt=F32, kind="ExternalInput": nc.dram_tensor(name, list(shape), dt, kind=kind).ap()
    NTL = S // 128
    SS = NPG * 128 + 128
    NTS = SS // 128
    NPHYS = nphys or (128 * NPG * 5 + 3) // 4
    xT = dr("xT", [1024, S])
    wA = dr("wA", [1024, OFF_A[-1]])
    bA = dr("bA", [128, 17])
    wT = dr("wT", [1024, 652])
    bT = dr("bT", [128, 652])
    dtb = dr("dtb", [128, 512])
    alog = dr("alog", [128, 512])
    cw = dr("cw", [128, 6, 4])
    cb = dr("cb", [128, 6])
    dsk = dr("dsk", [128, 4])
    nw = dr("nw", [128, 4])
    w1kv = dr("w1kv", [128, 32, 128])
    pekv = dr("pekv", [128, 32])
    b1 = dr("b1", [128, 2])
    w2kv = dr("w2kv", [128, 128])
    b2k = dr("b2k", [64, 1])
    b2v = dr("b2v", [128, 64])
    kaug = dr("kaug", [7, max(S, SS)])
    kaug_c = dr("kaug_c", [7, 512])
    qaug = dr("qaug", [7, 4, max(S, SS)])
    caus4 = dr("caus4", [128, 512])
    low4 = dr("low4", [128, 512])
    EW = dr("EW", [128, max(S, SS)])
    Zm = dr("Zm", [128, 2432])
    Ftab = dr("F", [128, 256])
    ov = dr("ov", [128, 4, 128])
    Ut = dr("U", [128, 128])
    ones_d = dr("ones", [128, 128])
    ident = dr("ident", [128, 128])
    xTs = dr("xTs", [1024, NS])
    cache = dr("cache", [NPHYS * 128, 256])
    pt_rep = dr("pt_rep", [128, NS * NPG], I32)
    kvwin_s = dr("kvwin_s", [NS, 512, 128])
    ssm_s = dr("ssm_s", [NS, 512, 128])
    convT_s = dr("convT_s", [3, 768, NS])
    colp = dr("colp", [128, 3, 4])
    yT = dr("yT", [768, S], kind="ExternalOutput")
    kvT = dr("kvT", [256, S], kind="ExternalOutput")
    winT = dr("winT", [128, 512], kind="ExternalOutput")
    ssm_p = dr("ssm_p", [512, 128], kind="ExternalOutput")
    convT_p = dr("convT_p", [768, 3], kind="ExternalOutput")
    ysT = dr("ysT", [512, NS], kind="ExternalOutput")
    ynS = dr("ynS", [NS, 256], kind="ExternalOutput")
    kvS = dr("kvS", [256, NS], kind="ExternalOutput")
    winS = dr("winS", [NS, 511, 128], kind="ExternalOutput")
    ssm_o = dr("ssm_o", [NS, 512, 128], kind="ExternalOutput")
    convS = dr("convS", [3, 768, NS], kind="ExternalOutput")
    winnewT = dr("winnewT", [128, NS], kind="ExternalOutput")

    with ExitStack() as es:
        P = Prog(nc, es)
        op = P.op
        pst = [P.ps("ps%d" % i, [128, 1024], F32) for i in range(4)]
        psh = []
        for i in range(4):
            psh.append((pst[i][0], 0, P.buf("psb%da" % i)))
            psh.append((pst[i][0], 512, P.buf("psb%db" % i)))
        rr = [0]

        def next_ps():
            r = psh[6 + rr[0] % 2]
            rr[0] += 1
            return r

        def const(name, src, shape, dt=F32, q="sp"):
            t_, b_ = P.sb(name, shape, dt)
            P.dma("pool" if dt != F32 else q, t_[:], src, writes=[b_])
            return t_, b_

        U_t, U_b = const("U", Ut, [128, 128])
        one_t, one_b = const("ones", ones_d, [128, 128])
        id_t, id_b = const("identf", ident, [128, 128])
        idb_t, idb_b = const("identb", ident, [128, 128], BF16)
        caus_t, caus_b = const("caus4", caus4, [128, 512], BF16)
        low_t, low_b = const("low4", low4, [128, 512], BF16)
        EW_t, EW_b = const("EW", EW, [128, max(S, SS)], BF16)
        Zm_t, Zm_b = const("Zm", Zm, [128, 2432], BF16)
        F_t, F_b = const("Ft", Ftab, [128, 256])
        wA_t, wA_b = P.sb("wA", [128, 8, OFF_A[-1]], BF16)
        for k in range(8):
            P.dma("pool", wA_t[:, k, :], wA[k * 128:(k + 1) * 128, :], writes=[wA_b])
        wT_t, wT_b = P.sb("wT", [128, 8, 652], BF16)
        for k in range(8):
            P.dma("pool", wT_t[:, k, :], wT[k * 128:(k + 1) * 128, :], writes=[wT_b])
        bA_t, bA_b = const("bA", bA, [128, 17])
        bT_t, bT_b = const("bT", bT, [128, 652])
        dtb_t, dtb_b = const("dtb", dtb, [128, 512])
        A_t, A_b = const("Arep", alog, [128, 512])
        cw_t, cw_b = const("cw", cw, [128, 6, 4])
        cb_t, cb_b = const("cb", cb, [128, 6])
        dsk_t, dsk_b = const("dsk", dsk, [128, 4])
        nw_t, nw_b = const("nw", nw, [128, 4])
        w1_t, w1_b = const("w1kv", w1kv, [128, 32, 128], BF16)
        pe_t, pe_b = const("pekv", pekv, [128, 32])
        b1_t, b1_b = const("b1", b1, [128, 2])
        w2_t, w2_b = const("w2kv", w2kv, [128, 128], BF16)
        b2k_t, b2k_b = const("b2k", b2k, [64, 1])
        b2v_t, b2v_b = const("b2v", b2v, [128, 64])
        op("act", lambda e: e.activation(out=A_t[:], in_=A_t[:], func=AF.Exp), reads=[A_b], writes=[A_b])
        op("dve", lambda e: e.tensor_scalar(out=A_t[:], in0=A_t[:], scalar1=-1.0, scalar2=None, op0=ALU.mult), reads=[A_b], writes=[A_b])
        op("dve", lambda e: e.tensor_tensor(out=dtb_t[:], in0=dtb_t[:], in1=bT_t[:, 0:512], op=ALU.add), reads=[dtb_b, bT_b], writes=[dtb_b])
        b1p_t, b1p_b = P.sb("b1p", [128, 2], F32)
        es_tmp = ExitStack()
        P.es = es_tmp
        w1f_t, w1f_b = const("w1kvf", w1kv, [128, 32, 128])
        P.es = es
        for tkv in range(2):
            pt, po, pb = next_ps()
            for l in range(32):
                op("pe", lambda e: e.matmul(pt[:, po:po + 1], lhsT=w1f_t[tkv * 64:(tkv + 1) * 64, l, :], rhs=pe_t[tkv * 64:(tkv + 1) * 64, l:l + 1],
                                            start=(l == 0), stop=(l == 31)), reads=[w1f_b, pe_b], writes=[pb])
            op("dve", lambda e: e.tensor_tensor(out=b1p_t[:, tkv:tkv + 1], in0=pt[:, po:po + 1], in1=b1_t[:, tkv:tkv + 1], op=ALU.add),
               reads=[pb, b1_b], writes=[b1p_b])
        P.barrier()
        es_tmp.close()

        def alloc_kv(pref, Sx, NCx):
            d = {}
            d["Sx"] = Sx
            d["kvc"] = P.sb(pref + "kvc", [128, Sx], BF16)
            d["ksl"] = P.sb(pref + "ksl", [71, Sx], BF16)
            d["kwn"] = P.sb(pref + "kwn", [71, 1024], BF16)
            d["vsl"] = P.sb(pref + "vsl", [128, Sx // 128, 65], BF16)
            d["vwn"] = P.sb(pref + "vwn", [128, 8, 65], BF16)
            d["kc"] = P.sb(pref + "kc", [71, NCx * 128], BF16)
            d["vc"] = P.sb(pref + "vc", [128, NCx, 193], BF16)
            d["qa"] = P.sb(pref + "qa", [71, 4, 128], BF16)
            d["NCx"] = NCx
            P.dma("pool", d["ksl"][0][64:71, :], kaug[:, 0:Sx], writes=[d["ksl"][1]])
            P.dma("pool", d["kc"][0][64:71, :], kaug_c[:, 0:NCx * 128], writes=[d["kc"][1]])
            P.dma("pool", d["vc"][0][:, :, 65:193], ov[:, 0:NCx, :], writes=[d["vc"][1]])
            op("dve", lambda e: e.memset(d["vsl"][0][:, :, 64:65], 1.0), writes=[d["vsl"][1]])
            op("dve", lambda e: e.memset(d["vwn"][0][:, :, 64:65], 1.0), writes=[d["vwn"][1]])
            op("dve", lambda e: e.memset(d["vc"][0][:, :, 64:65], 1.0), writes=[d["vc"][1]])
            return d

        scr = {}
        scr["hid"] = P.sb("hid", [128, 512], BF16)
        scr["pT"] = [P.sb("pT%d" % i, [128, 512], BF16) for i in range(3)]
        scr["imp"] = P.sb("imp", [128, 4, 128], F32)
        scr["sadd"] = P.sb("sadd", [128, 512], F32)
        scr["m8"] = P.sb("m8", [128, 16], F32)
        scr["nst4"] = P.sb("nst4", [128, 4, 128], BF16)
        scr["rd"] = P.sb("rd", [128, 16], F32)
        scr["osb"] = P.sb("osb", [128, 3, 4, 64], F32)
        scr["ny"] = P.sb("ny", [128, 256], F32)
        scr["nyT"] = P.sb("nyT", [128, 2, 128], F32)
        pcnt = [0]

        def compress(kv, n0, nn):
            kvc_t, kvc_b = kv["kvc"]
            kc_t, kc_b = kv["kc"]
            vc_t, vc_b = kv["vc"]
            h_t, h_b = scr["hid"]
            for tkv in range(2):
                pt, po, pb = next_ps()
                lo = tkv * 64
                for l in range(32):
                    rhs = kvc_t[lo:lo + 64, n0 * 16 + l:n0 * 16 + l + (nn - 1) * 16 + 1:16]
                    op("pe", lambda e: e.matmul(pt[:, po:po + nn], lhsT=w1_t[lo:lo + 64, l, :], rhs=rhs, start=(l == 0), stop=(l == 31)),
                       reads=[w1_b, kvc_b], writes=[pb])
                op("act", lambda e: e.activation(out=h_t[:, :nn], in_=pt[:, po:po + nn], func=AF.Silu, bias=b1p_t[:, tkv:tkv + 1], scale=1.0),
                   reads=[pb, b1p_b], writes=[h_b])
                if tkv == 0:
                    p2, o2, b2 = next_ps()
                    op("pe", lambda e: e.matmul(p2[0:64, o2:o2 + nn], lhsT=w2_t[:, 0:64], rhs=h_t[:, :nn], start=True, stop=True),
                       reads=[w2_b, h_b], writes=[b2])
                    op("act", lambda e: e.activation(out=kc_t[0:64, n0:n0 + nn], in_=p2[0:64, o2:o2 + nn], func=AF.Identity, bias=b2k_t[:, 0:1], scale=1.0),
                       reads=[b2, b2k_b], writes=[kc_b])
                else:
                    for c0 in range(0, nn, 128):
                        w = min(128, nn - c0)
                        assert (n0 + c0) % 128 == 0
                        p2, o2, b2 = next_ps()
                        op("pe", lambda e: e.matmul(p2[0:w, o2:o2 + 64], lhsT=h_t[:, c0:c0 + w], rhs=w2_t[:, 64:128], start=True, stop=True),
                           reads=[w2_b, h_b], writes=[b2])
                        op("dve", lambda e: e.tensor_tensor(out=vc_t[0:w, (n0 + c0) // 128, 0:64], in0=p2[0:w, o2:o2 + 64], in1=b2v_t[0:w, :], op=ALU.add),
                           reads=[b2, b2v_b], writes=[vc_b])

        def attn_branch(kv, t, kname, vname, ktiles, mask_fn, acc, ncols, vcols):
            k_t, k_b = kv[kname]
            v_t, v_b = kv[vname]
            qa_t, qa_b = kv["qa"]
            n = len(ktiles)

            def qk(idx):
                kt = ktiles[idx]
                kk = kt % 8 if kname == "kwn" else kt
                pt, po, pb = next_ps()
                extra = mask_fn(kt)
                mm_extra = [x for x in extra if x[0] == "mm"]
                post = [x for x in extra if x[0] == "mul"]
                pre_add = [x for x in extra if x[0] == "add"]
                op("pe", lambda e: e.matmul(pt[:, po:po + 512], lhsT=k_t[0:71, kk * 128:(kk + 1) * 128], rhs=qa_t[0:71, :, :],
                                            start=True, stop=(len(mm_extra) == 0)), reads=[k_b, qa_b], writes=[pb])
                for i_, (_, l_, r_, rd_) in enumerate(mm_extra):
                    op("pe", lambda e: e.matmul(pt[:, po:po + 512], lhsT=l_, rhs=r_, start=False, stop=(i_ == len(mm_extra) - 1)),
                       reads=rd_, writes=[pb])
                p_t, p_b = scr["pT"][pcnt[0] % 3]
                pcnt[0] += 1
                if pre_add:
                    sc_t, sc_b = scr["sadd"]
                    (_, m_ap, rd_) = pre_add[0]
                    op("dve", lambda e: e.tensor_tensor(out=sc_t[:].rearrange("p (h q) -> p h q", h=4), in0=pt[:, po:po + 512].rearrange("p (h q) -> p h q", h=4),
                                                        in1=m_ap, op=ALU.add), reads=[pb] + rd_, writes=[sc_b])
                    op("act", lambda e: e.activation(out=p_t[:], in_=sc_t[:], func=AF.Exp, scale=0.125), reads=[sc_b], writes=[p_b])
                else:
                    op("act", lambda e: e.activation(out=p_t[:], in_=pt[:, po:po + 512], func=AF.Exp, scale=0.125), reads=[pb], writes=[p_b])
                for (_, m_ap, rd_) in post:
                    op("dve", lambda e: e.tensor_tensor(out=p_t[:].rearrange("p (h q) -> p h q", h=4), in0=p_t[:].rearrange("p (h q) -> p h q", h=4),
                                                        in1=m_ap, op=ALU.mult), reads=[p_b] + rd_, writes=[p_b])
                return (kk, p_t, p_b)

            def pv(idx, st):
                kk, p_t, p_b = st
                for h in range(4):
                    at, ao, ab = acc[h // 2]
                    co = ao + (h % 2) * ncols
                    op("pe", lambda e: e.matmul(at[:, co:co + vcols], lhsT=p_t[:, h * 128:(h + 1) * 128], rhs=v_t[:, kk, 0:vcols],
                                                start=(idx == 0 and h % 2 == 0), stop=(idx == n - 1 and h % 2 == 1)), reads=[p_b, v_b], writes=[ab])
            st = qk(0)
            for idx in range(n):
                nxt = qk(idx + 1) if idx + 1 < n else None
                pv(idx, st)
                st = nxt

        def nsa_qtile(kv, t, gate_ap, gate_b, out_fn):
            i_t, i_b = scr["imp"]
            m_t, m_b = scr["m8"]
            n_t, n_b = scr["nst4"]
            r_t, r_b = scr["rd"]
            o_t, o_b = scr["osb"]
            nvalid = min(8 * t + 7, kv["NCx"] * 128)
            cchunks = list(range((nvalid + 127) // 128))
            accc = [psh[0], psh[1]]

            def cmask(c):
                K = 2048 * c - 128 * t + 31
                if 16 * 127 + K <= 0:
                    return []
                o = 128 - K
                assert 0 <= o <= 2432 - 128
                return [("add", Zm_t[:, o:o + 128].unsqueeze(1).to_broadcast([128, 4, 128]), [Zm_b])]
            if cchunks:
                attn_branch(kv, t, "kc", "vc", cchunks, cmask, accc, 193, 193)
            else:
                for (at, ao, ab) in accc:
                    op("dve", lambda e: e.memset(at[:, ao:ao + 386], 0.0), writes=[ab])
            for h in range(4):
                at, ao, ab = accc[h // 2]
                co = ao + (h % 2) * 193
                op("dve", lambda e: e.tensor_scalar(out=r_t[:, h:h + 1], in0=at[:, co + 64:co + 65], scalar1=1e-30, scalar2=None, op0=ALU.max),
                   reads=[ab], writes=[r_b])
            op("dve", lambda e: e.reciprocal(out=r_t[:, 0:4], in_=r_t[:, 0:4]), reads=[r_b], writes=[r_b])
            for h in range(4):
                at, ao, ab = accc[h // 2]
                co = ao + (h % 2) * 193
                if h == 0:
                    op("dve", lambda e: e.tensor_scalar(out=i_t[:, 0, :], in0=at[:, co + 65:co + 193], scalar1=r_t[:, 0:1], scalar2=None, op0=ALU.mult),
                       reads=[ab, r_b], writes=[i_b])
                else:
                    op("dve", lambda e: e.scalar_tensor_tensor(out=i_t[:, 0, :], in0=at[:, co + 65:co + 193], scalar=r_t[:, h:h + 1], in1=i_t[:, 0, :],
                                                               op0=ALU.mult, op1=ALU.add), reads=[ab, r_b, i_b], writes=[i_b])
                op("dve", lambda e: e.tensor_copy(out=o_t[:, 0, h, :], in_=at[:, co:co + 64]), reads=[ab], writes=[o_b])
            o2 = 126 - 2 * t
            op("dve", lambda e: e.tensor_tensor(out=i_t[:, 0, :], in0=i_t[:, 0, :], in1=F_t[:, o2:o2 + 128], op=ALU.add), reads=[i_b, F_b], writes=[i_b])
            op("dve", lambda e: e.memset(i_t[:, 0, 0:1], 1e30), writes=[i_b])
            op("dve", lambda e: e.max(out=m_t[:, 0:8], in_=i_t[:, 0, :]), reads=[i_b], writes=[m_b])
            op("dve", lambda e: e.match_replace(out=i_t[:, 1, :], in_to_replace=m_t[:, 0:8], in_values=i_t[:, 0, :], imm_value=-3.0e38),
               reads=[i_b, m_b], writes=[i_b])
            op("dve", lambda e: e.max(out=m_t[:, 8:16], in_=i_t[:, 1, :]), reads=[i_b], writes=[m_b])
            op("dve", lambda e: e.tensor_scalar(out=m_t[:, 15:16], in0=m_t[:, 15:16], scalar1=-1e29, scalar2=None, op0=ALU.max), reads=[m_b], writes=[m_b])
            op("dve", lambda e: e.tensor_scalar(out=i_t[:, 2, :], in0=i_t[:, 0, :], scalar1=m_t[:, 15:16], scalar2=None, op0=ALU.is_ge),
               reads=[i_b, m_b], writes=[i_b])
            op("dve", lambda e: e.tensor_scalar(out=i_t[:, 2, :], in0=i_t[:, 2, :], scalar1=1.0, scalar2=-NEG, op0=ALU.subtract, op1=ALU.mult),
               reads=[i_b], writes=[i_b])
            pt, po, pb = next_ps()
            op("pe", lambda e: e.transpose(out=pt[:, po:po + 128], in_=i_t[:, 2, :], identity=id_t[:]), reads=[i_b, id_b], writes=[pb])
            op("dve", lambda e: e.tensor_copy(out=n_t[:], in_=pt[:, po:po + 128].unsqueeze(1).to_broadcast([128, 4, 128])), reads=[pb], writes=[n_b])
            accs = [psh[2], psh[3]]

            def smask(kt):
                r = [("mm", EW_t[:, kt * 128:(kt + 1) * 128], n_t[:].rearrange("p h q -> p (h q)"), [EW_b, n_b])]
                if kt == t:
                    r.append(("mm", idb_t[:], caus_t[:], [idb_b, caus_b]))
                return r
            attn_branch(kv, t, "ksl", "vsl", list(range(t + 1)), smask, accs, 65, 65)
            accw = [psh[4], psh[5]]

            def wmask(kt):
                if kt == t:
                    return [("mm", idb_t[:], caus_t[:], [idb_b, caus_b])]
                if kt == t - 4:
                    return [("mm", idb_t[:], low_t[:], [idb_b, low_b])]
                return []
            attn_branch(kv, t, "kwn", "vwn", [k_ for k_ in range(t - 4, t + 1) if k_ >= 0], wmask, accw, 65, 65)
            for bi_, acc_ in ((1, accs), (2, accw)):
                for h in range(4):
                    at, ao, ab = acc_[h // 2]
                    co = ao + (h % 2) * 65
                    op("dve", lambda e: e.tensor_copy(out=r_t[:, bi_ * 4 + h:bi_ * 4 + h + 1], in_=at[:, co + 64:co + 65]), reads=[ab], writes=[r_b])
                    op("dve", lambda e: e.tensor_copy(out=o_t[:, bi_, h, :], in_=at[:, co:co + 64]), reads=[ab], writes=[o_b])
            op("dve", lambda e: e.reciprocal(out=r_t[:, 4:12], in_=r_t[:, 4:12]), reads=[r_b], writes=[r_b])
            op("dve", lambda e: e.tensor_tensor(out=r_t[:, 0:12].rearrange("p (b h) -> p b h", b=3), in0=r_t[:, 0:12].rearrange("p (b h) -> p b h", b=3),
                                                in1=gate_ap.rearrange("p (h b) -> p b h", b=3), op=ALU.mult), reads=[r_b, gate_b], writes=[r_b])
            y_t, y_b = scr["ny"]
            for h in range(4):
                op("dve", lambda e: e.tensor_scalar(out=y_t[:, h * 64:(h + 1) * 64], in0=o_t[:, 0, h, :], scalar1=r_t[:, h:h + 1], scalar2=None, op0=ALU.mult),
                   reads=[o_b, r_b], writes=[y_b])
                for bi_ in (1, 2):
                    op("dve", lambda e: e.scalar_tensor_tensor(out=y_t[:, h * 64:(h + 1) * 64], in0=o_t[:, bi_, h, :], scalar=r_t[:, bi_ * 4 + h:bi_ * 4 + h + 1],
                                                               in1=y_t[:, h * 64:(h + 1) * 64], op0=ALU.mult, op1=ALU.add), reads=[o_b, r_b, y_b], writes=[y_b])
            out_fn(y_t, y_b)

        ssd = {}
        for nm, shp, dt_ in (("sq", [128, 4, 128], F32), ("rstd", [128, 128], F32), ("yo", [128, 4, 128], F32)):
            ssd[nm] = P.sb("ssd_" + nm, shp, dt_)
        def ssd_chunk(c, blk, col0):
            cs = slice(col0, col0 + 128)
            xTb_t, xTb_b = blk["xTb"]
            T = lambda nm: ssd[nm][0]
            Bf = lambda nm: ssd[nm][1]
            pt, po, pb = psh[0]
            for k in range(8):
                op("pe", lambda e: e.matmul(pt[:, po:po + 512], lhsT=xTb_t[:, k, cs], rhs=wT_t[:, k, 0:512], start=(k == 0), stop=(k == 7)),
                   reads=[xTb_b, wT_b], writes=[pb])
            op("dve", lambda e: e.tensor_tensor(out=T("dt")[:], in0=pt[:, po:po + 512], in1=dtb_t[:], op=ALU.add), reads=[pb, dtb_b], writes=[Bf("dt")])
            op("act", lambda e: e.activation(out=T("dt")[:], in_=T("dt")[:], func=AF.Exp), reads=[Bf("dt")], writes=[Bf("dt")])
            op("act", lambda e: e.activation(out=T("dt")[:], in_=T("dt")[:], func=AF.Ln, bias=1.0, scale=1.0), reads=[Bf("dt")], writes=[Bf("dt")])
            op("dve", lambda e: e.tensor_tensor(out=T("dtA")[:], in0=T("dt")[:], in1=A_t[:], op=ALU.mult), reads=[Bf("dt"), A_b], writes=[Bf("dtA")])
            p1, o1, b1_ = psh[1]
            xs_t, xs_b = blk["xsT"]
            for k in range(4):
                op("pe", lambda e: e.transpose(out=p1[:, o1 + k * 128:o1 + (k + 1) * 128], in_=xs_t[:, k, cs], identity=id_t[:]),
                   reads=[xs_b, id_b], writes=[b1_])
            op("dve", lambda e: e.tensor_copy(out=T("xs")[:], in_=p1[:, o1:o1 + 512]), reads=[b1_], writes=[Bf("xs")])
            p2, o2, b2_ = psh[2]
            BT_t, BT_b = blk["BT"]
            op("pe", lambda e: e.transpose(out=p2[:, o2:o2 + 128], in_=BT_t[:, cs], identity=id_t[:]), reads=[BT_b, id_b], writes=[b2_])
            op("dve", lambda e: e.tensor_copy(out=T("Btb")[:], in_=p2[:, o2:o2 + 128]), reads=[b2_], writes=[Bf("Btb")])
            p3, o3, b3_ = psh[3]
            op("pe", lambda e: e.matmul(p3[:, o3:o3 + 512], lhsT=U_t[:], rhs=T("dtA")[:], start=True, stop=True), reads=[U_b, Bf("dtA")], writes=[b3_])
            p4, o4, b4_ = psh[4]
            op("pe", lambda e: e.matmul(p4[:, o4:o4 + 512], lhsT=one_t[:], rhs=T("dtA")[:], start=True, stop=True), reads=[one_b, Bf("dtA")], writes=[b4_])
            op("dve", lambda e: e.tensor_copy(out=T("acs")[:], in_=p3[:, o3:o3 + 512]), reads=[b3_], writes=[Bf("acs")])
            op("dve", lambda e: e.tensor_tensor(out=T("w")[:], in0=p4[:, o4:o4 + 512], in1=T("acs")[:], op=ALU.subtract), reads=[b4_, Bf("acs")], writes=[Bf("w")])
            op("act", lambda e: e.activation(out=T("w")[:], in_=T("w")[:], func=AF.Exp), reads=[Bf("w")], writes=[Bf("w")])
            op("act", lambda e: e.activation(out=T("dec")[:], in_=p4[:, o4:o4 + 512], func=AF.Exp), reads=[b4_], writes=[Bf("dec")])
            op("dve", lambda e: e.tensor_tensor(out=T("xdt")[:], in0=T("xs")[:], in1=T("dt")[:], op=ALU.mult), reads=[Bf("xs"), Bf("dt")], writes=[Bf("xdt")])
            op("pool", lambda e: e.tensor_copy(out=T("xdtb")[:], in_=T("xdt")[:]), reads=[Bf("xdt")], writes=[Bf("xdtb")])
            op("dve", lambda e: e.tensor_tensor(out=T("xdtw")[:], in0=T("xdt")[:], in1=T("w")[:], op=ALU.mult), reads=[Bf("xdt"), Bf("w")], writes=[Bf("xdtw")])
            p5, o5, b5_ = psh[5]
            op("pe", lambda e: e.matmul(p5[:, o5:o5 + 512], lhsT=T("Btb")[:], rhs=T("xdtw")[:], start=True, stop=True), reads=[Bf("Btb"), Bf("xdtw")], writes=[b5_])
            p6, o6, b6_ = psh[6]
            CTb_t, CTb_b = blk["CTb"]
            BTb_t, BTb_b = blk["BTb"]
            for k in range(4):
                op("pe", lambda e: e.matmul(p6[:, o6 + k * 128:o6 + (k + 1) * 128], lhsT=T("hTb")[:, k * 128:(k + 1) * 128], rhs=CTb_t[:, cs], start=True, stop=True),
                   reads=[Bf("hTb"), CTb_b], writes=[b6_])
            p7, o7, b7_ = psh[7]
            for k in range(4):
                op("pe", lambda e: e.matmul(p7[:, o7 + k * 128:o7 + (k + 1) * 128], lhsT=T("dtA")[:, k * 128:(k + 1) * 128], rhs=U_t[:], start=True, stop=True),
                   reads=[Bf("dtA"), U_b], writes=[b7_])
            op("act", lambda e: e.activation(out=T("ea")[:].rearrange("p k l -> p (k l)"), in_=p7[:, o7:o7 + 512], func=AF.Exp), reads=[b7_], writes=[Bf("ea")])
            op("pe", lambda e: e.matmul(pt[:, po:po + 128], lhsT=BTb_t[:, cs], rhs=CTb_t[:, cs], start=True, stop=True), reads=[BTb_b, CTb_b], writes=[pb])
            op("dve", lambda e: e.tensor_tensor(out=T("cbm")[:], in0=pt[:, po:po + 128], in1=U_t[:], op=ALU.mult), reads=[pb, U_b], writes=[Bf("cbm")])
            dcol = T("dtA")[:].rearrange("p (j q) -> p j q", q=64)[:, :, 0:1]
            op("dve", lambda e: e.tensor_copy(out=T("L1")[:], in_=dcol.to_broadcast([128, 8, 128])), reads=[Bf("dtA")], writes=[Bf("L1")])
            op("dve", lambda e: e.scalar_tensor_tensor(out=T("L2")[:], in0=T("L1")[:], scalar=-1.0, in1=U_t[:].unsqueeze(1).to_broadcast([128, 8, 128]),
                                                       op0=ALU.mult, op1=ALU.mult), reads=[Bf("L1"), U_b], writes=[Bf("L2")])
            for half, (pd, od, bd) in enumerate((psh[1], psh[2])):
                for jj in range(4):
                    j = half * 4 + jj
                    op("pe", lambda e: e.matmul(pd[:, od + jj * 128:od + (jj + 1) * 128], lhsT=T("L1")[:, j, :], rhs=U_t[:], start=True, stop=False),
                       reads=[Bf("L1"), U_b], writes=[bd])
                    op("pe", lambda e: e.matmul(pd[:, od + jj * 128:od + (jj + 1) * 128], lhsT=T("L2")[:, j, :], rhs=one_t[:], start=False, stop=True),
                       reads=[Bf("L2"), one_b], writes=[bd])
                Ef = T("E")[:].rearrange("p k l -> p (k l)")
                op("dve", lambda e: e.tensor_scalar(out=Ef, in0=pd[:, od:od + 512], scalar1=0.0, scalar2=None, op0=ALU.min), reads=[bd], writes=[Bf("E")])
                op("act", lambda e: e.activation(out=Ef, in_=Ef, func=AF.Exp), reads=[Bf("E")], writes=[Bf("E")])
                op("dve", lambda e: e.tensor_tensor(out=T("Mj")[:, half * 4:(half + 1) * 4, :], in0=T("E")[:], in1=T("cbm")[:].unsqueeze(1).to_broadcast([128, 4, 128]),
                                                    op=ALU.mult), reads=[Bf("E"), Bf("cbm")], writes=[Bf("Mj")])
            for j in range(8):
                k, hf = j // 2, j % 2
                op("pe", lambda e: e.matmul(p3[hf * 64:(hf + 1) * 64, o3 + k * 128:o3 + (k + 1) * 128], lhsT=T("xdtb")[:, j * 64:(j + 1) * 64], rhs=T("Mj")[:, j, :],
                                            start=True, stop=True), reads=[Bf("xdtb"), Bf("Mj")], writes=[b3_])
            yf = T("y")[:].rearrange("p k l -> p (k l)")
            op("dve", lambda e: e.tensor_tensor(out=yf, in0=p6[:, o6:o6 + 512], in1=T("ea")[:].rearrange("p k l -> p (k l)"), op=ALU.mult),
               reads=[b6_, Bf("ea")], writes=[Bf("y")])
            op("dve", lambda e: e.tensor_tensor(out=yf, in0=p3[:, o3:o3 + 512], in1=yf, op=ALU.add), reads=[b3_, Bf("y")], writes=[Bf("y")])
            sz_t, sz_b = blk["szT"]
            for k in range(4):
                op("dve", lambda e: e.scalar_tensor_tensor(out=T("y")[:, k, :], in0=xs_t[:, k, cs], scalar=dsk_t[:, k:k + 1], in1=T("y")[:, k, :],
                                                           op0=ALU.mult, op1=ALU.add), reads=[xs_b, dsk_b, Bf("y")], writes=[Bf("y")])
            op("dve", lambda e: e.tensor_tensor(out=T("y")[:], in0=T("y")[:], in1=sz_t[:, :, cs], op=ALU.mult), reads=[Bf("y"), sz_b], writes=[Bf("y")])
            _rms_out(T("y"), Bf("y"), 128, lambda k: yT[k * 128:(k + 1) * 128, c * 128:(c + 1) * 128], p4, o4, b4_)
            op("dve", lambda e: e.tensor_tensor(out=T("tmp")[:], in0=T("hT")[:], in1=T("dec")[:], op=ALU.mult), reads=[Bf("hT"), Bf("dec")], writes=[Bf("tmp")])
            op("dve", lambda e: e.tensor_tensor(out=T("hT")[:], in0=p5[:, o5:o5 + 512], in1=T("tmp")[:], op=ALU.add), reads=[b5_, Bf("tmp")], writes=[Bf("hT")])
            op("pool", lambda e: e.tensor_copy(out=T("hTb")[:], in_=T("hT")[:]), reads=[Bf("hT")], writes=[Bf("hTb")])

        def _rms_out(y_t, y_b, W, dst_fn, pq, oq, bq):
            sq_t, sq_b = ssd["sq"]
            r_t, r_b = ssd["rstd"]
            o_t, o_b = ssd["yo"]
            op("pool", lambda e: e.tensor_tensor(out=sq_t[:, :, :W], in0=y_t[:, :, :W], in1=y_t[:, :, :W], op=ALU.mult), reads=[y_b], writes=[sq_b])
            for k in range(4):
                op("pe", lambda e: e.matmul(pq[:, oq:oq + W], lhsT=one_t[:], rhs=sq_t[:, k, :W], start=(k == 0), stop=(k == 3)), reads=[one_b, sq_b], writes=[bq])
            op("dve", lambda e: e.tensor_scalar(out=r_t[:, :W], in0=pq[:, oq:oq + W], scalar1=1.0 / 512.0, scalar2=NORM_EPS, op0=ALU.mult, op1=ALU.add),
               reads=[bq], writes=[r_b])
            op("act", lambda e: e.activation(out=r_t[:, :W], in_=r_t[:, :W], func=AF.Sqrt), reads=[r_b], writes=[r_b])
            op("dve", lambda e: e.reciprocal(out=r_t[:, :W], in_=r_t[:, :W]), reads=[r_b], writes=[r_b])
            for k in range(4):
                op("dve", lambda e: e.scalar_tensor_tensor(out=o_t[:, k, :W], in0=y_t[:, k, :W], scalar=nw_t[:, k:k + 1], in1=r_t[:, :W], op0=ALU.mult, op1=ALU.mult),
                   reads=[y_b, nw_b, r_b], writes=[o_b])
            for k in range(4):
                P.dma("sp", dst_fn(k), o_t[:, k, :W], reads=[o_b])

        blkb = {}
        blkb["xTb"] = P.sb("xTb", [128, 8, BW], BF16)
        blkb["pre"] = P.sb("pre", [128, 6, BW + 3], F32)
        blkb["xsT"] = P.sb("xsT", [128, 4, BW], F32)
        blkb["BT"] = P.sb("BT", [128, BW], F32)
        blkb["BTb"] = P.sb("BTb", [128, BW], BF16)
        blkb["CTb"] = P.sb("CTb", [128, BW], BF16)
        blkb["szT"] = P.sb("szT", [128, 4, BW], F32)
        blkb["qb"] = P.sb("qb", [64, 4, BW], BF16)
        blkb["stg"] = [P.sb("stg0", [128, BW], F32)] * 3
        blkb["cacc"] = P.sb("cacc", [128, BW], F32)
        blkb["gate"] = P.sb("gateq", [128, 12], F32)
        blkb["vtok"] = P.sb("vtok", [128, 192], F32)
        op("dve", lambda e: e.memset(blkb["pre"][0][:, :, 0:3], 0.0), writes=[blkb["pre"][1]])

        def inproj_block(xsrc, c0, W, kv, kvcol0, outs, wpos=None):
            xTb_t, xTb_b = blkb["xTb"]
            for k in range(8):
                P.dma("pool", xTb_t[:, k, :W], xsrc[k * 128:(k + 1) * 128, c0:c0 + W], writes=[xTb_b])
            pre_t, pre_b = blkb["pre"]
            sz_t, sz_b = blkb["szT"]
            q_t, q_b = blkb["qb"]
            for ct in range(17):
                M = COLS_A[ct]
                pt, po, pb = next_ps()
                for k in range(8):
                    op("pe", lambda e: e.matmul(pt[0:M, po:po + W], lhsT=wA_t[:, k, OFF_A[ct]:OFF_A[ct] + M], rhs=xTb_t[:, k, :W], start=(k == 0), stop=(k == 7)),
                       reads=[wA_b, xTb_b], writes=[pb])
                bias = bA_t[0:M, ct:ct + 1]
                if ct < 4:
                    op("act", lambda e: e.activation(out=sz_t[:, ct, :W], in_=pt[:, po:po + W], func=AF.Silu, bias=bias, scale=1.0), reads=[pb, bA_b], writes=[sz_b])
                elif ct < 10:
                    op("act", lambda e: e.activation(out=pre_t[:, ct - 4, 3:3 + W], in_=pt[:, po:po + W], func=AF.Identity, bias=bias, scale=1.0),
                       reads=[pb, bA_b], writes=[pre_b])
                elif ct < 14:
                    op("act", lambda e: e.activation(out=q_t[:, ct - 10, :W], in_=pt[0:64, po:po + W], func=AF.Identity, bias=bias, scale=1.0),
                       reads=[pb, bA_b], writes=[q_b])
                else:
                    st_t, st_b = blkb["stg"][ct - 14]
                    op("act", lambda e: e.activation(out=st_t[:, :W], in_=pt[:, po:po + W], func=AF.Identity, bias=bias, scale=1.0), reads=[pb, bA_b], writes=[st_b])
                    if ct == 14:
                        op("pool", lambda e: e.tensor_copy(out=kv["kvc"][0][:, kvcol0:kvcol0 + W], in_=st_t[:, :W]), reads=[st_b], writes=[kv["kvc"][1]])
                    elif ct == 15:
                        op("pool", lambda e: e.tensor_copy(out=kv["ksl"][0][0:64, kvcol0:kvcol0 + W], in_=st_t[0:64, :W]), reads=[st_b], writes=[kv["ksl"][1]])
                    else:
                        rc = kvcol0 % 1024 if wpos is not None else kvcol0
                        op("pool", lambda e: e.tensor_copy(out=kv["kwn"][0][0:64, rc:rc + W], in_=st_t[0:64, :W]), reads=[st_b], writes=[kv["kwn"][1]])
                        if wpos is not None:
                            P.dma("pool", kv["kwn"][0][64:71, rc:rc + W], kaug[:, wpos:wpos + W], writes=[kv["kwn"][1]])
                    outs(ct - 14, st_t, st_b)

        def conv_block(W, hist_fn=None):
            pre_t, pre_b = blkb["pre"]
            a_t, a_b = blkb["cacc"]
            xs_t, xs_b = blkb["xsT"]
            for ci in range(6):
                op("dve", lambda e: e.tensor_scalar(out=a_t[:, :W], in0=pre_t[:, ci, 0:W], scalar1=cw_t[:, ci, 0:1], scalar2=None, op0=ALU.mult),
                   reads=[pre_b, cw_b], writes=[a_b])
                for k in range(1, 4):
                    op("dve", lambda e: e.scalar_tensor_tensor(out=a_t[:, :W], in0=pre_t[:, ci, k:k + W], scalar=cw_t[:, ci, k:k + 1], in1=a_t[:, :W],
                                                               op0=ALU.mult, op1=ALU.add), reads=[pre_b, cw_b, a_b], writes=[a_b])
                if ci < 4:
                    op("act", lambda e: e.activation(out=xs_t[:, ci, :W], in_=a_t[:, :W], func=AF.Silu, bias=cb_t[:, ci:ci + 1], scale=1.0),
                       reads=[a_b, cb_b], writes=[xs_b])
                elif ci == 4:
                    op("act", lambda e: e.activation(out=blkb["BT"][0][:, :W], in_=a_t[:, :W], func=AF.Silu, bias=cb_t[:, ci:ci + 1], scale=1.0),
                       reads=[a_b, cb_b], writes=[blkb["BT"][1]])
                    op("pool", lambda e: e.tensor_copy(out=blkb["BTb"][0][:, :W], in_=blkb["BT"][0][:, :W]), reads=[blkb["BT"][1]], writes=[blkb["BTb"][1]])
                else:
                    op("act", lambda e: e.activation(out=blkb["CTb"][0][:, :W], in_=a_t[:, :W], func=AF.Silu, bias=cb_t[:, ci:ci + 1], scale=1.0),
                       reads=[a_b, cb_b], writes=[blkb["CTb"][1]])

        def tok_proj(c_ap_fn, W, kv, tile_idx, want_dt=False):
            xTb_t, xTb_b = blkb["xTb"]
            pt, po, pb = next_ps()
            for k in range(8):
                op("pe", lambda e: e.matmul(pt[0:W, po:po + 140], lhsT=c_ap_fn(k), rhs=wT_t[:, k, 512:652], start=(k == 0), stop=(k == 7)),
                   reads=[xTb_b, wT_b], writes=[pb])
            g_t, g_b = blkb["gate"]
            v_t, v_b = blkb["vtok"]
            op("dve", lambda e: e.tensor_tensor(out=v_t[0:W, 0:140], in0=pt[0:W, po:po + 140], in1=bT_t[0:W, 512:652], op=ALU.add), reads=[pb, bT_b], writes=[v_b])
            op("act", lambda e: e.activation(out=g_t[0:W, :], in_=v_t[0:W, 0:12], func=AF.Sigmoid), reads=[v_b], writes=[g_b])
            if kv is not None:
                op("pool", lambda e: e.tensor_copy(out=kv["vsl"][0][0:W, tile_idx, 0:64], in_=v_t[0:W, 12:76]), reads=[v_b], writes=[kv["vsl"][1]])
                op("pool", lambda e: e.tensor_copy(out=kv["vwn"][0][0:W, tile_idx % 8, 0:64], in_=v_t[0:W, 76:140]), reads=[v_b], writes=[kv["vwn"][1]])

        es_p = ExitStack()
        P.es = es_p
        ssd["hT"] = P.sb("hT", [128, 512], F32)
        ssd["hTb"] = P.sb("hTb", [128, 512], BF16)
        op("dve", lambda e: e.memset(ssd["hT"][0][:], 0.0), writes=[ssd["hT"][1]])
        op("dve", lambda e: e.memset(ssd["hTb"][0][:], 0.0), writes=[ssd["hTb"][1]])
        for nm, shp, dt_ in (("dt", [128, 512], F32), ("dtA", [128, 512], F32), ("xs", [128, 512], F32), ("Btb", [128, 128], BF16),
                             ("acs", [128, 512], F32), ("w", [128, 512], F32), ("dec", [128, 512], F32), ("xdt", [128, 512], F32),
                             ("xdtb", [128, 512], BF16), ("xdtw", [128, 512], BF16), ("ea", [128, 4, 128], F32), ("cbm", [128, 128], F32),
                             ("L1", [128, 8, 128], F32), ("L2", [128, 8, 128], F32), ("E", [128, 4, 128], F32), ("Mj", [128, 8, 128], BF16),
                             ):
            ssd[nm] = P.sb("ssd_" + nm, shp, dt_)
        ssd["tmp"] = ssd["acs"]
        ssd["y"] = P.sb("ssd_y", [128, 4, 128], F32)

        pkv = alloc_kv("p_", S, 4)
        P.es = es
        for nm in ("kc",):
            op("dve", lambda e: e.memset(pkv[nm][0][0:64, :], 0.0), writes=[pkv[nm][1]])
        op("dve", lambda e: e.memset(pkv["vc"][0][:, :, 0:64], 0.0), writes=[pkv["vc"][1]])
        done_n = 0
        NB = S // BW
        TPB = BW // 128
        for tb in range(NB):
            c0 = tb * BW

            def outs(i, st_t, st_b, c0=c0, tb=tb):
                if i < 2:
                    P.dma("sp", kvT[i * 128:(i + 1) * 128, c0:c0 + BW], st_t[:, :BW], reads=[st_b])
                elif c0 >= S - 512:
                    P.dma("sp", winT[:, c0 - (S - 512):c0 - (S - 512) + BW], st_t[:, :BW], reads=[st_b])
            inproj_block(xT, c0, BW, pkv, c0, outs, wpos=c0)
            if tb == NB - 1:
                P.dma("sp", convT_p.rearrange("(c p) k -> p c k", p=128), blkb["pre"][0][:, :, BW:BW + 3], reads=[blkb["pre"][1]])
            conv_block(BW)
            nmax = min((BW // 16) * (tb + 1) - 1, 511)
            n = done_n
            while n < nmax:
                cch = n // 128
                hi = min(nmax, cch * 128 + 128)
                compress(pkv, cch * 128, hi - cch * 128)
                n = hi
            done_n = nmax
            for tl in range(TPB):
                t = tb * TPB + tl
                cs = slice(tl * 128, (tl + 1) * 128)
                ssd_chunk(t, blkb, tl * 128)
                tok_proj(lambda k: blkb["xTb"][0][:, k, cs], 128, pkv, t)
                qa_t, qa_b = pkv["qa"]
                P.dma("pool", qa_t[64:71, :, :], qaug[:, :, t * 128:(t + 1) * 128], writes=[qa_b])
                op("pool", lambda e: e.tensor_copy(out=qa_t[0:64, :, :], in_=blkb["qb"][0][:, :, cs]), reads=[blkb["qb"][1]], writes=[qa_b])

                def out_fn(y_t, y_b, t=t):
                    n_t, n_b = scr["nyT"]
                    pt, po, pb = next_ps()
                    for k in range(2):
                        op("pe", lambda e: e.transpose(out=pt[:, po + k * 128:po + (k + 1) * 128], in_=y_t[:, k * 128:(k + 1) * 128], identity=id_t[:]),
                           reads=[y_b, id_b], writes=[pb])
                    op("dve", lambda e: e.tensor_copy(out=n_t[:].rearrange("p k q -> p (k q)"), in_=pt[:, po:po + 256]), reads=[pb], writes=[n_b])
                    P.dma("sp", yT[512:768, t * 128:(t + 1) * 128].rearrange("(k p) q -> p k q", p=128), n_t[:], reads=[n_b])
                nsa_qtile(pkv, t, blkb["gate"][0][:, :], blkb["gate"][1], out_fn)
            op("dve", lambda e: e.tensor_copy(out=blkb["pre"][0][:, :, 0:3], in_=blkb["pre"][0][:, :, BW:BW + 3]), reads=[blkb["pre"][1]], writes=[blkb["pre"][1]])
        pt, po, pb = next_ps()
        for k in range(4):
            op("pe", lambda e: e.transpose(out=pt[:, po + k * 128:po + (k + 1) * 128], in_=ssd["hT"][0][:, k * 128:(k + 1) * 128], identity=id_t[:]),
               reads=[ssd["hT"][1], id_b], writes=[pb])
        op("dve", lambda e: e.tensor_copy(out=ssd["tmp"][0][:], in_=pt[:, po:po + 512]), reads=[pb], writes=[ssd["tmp"][1]])
        P.dma("sp", ssm_p.rearrange("(k p) n -> p k n", p=128), ssd["tmp"][0][:].rearrange("p (k n) -> p k n", k=4), reads=[ssd["tmp"][1]])
        if stop == "prompt":
            P.finish()
            raise _Stop(nc)

        P.barrier()
        es_p.close()
        skv = alloc_kv("s_", SS, 2)
        P.dma("pool", skv["kwn"][0][64:71, 512:1024], kaug[:, (NPG - 4) * 128:NPG * 128], writes=[skv["kwn"][1]], key="dl_s_kwn_aug")
        P.dma("pool", skv["kwn"][0][64:71, 0:128], kaug[:, NPG * 128:NPG * 128 + 128], writes=[skv["kwn"][1]], key="dl_s_kwn_aug")
        for nm in ("kvc", "vsl", "vwn", "vc"):
            pass
        for nm, rows in (("kvc", 128), ("ksl", 64), ("kwn", 64), ("kc", 64)):
            op("dve", lambda e: e.memset(skv[nm][0][0:rows, :], 0.0), writes=[skv[nm][1]])
        for nm in ("vsl", "vwn", "vc"):
            op("dve", lambda e: e.memset(skv[nm][0][:, :, 0:64], 0.0), writes=[skv[nm][1]])
        qa_t, qa_b = skv["qa"]
        TQ = NTS - 1
        op("dve", lambda e: e.memset(qa_t[0:64, :, :], 0.0), writes=[qa_b])
        P.dma("pool", qa_t[64:71, :, :], qaug[:, :, TQ * 128:(TQ + 1) * 128], writes=[qa_b])
        newkv = {"kvc": P.sb("n_kvc", [128, NS], BF16), "ksl": P.sb("n_ksl", [64, NS], BF16), "kwn": P.sb("n_kwn", [64, NS], BF16)}
        colp_t, colp_b = const("colp", colp, [128, 3, 4])
        hs_t, hs_b = P.sb("hs", [128, 3, 6, NS], F32)
        for k in range(3):
            P.dma("sp", hs_t[:, k, :, :], convT_s[k].rearrange("(c p) s -> p c s", p=128), writes=[hs_b])

        def outs_s(i, st_t, st_b):
            if i < 2:
                P.dma("sp", kvS[i * 128:(i + 1) * 128, :], st_t[:, :NS], reads=[st_b])
            else:
                P.dma("sp", winnewT[:, :], st_t[:, :NS], reads=[st_b])
        inproj_block(xTs, 0, NS, newkv, 0, outs_s)
        pre_t, pre_b = blkb["pre"]
        P.dma("sp", convS[0].rearrange("(c p) s -> p c s", p=128), hs_t[:, 1, :, :], reads=[hs_b])
        P.dma("sp", convS[1].rearrange("(c p) s -> p c s", p=128), hs_t[:, 2, :, :], reads=[hs_b])
        P.dma("sp", convS[2].rearrange("(c p) s -> p c s", p=128), pre_t[:, :, 3:3 + NS], reads=[pre_b])
        a_t, a_b = blkb["cacc"]
        xs_t, xs_b = blkb["xsT"]
        sBT, sBT_b = blkb["BT"]
        sCT, sCT_b = P.sb("sCT", [128, NS], F32)
        for ci in range(6):
            op("dve", lambda e: e.tensor_scalar(out=a_t[:, :NS], in0=hs_t[:, 0, ci, :], scalar1=cw_t[:, ci, 0:1], scalar2=None, op0=ALU.mult),
               reads=[hs_b, cw_b], writes=[a_b])
            for k in range(1, 4):
                src = hs_t[:, k, ci, :] if k < 3 else pre_t[:, ci, 3:3 + NS]
                op("dve", lambda e: e.scalar_tensor_tensor(out=a_t[:, :NS], in0=src, scalar=cw_t[:, ci, k:k + 1], in1=a_t[:, :NS], op0=ALU.mult, op1=ALU.add),
                   reads=[hs_b, pre_b, cw_b, a_b], writes=[a_b])
            dst, dst_b = (xs_t[:, ci, :NS], xs_b) if ci < 4 else ((sBT[:, :NS], sBT_b) if ci == 4 else (sCT[:, :NS], sCT_b))
            op("act", lambda e: e.activation(out=dst, in_=a_t[:, :NS], func=AF.Silu, bias=cb_t[:, ci:ci + 1], scale=1.0), reads=[a_b, cb_b], writes=[dst_b])
        tok_proj(lambda k: blkb["xTb"][0][:, k, :NS], NS, None, 0)
        gtok_t, gtok_b = P.sb("gtok", [128, 12], F32)
        vtokb_t, vtokb_b = P.sb("vtokb", [128, 128], BF16)
        op("dve", lambda e: e.tensor_copy(out=gtok_t[0:NS, :], in_=blkb["gate"][0][0:NS, :]), reads=[blkb["gate"][1]], writes=[gtok_b])
        op("dve", lambda e: e.tensor_copy(out=vtokb_t[0:NS, :], in_=blkb["vtok"][0][0:NS, 12:140]), reads=[blkb["vtok"][1]], writes=[vtokb_b])
        dtT_t, dtT_b = P.sb("dtT", [128, 4, NS], F32)
        dec_t, dec_b = P.sb("decT", [128, 4, NS], F32)
        xdtT_t, xdtT_b = P.sb("xdtT", [128, 4, NS], F32)
        ysm_t, ysm_b = P.sb("ysm", [128, 4, NS], F32)
        bsum_t, bsum_b = P.sb("bsum", [128, 4], F32)
        Acol_t, Acol_b = P.sb("Acol", [128, 4], F32)
        op("dve", lambda e: e.tensor_tensor(out=bsum_t[:], in0=colp_t[:, 0, :], in1=colp_t[:, 1, :], op=ALU.add), reads=[colp_b], writes=[bsum_b])
        op("act", lambda e: e.activation(out=Acol_t[:], in_=colp_t[:, 2, :], func=AF.Exp), reads=[colp_b], writes=[Acol_b])
        op("dve", lambda e: e.tensor_scalar(out=Acol_t[:], in0=Acol_t[:], scalar1=-1.0, scalar2=None, op0=ALU.mult), reads=[Acol_b], writes=[Acol_b])
        for k4 in range(4):
            pt, po, pb = next_ps()
            for k in range(8):
                op("pe", lambda e: e.matmul(pt[:, po:po + NS], lhsT=wT_t[:, k, k4 * 128:(k4 + 1) * 128], rhs=blkb["xTb"][0][:, k, :NS], start=(k == 0), stop=(k == 7)),
                   reads=[wT_b, blkb["xTb"][1]], writes=[pb])
            op("act", lambda e: e.activation(out=dtT_t[:, k4, :], in_=pt[:, po:po + NS], func=AF.Exp, bias=bsum_t[:, k4:k4 + 1], scale=1.0),
               reads=[pb, bsum_b], writes=[dtT_b])
        op("act", lambda e: e.activation(out=dtT_t[:], in_=dtT_t[:], func=AF.Ln, bias=1.0, scale=1.0), reads=[dtT_b], writes=[dtT_b])
        for k4 in range(4):
            op("act", lambda e: e.activation(out=dec_t[:, k4, :], in_=dtT_t[:, k4, :], func=AF.Exp, scale=Acol_t[:, k4:k4 + 1]), reads=[dtT_b, Acol_b], writes=[dec_b])
        op("dve", lambda e: e.tensor_tensor(out=xdtT_t[:], in0=xs_t[:, :, :NS], in1=dtT_t[:], op=ALU.mult), reads=[xs_b, dtT_b], writes=[xdtT_b])
        hst = [P.sb("hst%d" % i, [128, 4, 128], F32) for i in range(2)]
        dgb, dgb_b = P.sb("dgb", [128, 256], F32)
        st1, st1_b = P.sb("sst1", [128, 4, 128], F32)
        st2, st2_b = P.sb("sst2", [128, 4, 128], F32)
        for s_i in range(NS):
            h_t, h_b = hst[s_i % 2]
            P.dma("sp", h_t[:], ssm_s[s_i].rearrange("(k p) n -> p k n", p=128), writes=[h_b])
            op("dve", lambda e: e.tensor_scalar(out=dgb[:, 0:128], in0=id_t[:], scalar1=sBT[:, s_i:s_i + 1], scalar2=None, op0=ALU.mult),
               reads=[id_b, sBT_b], writes=[dgb_b])
            op("dve", lambda e: e.tensor_scalar(out=dgb[:, 128:256], in0=id_t[:], scalar1=sCT[:, s_i:s_i + 1], scalar2=None, op0=ALU.mult),
               reads=[id_b, sCT_b], writes=[dgb_b])
            pt, po, pb = next_ps()
            op("pe", lambda e: e.matmul(pt[:, po:po + 256], lhsT=one_t[:], rhs=dgb[:], start=True, stop=True), reads=[one_b, dgb_b], writes=[pb])
            op("dve", lambda e: e.tensor_tensor(out=st1[:], in0=h_t[:], in1=dec_t[:, :, s_i:s_i + 1].to_broadcast([128, 4, 128]), op=ALU.mult),
               reads=[h_b, dec_b], writes=[st1_b])
            op("dve", lambda e: e.tensor_tensor(out=st2[:], in0=pt[:, po:po + 128].unsqueeze(1).to_broadcast([128, 4, 128]),
                                                in1=xdtT_t[:, :, s_i:s_i + 1].to_broadcast([128, 4, 128]), op=ALU.mult), reads=[pb, xdtT_b], writes=[st2_b])
            op("dve", lambda e: e.tensor_tensor(out=h_t[:], in0=st1[:], in1=st2[:], op=ALU.add), reads=[st1_b, st2_b], writes=[h_b])
            P.dma("sp", ssm_o[s_i].rearrange("(k p) n -> p k n", p=128), h_t[:], reads=[h_b])
            op("dve", lambda e: e.tensor_tensor(out=st1[:], in0=h_t[:], in1=pt[:, po + 128:po + 256].unsqueeze(1).to_broadcast([128, 4, 128]), op=ALU.mult),
               reads=[h_b, pb], writes=[st1_b])
            op("dve", lambda e: e.tensor_reduce(out=ysm_t[:, :, s_i], in_=st1[:], axis=AX.X, op=ALU.add), reads=[st1_b], writes=[ysm_b])
        for k in range(4):
            op("dve", lambda e: e.scalar_tensor_tensor(out=ysm_t[:, k, :], in0=xs_t[:, k, :NS], scalar=dsk_t[:, k:k + 1], in1=ysm_t[:, k, :], op0=ALU.mult, op1=ALU.add),
               reads=[xs_b, dsk_b, ysm_b], writes=[ysm_b])
        op("dve", lambda e: e.tensor_tensor(out=ysm_t[:], in0=ysm_t[:], in1=blkb["szT"][0][:, :, :NS], op=ALU.mult), reads=[ysm_b, blkb["szT"][1]], writes=[ysm_b])
        pq, oq, bq = next_ps()
        _rms_out(ysm_t, ysm_b, NS, lambda k: ysT[k * 128:(k + 1) * 128, :], pq, oq, bq)
        idx_t, idx_b = P.sb("idx", [128, NS * NPG], I32)
        idxf_t, idxf_b = P.sb("idxf", [128, NS * NPG], F32)
        iot_t, iot_b = P.sb("iot", [128, 1], I32)
        iotf_t, iotf_b = P.sb("iotf", [128, 1], F32)
        P.dma("sp", idx_t[:], pt_rep, writes=[idx_b])
        op("pool", lambda e: e.iota(iot_t[:], pattern=[[0, 1]], base=0, channel_multiplier=1), writes=[iot_b])
        op("dve", lambda e: e.tensor_copy(out=iotf_t[:], in_=iot_t[:]), reads=[iot_b], writes=[iotf_b])
        op("dve", lambda e: e.tensor_copy(out=idxf_t[:], in_=idx_t[:]), reads=[idx_b], writes=[idxf_b])
        op("dve", lambda e: e.tensor_scalar(out=idxf_t[:], in0=idxf_t[:], scalar1=128.0, scalar2=iotf_t[:, 0:1], op0=ALU.mult, op1=ALU.add),
           reads=[idxf_b, iotf_b], writes=[idxf_b])
        op("dve", lambda e: e.tensor_copy(out=idx_t[:], in_=idxf_t[:]), reads=[idxf_b], writes=[idx_b])
        G = [P.sb("G0", [128, NPG, 256], F32)] * 2
        Wt = [P.sb("Wt%d" % i, [128, 4, 128], F32) for i in range(2)]
        gq_t, gq_b = P.sb("gq", [128, 12], F32)
        op("dve", lambda e: e.memset(gq_t[:], 0.0), writes=[gq_b])
        for s_i in range(NS):
            g_t, g_b = G[s_i % 2]
            w_t, w_b = Wt[s_i % 2]
            P._deps("pool", [idx_b], [g_b])
            for j in range(NPG):
                inst = nc.gpsimd.indirect_dma_start(out=g_t[:, j, :], out_offset=None, in_=cache,
                                                    in_offset=bass.IndirectOffsetOnAxis(ap=idx_t[:, s_i * NPG + j:s_i * NPG + j + 1], axis=0))
                key = "dl_" + g_b.name
                P._sem(key)
                P.cnt[key] += 16
                inst.then_inc(P.sems[key], 16)
            g_b.w = ("dl_" + g_b.name, P.cnt["dl_" + g_b.name])
            g_b.r = {}
            idx_b.r["dl_" + g_b.name] = P.cnt["dl_" + g_b.name]
            P.dma("sp", w_t[:], kvwin_s[s_i].rearrange("(c p) f -> p c f", p=128), writes=[w_b])
            P.dma("sp", winS[s_i], kvwin_s[s_i, 1:512, :], key="ds_winS")
            for j0 in range(0, NPG, 4):
                pt, po, pb = next_ps()
                for jj in range(4):
                    op("pe", lambda e: e.transpose(out=pt[:, po + jj * 128:po + (jj + 1) * 128], in_=g_t[:, j0 + jj, 0:128], identity=id_t[:]),
                       reads=[g_b, id_b], writes=[pb])
                op("dve", lambda e: e.tensor_copy(out=skv["kvc"][0][:, j0 * 128:(j0 + 4) * 128], in_=pt[:, po:po + 512]), reads=[pb], writes=[skv["kvc"][1]])
                pt, po, pb = next_ps()
                for jj in range(4):
                    op("pe", lambda e: e.transpose(out=pt[0:64, po + jj * 128:po + (jj + 1) * 128], in_=g_t[:, j0 + jj, 128:192], identity=id_t[:]),
                       reads=[g_b, id_b], writes=[pb])
                op("dve", lambda e: e.tensor_copy(out=skv["ksl"][0][0:64, j0 * 128:(j0 + 4) * 128], in_=pt[0:64, po:po + 512]), reads=[pb], writes=[skv["ksl"][1]])
            op("pool", lambda e: e.tensor_copy(out=skv["vsl"][0][:, 0:NPG, 0:64], in_=g_t[:, :, 192:256]), reads=[g_b], writes=[skv["vsl"][1]])
            pt, po, pb = next_ps()
            for cc in range(4):
                op("pe", lambda e: e.transpose(out=pt[0:64, po + cc * 128:po + (cc + 1) * 128], in_=w_t[:, cc, 0:64], identity=id_t[:]),
                   reads=[w_b, id_b], writes=[pb])
            op("dve", lambda e: e.tensor_copy(out=skv["kwn"][0][0:64, 512:1024], in_=pt[0:64, po:po + 512]), reads=[pb], writes=[skv["kwn"][1]])
            op("pool", lambda e: e.tensor_copy(out=skv["vwn"][0][:, 4:8, 0:64], in_=w_t[:, :, 64:128]), reads=[w_b], writes=[skv["vwn"][1]])
            NP0 = NPG * 128
            op("pool", lambda e: e.tensor_copy(out=skv["ksl"][0][0:64, NP0:NP0 + 1], in_=newkv["ksl"][0][:, s_i:s_i + 1]), reads=[newkv["ksl"][1]], writes=[skv["ksl"][1]])
            op("pool", lambda e: e.tensor_copy(out=skv["kwn"][0][0:64, 0:1], in_=newkv["kwn"][0][:, s_i:s_i + 1]), reads=[newkv["kwn"][1]], writes=[skv["kwn"][1]])
            P.dma("sp", skv["vsl"][0][0:1, NPG, 0:64], vtokb_t[s_i:s_i + 1, 0:64], reads=[vtokb_b], writes=[skv["vsl"][1]], key="dl_svsl")
            P.dma("sp", skv["vwn"][0][0:1, 0, 0:64], vtokb_t[s_i:s_i + 1, 64:128], reads=[vtokb_b], writes=[skv["vwn"][1]], key="dl_svwn")
            P.dma("sp", gq_t[0:1, :], gtok_t[s_i:s_i + 1, :], reads=[gtok_b], writes=[gq_b], key="dl_gq")
            op("pool", lambda e: e.tensor_copy(out=qa_t[0:64, :, 0:1], in_=blkb["qb"][0][:, :, s_i:s_i + 1]), reads=[blkb["qb"][1]], writes=[qa_b])
            compress(skv, 0, 128)

            def out_s(y_t, y_b, s_i=s_i):
                P.dma("sp", ynS[s_i:s_i + 1, :], y_t[0:1, :], reads=[y_b])
            nsa_qtile(skv, TQ, gq_t[:, :], gq_b, out_s)
        P.finish()
    return nc


def _chan(g):
    return np.concatenate([np.arange(g * 512, (g + 1) * 512), 2048 + np.arange(g * 128, (g + 1) * 128), 2560 + np.arange(g * 128, (g + 1) * 128)])


def prep_l1_weights(inp, g, Smax):
    w_in, b_in = inp["w_in"], inp["b_in"]
    colsA = np.concatenate([
        np.arange(g * 512, (g + 1) * 512), 2048 + _chan(g),
        5152 + g * 256 + np.arange(256),
        np.concatenate([6176 + tt * 256 + g * 64 + np.arange(64) for tt in range(6)])])
    wA = np.ascontiguousarray(w_in[:, colsA])
    bfull = b_in[colsA]
    bA = np.zeros((128, 17), np.float32)
    off = 0
    for ct, m in enumerate(COLS_A):
        bA[:m, ct] = bfull[off:off + m]
        off += m
    hd = np.repeat(np.arange(8), 64)
    colsT = np.concatenate([5120 + g * 8 + hd, 7712 + g * 12 + np.arange(12), 6176 + 3 * 256 + g * 64 + np.arange(64), 6176 + 5 * 256 + g * 64 + np.arange(64)])
    wT = np.ascontiguousarray(w_in[:, colsT])
    bT = np.ascontiguousarray(np.broadcast_to(b_in[colsT][None, :], (128, 652)))
    heads = g * 8 + hd
    dtb = np.ascontiguousarray(np.broadcast_to(inp["dt_bias"][heads][None, :], (128, 512)))
    alog = np.ascontiguousarray(np.broadcast_to(inp["a_log"][heads][None, :], (128, 512)))
    ch = _chan(g)
    cw = np.ascontiguousarray(inp["conv_w"][:, ch].reshape(4, 6, 128).transpose(2, 1, 0))
    cb = np.ascontiguousarray(inp["conv_b"][ch].reshape(6, 128).T)
    hp = heads.reshape(4, 128).T
    dsk = np.ascontiguousarray(inp["d_skip"][hp])
    nw = np.ascontiguousarray(inp["ssd_norm_w"][g * 512:(g + 1) * 512].reshape(4, 128).T)
    colp = np.stack([b_in[5120 + hp], inp["dt_bias"][hp], inp["a_log"][hp]], 1).astype(np.float32)
    w1 = inp["cmp_w1"]
    w1kv = np.ascontiguousarray(w1.transpose(0, 2, 1, 3).reshape(128, 32, 128))
    pekv = np.ascontiguousarray(inp["cmp_pe"].transpose(0, 2, 1).reshape(128, 32))
    b1 = np.ascontiguousarray(inp["cmp_b1"].T)
    w2kv = np.ascontiguousarray(np.concatenate([inp["cmp_w2"][0], inp["cmp_w2"][1]], 1))
    b2k = np.ascontiguousarray(inp["cmp_b2"][0][:, None])
    b2v = np.ascontiguousarray(np.broadcast_to(inp["cmp_b2"][1][None, :], (128, 64)))
    t = _nsa_tables(Smax)
    d = dict(wA=wA, bA=bA, wT=wT, bT=bT, dtb=dtb, alog=alog, cw=cw, cb=cb, dsk=dsk, nw=nw, colp=np.ascontiguousarray(colp),
             w1kv=w1kv, pekv=pekv, b1=b1, w2kv=w2kv, b2k=b2k, b2v=b2v,
             kaug=t["kaug"], kaug_c=t["kaug_c"], qaug=np.ascontiguousarray(t["qaug"][g]), caus4=t["caus4"], low4=t["low4"], EW=t["EW"], Zm=t["Zm"],
             F=t["F"], ov=t["ov"], U=t["U"], ones=t["ones"], ident=t["ident"])
    return {k: np.ascontiguousarray(v, dtype=np.float32) for k, v in d.items()}


def prep_l1_data(inp, b, g, S, samples, NPG):
    NS = len(samples)
    ch = _chan(g)
    d = {}
    d["xT"] = np.ascontiguousarray(inp["x_prompt"][b, :S].T)
    d["xTs"] = np.ascontiguousarray(inp["x_sample"][samples, 0].T)
    d["pt_rep"] = np.ascontiguousarray(np.broadcast_to(inp["page_table"][samples].reshape(1, -1), (128, NS * NPG))).astype(np.int32)
    d["kvwin_s"] = np.ascontiguousarray(inp["cache_kv_win"][samples][:, :, :, g, :].reshape(NS, 512, 128))
    d["ssm_s"] = np.ascontiguousarray(inp["state_ssm"][samples, g * 8:(g + 1) * 8].reshape(NS, 512, 128))
    d["convT_s"] = np.ascontiguousarray(inp["state_conv"][samples][:, :, ch].transpose(1, 2, 0))
    return d


def cache_group(inp, g):
    c = inp["cache_kv_paged"]
    return np.ascontiguousarray(c[:, :, :, g, :].reshape(c.shape[0] * 128, 256))


def l2_weights(w):
    bc = np.zeros((128, 6, 1024), np.float32)
    for i, k in enumerate(["b_out", "ln1_g", "ln1_b", "ln2_g", "ln2_b"]):
        bc[:, i, :] = w[k][None, :]
    bc[:, 5, :64] = w["b_router"][None, :]
    return dict(
        w_gm=np.ascontiguousarray(w["w_in"][:, 7760:]), b_gm=np.ascontiguousarray(w["b_in"][7760:].reshape(16, 128).T),
        w_sd=w["w_ssd_down"], w_nd=w["w_nsa_down"], w_out=w["w_out"], bcast=bc, w_r=w["w_router"],
        w_e1=np.concatenate([w["w_e1"], w["w_s1"][None]], 0), w_e3=np.concatenate([w["w_e3"], w["w_s3"][None]], 0),
        w_e2=np.concatenate([w["w_e2"], w["w_s2"][None]], 0), ident=np.eye(128, dtype=np.float32))


def kernel(**inp):
    inp = {k: np.asarray(v) for k, v in inp.items()}
    B, S = inp["x_prompt"].shape[:2]
    DB = inp["x_sample"].shape[0]
    NPG = inp["page_table"].shape[1]
    NS = DB // 2
    SS = NPG * 128 + 128
    nc1 = build_l1(S, NS, NPG, nphys=inp["cache_kv_paged"].shape[0])
    caches = [cache_group(inp, g) for g in range(4)]
    wts = [prep_l1_weights(inp, g, max(S, SS)) for g in range(4)]
    maps = []
    for c in range(8):
        b, g = c // 4, c % 4
        samples = np.arange(b * NS, (b + 1) * NS)
        m = dict(wts[g])
        m.update(prep_l1_data(inp, b, g, S, samples, NPG))
        m["cache"] = caches[g]
        maps.append(m)
    r1 = run_bass_kernel_spmd(nc1, maps, core_ids=list(range(8))).results
    del maps, caches
    f32 = np.float32
    ssd_y = np.zeros((B, S, 2048), f32); nsa_y = np.zeros((B, S, 1024), f32)
    kv_p = np.zeros((B, S, 4, 4, 64), f32); win_p = np.zeros((B, 512, 2, 4, 64), f32)
    ssm_pr = np.zeros((B, 32, 64, 128), f32); conv_p = np.zeros((B, 3, 3072), f32)
    ssd_ys = np.zeros((DB, 2048), f32); nsa_ys = np.zeros((DB, 1024), f32)
    kv_s = np.zeros((DB, 1, 4, 4, 64), f32); win_s = np.zeros((DB, 512, 2, 4, 64), f32)
    ssm_sm = np.zeros((DB, 32, 64, 128), f32); conv_s = np.zeros((DB, 3, 3072), f32)
    for c in range(8):
        b, g = c // 4, c % 4
        r = r1[c]
        sm = slice(b * NS, (b + 1) * NS)
        ch = _chan(g)
        ssd_y[b, :, g * 512:(g + 1) * 512] = r["yT"][0:512].T
        nsa_y[b, :, g * 256:(g + 1) * 256] = r["yT"][512:768].T
        for tt in range(4):
            kv_p[b, :, tt, g, :] = r["kvT"][tt * 64:(tt + 1) * 64].T
            kv_s[sm, 0, tt, g, :] = r["kvS"][tt * 64:(tt + 1) * 64].T
        for t2 in range(2):
            win_p[b, :, t2, g, :] = r["winT"][t2 * 64:(t2 + 1) * 64].T
            win_s[sm, 0:511, t2, g, :] = r["winS"][:, :, t2 * 64:(t2 + 1) * 64]
            win_s[sm, 511, t2, g, :] = r["winnewT"][t2 * 64:(t2 + 1) * 64].T
        ssm_pr[b, g * 8:(g + 1) * 8] = r["ssm_p"].reshape(8, 64, 128)
        conv_p[b][:, ch] = r["convT_p"].T
        ssd_ys[sm, g * 512:(g + 1) * 512] = r["ysT"].T
        nsa_ys[sm, g * 256:(g + 1) * 256] = r["ynS"]
        ssm_sm[sm, g * 8:(g + 1) * 8] = r["ssm_o"].reshape(NS, 8, 64, 128)
        conv_s[sm][:, :, ch] = 0
        cs_ = conv_s[sm]
        cs_[:, :, ch] = r["convS"].transpose(2, 0, 1)
        conv_s[sm] = cs_
    TQ = S // 4
    SQ = DB // 8
    NT_TILES = (TQ + SQ + 127) // 128
    NT = NT_TILES * 128
    nc2 = build_l2(NT_TILES)
    w2 = l2_weights(inp)
    maps = []
    for c in range(8):
        b, k = c // 4, c % 4
        srows = np.arange(b * NS + k * SQ, b * NS + (k + 1) * SQ)
        x = np.zeros((NT, 1024), f32)
        x[:TQ] = inp["x_prompt"][b, k * TQ:(k + 1) * TQ]
        x[TQ:TQ + SQ] = inp["x_sample"][srows, 0]
        ym = np.zeros((NT, 3072), f32)
        ym[:TQ, :2048] = ssd_y[b, k * TQ:(k + 1) * TQ]
        ym[:TQ, 2048:] = nsa_y[b, k * TQ:(k + 1) * TQ]
        ym[TQ:TQ + SQ, :2048] = ssd_ys[srows]
        ym[TQ:TQ + SQ, 2048:] = nsa_ys[srows]
        m = dict(w2)
        m.update(xT=np.ascontiguousarray(x.T), x_tok=x, yT=np.ascontiguousarray(ym.T))
        maps.append(m)
    r2 = run_bass_kernel_spmd(nc2, maps, core_ids=list(range(8))).results
    y_p = np.zeros((B, S, 1024), f32)
    y_s = np.zeros((DB, 1, 1024), f32)
    for c in range(8):
        b, k = c // 4, c % 4
        srows = np.arange(b * NS + k * SQ, b * NS + (k + 1) * SQ)
        y_p[b, k * TQ:(k + 1) * TQ] = r2[c]["y"][:TQ]
        y_s[srows, 0] = r2[c]["y"][TQ:TQ + SQ]
    return (y_p, y_s, kv_p, kv_s, win_p, win_s, ssm_pr, ssm_sm, conv_p, conv_s)
```

```python
from contextlib import ExitStack
import numpy as np
import concourse.bass as bass
import concourse.mybir as mybir
from concourse.bass_utils import run_bass_kernel_spmd

F32 = mybir.dt.float32
BF16 = mybir.dt.bfloat16
I32 = mybir.dt.int32
U32 = mybir.dt.uint32
AF = mybir.ActivationFunctionType
ALU = mybir.AluOpType
AX = mybir.AxisListType

D_MODEL = 1024
ALPHA = 2.0 ** 0.25
NORM_EPS = 1e-5
N_EXPERTS = 64
ROUTE_SCALE = 2.5
BIG = 1.0e9


class Buf:
    __slots__ = ("name", "w", "r")

    def __init__(self, name):
        self.name = name
        self.w = None
        self.r = {}


class Prog:
    def __init__(self, nc, es):
        self.nc = nc
        self.es = es
        self.sem_es = es
        self.eng = {"pe": nc.tensor, "dve": nc.vector, "act": nc.scalar, "pool": nc.gpsimd, "sp": nc.sync}
        self.sems = {}
        self.cnt = {}
        self.seen = {e: {} for e in self.eng}
        self.nbuf = 0
        self.store_keys = set()
        for e in self.eng:
            self._sem(e)

    def _sem(self, key):
        if key not in self.sems:
            self.sems[key] = self.sem_es.enter_context(self.nc.semaphore("s_" + key))
            self.cnt[key] = 0
        return self.sems[key]

    def buf(self, name=None):
        self.nbuf += 1
        return Buf(name or ("b%d" % self.nbuf))

    def sb(self, name, shape, dt):
        t = self.es.enter_context(self.nc.sbuf_tensor("sb_" + name, list(shape), dt))
        return t, Buf(name)

    def ps(self, name, shape, dt=F32):
        t = self.es.enter_context(self.nc.psum_tensor("pp_" + name, list(shape), dt))
        return t, Buf(name)

    def _wait(self, e, key, val):
        if e == "pe" and key == "pe":
            return
        if self.seen[e].get(key, 0) >= val:
            return
        self.eng[e].wait_ge(self.sems[key], val)
        self.seen[e][key] = val

    def _deps(self, e, reads, writes):
        for b in reads:
            if b.w is not None:
                self._wait(e, b.w[0], b.w[1])
        for b in writes:
            if b.w is not None:
                self._wait(e, b.w[0], b.w[1])
            for k, v in b.r.items():
                self._wait(e, k, v)

    def op(self, e, fn, reads=(), writes=()):
        self._deps(e, reads, writes)
        inst = fn(self.eng[e])
        self.cnt[e] += 1
        inst.then_inc(self.sems[e], 1)
        c = self.cnt[e]
        for b in reads:
            b.r[e] = c
        for b in writes:
            b.w = (e, c)
            b.r = {}
        self.seen[e][e] = max(self.seen[e].get(e, 0), 0)

    def dma(self, q, out, in_, reads=(), writes=(), key=None, **kw):
        if key is None:
            key = ("dl_" + writes[0].name) if writes else ("ds_" + reads[0].name)
        self._sem(key)
        if not writes:
            self.store_keys.add(key)
        self._deps(q, reads, writes)
        inst = self.eng[q].dma_start(out=out, in_=in_, **kw)
        self.cnt[key] += 16
        inst.then_inc(self.sems[key], 16)
        c = self.cnt[key]
        for b in reads:
            b.r[key] = c
        for b in writes:
            b.w = (key, c)
            b.r = {}

    def barrier(self):
        for e in self.eng:
            for key, v in self.cnt.items():
                if v > 0 and key != e:
                    self._wait(e, key, v)

    def finish(self):
        for key, v in self.cnt.items():
            if key.startswith("ds_") or key.startswith("dl_"):
                if v > 0:
                    self._wait("sp", key, v)
        for e in ("pe", "dve", "act", "pool"):
            if self.cnt[e] > 0:
                self._wait("sp", e, self.cnt[e])


class _Stop(Exception):
    pass


def build_l2(NT_TILES, n_exp=N_EXPERTS, stop=None):
    try:
        return _build_l2(NT_TILES, n_exp, stop)
    except _Stop as ex:
        return ex.args[0]


def _build_l2(NT_TILES, n_exp, stop):
    NT = NT_TILES * 128
    nc = bass.Bass("TRN2", target_bir_lowering=False)
    dr = lambda name, shape, dt=F32, kind="ExternalInput": nc.dram_tensor(name, list(shape), dt, kind=kind).ap()
    xT = dr("xT", [1024, NT])
    x_tok = dr("x_tok", [NT, 1024])
    yT = dr("yT", [3072, NT])
    w_gm = dr("w_gm", [1024, 2048])
    b_gm = dr("b_gm", [128, 16])
    w_sd = dr("w_sd", [2048, 1024])
    w_nd = dr("w_nd", [1024, 1024])
    w_out = dr("w_out", [1024, 1024])
    bc = dr("bcast", [128, 6, 1024])
    w_r = dr("w_r", [1024, 64])
    w_e1 = dr("w_e1", [n_exp + 1, 1024, 256])
    w_e3 = dr("w_e3", [n_exp + 1, 1024, 256])
    w_e2 = dr("w_e2", [n_exp + 1, 256, 1024])
    ident = dr("ident", [128, 128])
    y = dr("y", [NT, 1024], kind="ExternalOutput")

    blocks = []
    t = 0
    while t < NT_TILES:
        n = min(4, NT_TILES - t)
        blocks.append((t, n))
        t += n

    with ExitStack() as es:
        P = Prog(nc, es)
        mixT, mixT_b = P.sb("mixT", [128, 8, NT], BF16)
        mix_bufs = [P.buf("mixb%d" % i) for i in range(NT_TILES)]
        idf, idf_b = P.sb("idf", [128, 128], F32)
        P.dma("sp", idf[:], ident, writes=[idf_b])
        pst = [P.ps("ps%d" % i, [128, 1024], F32) for i in range(4)]
        psh = []
        for i in range(4):
            psh.append((pst[i][0], 0, P.buf("psb%da" % i)))
            psh.append((pst[i][0], 512, P.buf("psb%db" % i)))
        rr = [0]

        def next_ps():
            r = psh[rr[0] % 8]
            rr[0] += 1
            return r

        with ExitStack() as esA:
            old_es = P.es
            P.es = esA
            wgm, wgm_b = P.sb("wgm", [128, 8, 2048], BF16)
            wsd, wsd_b = P.sb("wsd", [128, 16, 1024], BF16)
            wnd, wnd_b = P.sb("wnd", [128, 8, 1024], BF16)
            bgm, bgm_b = P.sb("bgm", [128, 16], F32)
            for k in range(8):
                P.dma("pool", wgm[:, k, :], w_gm[k * 128:(k + 1) * 128, :], writes=[wgm_b])
            for k in range(16):
                P.dma("pool", wsd[:, k, :], w_sd[k * 128:(k + 1) * 128, :], writes=[wsd_b])
            for k in range(8):
                P.dma("pool", wnd[:, k, :], w_nd[k * 128:(k + 1) * 128, :], writes=[wnd_b])
            P.dma("sp", bgm[:], b_gm, writes=[bgm_b])
            xTb = [P.sb("xTb%d" % i, [128, 8, 512], BF16) for i in range(1)]
            yTb = [P.sb("yTb%d" % i, [128, 24, 512], BF16) for i in range(1)]
            gT = [P.sb("gT%d" % i, [128, 16, 512], F32) for i in range(1)]
            t1 = [P.sb("t1_%d" % i, [128, 512], F32) for i in range(2)]
            t2 = [P.sb("t2_%d" % i, [128, 512], F32) for i in range(2)]
            for bi, (t0, ntl) in enumerate(blocks):
                W = ntl * 128
                c0 = t0 * 128
                xb_t, xb_b = xTb[0]
                yb_t, yb_b = yTb[0]
                for k in range(8):
                    P.dma("pool", xb_t[:, k, :W], xT[k * 128:(k + 1) * 128, c0:c0 + W], writes=[xb_b])
                for k in range(24):
                    P.dma("pool", yb_t[:, k, :W], yT[k * 128:(k + 1) * 128, c0:c0 + W], writes=[yb_b])
                g_t, g_b = gT[0]
                mb = [mix_bufs[t0 + i] for i in range(ntl)]
                for ct in range(16):
                    pt, po, pb = next_ps()
                    for k in range(8):
                        P.op("pe", lambda e: e.matmul(pt[:, po:po + W], lhsT=wgm[:, k, ct * 128:(ct + 1) * 128],
                                                      rhs=xb_t[:, k, :W], start=(k == 0), stop=(k == 7)),
                             reads=[wgm_b, xb_b], writes=[pb])
                    P.op("act", lambda e: e.activation(out=g_t[:, ct, :W], in_=pt[:, po:po + W], func=AF.Sigmoid,
                                                       bias=bgm[:, ct:ct + 1], scale=1.0),
                         reads=[pb, bgm_b], writes=[g_b])
                for ct in range(8):
                    pa_t, pa_o, pa_b = next_ps()
                    for k in range(16):
                        P.op("pe", lambda e: e.matmul(pa_t[:, pa_o:pa_o + W], lhsT=wsd[:, k, ct * 128:(ct + 1) * 128],
                                                      rhs=yb_t[:, k, :W], start=(k == 0), stop=(k == 15)),
                             reads=[wsd_b, yb_b], writes=[pa_b])
                    pn_t, pn_o, pn_b = next_ps()
                    for k in range(8):
                        P.op("pe", lambda e: e.matmul(pn_t[:, pn_o:pn_o + W], lhsT=wnd[:, k, ct * 128:(ct + 1) * 128],
                                                      rhs=yb_t[:, 16 + k, :W], start=(k == 0), stop=(k == 7)),
                             reads=[wnd_b, yb_b], writes=[pn_b])
                    a_t, a_b = t1[ct % 2]
                    b_t, b_b = t2[ct % 2]
                    P.op("dve", lambda e: e.tensor_tensor(out=a_t[:, :W], in0=pa_t[:, pa_o:pa_o + W], in1=g_t[:, ct, :W], op=ALU.mult),
                         reads=[pa_b, g_b], writes=[a_b])
                    P.op("dve", lambda e: e.tensor_tensor(out=b_t[:, :W], in0=pn_t[:, pn_o:pn_o + W], in1=g_t[:, 8 + ct, :W], op=ALU.mult),
                         reads=[pn_b, g_b], writes=[b_b])
                    P.op("pool", lambda e: e.tensor_tensor(out=mixT[:, ct, c0:c0 + W], in0=a_t[:, :W], in1=b_t[:, :W], op=ALU.add),
                         reads=[a_b, b_b], writes=mb)
            P.barrier()
            if stop == "A1":
                P.finish()
                raise _Stop(nc)
            P.es = old_es
        acc = [P.sb("acc%d" % i, [128, 1024], F32) for i in range(NT_TILES)]
        hT, hT_b = P.sb("hT", [128, 8, NT], BF16)
        hT_bufs = [P.buf("hTb%d" % i) for i in range(NT_TILES)]
        gates = [P.sb("gate%d" % i, [128, n_exp + 1], F32) for i in range(NT_TILES)]
        bct, bct_b = P.sb("bct", [128, 6, 1024], F32)
        P.dma("sp", bct[:], bc, writes=[bct_b])
        with ExitStack() as esA:
            old_es = P.es
            P.es = esA
            wo, wo_b = P.sb("wo", [128, 8, 1024], BF16)
            wr, wr_b = P.sb("wr", [128, 8, 64], F32)
            for k in range(8):
                P.dma("pool", wo[:, k, :], w_out[k * 128:(k + 1) * 128, :], writes=[wo_b])
            P.dma("sp", wr[:], w_r.rearrange("(k p) e -> p k e", p=128), writes=[wr_b])
            xt = [P.sb("xt%d" % i, [128, 1024], F32) for i in range(2)]
            hTf = [P.sb("hTf%d" % i, [128, 8, 128], F32) for i in range(2)]
            st6 = [P.sb("st6_%d" % i, [128, 2, 6], F32) for i in range(2)]
            mv = [P.sb("mv%d" % i, [128, 2], F32) for i in range(2)]
            rs = [P.sb("rs%d" % i, [128, 2], F32) for i in range(2)]
            rt = [P.sb("rt%d" % i, [128, 8, 64], F32) for i in range(2)]
            r8 = [P.sb("r8_%d" % i, [128, 8, 8], F32) for i in range(2)]
            for ti in range(NT_TILES):
                s = ti % 2
                x_t, x_b = xt[s]
                P.dma("sp", x_t[:], x_tok[ti * 128:(ti + 1) * 128, :], writes=[x_b])
                halves = []
                for hh in range(2):
                    pt, po, pb = next_ps()
                    for k in range(8):
                        P.op("pe", lambda e: e.matmul(pt[:, po:po + 512], lhsT=mixT[:, k, ti * 128:(ti + 1) * 128],
                                                      rhs=wo[:, k, hh * 512:(hh + 1) * 512], start=(k == 0), stop=(k == 7)),
                             reads=[mix_bufs[ti], wo_b], writes=[pb])
                    halves.append((pt, po, pb))
                for hh in range(2):
                    pt, po, pb = halves[hh]
                    P.op("dve", lambda e: e.scalar_tensor_tensor(out=x_t[:, hh * 512:(hh + 1) * 512], in0=x_t[:, hh * 512:(hh + 1) * 512],
                                                                 scalar=ALPHA, in1=pt[:, po:po + 512], op0=ALU.mult, op1=ALU.add),
                         reads=[x_b, pb], writes=[x_b])
                P.op("pool", lambda e: e.tensor_tensor(out=x_t[:], in0=x_t[:], in1=bct[:, 0, :], op=ALU.add),
                     reads=[x_b, bct_b], writes=[x_b])
                a_t, a_b = acc[ti]
                if stop == "A2pre":
                    P.finish()
                    raise _Stop(nc)
                _layer_norm(P, x_t, x_b, a_t, a_b, bct, bct_b, 1, 2, st6[s], mv[s], rs[s])
                if stop == "A2a":
                    P.finish()
                    raise _Stop(nc)
                f_t, f_b = hTf[s]
                for hh in range(2):
                    pt, po, pb = next_ps()
                    for j in range(4):
                        k = hh * 4 + j
                        P.op("pe", lambda e: e.transpose(out=pt[:, po + j * 128:po + (j + 1) * 128], in_=a_t[:, k * 128:(k + 1) * 128],
                                                         identity=idf[:]),
                             reads=[a_b, idf_b], writes=[pb])
                    P.op("dve", lambda e: e.tensor_copy(out=f_t[:, hh * 4:(hh + 1) * 4, :], in_=pt[:, po:po + 512].rearrange("p (j t) -> p j t", j=4)),
                         reads=[pb], writes=[f_b])
                    P.op("dve", lambda e: e.tensor_copy(out=hT[:, hh * 4:(hh + 1) * 4, ti * 128:(ti + 1) * 128],
                                                        in_=pt[:, po:po + 512].rearrange("p (j t) -> p j t", j=4)),
                         reads=[pb], writes=[hT_bufs[ti]])
                if stop == "A2b":
                    P.finish()
                    raise _Stop(nc)
                P.op("pool", lambda e: e.tensor_scalar(out=a_t[:], in0=a_t[:], scalar1=ALPHA, scalar2=None, op0=ALU.mult),
                     reads=[a_b], writes=[a_b])
                pt, po, pb = next_ps()
                for k in range(8):
                    P.op("pe", lambda e: e.matmul(pt[:, po:po + 64], lhsT=f_t[:, k, :], rhs=wr[:, k, :], start=(k == 0), stop=(k == 7)),
                         reads=[f_b, wr_b], writes=[pb])
                if stop == "A2c":
                    P.finish()
                    raise _Stop(nc)
                _routing(P, pt, po, pb, gates[ti], bct, bct_b, rt[s], r8[s], n_exp)
            P.barrier()
            if stop == "A2":
                P.finish()
                raise _Stop(nc)
            P.es = old_es
        with ExitStack() as esB:
            old_es = P.es
            P.es = esB
            w1 = [P.sb("w1_%d" % i, [128, 8, 256], BF16) for i in range(2)]
            w3 = [P.sb("w3_%d" % i, [128, 8, 256], BF16) for i in range(2)]
            w2 = [P.sb("w2_%d" % i, [128, 2, 1024], BF16) for i in range(2)]
            sil = [P.sb("sil%d" % i, [128, 2, 512], F32) for i in range(2)]
            aT = [P.sb("aT%d" % i, [128, 2, 512], BF16) for i in range(2)]
            cnt = 0
            items = []
            for e_i in range(n_exp + 1):
                for bi, (t0, ntl) in enumerate(blocks):
                    items.append((e_i, t0, ntl, bi == 0))

            def stage1(it, cnt):
                e_i, t0, ntl, first = it
                s = e_i % 2
                w1_t, w1_b = w1[s]
                w3_t, w3_b = w3[s]
                w2_t, w2_b = w2[s]
                if first:
                    P.dma("pool", w1_t[:], w_e1[e_i].rearrange("(k p) f -> p k f", p=128), writes=[w1_b])
                    P.dma("pool", w3_t[:], w_e3[e_i].rearrange("(k p) f -> p k f", p=128), writes=[w3_b])
                    P.dma("pool", w2_t[:], w_e2[e_i].rearrange("(k p) f -> p k f", p=128), writes=[w2_b])
                W = ntl * 128
                c0 = t0 * 128
                hb = [hT_bufs[t0 + i] for i in range(ntl)]
                h1 = [next_ps() for _ in range(2)]
                h3 = [next_ps() for _ in range(2)]
                for f in range(2):
                    pt, po, pb = h1[f]
                    for k in range(8):
                        P.op("pe", lambda e: e.matmul(pt[:, po:po + W], lhsT=w1_t[:, k, f * 128:(f + 1) * 128], rhs=hT[:, k, c0:c0 + W],
                                                      start=(k == 0), stop=(k == 7)), reads=[w1_b] + hb, writes=[pb])
                    pt3, po3, pb3 = h3[f]
                    for k in range(8):
                        P.op("pe", lambda e: e.matmul(pt3[:, po3:po3 + W], lhsT=w3_t[:, k, f * 128:(f + 1) * 128], rhs=hT[:, k, c0:c0 + W],
                                                      start=(k == 0), stop=(k == 7)), reads=[w3_b] + hb, writes=[pb3])
                sl_t, sl_b = sil[cnt % 2]
                at_t, at_b = aT[cnt % 2]
                for f in range(2):
                    pt, po, pb = h1[f]
                    P.op("act", lambda e: e.activation(out=sl_t[:, f, :W], in_=pt[:, po:po + W], func=AF.Silu), reads=[pb], writes=[sl_b])
                for f in range(2):
                    pt3, po3, pb3 = h3[f]
                    P.op("dve", lambda e: e.tensor_tensor(out=at_t[:, f, :W], in0=pt3[:, po3:po3 + W], in1=sl_t[:, f, :W], op=ALU.mult),
                         reads=[pb3, sl_b], writes=[at_b])
                return (at_t, at_b, w2_t, w2_b)

            def stage2(it, st):
                e_i, t0, ntl, first = it
                at_t, at_b, w2_t, w2_b = st
                for tl in range(ntl):
                    ti = t0 + tl
                    a_t, a_b = acc[ti]
                    g_t, g_b = gates[ti]
                    for hh in range(2):
                        pt, po, pb = next_ps()
                        for f in range(2):
                            P.op("pe", lambda e: e.matmul(pt[:, po:po + 512], lhsT=at_t[:, f, tl * 128:(tl + 1) * 128],
                                                          rhs=w2_t[:, f, hh * 512:(hh + 1) * 512], start=(f == 0), stop=(f == 1)),
                                 reads=[at_b, w2_b], writes=[pb])
                        P.op("dve", lambda e: e.scalar_tensor_tensor(out=a_t[:, hh * 512:(hh + 1) * 512], in0=pt[:, po:po + 512],
                                                                     scalar=g_t[:, e_i:e_i + 1], in1=a_t[:, hh * 512:(hh + 1) * 512],
                                                                     op0=ALU.mult, op1=ALU.add), reads=[pb, g_b, a_b], writes=[a_b])
            st = stage1(items[0], 0)
            for i in range(len(items)):
                nxt = stage1(items[i + 1], i + 1) if i + 1 < len(items) else None
                stage2(items[i], st)
                st = nxt
            P.barrier()
            if stop == "B":
                P.finish()
                raise _Stop(nc)
            P.es = old_es
        with ExitStack() as esC:
            old_es = P.es
            P.es = esC
            ot = [P.sb("ot%d" % i, [128, 1024], F32) for i in range(2)]
            st6 = [P.sb("cst6_%d" % i, [128, 2, 6], F32) for i in range(2)]
            mv = [P.sb("cmv%d" % i, [128, 2], F32) for i in range(2)]
            rs = [P.sb("crs%d" % i, [128, 2], F32) for i in range(2)]
            for ti in range(NT_TILES):
                s = ti % 2
                a_t, a_b = acc[ti]
                o_t, o_b = ot[s]
                _layer_norm(P, a_t, a_b, o_t, o_b, bct, bct_b, 3, 4, st6[s], mv[s], rs[s])
                P.dma("sp", y[ti * 128:(ti + 1) * 128, :], o_t[:], reads=[o_b])
            P.finish()
            P.es = old_es
    return nc


def _layer_norm(P, x_t, x_b, o_t, o_b, bct, bct_b, gi, bi, st6, mv, rs):
    s_t, s_b = st6
    m_t, m_b = mv
    r_t, r_b = rs
    for hh in range(2):
        P.op("dve", lambda e, hh=hh: e.bn_stats(out=s_t[:, hh, :], in_=x_t[:, hh * 512:(hh + 1) * 512]), reads=[x_b], writes=[s_b])
    P.op("dve", lambda e: e.bn_aggr(out=m_t[:], in_=s_t[:].rearrange("p a b -> p (a b)")), reads=[s_b], writes=[m_b])
    P.op("dve", lambda e: e.tensor_scalar(out=r_t[:, 0:1], in0=m_t[:, 1:2], scalar1=NORM_EPS, scalar2=None, op0=ALU.add), reads=[m_b], writes=[r_b])
    P.op("act", lambda e: e.activation(out=r_t[:, 1:2], in_=r_t[:, 0:1], func=AF.Sqrt), reads=[r_b], writes=[r_b])
    P.op("dve", lambda e: e.reciprocal(out=r_t[:, 0:1], in_=r_t[:, 1:2]), reads=[r_b], writes=[r_b])
    P.op("dve", lambda e: e.tensor_scalar(out=o_t[:], in0=x_t[:], scalar1=m_t[:, 0:1], scalar2=r_t[:, 0:1], op0=ALU.subtract, op1=ALU.mult),
         reads=[x_b, m_b, r_b], writes=[o_b])
    P.op("pool", lambda e: e.tensor_tensor(out=o_t[:], in0=o_t[:], in1=bct[:, gi, :], op=ALU.mult), reads=[o_b, bct_b], writes=[o_b])
    P.op("pool", lambda e: e.tensor_tensor(out=o_t[:], in0=o_t[:], in1=bct[:, bi, :], op=ALU.add), reads=[o_b, bct_b], writes=[o_b])


def _routing(P, pt, po, pb, gate, bct, bct_b, rt, r8, n_exp):
    g_t, g_b = gate
    t_t, t_b = rt
    q_t, q_b = r8
    sc = t_t[:, 0, :]
    bi = t_t[:, 1, :]
    tmp = t_t[:, 2, :]
    msk = t_t[:, 3, :]
    sel = t_t[:, 4, :]
    v3 = lambda ap: ap.rearrange("p (a b) -> p a b", a=8)
    P.op("act", lambda e: e.activation(out=sc, in_=pt[:, po:po + 64], func=AF.Sigmoid), reads=[pb], writes=[t_b])
    P.op("dve", lambda e: e.tensor_tensor(out=bi, in0=sc, in1=bct[:, 5, 0:64], op=ALU.add), reads=[t_b, bct_b], writes=[t_b])
    P.op("dve", lambda e: e.tensor_reduce(out=q_t[:, 0, :], in_=v3(bi), axis=AX.X, op=ALU.max), reads=[t_b], writes=[q_b])
    P.op("dve", lambda e: e.tensor_tensor(out=v3(tmp), in0=v3(bi), in1=q_t[:, 0, :].unsqueeze(2).to_broadcast([128, 8, 8]), op=ALU.is_equal),
         reads=[t_b, q_b], writes=[t_b])
    P.op("dve", lambda e: e.scalar_tensor_tensor(out=tmp, in0=tmp, scalar=-BIG, in1=bi, op0=ALU.mult, op1=ALU.add), reads=[t_b], writes=[t_b])
    P.op("dve", lambda e: e.tensor_reduce(out=q_t[:, 1, :], in_=v3(tmp), axis=AX.X, op=ALU.max), reads=[t_b], writes=[q_b])
    P.op("dve", lambda e: e.tensor_tensor(out=q_t[:, 2, :], in0=q_t[:, 0, :], in1=q_t[:, 1, :], op=ALU.add), reads=[q_b], writes=[q_b])
    P.op("dve", lambda e: e.max(out=q_t[:, 3, :], in_=q_t[:, 2, :]), reads=[q_b], writes=[q_b])
    P.op("dve", lambda e: e.tensor_scalar(out=q_t[:, 4, :], in0=q_t[:, 2, :], scalar1=q_t[:, 3, 3:4], scalar2=None, op0=ALU.is_ge), reads=[q_b], writes=[q_b])
    P.op("dve", lambda e: e.tensor_scalar(out=q_t[:, 4, :], in0=q_t[:, 4, :], scalar1=1.0, scalar2=BIG, op0=ALU.subtract, op1=ALU.mult), reads=[q_b], writes=[q_b])
    P.op("dve", lambda e: e.tensor_tensor(out=v3(msk), in0=v3(bi), in1=q_t[:, 4, :].unsqueeze(2).to_broadcast([128, 8, 8]), op=ALU.add),
         reads=[t_b, q_b], writes=[t_b])
    P.op("dve", lambda e: e.max(out=q_t[:, 5, :], in_=msk), reads=[t_b], writes=[q_b])
    P.op("dve", lambda e: e.tensor_scalar(out=sel, in0=msk, scalar1=q_t[:, 5, 7:8], scalar2=None, op0=ALU.is_ge), reads=[t_b, q_b], writes=[t_b])
    P.op("dve", lambda e: e.tensor_tensor(out=sel, in0=sel, in1=sc, op=ALU.mult), reads=[t_b], writes=[t_b])
    P.op("dve", lambda e: e.tensor_reduce(out=q_t[:, 6, 0:1], in_=sel, axis=AX.X, op=ALU.add), reads=[t_b], writes=[q_b])
    P.op("dve", lambda e: e.reciprocal(out=q_t[:, 6, 1:2], in_=q_t[:, 6, 0:1]), reads=[q_b], writes=[q_b])
    P.op("dve", lambda e: e.tensor_scalar(out=g_t[:, 0:n_exp], in0=sel, scalar1=q_t[:, 6, 1:2], scalar2=ROUTE_SCALE, op0=ALU.mult, op1=ALU.mult),
         reads=[t_b, q_b], writes=[g_b])
    P.op("dve", lambda e: e.memset(g_t[:, n_exp:n_exp + 1], 1.0), reads=[], writes=[g_b])


NEG = -240000.0
COLS_A = [128] * 10 + [64] * 4 + [128] * 3
OFF_A = [int(v) for v in np.cumsum([0] + COLS_A)]


def _nsa_tables(S):
    h = np.arange(1, 17, dtype=np.float64)
    slopes = 2.0 ** (-8.0 * h / 16.0)
    import ml_dtypes
    bf = lambda a: np.asarray(a, np.float32).astype(ml_dtypes.bfloat16).astype(np.float32)
    t = {}
    pos = np.arange(S)
    ka = np.zeros((7, S), np.float32)
    ka[0] = ka[1] = (pos // 128) * 128
    ka[2] = ka[3] = pos % 128
    ka[4:] = 1.0
    t["kaug"] = ka
    nn = np.arange(512)
    ends = nn * 16 + 31
    kc = np.zeros((7, 512), np.float32)
    kc[0] = kc[1] = (ends // 128) * 128
    kc[2] = kc[3] = ends % 128
    kc[4:] = 1.0
    t["kaug_c"] = kc
    qa = np.zeros((4, 7, 4, S), np.float32)
    for g in range(4):
        for j in range(4):
            sl = np.float32(slopes[g * 4 + j] * 8.0)
            sh = bf(sl)
            slo = bf(sl - sh)
            Q = (-(np.float64(sh) + np.float64(slo)) * pos).astype(np.float32)
            qh = bf(Q)
            ql = bf(Q - qh)
            qll = bf(Q - qh - ql)
            qa[g, 0, j] = sh
            qa[g, 1, j] = slo
            qa[g, 2, j] = sh
            qa[g, 3, j] = slo
            qa[g, 4, j] = qh
            qa[g, 5, j] = ql
            qa[g, 6, j] = qll
    t["qaug"] = qa
    i = np.arange(128)[:, None]
    rq = np.arange(128)[None, :]
    t["caus4"] = np.tile(np.where(i <= rq, 0.0, NEG).astype(np.float32), (1, 4))
    t["low4"] = np.tile(np.where(i > rq, 0.0, NEG).astype(np.float32), (1, 4))
    y = np.arange(S)[None, :]
    t["EW"] = (y // 64 == np.arange(128)[:, None]).astype(np.float32)
    yy = np.arange(2432)[None, :]
    t["Zm"] = np.where(yy >= 16 * i + 128, 0.0, NEG).astype(np.float32)
    yf = np.arange(256)[None, :]
    rel = yf - 126
    c = (np.arange(128)[:, None] >= 64).astype(np.int64)
    F = np.zeros((128, 256), np.float32)
    F[(rel == c) | (rel == c - 1)] = 1e30
    F[rel > c] = -1e30
    t["F"] = F
    n = np.arange(512)[:, None] * 16
    m = np.arange(128)[None, :] * 64
    t["ov"] = ((n < m + 64) & (n + 32 > m)).astype(np.float32).reshape(4, 128, 128).transpose(1, 0, 2).copy()
    t["U"] = (i <= rq).astype(np.float32)
    t["ones"] = np.ones((128, 128), np.float32)
    t["ident"] = np.eye(128, dtype=np.float32)
    return t


def build_l1(S, NS, NPG=16, stop=None, nphys=None):
    try:
        return _build_l1(S, NS, NPG, stop, nphys)
    except _Stop as ex:
        return ex.args[0]


def _build_l1(S, NS, NPG, stop, nphys):
    BW = 256
    nc = bass.Bass("TRN2", target_bir_lowering=False)
    dr = lambda name, shape, dt=F32, kind="ExternalInput": nc.dram_tensor(name, list(shape), dt, kind=kind).ap()
    NTL = S // 128
    SS = NPG * 128 + 128
    NTS = SS // 128
    NPHYS = nphys or (128 * NPG * 5 + 3) // 4
    xT = dr("xT", [1024, S])
    wA = dr("wA", [1024, OFF_A[-1]])
    bA = dr("bA", [128, 17])
    wT = dr("wT", [1024, 652])
    bT = dr("bT", [128, 652])
    dtb = dr("dtb", [128, 512])
    alog = dr("alog", [128, 512])
    cw = dr("cw", [128, 6, 4])
    cb = dr("cb", [128, 6])
    dsk = dr("dsk", [128, 4])
    nw = dr("nw", [128, 4])
    w1kv = dr("w1kv", [128, 32, 128])
    pekv = dr("pekv", [128, 32])
    b1 = dr("b1", [128, 2])
    w2kv = dr("w2kv", [128, 128])
    b2k = dr("b2k", [64, 1])
    b2v = dr("b2v", [128, 64])
    kaug = dr("kaug", [7, max(S, SS)])
    kaug_c = dr("kaug_c", [7, 512])
    qaug = dr("qaug", [7, 4, max(S, SS)])
    caus4 = dr("caus4", [128, 512])
    low4 = dr("low4", [128, 512])
    EW = dr("EW", [128, max(S, SS)])
    Zm = dr("Zm", [128, 2432])
    Ftab = dr("F", [128, 256])
    ov = dr("ov", [128, 4, 128])
    Ut = dr("U", [128, 128])
    ones_d = dr("ones", [128, 128])
    ident = dr("ident", [128, 128])
    xTs = dr("xTs", [1024, NS])
    cache = dr("cache", [NPHYS * 128, 256])
    pt_rep = dr("pt_rep", [128, NS * NPG], I32)
    kvwin_s = dr("kvwin_s", [NS, 512, 128])
    ssm_s = dr("ssm_s", [NS, 512, 128])
    convT_s = dr("convT_s", [3, 768, NS])
    colp = dr("colp", [128, 3, 4])
    yT = dr("yT", [768, S], kind="ExternalOutput")
    kvT = dr("kvT", [256, S], kind="ExternalOutput")
    winT = dr("winT", [128, 512], kind="ExternalOutput")
    ssm_p = dr("ssm_p", [512, 128], kind="ExternalOutput")
    convT_p = dr("convT_p", [768, 3], kind="ExternalOutput")
    ysT = dr("ysT", [512, NS], kind="ExternalOutput")
    ynS = dr("ynS", [NS, 256], kind="ExternalOutput")
    kvS = dr("kvS", [256, NS], kind="ExternalOutput")
    winS = dr("winS", [NS, 511, 128], kind="ExternalOutput")
    ssm_o = dr("ssm_o", [NS, 512, 128], kind="ExternalOutput")
    convS = dr("convS", [3, 768, NS], kind="ExternalOutput")
    winnewT = dr("winnewT", [128, NS], kind="ExternalOutput")

    with ExitStack() as es:
        P = Prog(nc, es)
        op = P.op
        pst = [P.ps("ps%d" % i, [128, 1024], F32) for i in range(4)]
        psh = []
        for i in range(4):
            psh.append((pst[i][0], 0, P.buf("psb%da" % i)))
            psh.append((pst[i][0], 512, P.buf("psb%db" % i)))
        rr = [0]

        def next_ps():
            r = psh[6 + rr[0] % 2]
            rr[0] += 1
            return r

        def const(name, src, shape, dt=F32, q="sp"):
            t_, b_ = P.sb(name, shape, dt)
            P.dma("pool" if dt != F32 else q, t_[:], src, writes=[b_])
            return t_, b_

        U_t, U_b = const("U", Ut, [128, 128])
        one_t, one_b = const("ones", ones_d, [128, 128])
        id_t, id_b = const("identf", ident, [128, 128])
        idb_t, idb_b = const("identb", ident, [128, 128], BF16)
        caus_t, caus_b = const("caus4", caus4, [128, 512], BF16)
        low_t, low_b = const("low4", low4, [128, 512], BF16)
        EW_t, EW_b = const("EW", EW, [128, max(S, SS)], BF16)
        Zm_t, Zm_b = const("Zm", Zm, [128, 2432], BF16)
        F_t, F_b = const("Ft", Ftab, [128, 256])
        wA_t, wA_b = P.sb("wA", [128, 8, OFF_A[-1]], BF16)
        for k in range(8):
            P.dma("pool", wA_t[:, k, :], wA[k * 128:(k + 1) * 128, :], writes=[wA_b])
        wT_t, wT_b = P.sb("wT", [128, 8, 652], BF16)
        for k in range(8):
            P.dma("pool", wT_t[:, k, :], wT[k * 128:(k + 1) * 128, :], writes=[wT_b])
        bA_t, bA_b = const("bA", bA, [128, 17])
        bT_t, bT_b = const("bT", bT, [128, 652])
        dtb_t, dtb_b = const("dtb", dtb, [128, 512])
        A_t, A_b = const("Arep", alog, [128, 512])
        cw_t, cw_b = const("cw", cw, [128, 6, 4])
        cb_t, cb_b = const("cb", cb, [128, 6])
        dsk_t, dsk_b = const("dsk", dsk, [128, 4])
        nw_t, nw_b = const("nw", nw, [128, 4])
        w1_t, w1_b = const("w1kv", w1kv, [128, 32, 128], BF16)
        pe_t, pe_b = const("pekv", pekv, [128, 32])
        b1_t, b1_b = const("b1", b1, [128, 2])
        w2_t, w2_b = const("w2kv", w2kv, [128, 128], BF16)
        b2k_t, b2k_b = const("b2k", b2k, [64, 1])
        b2v_t, b2v_b = const("b2v", b2v, [128, 64])
        op("act", lambda e: e.activation(out=A_t[:], in_=A_t[:], func=AF.Exp), reads=[A_b], writes=[A_b])
        op("dve", lambda e: e.tensor_scalar(out=A_t[:], in0=A_t[:], scalar1=-1.0, scalar2=None, op0=ALU.mult), reads=[A_b], writes=[A_b])
        op("dve", lambda e: e.tensor_tensor(out=dtb_t[:], in0=dtb_t[:], in1=bT_t[:, 0:512], op=ALU.add), reads=[dtb_b, bT_b], writes=[dtb_b])
        b1p_t, b1p_b = P.sb("b1p", [128, 2], F32)
        es_tmp = ExitStack()
        P.es = es_tmp
        w1f_t, w1f_b = const("w1kvf", w1kv, [128, 32, 128])
        P.es = es
        for tkv in range(2):
            pt, po, pb = next_ps()
            for l in range(32):
                op("pe", lambda e: e.matmul(pt[:, po:po + 1], lhsT=w1f_t[tkv * 64:(tkv + 1) * 64, l, :], rhs=pe_t[tkv * 64:(tkv + 1) * 64, l:l + 1],
                                            start=(l == 0), stop=(l == 31)), reads=[w1f_b, pe_b], writes=[pb])
            op("dve", lambda e: e.tensor_tensor(out=b1p_t[:, tkv:tkv + 1], in0=pt[:, po:po + 1], in1=b1_t[:, tkv:tkv + 1], op=ALU.add),
               reads=[pb, b1_b], writes=[b1p_b])
        P.barrier()
        es_tmp.close()

        def alloc_kv(pref, Sx, NCx):
            d = {}
            d["Sx"] = Sx
            d["kvc"] = P.sb(pref + "kvc", [128, Sx], BF16)
            d["ksl"] = P.sb(pref + "ksl", [71, Sx], BF16)
            d["kwn"] = P.sb(pref + "kwn", [71, 1024], BF16)
            d["vsl"] = P.sb(pref + "vsl", [128, Sx // 128, 65], BF16)
            d["vwn"] = P.sb(pref + "vwn", [128, 8, 65], BF16)
            d["kc"] = P.sb(pref + "kc", [71, NCx * 128], BF16)
            d["vc"] = P.sb(pref + "vc", [128, NCx, 193], BF16)
            d["qa"] = P.sb(pref + "qa", [71, 4, 128], BF16)
            d["NCx"] = NCx
            P.dma("pool", d["ksl"][0][64:71, :], kaug[:, 0:Sx], writes=[d["ksl"][1]])
            P.dma("pool", d["kc"][0][64:71, :], kaug_c[:, 0:NCx * 128], writes=[d["kc"][1]])
            P.dma("pool", d["vc"][0][:, :, 65:193], ov[:, 0:NCx, :], writes=[d["vc"][1]])
            op("dve", lambda e: e.memset(d["vsl"][0][:, :, 64:65], 1.0), writes=[d["vsl"][1]])
            op("dve", lambda e: e.memset(d["vwn"][0][:, :, 64:65], 1.0), writes=[d["vwn"][1]])
            op("dve", lambda e: e.memset(d["vc"][0][:, :, 64:65], 1.0), writes=[d["vc"][1]])
            return d

        scr = {}
        scr["hid"] = P.sb("hid", [128, 512], BF16)
        scr["pT"] = [P.sb("pT%d" % i, [128, 512], BF16) for i in range(3)]
        scr["imp"] = P.sb("imp", [128, 4, 128], F32)
        scr["sadd"] = P.sb("sadd", [128, 512], F32)
        scr["m8"] = P.sb("m8", [128, 16], F32)
        scr["nst4"] = P.sb("nst4", [128, 4, 128], BF16)
        scr["rd"] = P.sb("rd", [128, 16], F32)
        scr["osb"] = P.sb("osb", [128, 3, 4, 64], F32)
        scr["ny"] = P.sb("ny", [128, 256], F32)
        scr["nyT"] = P.sb("nyT", [128, 2, 128], F32)
        pcnt = [0]

        def compress(kv, n0, nn):
            kvc_t, kvc_b = kv["kvc"]
            kc_t, kc_b = kv["kc"]
            vc_t, vc_b = kv["vc"]
            h_t, h_b = scr["hid"]
            for tkv in range(2):
                pt, po, pb = next_ps()
                lo = tkv * 64
                for l in range(32):
                    rhs = kvc_t[lo:lo + 64, n0 * 16 + l:n0 * 16 + l + (nn - 1) * 16 + 1:16]
                    op("pe", lambda e: e.matmul(pt[:, po:po + nn], lhsT=w1_t[lo:lo + 64, l, :], rhs=rhs, start=(l == 0), stop=(l == 31)),
                       reads=[w1_b, kvc_b], writes=[pb])
                op("act", lambda e: e.activation(out=h_t[:, :nn], in_=pt[:, po:po + nn], func=AF.Silu, bias=b1p_t[:, tkv:tkv + 1], scale=1.0),
                   reads=[pb, b1p_b], writes=[h_b])
                if tkv == 0:
                    p2, o2, b2 = next_ps()
                    op("pe", lambda e: e.matmul(p2[0:64, o2:o2 + nn], lhsT=w2_t[:, 0:64], rhs=h_t[:, :nn], start=True, stop=True),
                       reads=[w2_b, h_b], writes=[b2])
                    op("act", lambda e: e.activation(out=kc_t[0:64, n0:n0 + nn], in_=p2[0:64, o2:o2 + nn], func=AF.Identity, bias=b2k_t[:, 0:1], scale=1.0),
                       reads=[b2, b2k_b], writes=[kc_b])
                else:
                    for c0 in range(0, nn, 128):
                        w = min(128, nn - c0)
                        assert (n0 + c0) % 128 == 0
                        p2, o2, b2 = next_ps()
                        op("pe", lambda e: e.matmul(p2[0:w, o2:o2 + 64], lhsT=h_t[:, c0:c0 + w], rhs=w2_t[:, 64:128], start=True, stop=True),
                           reads=[w2_b, h_b], writes=[b2])
                        op("dve", lambda e: e.tensor_tensor(out=vc_t[0:w, (n0 + c0) // 128, 0:64], in0=p2[0:w, o2:o2 + 64], in1=b2v_t[0:w, :], op=ALU.add),
                           reads=[b2, b2v_b], writes=[vc_b])

        def attn_branch(kv, t, kname, vname, ktiles, mask_fn, acc, ncols, vcols):
            k_t, k_b = kv[kname]
            v_t, v_b = kv[vname]
            qa_t, qa_b = kv["qa"]
            n = len(ktiles)

            def qk(idx):
                kt = ktiles[idx]
                kk = kt % 8 if kname == "kwn" else kt
                pt, po, pb = next_ps()
                extra = mask_fn(kt)
                mm_extra = [x for x in extra if x[0] == "mm"]
                post = [x for x in extra if x[0] == "mul"]
                pre_add = [x for x in extra if x[0] == "add"]
                op("pe", lambda e: e.matmul(pt[:, po:po + 512], lhsT=k_t[0:71, kk * 128:(kk + 1) * 128], rhs=qa_t[0:71, :, :],
                                            start=True, stop=(len(mm_extra) == 0)), reads=[k_b, qa_b], writes=[pb])
                for i_, (_, l_, r_, rd_) in enumerate(mm_extra):
                    op("pe", lambda e: e.matmul(pt[:, po:po + 512], lhsT=l_, rhs=r_, start=False, stop=(i_ == len(mm_extra) - 1)),
                       reads=rd_, writes=[pb])
                p_t, p_b = scr["pT"][pcnt[0] % 3]
                pcnt[0] += 1
                if pre_add:
                    sc_t, sc_b = scr["sadd"]
                    (_, m_ap, rd_) = pre_add[0]
                    op("dve", lambda e: e.tensor_tensor(out=sc_t[:].rearrange("p (h q) -> p h q", h=4), in0=pt[:, po:po + 512].rearrange("p (h q) -> p h q", h=4),
                                                        in1=m_ap, op=ALU.add), reads=[pb] + rd_, writes=[sc_b])
                    op("act", lambda e: e.activation(out=p_t[:], in_=sc_t[:], func=AF.Exp, scale=0.125), reads=[sc_b], writes=[p_b])
                else:
                    op("act", lambda e: e.activation(out=p_t[:], in_=pt[:, po:po + 512], func=AF.Exp, scale=0.125), reads=[pb], writes=[p_b])
                for (_, m_ap, rd_) in post:
                    op("dve", lambda e: e.tensor_tensor(out=p_t[:].rearrange("p (h q) -> p h q", h=4), in0=p_t[:].rearrange("p (h q) -> p h q", h=4),
                                                        in1=m_ap, op=ALU.mult), reads=[p_b] + rd_, writes=[p_b])
                return (kk, p_t, p_b)

            def pv(idx, st):
                kk, p_t, p_b = st
                for h in range(4):
                    at, ao, ab = acc[h // 2]
                    co = ao + (h % 2) * ncols
                    op("pe", lambda e: e.matmul(at[:, co:co + vcols], lhsT=p_t[:, h * 128:(h + 1) * 128], rhs=v_t[:, kk, 0:vcols],
                                                start=(idx == 0 and h % 2 == 0), stop=(idx == n - 1 and h % 2 == 1)), reads=[p_b, v_b], writes=[ab])
            st = qk(0)
            for idx in range(n):
                nxt = qk(idx + 1) if idx + 1 < n else None
                pv(idx, st)
                st = nxt

        def nsa_qtile(kv, t, gate_ap, gate_b, out_fn):
            i_t, i_b = scr["imp"]
            m_t, m_b = scr["m8"]
            n_t, n_b = scr["nst4"]
            r_t, r_b = scr["rd"]
            o_t, o_b = scr["osb"]
            nvalid = min(8 * t + 7, kv["NCx"] * 128)
            cchunks = list(range((nvalid + 127) // 128))
            accc = [psh[0], psh[1]]

            def cmask(c):
                K = 2048 * c - 128 * t + 31
                if 16 * 127 + K <= 0:
                    return []
                o = 128 - K
                assert 0 <= o <= 2432 - 128
                return [("add", Zm_t[:, o:o + 128].unsqueeze(1).to_broadcast([128, 4, 128]), [Zm_b])]
            if cchunks:
                attn_branch(kv, t, "kc", "vc", cchunks, cmask, accc, 193, 193)
            else:
                for (at, ao, ab) in accc:
                    op("dve", lambda e: e.memset(at[:, ao:ao + 386], 0.0), writes=[ab])
            for h in range(4):
                at, ao, ab = accc[h // 2]
                co = ao + (h % 2) * 193
                op("dve", lambda e: e.tensor_scalar(out=r_t[:, h:h + 1], in0=at[:, co + 64:co + 65], scalar1=1e-30, scalar2=None, op0=ALU.max),
                   reads=[ab], writes=[r_b])
            op("dve", lambda e: e.reciprocal(out=r_t[:, 0:4], in_=r_t[:, 0:4]), reads=[r_b], writes=[r_b])
            for h in range(4):
                at, ao, ab = accc[h // 2]
                co = ao + (h % 2) * 193
                if h == 0:
                    op("dve", lambda e: e.tensor_scalar(out=i_t[:, 0, :], in0=at[:, co + 65:co + 193], scalar1=r_t[:, 0:1], scalar2=None, op0=ALU.mult),
                       reads=[ab, r_b], writes=[i_b])
                else:
                    op("dve", lambda e: e.scalar_tensor_tensor(out=i_t[:, 0, :], in0=at[:, co + 65:co + 193], scalar=r_t[:, h:h + 1], in1=i_t[:, 0, :],
                                                               op0=ALU.mult, op1=ALU.add), reads=[ab, r_b, i_b], writes=[i_b])
                op("dve", lambda e: e.tensor_copy(out=o_t[:, 0, h, :], in_=at[:, co:co + 64]), reads=[ab], writes=[o_b])
            accw = [psh[4], psh[5]]

            def wmask(kt):
                if kt == t:
                    return [("mm", idb_t[:], caus_t[:], [idb_b, caus_b])]
                if kt == t - 4:
                    return [("mm", idb_t[:], low_t[:], [idb_b, low_b])]
                return []
            attn_branch(kv, t, "kwn", "vwn", [k_ for k_ in range(t - 4, t + 1) if k_ >= 0], wmask, accw, 65, 65)
            o2 = 126 - 2 * t
            op("dve", lambda e: e.tensor_tensor(out=i_t[:, 0, :], in0=i_t[:, 0, :], in1=F_t[:, o2:o2 + 128], op=ALU.add), reads=[i_b, F_b], writes=[i_b])
            op("dve", lambda e: e.memset(i_t[:, 0, 0:1], 1e30), writes=[i_b])
            op("dve", lambda e: e.max(out=m_t[:, 0:8], in_=i_t[:, 0, :]), reads=[i_b], writes=[m_b])
            op("dve", lambda e: e.match_replace(out=i_t[:, 1, :], in_to_replace=m_t[:, 0:8], in_values=i_t[:, 0, :], imm_value=-3.0e38),
               reads=[i_b, m_b], writes=[i_b])
            op("dve", lambda e: e.max(out=m_t[:, 8:16], in_=i_t[:, 1, :]), reads=[i_b], writes=[m_b])
            op("dve", lambda e: e.tensor_scalar(out=m_t[:, 15:16], in0=m_t[:, 15:16], scalar1=-1e29, scalar2=None, op0=ALU.max), reads=[m_b], writes=[m_b])
            op("dve", lambda e: e.tensor_scalar(out=i_t[:, 2, :], in0=i_t[:, 0, :], scalar1=m_t[:, 15:16], scalar2=None, op0=ALU.is_ge),
               reads=[i_b, m_b], writes=[i_b])
            op("dve", lambda e: e.tensor_scalar(out=i_t[:, 2, :], in0=i_t[:, 2, :], scalar1=1.0, scalar2=-NEG, op0=ALU.subtract, op1=ALU.mult),
               reads=[i_b], writes=[i_b])
            pt, po, pb = next_ps()
            op("pe", lambda e: e.transpose(out=pt[:, po:po + 128], in_=i_t[:, 2, :], identity=id_t[:]), reads=[i_b, id_b], writes=[pb])
            op("dve", lambda e: e.tensor_copy(out=n_t[:], in_=pt[:, po:po + 128].unsqueeze(1).to_broadcast([128, 4, 128])), reads=[pb], writes=[n_b])
            accs = [psh[2], psh[3]]

            def smask(kt):
                r = [("mm", EW_t[:, kt * 128:(kt + 1) * 128], n_t[:].rearrange("p h q -> p (h q)"), [EW_b, n_b])]
                if kt == t:
                    r.append(("mm", idb_t[:], caus_t[:], [idb_b, caus_b]))
                return r
            attn_branch(kv, t, "ksl", "vsl", list(range(t + 1)), smask, accs, 65, 65)
            for bi_, acc_ in ((1, accs), (2, accw)):
                for h in range(4):
                    at, ao, ab = acc_[h // 2]
                    co = ao + (h % 2) * 65
                    op("dve", lambda e: e.tensor_copy(out=r_t[:, bi_ * 4 + h:bi_ * 4 + h + 1], in_=at[:, co + 64:co + 65]), reads=[ab], writes=[r_b])
                    op("dve", lambda e: e.tensor_copy(out=o_t[:, bi_, h, :], in_=at[:, co:co + 64]), reads=[ab], writes=[o_b])
            op("dve", lambda e: e.reciprocal(out=r_t[:, 4:12], in_=r_t[:, 4:12]), reads=[r_b], writes=[r_b])
            op("dve", lambda e: e.tensor_tensor(out=r_t[:, 0:12].rearrange("p (b h) -> p b h", b=3), in0=r_t[:, 0:12].rearrange("p (b h) -> p b h", b=3),
                                                in1=gate_ap.rearrange("p (h b) -> p b h", b=3), op=ALU.mult), reads=[r_b, gate_b], writes=[r_b])
            y_t, y_b = scr["ny"]
            for h in range(4):
                op("dve", lambda e: e.tensor_scalar(out=y_t[:, h * 64:(h + 1) * 64], in0=o_t[:, 0, h, :], scalar1=r_t[:, h:h + 1], scalar2=None, op0=ALU.mult),
                   reads=[o_b, r_b], writes=[y_b])
                for bi_ in (1, 2):
                    op("dve", lambda e: e.scalar_tensor_tensor(out=y_t[:, h * 64:(h + 1) * 64], in0=o_t[:, bi_, h, :], scalar=r_t[:, bi_ * 4 + h:bi_ * 4 + h + 1],
                                                               in1=y_t[:, h * 64:(h + 1) * 64], op0=ALU.mult, op1=ALU.add), reads=[o_b, r_b, y_b], writes=[y_b])
            out_fn(y_t, y_b)

        ssd = {}
        for nm, shp, dt_ in (("sq", [128, 4, 128], F32), ("rstd", [128, 128], F32), ("yo", [128, 4, 128], F32)):
            ssd[nm] = P.sb("ssd_" + nm, shp, dt_)
        def ssd_chunk(c, blk, col0):
            cs = slice(col0, col0 + 128)
            xTb_t, xTb_b = blk["xTb"]
            T = lambda nm: ssd[nm][0]
            Bf = lambda nm: ssd[nm][1]
            pt, po, pb = psh[0]
            for k in range(8):
                op("pe", lambda e: e.matmul(pt[:, po:po + 512], lhsT=xTb_t[:, k, cs], rhs=wT_t[:, k, 0:512], start=(k == 0), stop=(k == 7)),
                   reads=[xTb_b, wT_b], writes=[pb])
            op("dve", lambda e: e.tensor_tensor(out=T("dt")[:], in0=pt[:, po:po + 512], in1=dtb_t[:], op=ALU.add), reads=[pb, dtb_b], writes=[Bf("dt")])
            op("act", lambda e: e.activation(out=T("dt")[:], in_=T("dt")[:], func=AF.Exp), reads=[Bf("dt")], writes=[Bf("dt")])
            op("act", lambda e: e.activation(out=T("dt")[:], in_=T("dt")[:], func=AF.Ln, bias=1.0, scale=1.0), reads=[Bf("dt")], writes=[Bf("dt")])
            op("dve", lambda e: e.tensor_tensor(out=T("dtA")[:], in0=T("dt")[:], in1=A_t[:], op=ALU.mult), reads=[Bf("dt"), A_b], writes=[Bf("dtA")])
            p1, o1, b1_ = psh[1]
            xs_t, xs_b = blk["xsT"]
            for k in range(4):
                op("pe", lambda e: e.transpose(out=p1[:, o1 + k * 128:o1 + (k + 1) * 128], in_=xs_t[:, k, cs], identity=id_t[:]),
                   reads=[xs_b, id_b], writes=[b1_])
            op("dve", lambda e: e.tensor_copy(out=T("xs")[:], in_=p1[:, o1:o1 + 512]), reads=[b1_], writes=[Bf("xs")])
            p2, o2, b2_ = psh[2]
            BT_t, BT_b = blk["BT"]
            op("pe", lambda e: e.transpose(out=p2[:, o2:o2 + 128], in_=BT_t[:, cs], identity=id_t[:]), reads=[BT_b, id_b], writes=[b2_])
            op("dve", lambda e: e.tensor_copy(out=T("Btb")[:], in_=p2[:, o2:o2 + 128]), reads=[b2_], writes=[Bf("Btb")])
            p3, o3, b3_ = psh[3]
            op("pe", lambda e: e.matmul(p3[:, o3:o3 + 512], lhsT=U_t[:], rhs=T("dtA")[:], start=True, stop=True), reads=[U_b, Bf("dtA")], writes=[b3_])
            p4, o4, b4_ = psh[4]
            op("pe", lambda e: e.matmul(p4[:, o4:o4 + 512], lhsT=one_t[:], rhs=T("dtA")[:], start=True, stop=True), reads=[one_b, Bf("dtA")], writes=[b4_])
            op("dve", lambda e: e.tensor_copy(out=T("acs")[:], in_=p3[:, o3:o3 + 512]), reads=[b3_], writes=[Bf("acs")])
            op("dve", lambda e: e.tensor_tensor(out=T("w")[:], in0=p4[:, o4:o4 + 512], in1=T("acs")[:], op=ALU.subtract), reads=[b4_, Bf("acs")], writes=[Bf("w")])
            op("act", lambda e: e.activation(out=T("w")[:], in_=T("w")[:], func=AF.Exp), reads=[Bf("w")], writes=[Bf("w")])
            op("act", lambda e: e.activation(out=T("dec")[:], in_=p4[:, o4:o4 + 512], func=AF.Exp), reads=[b4_], writes=[Bf("dec")])
            op("dve", lambda e: e.tensor_tensor(out=T("xdt")[:], in0=T("xs")[:], in1=T("dt")[:], op=ALU.mult), reads=[Bf("xs"), Bf("dt")], writes=[Bf("xdt")])
            op("pool", lambda e: e.tensor_copy(out=T("xdtb")[:], in_=T("xdt")[:]), reads=[Bf("xdt")], writes=[Bf("xdtb")])
            op("dve", lambda e: e.tensor_tensor(out=T("xdtw")[:], in0=T("xdt")[:], in1=T("w")[:], op=ALU.mult), reads=[Bf("xdt"), Bf("w")], writes=[Bf("xdtw")])
            p5, o5, b5_ = psh[5]
            op("pe", lambda e: e.matmul(p5[:, o5:o5 + 512], lhsT=T("Btb")[:], rhs=T("xdtw")[:], start=True, stop=True), reads=[Bf("Btb"), Bf("xdtw")], writes=[b5_])
            p6, o6, b6_ = psh[6]
            CTb_t, CTb_b = blk["CTb"]
            BTb_t, BTb_b = blk["BTb"]
            for k in range(4):
                op("pe", lambda e: e.matmul(p6[:, o6 + k * 128:o6 + (k + 1) * 128], lhsT=T("hTb")[:, k * 128:(k + 1) * 128], rhs=CTb_t[:, cs], start=True, stop=True),
                   reads=[Bf("hTb"), CTb_b], writes=[b6_])
            p7, o7, b7_ = psh[7]
            for k in range(4):
                op("pe", lambda e: e.matmul(p7[:, o7 + k * 128:o7 + (k + 1) * 128], lhsT=T("dtA")[:, k * 128:(k + 1) * 128], rhs=U_t[:], start=True, stop=True),
                   reads=[Bf("dtA"), U_b], writes=[b7_])
            op("act", lambda e: e.activation(out=T("ea")[:].rearrange("p k l -> p (k l)"), in_=p7[:, o7:o7 + 512], func=AF.Exp), reads=[b7_], writes=[Bf("ea")])
            op("pe", lambda e: e.matmul(pt[:, po:po + 128], lhsT=BTb_t[:, cs], rhs=CTb_t[:, cs], start=True, stop=True), reads=[BTb_b, CTb_b], writes=[pb])
            op("dve", lambda e: e.tensor_tensor(out=T("cbm")[:], in0=pt[:, po:po + 128], in1=U_t[:], op=ALU.mult), reads=[pb, U_b], writes=[Bf("cbm")])
            dcol = T("dtA")[:].rearrange("p (j q) -> p j q", q=64)[:, :, 0:1]
            op("dve", lambda e: e.tensor_copy(out=T("L1")[:], in_=dcol.to_broadcast([128, 8, 128])), reads=[Bf("dtA")], writes=[Bf("L1")])
            op("dve", lambda e: e.scalar_tensor_tensor(out=T("L2")[:], in0=T("L1")[:], scalar=-1.0, in1=U_t[:].unsqueeze(1).to_broadcast([128, 8, 128]),
                                                       op0=ALU.mult, op1=ALU.mult), reads=[Bf("L1"), U_b], writes=[Bf("L2")])
            for half, (pd, od, bd) in enumerate((psh[1], psh[2])):
                for jj in range(4):
                    j = half * 4 + jj
                    op("pe", lambda e: e.matmul(pd[:, od + jj * 128:od + (jj + 1) * 128], lhsT=T("L1")[:, j, :], rhs=U_t[:], start=True, stop=False),
                       reads=[Bf("L1"), U_b], writes=[bd])
                    op("pe", lambda e: e.matmul(pd[:, od + jj * 128:od + (jj + 1) * 128], lhsT=T("L2")[:, j, :], rhs=one_t[:], start=False, stop=True),
                       reads=[Bf("L2"), one_b], writes=[bd])
                Ef = T("E")[:].rearrange("p k l -> p (k l)")
                op("dve", lambda e: e.tensor_scalar(out=Ef, in0=pd[:, od:od + 512], scalar1=0.0, scalar2=None, op0=ALU.min), reads=[bd], writes=[Bf("E")])
                op("act", lambda e: e.activation(out=Ef, in_=Ef, func=AF.Exp), reads=[Bf("E")], writes=[Bf("E")])
                op("dve", lambda e: e.tensor_tensor(out=T("Mj")[:, half * 4:(half + 1) * 4, :], in0=T("E")[:], in1=T("cbm")[:].unsqueeze(1).to_broadcast([128, 4, 128]),
                                                    op=ALU.mult), reads=[Bf("E"), Bf("cbm")], writes=[Bf("Mj")])
            for j in range(8):
                k, hf = j // 2, j % 2
                op("pe", lambda e: e.matmul(p3[hf * 64:(hf + 1) * 64, o3 + k * 128:o3 + (k + 1) * 128], lhsT=T("xdtb")[:, j * 64:(j + 1) * 64], rhs=T("Mj")[:, j, :],
                                            start=True, stop=True), reads=[Bf("xdtb"), Bf("Mj")], writes=[b3_])
            yf = T("y")[:].rearrange("p k l -> p (k l)")
            op("dve", lambda e: e.tensor_tensor(out=yf, in0=p6[:, o6:o6 + 512], in1=T("ea")[:].rearrange("p k l -> p (k l)"), op=ALU.mult),
               reads=[b6_, Bf("ea")], writes=[Bf("y")])
            op("dve", lambda e: e.tensor_tensor(out=yf, in0=p3[:, o3:o3 + 512], in1=yf, op=ALU.add), reads=[b3_, Bf("y")], writes=[Bf("y")])
            sz_t, sz_b = blk["szT"]
            for k in range(4):
                op("dve", lambda e: e.scalar_tensor_tensor(out=T("y")[:, k, :], in0=xs_t[:, k, cs], scalar=dsk_t[:, k:k + 1], in1=T("y")[:, k, :],
                                                           op0=ALU.mult, op1=ALU.add), reads=[xs_b, dsk_b, Bf("y")], writes=[Bf("y")])
            op("dve", lambda e: e.tensor_tensor(out=T("y")[:], in0=T("y")[:], in1=sz_t[:, :, cs], op=ALU.mult), reads=[Bf("y"), sz_b], writes=[Bf("y")])
            _rms_out(T("y"), Bf("y"), 128, lambda k: yT[k * 128:(k + 1) * 128, c * 128:(c + 1) * 128], p4, o4, b4_)
            op("dve", lambda e: e.tensor_tensor(out=T("tmp")[:], in0=T("hT")[:], in1=T("dec")[:], op=ALU.mult), reads=[Bf("hT"), Bf("dec")], writes=[Bf("tmp")])
            op("dve", lambda e: e.tensor_tensor(out=T("hT")[:], in0=p5[:, o5:o5 + 512], in1=T("tmp")[:], op=ALU.add), reads=[b5_, Bf("tmp")], writes=[Bf("hT")])
            op("pool", lambda e: e.tensor_copy(out=T("hTb")[:], in_=T("hT")[:]), reads=[Bf("hT")], writes=[Bf("hTb")])

        def _rms_out(y_t, y_b, W, dst_fn, pq, oq, bq):
            sq_t, sq_b = ssd["sq"]
            r_t, r_b = ssd["rstd"]
            o_t, o_b = ssd["yo"]
            op("pool", lambda e: e.tensor_tensor(out=sq_t[:, :, :W], in0=y_t[:, :, :W], in1=y_t[:, :, :W], op=ALU.mult), reads=[y_b], writes=[sq_b])
            for k in range(4):
                op("pe", lambda e: e.matmul(pq[:, oq:oq + W], lhsT=one_t[:], rhs=sq_t[:, k, :W], start=(k == 0), stop=(k == 3)), reads=[one_b, sq_b], writes=[bq])
            op("dve", lambda e: e.tensor_scalar(out=r_t[:, :W], in0=pq[:, oq:oq + W], scalar1=1.0 / 512.0, scalar2=NORM_EPS, op0=ALU.mult, op1=ALU.add),
               reads=[bq], writes=[r_b])
            op("act", lambda e: e.activation(out=r_t[:, :W], in_=r_t[:, :W], func=AF.Ln), reads=[r_b], writes=[r_b])
            op("act", lambda e: e.activation(out=r_t[:, :W], in_=r_t[:, :W], func=AF.Exp, scale=-0.5), reads=[r_b], writes=[r_b])
            for k in range(4):
                op("dve", lambda e: e.scalar_tensor_tensor(out=o_t[:, k, :W], in0=y_t[:, k, :W], scalar=nw_t[:, k:k + 1], in1=r_t[:, :W], op0=ALU.mult, op1=ALU.mult),
                   reads=[y_b, nw_b, r_b], writes=[o_b])
            for k in range(4):
                P.dma("sp", dst_fn(k), o_t[:, k, :W], reads=[o_b])

        blkb = {}
        blkb["xTb"] = P.sb("xTb", [128, 8, BW], BF16)
        blkb["pre"] = P.sb("pre", [128, 6, BW + 3], F32)
        blkb["xsT"] = P.sb("xsT", [128, 4, BW], F32)
        blkb["BT"] = P.sb("BT", [128, BW], F32)
        blkb["BTb"] = P.sb("BTb", [128, BW], BF16)
        blkb["CTb"] = P.sb("CTb", [128, BW], BF16)
        blkb["szT"] = P.sb("szT", [128, 4, BW], F32)
        blkb["qb"] = P.sb("qb", [64, 4, BW], BF16)
        blkb["stg"] = [P.sb("stg0", [128, BW], F32)] * 3
        blkb["cacc"] = P.sb("cacc", [128, BW], F32)
        blkb["gate"] = P.sb("gateq", [128, 12], F32)
        blkb["vtok"] = P.sb("vtok", [128, 192], F32)
        op("dve", lambda e: e.memset(blkb["pre"][0][:, :, 0:3], 0.0), writes=[blkb["pre"][1]])

        def inproj_block(xsrc, c0, W, kv, kvcol0, outs, wpos=None):
            xTb_t, xTb_b = blkb["xTb"]
            for k in range(8):
                P.dma("pool", xTb_t[:, k, :W], xsrc[k * 128:(k + 1) * 128, c0:c0 + W], writes=[xTb_b])
            pre_t, pre_b = blkb["pre"]
            sz_t, sz_b = blkb["szT"]
            q_t, q_b = blkb["qb"]
            for ct in range(17):
                M = COLS_A[ct]
                pt, po, pb = next_ps()
                for k in range(8):
                    op("pe", lambda e: e.matmul(pt[0:M, po:po + W], lhsT=wA_t[:, k, OFF_A[ct]:OFF_A[ct] + M], rhs=xTb_t[:, k, :W], start=(k == 0), stop=(k == 7)),
                       reads=[wA_b, xTb_b], writes=[pb])
                bias = bA_t[0:M, ct:ct + 1]
                if ct < 4:
                    op("act", lambda e: e.activation(out=sz_t[:, ct, :W], in_=pt[:, po:po + W], func=AF.Silu, bias=bias, scale=1.0), reads=[pb, bA_b], writes=[sz_b])
                elif ct < 10:
                    op("act", lambda e: e.activation(out=pre_t[:, ct - 4, 3:3 + W], in_=pt[:, po:po + W], func=AF.Identity, bias=bias, scale=1.0),
                       reads=[pb, bA_b], writes=[pre_b])
                elif ct < 14:
                    op("act", lambda e: e.activation(out=q_t[:, ct - 10, :W], in_=pt[0:64, po:po + W], func=AF.Identity, bias=bias, scale=1.0),
                       reads=[pb, bA_b], writes=[q_b])
                else:
                    st_t, st_b = blkb["stg"][ct - 14]
                    op("act", lambda e: e.activation(out=st_t[:, :W], in_=pt[:, po:po + W], func=AF.Identity, bias=bias, scale=1.0), reads=[pb, bA_b], writes=[st_b])
                    if ct == 14:
                        op("pool", lambda e: e.tensor_copy(out=kv["kvc"][0][:, kvcol0:kvcol0 + W], in_=st_t[:, :W]), reads=[st_b], writes=[kv["kvc"][1]])
                    elif ct == 15:
                        op("pool", lambda e: e.tensor_copy(out=kv["ksl"][0][0:64, kvcol0:kvcol0 + W], in_=st_t[0:64, :W]), reads=[st_b], writes=[kv["ksl"][1]])
                    else:
                        rc = kvcol0 % 1024 if wpos is not None else kvcol0
                        op("pool", lambda e: e.tensor_copy(out=kv["kwn"][0][0:64, rc:rc + W], in_=st_t[0:64, :W]), reads=[st_b], writes=[kv["kwn"][1]])
                        if wpos is not None:
                            P.dma("pool", kv["kwn"][0][64:71, rc:rc + W], kaug[:, wpos:wpos + W], writes=[kv["kwn"][1]])
                    outs(ct - 14, st_t, st_b)

        def conv_block(W, hist_fn=None):
            pre_t, pre_b = blkb["pre"]
            a_t, a_b = blkb["cacc"]
            xs_t, xs_b = blkb["xsT"]
            for ci in range(6):
                op("dve", lambda e: e.tensor_scalar(out=a_t[:, :W], in0=pre_t[:, ci, 0:W], scalar1=cw_t[:, ci, 0:1], scalar2=None, op0=ALU.mult),
                   reads=[pre_b, cw_b], writes=[a_b])
                for k in range(1, 4):
                    op("dve", lambda e: e.scalar_tensor_tensor(out=a_t[:, :W], in0=pre_t[:, ci, k:k + W], scalar=cw_t[:, ci, k:k + 1], in1=a_t[:, :W],
                                                               op0=ALU.mult, op1=ALU.add), reads=[pre_b, cw_b, a_b], writes=[a_b])
                if ci < 4:
                    op("act", lambda e: e.activation(out=xs_t[:, ci, :W], in_=a_t[:, :W], func=AF.Silu, bias=cb_t[:, ci:ci + 1], scale=1.0),
                       reads=[a_b, cb_b], writes=[xs_b])
                elif ci == 4:
                    op("act", lambda e: e.activation(out=blkb["BT"][0][:, :W], in_=a_t[:, :W], func=AF.Silu, bias=cb_t[:, ci:ci + 1], scale=1.0),
                       reads=[a_b, cb_b], writes=[blkb["BT"][1]])
                    op("pool", lambda e: e.tensor_copy(out=blkb["BTb"][0][:, :W], in_=blkb["BT"][0][:, :W]), reads=[blkb["BT"][1]], writes=[blkb["BTb"][1]])
                else:
                    op("act", lambda e: e.activation(out=blkb["CTb"][0][:, :W], in_=a_t[:, :W], func=AF.Silu, bias=cb_t[:, ci:ci + 1], scale=1.0),
                       reads=[a_b, cb_b], writes=[blkb["CTb"][1]])

        def tok_proj(c_ap_fn, W, kv, tile_idx, want_dt=False):
            xTb_t, xTb_b = blkb["xTb"]
            pt, po, pb = next_ps()
            for k in range(8):
                op("pe", lambda e: e.matmul(pt[0:W, po:po + 140], lhsT=c_ap_fn(k), rhs=wT_t[:, k, 512:652], start=(k == 0), stop=(k == 7)),
                   reads=[xTb_b, wT_b], writes=[pb])
            g_t, g_b = blkb["gate"]
            v_t, v_b = blkb["vtok"]
            op("dve", lambda e: e.tensor_tensor(out=v_t[0:W, 0:140], in0=pt[0:W, po:po + 140], in1=bT_t[0:W, 512:652], op=ALU.add), reads=[pb, bT_b], writes=[v_b])
            op("act", lambda e: e.activation(out=g_t[0:W, :], in_=v_t[0:W, 0:12], func=AF.Sigmoid), reads=[v_b], writes=[g_b])
            if kv is not None:
                op("pool", lambda e: e.tensor_copy(out=kv["vsl"][0][0:W, tile_idx, 0:64], in_=v_t[0:W, 12:76]), reads=[v_b], writes=[kv["vsl"][1]])
                op("pool", lambda e: e.tensor_copy(out=kv["vwn"][0][0:W, tile_idx % 8, 0:64], in_=v_t[0:W, 76:140]), reads=[v_b], writes=[kv["vwn"][1]])

        es_p = ExitStack()
        P.es = es_p
        ssd["hT"] = P.sb("hT", [128, 512], F32)
        ssd["hTb"] = P.sb("hTb", [128, 512], BF16)
        op("dve", lambda e: e.memset(ssd["hT"][0][:], 0.0), writes=[ssd["hT"][1]])
        op("dve", lambda e: e.memset(ssd["hTb"][0][:], 0.0), writes=[ssd["hTb"][1]])
        for nm, shp, dt_ in (("dt", [128, 512], F32), ("dtA", [128, 512], F32), ("xs", [128, 512], F32), ("Btb", [128, 128], BF16),
                             ("acs", [128, 512], F32), ("w", [128, 512], F32), ("dec", [128, 512], F32), ("xdt", [128, 512], F32),
                             ("xdtb", [128, 512], BF16), ("xdtw", [128, 512], BF16), ("ea", [128, 4, 128], F32), ("cbm", [128, 128], F32),
                             ("L1", [128, 8, 128], F32), ("L2", [128, 8, 128], F32), ("E", [128, 4, 128], F32), ("Mj", [128, 8, 128], BF16),
                             ):
            ssd[nm] = P.sb("ssd_" + nm, shp, dt_)
        ssd["tmp"] = ssd["acs"]
        ssd["y"] = P.sb("ssd_y", [128, 4, 128], F32)

        pkv = alloc_kv("p_", S, 4)
        P.es = es
        for nm in ("kc",):
            op("dve", lambda e: e.memset(pkv[nm][0][0:64, :], 0.0), writes=[pkv[nm][1]])
        op("dve", lambda e: e.memset(pkv["vc"][0][:, :, 0:64], 0.0), writes=[pkv["vc"][1]])
        done_n = 0
        NB = S // BW
        TPB = BW // 128
        for tb in range(NB):
            c0 = tb * BW

            def outs(i, st_t, st_b, c0=c0, tb=tb):
                if i < 2:
                    P.dma("sp", kvT[i * 128:(i + 1) * 128, c0:c0 + BW], st_t[:, :BW], reads=[st_b])
                elif c0 >= S - 512:
                    P.dma("sp", winT[:, c0 - (S - 512):c0 - (S - 512) + BW], st_t[:, :BW], reads=[st_b])
            inproj_block(xT, c0, BW, pkv, c0, outs, wpos=c0)
            if tb == NB - 1:
                P.dma("sp", convT_p.rearrange("(c p) k -> p c k", p=128), blkb["pre"][0][:, :, BW:BW + 3], reads=[blkb["pre"][1]])
            conv_block(BW)
            nmax = min((BW // 16) * (tb + 1) - 1, 511)
            n = done_n
            while n < nmax:
                cch = n // 128
                hi = min(nmax, cch * 128 + 128)
                compress(pkv, cch * 128, hi - cch * 128)
                n = hi
            done_n = nmax
            for tl in range(TPB):
                t = tb * TPB + tl
                cs = slice(tl * 128, (tl + 1) * 128)
                tok_proj(lambda k: blkb["xTb"][0][:, k, cs], 128, pkv, t)
                qa_t, qa_b = pkv["qa"]
                P.dma("pool", qa_t[64:71, :, :], qaug[:, :, t * 128:(t + 1) * 128], writes=[qa_b])
                op("pool", lambda e: e.tensor_copy(out=qa_t[0:64, :, :], in_=blkb["qb"][0][:, :, cs]), reads=[blkb["qb"][1]], writes=[qa_b])
                ssd_chunk(t, blkb, tl * 128)

                def out_fn(y_t, y_b, t=t):
                    n_t, n_b = scr["nyT"]
                    pt, po, pb = next_ps()
                    for k in range(2):
                        op("pe", lambda e: e.transpose(out=pt[:, po + k * 128:po + (k + 1) * 128], in_=y_t[:, k * 128:(k + 1) * 128], identity=id_t[:]),
                           reads=[y_b, id_b], writes=[pb])
                    op("dve", lambda e: e.tensor_copy(out=n_t[:].rearrange("p k q -> p (k q)"), in_=pt[:, po:po + 256]), reads=[pb], writes=[n_b])
                    P.dma("sp", yT[512:768, t * 128:(t + 1) * 128].rearrange("(k p) q -> p k q", p=128), n_t[:], reads=[n_b])
                nsa_qtile(pkv, t, blkb["gate"][0][:, :], blkb["gate"][1], out_fn)
            op("dve", lambda e: e.tensor_copy(out=blkb["pre"][0][:, :, 0:3], in_=blkb["pre"][0][:, :, BW:BW + 3]), reads=[blkb["pre"][1]], writes=[blkb["pre"][1]])
        pt, po, pb = next_ps()
        for k in range(4):
            op("pe", lambda e: e.transpose(out=pt[:, po + k * 128:po + (k + 1) * 128], in_=ssd["hT"][0][:, k * 128:(k + 1) * 128], identity=id_t[:]),
               reads=[ssd["hT"][1], id_b], writes=[pb])
        op("dve", lambda e: e.tensor_copy(out=ssd["tmp"][0][:], in_=pt[:, po:po + 512]), reads=[pb], writes=[ssd["tmp"][1]])
        P.dma("sp", ssm_p.rearrange("(k p) n -> p k n", p=128), ssd["tmp"][0][:].rearrange("p (k n) -> p k n", k=4), reads=[ssd["tmp"][1]])
        if stop == "prompt":
            P.finish()
            raise _Stop(nc)

        P.barrier()
        es_p.close()
        skv = alloc_kv("s_", SS, 2)
        P.dma("pool", skv["kwn"][0][64:71, 512:1024], kaug[:, (NPG - 4) * 128:NPG * 128], writes=[skv["kwn"][1]], key="dl_s_kwn_aug")
        P.dma("pool", skv["kwn"][0][64:71, 0:128], kaug[:, NPG * 128:NPG * 128 + 128], writes=[skv["kwn"][1]], key="dl_s_kwn_aug")
        for nm in ("kvc", "vsl", "vwn", "vc"):
            pass
        for nm, rows in (("kvc", 128), ("ksl", 64), ("kwn", 64), ("kc", 64)):
            op("dve", lambda e: e.memset(skv[nm][0][0:rows, :], 0.0), writes=[skv[nm][1]])
        for nm in ("vsl", "vwn", "vc"):
            op("dve", lambda e: e.memset(skv[nm][0][:, :, 0:64], 0.0), writes=[skv[nm][1]])
        qa_t, qa_b = skv["qa"]
        TQ = NTS - 1
        op("dve", lambda e: e.memset(qa_t[0:64, :, :], 0.0), writes=[qa_b])
        P.dma("pool", qa_t[64:71, :, :], qaug[:, :, TQ * 128:(TQ + 1) * 128], writes=[qa_b])
        newkv = {"kvc": P.sb("n_kvc", [128, NS], BF16), "ksl": P.sb("n_ksl", [64, NS], BF16), "kwn": P.sb("n_kwn", [64, NS], BF16)}
        colp_t, colp_b = const("colp", colp, [128, 3, 4])
        hs_t, hs_b = P.sb("hs", [128, 3, 6, NS], F32)
        for k in range(3):
            P.dma("sp", hs_t[:, k, :, :], convT_s[k].rearrange("(c p) s -> p c s", p=128), writes=[hs_b])

        def outs_s(i, st_t, st_b):
            if i < 2:
                P.dma("sp", kvS[i * 128:(i + 1) * 128, :], st_t[:, :NS], reads=[st_b])
            else:
                P.dma("sp", winnewT[:, :], st_t[:, :NS], reads=[st_b])
        inproj_block(xTs, 0, NS, newkv, 0, outs_s)
        pre_t, pre_b = blkb["pre"]
        P.dma("sp", convS[0].rearrange("(c p) s -> p c s", p=128), hs_t[:, 1, :, :], reads=[hs_b])
        P.dma("sp", convS[1].rearrange("(c p) s -> p c s", p=128), hs_t[:, 2, :, :], reads=[hs_b])
        P.dma("sp", convS[2].rearrange("(c p) s -> p c s", p=128), pre_t[:, :, 3:3 + NS], reads=[pre_b])
        a_t, a_b = blkb["cacc"]
        xs_t, xs_b = blkb["xsT"]
        sBT, sBT_b = blkb["BT"]
        sCT, sCT_b = P.sb("sCT", [128, NS], F32)
        for ci in range(6):
            op("dve", lambda e: e.tensor_scalar(out=a_t[:, :NS], in0=hs_t[:, 0, ci, :], scalar1=cw_t[:, ci, 0:1], scalar2=None, op0=ALU.mult),
               reads=[hs_b, cw_b], writes=[a_b])
            for k in range(1, 4):
                src = hs_t[:, k, ci, :] if k < 3 else pre_t[:, ci, 3:3 + NS]
                op("dve", lambda e: e.scalar_tensor_tensor(out=a_t[:, :NS], in0=src, scalar=cw_t[:, ci, k:k + 1], in1=a_t[:, :NS], op0=ALU.mult, op1=ALU.add),
                   reads=[hs_b, pre_b, cw_b, a_b], writes=[a_b])
            dst, dst_b = (xs_t[:, ci, :NS], xs_b) if ci < 4 else ((sBT[:, :NS], sBT_b) if ci == 4 else (sCT[:, :NS], sCT_b))
            op("act", lambda e: e.activation(out=dst, in_=a_t[:, :NS], func=AF.Silu, bias=cb_t[:, ci:ci + 1], scale=1.0), reads=[a_b, cb_b], writes=[dst_b])
        tok_proj(lambda k: blkb["xTb"][0][:, k, :NS], NS, None, 0)
        gtok_t, gtok_b = P.sb("gtok", [128, 12], F32)
        vtokb_t, vtokb_b = P.sb("vtokb", [128, 128], BF16)
        op("dve", lambda e: e.tensor_copy(out=gtok_t[0:NS, :], in_=blkb["gate"][0][0:NS, :]), reads=[blkb["gate"][1]], writes=[gtok_b])
        op("dve", lambda e: e.tensor_copy(out=vtokb_t[0:NS, :], in_=blkb["vtok"][0][0:NS, 12:140]), reads=[blkb["vtok"][1]], writes=[vtokb_b])
        dtT_t, dtT_b = P.sb("dtT", [128, 4, NS], F32)
        dec_t, dec_b = P.sb("decT", [128, 4, NS], F32)
        xdtT_t, xdtT_b = P.sb("xdtT", [128, 4, NS], F32)
        ysm_t, ysm_b = P.sb("ysm", [128, 4, NS], F32)
        bsum_t, bsum_b = P.sb("bsum", [128, 4], F32)
        Acol_t, Acol_b = P.sb("Acol", [128, 4], F32)
        op("dve", lambda e: e.tensor_tensor(out=bsum_t[:], in0=colp_t[:, 0, :], in1=colp_t[:, 1, :], op=ALU.add), reads=[colp_b], writes=[bsum_b])
        op("act", lambda e: e.activation(out=Acol_t[:], in_=colp_t[:, 2, :], func=AF.Exp), reads=[colp_b], writes=[Acol_b])
        op("dve", lambda e: e.tensor_scalar(out=Acol_t[:], in0=Acol_t[:], scalar1=-1.0, scalar2=None, op0=ALU.mult), reads=[Acol_b], writes=[Acol_b])
        for k4 in range(4):
            pt, po, pb = next_ps()
            for k in range(8):
                op("pe", lambda e: e.matmul(pt[:, po:po + NS], lhsT=wT_t[:, k, k4 * 128:(k4 + 1) * 128], rhs=blkb["xTb"][0][:, k, :NS], start=(k == 0), stop=(k == 7)),
                   reads=[wT_b, blkb["xTb"][1]], writes=[pb])
            op("act", lambda e: e.activation(out=dtT_t[:, k4, :], in_=pt[:, po:po + NS], func=AF.Exp, bias=bsum_t[:, k4:k4 + 1], scale=1.0),
               reads=[pb, bsum_b], writes=[dtT_b])
        op("act", lambda e: e.activation(out=dtT_t[:], in_=dtT_t[:], func=AF.Ln, bias=1.0, scale=1.0), reads=[dtT_b], writes=[dtT_b])
        for k4 in range(4):
            op("act", lambda e: e.activation(out=dec_t[:, k4, :], in_=dtT_t[:, k4, :], func=AF.Exp, scale=Acol_t[:, k4:k4 + 1]), reads=[dtT_b, Acol_b], writes=[dec_b])
        op("dve", lambda e: e.tensor_tensor(out=xdtT_t[:], in0=xs_t[:, :, :NS], in1=dtT_t[:], op=ALU.mult), reads=[xs_b, dtT_b], writes=[xdtT_b])
        hst = [P.sb("hst%d" % i, [128, 4, 128], F32) for i in range(2)]
        dgb, dgb_b = P.sb("dgb", [128, 256], F32)
        st1, st1_b = P.sb("sst1", [128, 4, 128], F32)
        st2, st2_b = P.sb("sst2", [128, 4, 128], F32)
        for s_i in range(NS):
            h_t, h_b = hst[s_i % 2]
            P.dma("sp", h_t[:], ssm_s[s_i].rearrange("(k p) n -> p k n", p=128), writes=[h_b])
            op("dve", lambda e: e.tensor_scalar(out=dgb[:, 0:128], in0=id_t[:], scalar1=sBT[:, s_i:s_i + 1], scalar2=None, op0=ALU.mult),
               reads=[id_b, sBT_b], writes=[dgb_b])
            op("dve", lambda e: e.tensor_scalar(out=dgb[:, 128:256], in0=id_t[:], scalar1=sCT[:, s_i:s_i + 1], scalar2=None, op0=ALU.mult),
               reads=[id_b, sCT_b], writes=[dgb_b])
            pt, po, pb = next_ps()
            op("pe", lambda e: e.matmul(pt[:, po:po + 256], lhsT=one_t[:], rhs=dgb[:], start=True, stop=True), reads=[one_b, dgb_b], writes=[pb])
            op("dve", lambda e: e.tensor_tensor(out=st1[:], in0=h_t[:], in1=dec_t[:, :, s_i:s_i + 1].to_broadcast([128, 4, 128]), op=ALU.mult),
               reads=[h_b, dec_b], writes=[st1_b])
            op("dve", lambda e: e.tensor_tensor(out=st2[:], in0=pt[:, po:po + 128].unsqueeze(1).to_broadcast([128, 4, 128]),
                                                in1=xdtT_t[:, :, s_i:s_i + 1].to_broadcast([128, 4, 128]), op=ALU.mult), reads=[pb, xdtT_b], writes=[st2_b])
            op("dve", lambda e: e.tensor_tensor(out=h_t[:], in0=st1[:], in1=st2[:], op=ALU.add), reads=[st1_b, st2_b], writes=[h_b])
            P.dma("sp", ssm_o[s_i].rearrange("(k p) n -> p k n", p=128), h_t[:], reads=[h_b])
            op("dve", lambda e: e.tensor_tensor(out=st1[:], in0=h_t[:], in1=pt[:, po + 128:po + 256].unsqueeze(1).to_broadcast([128, 4, 128]), op=ALU.mult),
               reads=[h_b, pb], writes=[st1_b])
            op("dve", lambda e: e.tensor_reduce(out=ysm_t[:, :, s_i], in_=st1[:], axis=AX.X, op=ALU.add), reads=[st1_b], writes=[ysm_b])
        for k in range(4):
            op("dve", lambda e: e.scalar_tensor_tensor(out=ysm_t[:, k, :], in0=xs_t[:, k, :NS], scalar=dsk_t[:, k:k + 1], in1=ysm_t[:, k, :], op0=ALU.mult, op1=ALU.add),
               reads=[xs_b, dsk_b, ysm_b], writes=[ysm_b])
        op("dve", lambda e: e.tensor_tensor(out=ysm_t[:], in0=ysm_t[:], in1=blkb["szT"][0][:, :, :NS], op=ALU.mult), reads=[ysm_b, blkb["szT"][1]], writes=[ysm_b])
        pq, oq, bq = next_ps()
        _rms_out(ysm_t, ysm_b, NS, lambda k: ysT[k * 128:(k + 1) * 128, :], pq, oq, bq)
        idx_t, idx_b = P.sb("idx", [128, NS * NPG], I32)
        idxf_t, idxf_b = P.sb("idxf", [128, NS * NPG], F32)
        iot_t, iot_b = P.sb("iot", [128, 1], I32)
        iotf_t, iotf_b = P.sb("iotf", [128, 1], F32)
        P.dma("sp", idx_t[:], pt_rep, writes=[idx_b])
        op("pool", lambda e: e.iota(iot_t[:], pattern=[[0, 1]], base=0, channel_multiplier=1), writes=[iot_b])
        op("dve", lambda e: e.tensor_copy(out=iotf_t[:], in_=iot_t[:]), reads=[iot_b], writes=[iotf_b])
        op("dve", lambda e: e.tensor_copy(out=idxf_t[:], in_=idx_t[:]), reads=[idx_b], writes=[idxf_b])
        op("dve", lambda e: e.tensor_scalar(out=idxf_t[:], in0=idxf_t[:], scalar1=128.0, scalar2=iotf_t[:, 0:1], op0=ALU.mult, op1=ALU.add),
           reads=[idxf_b, iotf_b], writes=[idxf_b])
        op("dve", lambda e: e.tensor_copy(out=idx_t[:], in_=idxf_t[:]), reads=[idxf_b], writes=[idx_b])
        G = [P.sb("G0", [128, NPG, 256], F32)] * 2
        Wt = [P.sb("Wt%d" % i, [128, 4, 128], F32) for i in range(2)]
        gq_t, gq_b = P.sb("gq", [128, 12], F32)
        op("dve", lambda e: e.memset(gq_t[:], 0.0), writes=[gq_b])
        for s_i in range(NS):
            g_t, g_b = G[s_i % 2]
            w_t, w_b = Wt[s_i % 2]
            P._deps("pool", [idx_b], [g_b])
            for j in range(NPG):
                inst = nc.gpsimd.indirect_dma_start(out=g_t[:, j, :], out_offset=None, in_=cache,
                                                    in_offset=bass.IndirectOffsetOnAxis(ap=idx_t[:, s_i * NPG + j:s_i * NPG + j + 1], axis=0))
                key = "dl_" + g_b.name
                P._sem(key)
                P.cnt[key] += 16
                inst.then_inc(P.sems[key], 16)
            g_b.w = ("dl_" + g_b.name, P.cnt["dl_" + g_b.name])
            g_b.r = {}
            idx_b.r["dl_" + g_b.name] = P.cnt["dl_" + g_b.name]
            P.dma("sp", w_t[:], kvwin_s[s_i].rearrange("(c p) f -> p c f", p=128), writes=[w_b])
            P.dma("sp", winS[s_i], kvwin_s[s_i, 1:512, :], key="ds_winS")
            for j0 in range(0, NPG, 4):
                pt, po, pb = next_ps()
                for jj in range(4):
                    op("pe", lambda e: e.transpose(out=pt[:, po + jj * 128:po + (jj + 1) * 128], in_=g_t[:, j0 + jj, 0:128], identity=id_t[:]),
                       reads=[g_b, id_b], writes=[pb])
                op("dve", lambda e: e.tensor_copy(out=skv["kvc"][0][:, j0 * 128:(j0 + 4) * 128], in_=pt[:, po:po + 512]), reads=[pb], writes=[skv["kvc"][1]])
                pt, po, pb = next_ps()
                for jj in range(4):
                    op("pe", lambda e: e.transpose(out=pt[0:64, po + jj * 128:po + (jj + 1) * 128], in_=g_t[:, j0 + jj, 128:192], identity=id_t[:]),
                       reads=[g_b, id_b], writes=[pb])
                op("dve", lambda e: e.tensor_copy(out=skv["ksl"][0][0:64, j0 * 128:(j0 + 4) * 128], in_=pt[0:64, po:po + 512]), reads=[pb], writes=[skv["ksl"][1]])
            op("pool", lambda e: e.tensor_copy(out=skv["vsl"][0][:, 0:NPG, 0:64], in_=g_t[:, :, 192:256]), reads=[g_b], writes=[skv["vsl"][1]])
            pt, po, pb = next_ps()
            for cc in range(4):
                op("pe", lambda e: e.transpose(out=pt[0:64, po + cc * 128:po + (cc + 1) * 128], in_=w_t[:, cc, 0:64], identity=id_t[:]),
                   reads=[w_b, id_b], writes=[pb])
            op("dve", lambda e: e.tensor_copy(out=skv["kwn"][0][0:64, 512:1024], in_=pt[0:64, po:po + 512]), reads=[pb], writes=[skv["kwn"][1]])
            op("pool", lambda e: e.tensor_copy(out=skv["vwn"][0][:, 4:8, 0:64], in_=w_t[:, :, 64:128]), reads=[w_b], writes=[skv["vwn"][1]])
            NP0 = NPG * 128
            op("pool", lambda e: e.tensor_copy(out=skv["ksl"][0][0:64, NP0:NP0 + 1], in_=newkv["ksl"][0][:, s_i:s_i + 1]), reads=[newkv["ksl"][1]], writes=[skv["ksl"][1]])
            op("pool", lambda e: e.tensor_copy(out=skv["kwn"][0][0:64, 0:1], in_=newkv["kwn"][0][:, s_i:s_i + 1]), reads=[newkv["kwn"][1]], writes=[skv["kwn"][1]])
            P.dma("sp", skv["vsl"][0][0:1, NPG, 0:64], vtokb_t[s_i:s_i + 1, 0:64], reads=[vtokb_b], writes=[skv["vsl"][1]], key="dl_svsl")
            P.dma("sp", skv["vwn"][0][0:1, 0, 0:64], vtokb_t[s_i:s_i + 1, 64:128], reads=[vtokb_b], writes=[skv["vwn"][1]], key="dl_svwn")
            P.dma("sp", gq_t[0:1, :], gtok_t[s_i:s_i + 1, :], reads=[gtok_b], writes=[gq_b], key="dl_gq")
            op("pool", lambda e: e.tensor_copy(out=qa_t[0:64, :, 0:1], in_=blkb["qb"][0][:, :, s_i:s_i + 1]), reads=[blkb["qb"][1]], writes=[qa_b])
            compress(skv, 0, 128)

            def out_s(y_t, y_b, s_i=s_i):
                P.dma("sp", ynS[s_i:s_i + 1, :], y_t[0:1, :], reads=[y_b])
            nsa_qtile(skv, TQ, gq_t[:, :], gq_b, out_s)
        P.finish()
    return nc


def _chan(g):
    return np.concatenate([np.arange(g * 512, (g + 1) * 512), 2048 + np.arange(g * 128, (g + 1) * 128), 2560 + np.arange(g * 128, (g + 1) * 128)])


def prep_l1_weights(inp, g, Smax):
    w_in, b_in = inp["w_in"], inp["b_in"]
    colsA = np.concatenate([
        np.arange(g * 512, (g + 1) * 512), 2048 + _chan(g),
        5152 + g * 256 + np.arange(256),
        np.concatenate([6176 + tt * 256 + g * 64 + np.arange(64) for tt in range(6)])])
    wA = np.ascontiguousarray(w_in[:, colsA])
    bfull = b_in[colsA]
    bA = np.zeros((128, 17), np.float32)
    off = 0
    for ct, m in enumerate(COLS_A):
        bA[:m, ct] = bfull[off:off + m]
        off += m
    hd = np.repeat(np.arange(8), 64)
    colsT = np.concatenate([5120 + g * 8 + hd, 7712 + g * 12 + np.arange(12), 6176 + 3 * 256 + g * 64 + np.arange(64), 6176 + 5 * 256 + g * 64 + np.arange(64)])
    wT = np.ascontiguousarray(w_in[:, colsT])
    bT = np.ascontiguousarray(np.broadcast_to(b_in[colsT][None, :], (128, 652)))
    heads = g * 8 + hd
    dtb = np.ascontiguousarray(np.broadcast_to(inp["dt_bias"][heads][None, :], (128, 512)))
    alog = np.ascontiguousarray(np.broadcast_to(inp["a_log"][heads][None, :], (128, 512)))
    ch = _chan(g)
    cw = np.ascontiguousarray(inp["conv_w"][:, ch].reshape(4, 6, 128).transpose(2, 1, 0))
    cb = np.ascontiguousarray(inp["conv_b"][ch].reshape(6, 128).T)
    hp = heads.reshape(4, 128).T
    dsk = np.ascontiguousarray(inp["d_skip"][hp])
    nw = np.ascontiguousarray(inp["ssd_norm_w"][g * 512:(g + 1) * 512].reshape(4, 128).T)
    colp = np.stack([b_in[5120 + hp], inp["dt_bias"][hp], inp["a_log"][hp]], 1).astype(np.float32)
    w1 = inp["cmp_w1"]
    w1kv = np.ascontiguousarray(w1.transpose(0, 2, 1, 3).reshape(128, 32, 128))
    pekv = np.ascontiguousarray(inp["cmp_pe"].transpose(0, 2, 1).reshape(128, 32))
    b1 = np.ascontiguousarray(inp["cmp_b1"].T)
    w2kv = np.ascontiguousarray(np.concatenate([inp["cmp_w2"][0], inp["cmp_w2"][1]], 1))
    b2k = np.ascontiguousarray(inp["cmp_b2"][0][:, None])
    b2v = np.ascontiguousarray(np.broadcast_to(inp["cmp_b2"][1][None, :], (128, 64)))
    t = _nsa_tables(Smax)
    d = dict(wA=wA, bA=bA, wT=wT, bT=bT, dtb=dtb, alog=alog, cw=cw, cb=cb, dsk=dsk, nw=nw, colp=np.ascontiguousarray(colp),
             w1kv=w1kv, pekv=pekv, b1=b1, w2kv=w2kv, b2k=b2k, b2v=b2v,
             kaug=t["kaug"], kaug_c=t["kaug_c"], qaug=np.ascontiguousarray(t["qaug"][g]), caus4=t["caus4"], low4=t["low4"], EW=t["EW"], Zm=t["Zm"],
             F=t["F"], ov=t["ov"], U=t["U"], ones=t["ones"], ident=t["ident"])
    return {k: np.ascontiguousarray(v, dtype=np.float32) for k, v in d.items()}


def prep_l1_data(inp, b, g, S, samples, NPG):
    NS = len(samples)
    ch = _chan(g)
    d = {}
    d["xT"] = np.ascontiguousarray(inp["x_prompt"][b, :S].T)
    d["xTs"] = np.ascontiguousarray(inp["x_sample"][samples, 0].T)
    d["pt_rep"] = np.ascontiguousarray(np.broadcast_to(inp["page_table"][samples].reshape(1, -1), (128, NS * NPG))).astype(np.int32)
    d["kvwin_s"] = np.ascontiguousarray(inp["cache_kv_win"][samples][:, :, :, g, :].reshape(NS, 512, 128))
    d["ssm_s"] = np.ascontiguousarray(inp["state_ssm"][samples, g * 8:(g + 1) * 8].reshape(NS, 512, 128))
    d["convT_s"] = np.ascontiguousarray(inp["state_conv"][samples][:, :, ch].transpose(1, 2, 0))
    return d


def cache_group(inp, g):
    c = inp["cache_kv_paged"]
    return np.ascontiguousarray(c[:, :, :, g, :].reshape(c.shape[0] * 128, 256))


def l2_weights(w):
    bc = np.zeros((128, 6, 1024), np.float32)
    for i, k in enumerate(["b_out", "ln1_g", "ln1_b", "ln2_g", "ln2_b"]):
        bc[:, i, :] = w[k][None, :]
    bc[:, 5, :64] = w["b_router"][None, :]
    return dict(
        w_gm=np.ascontiguousarray(w["w_in"][:, 7760:]), b_gm=np.ascontiguousarray(w["b_in"][7760:].reshape(16, 128).T),
        w_sd=w["w_ssd_down"], w_nd=w["w_nsa_down"], w_out=w["w_out"], bcast=bc, w_r=w["w_router"],
        w_e1=np.concatenate([w["w_e1"], w["w_s1"][None]], 0), w_e3=np.concatenate([w["w_e3"], w["w_s3"][None]], 0),
        w_e2=np.concatenate([w["w_e2"], w["w_s2"][None]], 0), ident=np.eye(128, dtype=np.float32))


def kernel(**inp):
    inp = {k: np.asarray(v) for k, v in inp.items()}
    B, S = inp["x_prompt"].shape[:2]
    DB = inp["x_sample"].shape[0]
    NPG = inp["page_table"].shape[1]
    NS = DB // 2
    SS = NPG * 128 + 128
    nc1 = build_l1(S, NS, NPG, nphys=inp["cache_kv_paged"].shape[0])
    caches = [cache_group(inp, g) for g in range(4)]
    wts = [prep_l1_weights(inp, g, max(S, SS)) for g in range(4)]
    maps = []
    for c in range(8):
        b, g = c // 4, c % 4
        samples = np.arange(b * NS, (b + 1) * NS)
        m = dict(wts[g])
        m.update(prep_l1_data(inp, b, g, S, samples, NPG))
        m["cache"] = caches[g]
        maps.append(m)
    r1 = run_bass_kernel_spmd(nc1, maps, core_ids=list(range(8))).results
    del maps, caches
    f32 = np.float32
    ssd_y = np.zeros((B, S, 2048), f32); nsa_y = np.zeros((B, S, 1024), f32)
    kv_p = np.zeros((B, S, 4, 4, 64), f32); win_p = np.zeros((B, 512, 2, 4, 64), f32)
    ssm_pr = np.zeros((B, 32, 64, 128), f32); conv_p = np.zeros((B, 3, 3072), f32)
    ssd_ys = np.zeros((DB, 2048), f32); nsa_ys = np.zeros((DB, 1024), f32)
    kv_s = np.zeros((DB, 1, 4, 4, 64), f32); win_s = np.zeros((DB, 512, 2, 4, 64), f32)
    ssm_sm = np.zeros((DB, 32, 64, 128), f32); conv_s = np.zeros((DB, 3, 3072), f32)
    for c in range(8):
        b, g = c // 4, c % 4
        r = r1[c]
        sm = slice(b * NS, (b + 1) * NS)
        ch = _chan(g)
        ssd_y[b, :, g * 512:(g + 1) * 512] = r["yT"][0:512].T
        nsa_y[b, :, g * 256:(g + 1) * 256] = r["yT"][512:768].T
        for tt in range(4):
            kv_p[b, :, tt, g, :] = r["kvT"][tt * 64:(tt + 1) * 64].T
            kv_s[sm, 0, tt, g, :] = r["kvS"][tt * 64:(tt + 1) * 64].T
        for t2 in range(2):
            win_p[b, :, t2, g, :] = r["winT"][t2 * 64:(t2 + 1) * 64].T
            win_s[sm, 0:511, t2, g, :] = r["winS"][:, :, t2 * 64:(t2 + 1) * 64]
            win_s[sm, 511, t2, g, :] = r["winnewT"][t2 * 64:(t2 + 1) * 64].T
        ssm_pr[b, g * 8:(g + 1) * 8] = r["ssm_p"].reshape(8, 64, 128)
        conv_p[b][:, ch] = r["convT_p"].T
        ssd_ys[sm, g * 512:(g + 1) * 512] = r["ysT"].T
        nsa_ys[sm, g * 256:(g + 1) * 256] = r["ynS"]
        ssm_sm[sm, g * 8:(g + 1) * 8] = r["ssm_o"].reshape(NS, 8, 64, 128)
        conv_s[sm][:, :, ch] = 0
        cs_ = conv_s[sm]
        cs_[:, :, ch] = r["convS"].transpose(2, 0, 1)
        conv_s[sm] = cs_
    TQ = S // 4
    SQ = DB // 8
    NT_TILES = (TQ + SQ + 127) // 128
    NT = NT_TILES * 128
    nc2 = build_l2(NT_TILES)
    w2 = l2_weights(inp)
    maps = []
    for c in range(8):
        b, k = c // 4, c % 4
        srows = np.arange(b * NS + k * SQ, b * NS + (k + 1) * SQ)
        x = np.zeros((NT, 1024), f32)
        x[:TQ] = inp["x_prompt"][b, k * TQ:(k + 1) * TQ]
        x[TQ:TQ + SQ] = inp["x_sample"][srows, 0]
        ym = np.zeros((NT, 3072), f32)
        ym[:TQ, :2048] = ssd_y[b, k * TQ:(k + 1) * TQ]
        ym[:TQ, 2048:] = nsa_y[b, k * TQ:(k + 1) * TQ]
        ym[TQ:TQ + SQ, :2048] = ssd_ys[srows]
        ym[TQ:TQ + SQ, 2048:] = nsa_ys[srows]
        m = dict(w2)
        m.update(xT=np.ascontiguousarray(x.T), x_tok=x, yT=np.ascontiguousarray(ym.T))
        maps.append(m)
    r2 = run_bass_kernel_spmd(nc2, maps, core_ids=list(range(8))).results
    y_p = np.zeros((B, S, 1024), f32)
    y_s = np.zeros((DB, 1, 1024), f32)
    for c in range(8):
        b, k = c // 4, c % 4
        srows = np.arange(b * NS + k * SQ, b * NS + (k + 1) * SQ)
        y_p[b, k * TQ:(k + 1) * TQ] = r2[c]["y"][:TQ]
        y_s[srows, 0] = r2[c]["y"][TQ:TQ + SQ]
    return (y_p, y_s, kv_p, kv_s, win_p, win_s, ssm_pr, ssm_sm, conv_p, conv_s)
```

```python
from contextlib import ExitStack
import numpy as np
import concourse.bass as bass
import concourse.mybir as mybir
from concourse.bass_utils import run_bass_kernel_spmd

F32 = mybir.dt.float32
BF16 = mybir.dt.bfloat16
I32 = mybir.dt.int32
U32 = mybir.dt.uint32
AF = mybir.ActivationFunctionType
ALU = mybir.AluOpType
AX = mybir.AxisListType

D_MODEL = 1024
ALPHA = 2.0 ** 0.25
NORM_EPS = 1e-5
N_EXPERTS = 64
ROUTE_SCALE = 2.5
BIG = 1.0e9


class Buf:
    __slots__ = ("name", "w", "r")

    def __init__(self, name):
        self.name = name
        self.w = None
        self.r = {}


class Prog:
    def __init__(self, nc, es):
        self.nc = nc
        self.es = es
        self.sem_es = es
        self.eng = {"pe": nc.tensor, "dve": nc.vector, "act": nc.scalar, "pool": nc.gpsimd, "sp": nc.sync}
        self.sems = {}
        self.cnt = {}
        self.seen = {e: {} for e in self.eng}
        self.nbuf = 0
        self.store_keys = set()
        for e in self.eng:
            self._sem(e)

    def _sem(self, key):
        if key not in self.sems:
            self.sems[key] = self.sem_es.enter_context(self.nc.semaphore("s_" + key))
            self.cnt[key] = 0
        return self.sems[key]

    def buf(self, name=None):
        self.nbuf += 1
        return Buf(name or ("b%d" % self.nbuf))

    def sb(self, name, shape, dt):
        t = self.es.enter_context(self.nc.sbuf_tensor("sb_" + name, list(shape), dt))
        return t, Buf(name)

    def ps(self, name, shape, dt=F32):
        t = self.es.enter_context(self.nc.psum_tensor("pp_" + name, list(shape), dt))
        return t, Buf(name)

    def _wait(self, e, key, val):
        if e == "pe" and key == "pe":
            return
        if self.seen[e].get(key, 0) >= val:
            return
        self.eng[e].wait_ge(self.sems[key], val)
        self.seen[e][key] = val

    def _deps(self, e, reads, writes):
        for b in reads:
            if b.w is not None:
                self._wait(e, b.w[0], b.w[1])
        for b in writes:
            if b.w is not None:
                self._wait(e, b.w[0], b.w[1])
            for k, v in b.r.items():
                self._wait(e, k, v)

    def op(self, e, fn, reads=(), writes=()):
        self._deps(e, reads, writes)
        inst = fn(self.eng[e])
        self.cnt[e] += 1
        inst.then_inc(self.sems[e], 1)
        c = self.cnt[e]
        for b in reads:
            b.r[e] = c
        for b in writes:
            b.w = (e, c)
            b.r = {}
        self.seen[e][e] = max(self.seen[e].get(e, 0), 0)

    def dma(self, q, out, in_, reads=(), writes=(), key=None, **kw):
        if key is None:
            key = ("dl_" + writes[0].name) if writes else ("ds_" + reads[0].name)
        self._sem(key)
        if not writes:
            self.store_keys.add(key)
        self._deps(q, reads, writes)
        inst = self.eng[q].dma_start(out=out, in_=in_, **kw)
        self.cnt[key] += 16
        inst.then_inc(self.sems[key], 16)
        c = self.cnt[key]
        for b in reads:
            b.r[key] = c
        for b in writes:
            b.w = (key, c)
            b.r = {}

    def barrier(self):
        for e in self.eng:
            for key, v in self.cnt.items():
                if v > 0 and key != e:
                    self._wait(e, key, v)

    def finish(self):
        for key, v in self.cnt.items():
            if key.startswith("ds_") or key.startswith("dl_"):
                if v > 0:
                    self._wait("sp", key, v)
        for e in ("pe", "dve", "act", "pool"):
            if self.cnt[e] > 0:
                self._wait("sp", e, self.cnt[e])


class _Stop(Exception):
    pass


def build_l2(NT_TILES, n_exp=N_EXPERTS, stop=None):
    try:
        return _build_l2(NT_TILES, n_exp, stop)
    except _Stop as ex:
        return ex.args[0]


def _build_l2(NT_TILES, n_exp, stop):
    NT = NT_TILES * 128
    nc = bass.Bass("TRN2", target_bir_lowering=False)
    dr = lambda name, shape, dt=F32, kind="ExternalInput": nc.dram_tensor(name, list(shape), dt, kind=kind).ap()
    xT = dr("xT", [1024, NT])
    x_tok = dr("x_tok", [NT, 1024])
    yT = dr("yT", [3072, NT])
    w_gm = dr("w_gm", [1024, 2048])
    b_gm = dr("b_gm", [128, 16])
    w_sd = dr("w_sd", [2048, 1024])
    w_nd = dr("w_nd", [1024, 1024])
    w_out = dr("w_out", [1024, 1024])
    bc = dr("bcast", [128, 6, 1024])
    w_r = dr("w_r", [1024, 64])
    w_e1 = dr("w_e1", [n_exp + 1, 1024, 256])
    w_e3 = dr("w_e3", [n_exp + 1, 1024, 256])
    w_e2 = dr("w_e2", [n_exp + 1, 256, 1024])
    ident = dr("ident", [128, 128])
    y = dr("y", [NT, 1024], kind="ExternalOutput")

    blocks = []
    t = 0
    while t < NT_TILES:
        n = min(4, NT_TILES - t)
        blocks.append((t, n))
        t += n

    with ExitStack() as es:
        P = Prog(nc, es)
        mixT, mixT_b = P.sb("mixT", [128, 8, NT], BF16)
        mix_bufs = [P.buf("mixb%d" % i) for i in range(NT_TILES)]
        idf, idf_b = P.sb("idf", [128, 128], F32)
        P.dma("sp", idf[:], ident, writes=[idf_b])
        pst = [P.ps("ps%d" % i, [128, 1024], F32) for i in range(4)]
        psh = []
        for i in range(4):
            psh.append((pst[i][0], 0, P.buf("psb%da" % i)))
            psh.append((pst[i][0], 512, P.buf("psb%db" % i)))
        rr = [0]

        def next_ps():
            r = psh[rr[0] % 8]
            rr[0] += 1
            return r

        with ExitStack() as esA:
            old_es = P.es
            P.es = esA
            wgm, wgm_b = P.sb("wgm", [128, 8, 2048], BF16)
            wsd, wsd_b = P.sb("wsd", [128, 16, 1024], BF16)
            wnd, wnd_b = P.sb("wnd", [128, 8, 1024], BF16)
            bgm, bgm_b = P.sb("bgm", [128, 16], F32)
            for k in range(8):
                P.dma("pool", wgm[:, k, :], w_gm[k * 128:(k + 1) * 128, :], writes=[wgm_b])
            for k in range(16):
                P.dma("pool", wsd[:, k, :], w_sd[k * 128:(k + 1) * 128, :], writes=[wsd_b])
            for k in range(8):
                P.dma("pool", wnd[:, k, :], w_nd[k * 128:(k + 1) * 128, :], writes=[wnd_b])
            P.dma("sp", bgm[:], b_gm, writes=[bgm_b])
            xTb = [P.sb("xTb%d" % i, [128, 8, 512], BF16) for i in range(1)]
            yTb = [P.sb("yTb%d" % i, [128, 24, 512], BF16) for i in range(1)]
            gT = [P.sb("gT%d" % i, [128, 16, 512], F32) for i in range(1)]
            t1 = [P.sb("t1_%d" % i, [128, 512], F32) for i in range(2)]
            t2 = [P.sb("t2_%d" % i, [128, 512], F32) for i in range(2)]
            for bi, (t0, ntl) in enumerate(blocks):
                W = ntl * 128
                c0 = t0 * 128
                xb_t, xb_b = xTb[0]
                yb_t, yb_b = yTb[0]
                for k in range(8):
                    P.dma("pool", xb_t[:, k, :W], xT[k * 128:(k + 1) * 128, c0:c0 + W], writes=[xb_b])
                for k in range(24):
                    P.dma("pool", yb_t[:, k, :W], yT[k * 128:(k + 1) * 128, c0:c0 + W], writes=[yb_b])
                g_t, g_b = gT[0]
                mb = [mix_bufs[t0 + i] for i in range(ntl)]
                for ct in range(16):
                    pt, po, pb = next_ps()
                    for k in range(8):
                        P.op("pe", lambda e: e.matmul(pt[:, po:po + W], lhsT=wgm[:, k, ct * 128:(ct + 1) * 128],
                                                      rhs=xb_t[:, k, :W], start=(k == 0), stop=(k == 7)),
                             reads=[wgm_b, xb_b], writes=[pb])
                    P.op("act", lambda e: e.activation(out=g_t[:, ct, :W], in_=pt[:, po:po + W], func=AF.Sigmoid,
                                                       bias=bgm[:, ct:ct + 1], scale=1.0),
                         reads=[pb, bgm_b], writes=[g_b])
                for ct in range(8):
                    pa_t, pa_o, pa_b = next_ps()
                    for k in range(16):
                        P.op("pe", lambda e: e.matmul(pa_t[:, pa_o:pa_o + W], lhsT=wsd[:, k, ct * 128:(ct + 1) * 128],
                                                      rhs=yb_t[:, k, :W], start=(k == 0), stop=(k == 15)),
                             reads=[wsd_b, yb_b], writes=[pa_b])
                    pn_t, pn_o, pn_b = next_ps()
                    for k in range(8):
                        P.op("pe", lambda e: e.matmul(pn_t[:, pn_o:pn_o + W], lhsT=wnd[:, k, ct * 128:(ct + 1) * 128],
                                                      rhs=yb_t[:, 16 + k, :W], start=(k == 0), stop=(k == 7)),
                             reads=[wnd_b, yb_b], writes=[pn_b])
                    a_t, a_b = t1[ct % 2]
                    b_t, b_b = t2[ct % 2]
                    P.op("dve", lambda e: e.tensor_tensor(out=a_t[:, :W], in0=pa_t[:, pa_o:pa_o + W], in1=g_t[:, ct, :W], op=ALU.mult),
                         reads=[pa_b, g_b], writes=[a_b])
                    P.op("dve", lambda e: e.tensor_tensor(out=b_t[:, :W], in0=pn_t[:, pn_o:pn_o + W], in1=g_t[:, 8 + ct, :W], op=ALU.mult),
                         reads=[pn_b, g_b], writes=[b_b])
                    P.op("pool", lambda e: e.tensor_tensor(out=mixT[:, ct, c0:c0 + W], in0=a_t[:, :W], in1=b_t[:, :W], op=ALU.add),
                         reads=[a_b, b_b], writes=mb)
            P.barrier()
            if stop == "A1":
                P.finish()
                raise _Stop(nc)
            P.es = old_es
        acc = [P.sb("acc%d" % i, [128, 1024], F32) for i in range(NT_TILES)]
        hT, hT_b = P.sb("hT", [128, 8, NT], BF16)
        hT_bufs = [P.buf("hTb%d" % i) for i in range(NT_TILES)]
        gates = [P.sb("gate%d" % i, [128, n_exp + 1], F32) for i in range(NT_TILES)]
        bct, bct_b = P.sb("bct", [128, 6, 1024], F32)
        P.dma("sp", bct[:], bc, writes=[bct_b])
        with ExitStack() as esA:
            old_es = P.es
            P.es = esA
            wo, wo_b = P.sb("wo", [128, 8, 1024], BF16)
            wr, wr_b = P.sb("wr", [128, 8, 64], F32)
            for k in range(8):
                P.dma("pool", wo[:, k, :], w_out[k * 128:(k + 1) * 128, :], writes=[wo_b])
            P.dma("sp", wr[:], w_r.rearrange("(k p) e -> p k e", p=128), writes=[wr_b])
            xt = [P.sb("xt%d" % i, [128, 1024], F32) for i in range(2)]
            hTf = [P.sb("hTf%d" % i, [128, 8, 128], F32) for i in range(2)]
            st6 = [P.sb("st6_%d" % i, [128, 2, 6], F32) for i in range(2)]
            mv = [P.sb("mv%d" % i, [128, 2], F32) for i in range(2)]
            rs = [P.sb("rs%d" % i, [128, 2], F32) for i in range(2)]
            rt = [P.sb("rt%d" % i, [128, 8, 64], F32) for i in range(2)]
            r8 = [P.sb("r8_%d" % i, [128, 8, 8], F32) for i in range(2)]
            for ti in range(NT_TILES):
                s = ti % 2
                x_t, x_b = xt[s]
                P.dma("sp", x_t[:], x_tok[ti * 128:(ti + 1) * 128, :], writes=[x_b])
                halves = []
                for hh in range(2):
                    pt, po, pb = next_ps()
                    for k in range(8):
                        P.op("pe", lambda e: e.matmul(pt[:, po:po + 512], lhsT=mixT[:, k, ti * 128:(ti + 1) * 128],
                                                      rhs=wo[:, k, hh * 512:(hh + 1) * 512], start=(k == 0), stop=(k == 7)),
                             reads=[mix_bufs[ti], wo_b], writes=[pb])
                    halves.append((pt, po, pb))
                for hh in range(2):
                    pt, po, pb = halves[hh]
                    P.op("dve", lambda e: e.scalar_tensor_tensor(out=x_t[:, hh * 512:(hh + 1) * 512], in0=x_t[:, hh * 512:(hh + 1) * 512],
                                                                 scalar=ALPHA, in1=pt[:, po:po + 512], op0=ALU.mult, op1=ALU.add),
                         reads=[x_b, pb], writes=[x_b])
                P.op("pool", lambda e: e.tensor_tensor(out=x_t[:], in0=x_t[:], in1=bct[:, 0, :], op=ALU.add),
                     reads=[x_b, bct_b], writes=[x_b])
                a_t, a_b = acc[ti]
                if stop == "A2pre":
                    P.finish()
                    raise _Stop(nc)
                _layer_norm(P, x_t, x_b, a_t, a_b, bct, bct_b, 1, 2, st6[s], mv[s], rs[s])
                if stop == "A2a":
                    P.finish()
                    raise _Stop(nc)
                f_t, f_b = hTf[s]
                for hh in range(2):
                    pt, po, pb = next_ps()
                    for j in range(4):
                        k = hh * 4 + j
                        P.op("pe", lambda e: e.transpose(out=pt[:, po + j * 128:po + (j + 1) * 128], in_=a_t[:, k * 128:(k + 1) * 128],
                                                         identity=idf[:]),
                             reads=[a_b, idf_b], writes=[pb])
                    P.op("dve", lambda e: e.tensor_copy(out=f_t[:, hh * 4:(hh + 1) * 4, :], in_=pt[:, po:po + 512].rearrange("p (j t) -> p j t", j=4)),
                         reads=[pb], writes=[f_b])
                    P.op("dve", lambda e: e.tensor_copy(out=hT[:, hh * 4:(hh + 1) * 4, ti * 128:(ti + 1) * 128],
                                                        in_=pt[:, po:po + 512].rearrange("p (j t) -> p j t", j=4)),
                         reads=[pb], writes=[hT_bufs[ti]])
                if stop == "A2b":
                    P.finish()
                    raise _Stop(nc)
                P.op("pool", lambda e: e.tensor_scalar(out=a_t[:], in0=a_t[:], scalar1=ALPHA, scalar2=None, op0=ALU.mult),
                     reads=[a_b], writes=[a_b])
                pt, po, pb = next_ps()
                for k in range(8):
                    P.op("pe", lambda e: e.matmul(pt[:, po:po + 64], lhsT=f_t[:, k, :], rhs=wr[:, k, :], start=(k == 0), stop=(k == 7)),
                         reads=[f_b, wr_b], writes=[pb])
                if stop == "A2c":
                    P.finish()
                    raise _Stop(nc)
                _routing(P, pt, po, pb, gates[ti], bct, bct_b, rt[s], r8[s], n_exp)
            P.barrier()
            if stop == "A2":
                P.finish()
                raise _Stop(nc)
            P.es = old_es
        with ExitStack() as esB:
            old_es = P.es
            P.es = esB
            w1 = [P.sb("w1_%d" % i, [128, 8, 256], BF16) for i in range(2)]
            w3 = [P.sb("w3_%d" % i, [128, 8, 256], BF16) for i in range(2)]
            w2 = [P.sb("w2_%d" % i, [128, 2, 1024], BF16) for i in range(2)]
            sil = [P.sb("sil%d" % i, [128, 2, 512], F32) for i in range(2)]
            aT = [P.sb("aT%d" % i, [128, 2, 512], BF16) for i in range(2)]
            cnt = 0
            items = []
            for e_i in range(n_exp + 1):
                for bi, (t0, ntl) in enumerate(blocks):
                    items.append((e_i, t0, ntl, bi == 0))

            def stage1(it, cnt):
                e_i, t0, ntl, first = it
                s = e_i % 2
                w1_t, w1_b = w1[s]
                w3_t, w3_b = w3[s]
                w2_t, w2_b = w2[s]
                if first:
                    P.dma("pool", w1_t[:], w_e1[e_i].rearrange("(k p) f -> p k f", p=128), writes=[w1_b])
                    P.dma("pool", w3_t[:], w_e3[e_i].rearrange("(k p) f -> p k f", p=128), writes=[w3_b])
                    P.dma("pool", w2_t[:], w_e2[e_i].rearrange("(k p) f -> p k f", p=128), writes=[w2_b])
                W = ntl * 128
                c0 = t0 * 128
                hb = [hT_bufs[t0 + i] for i in range(ntl)]
                h1 = [next_ps() for _ in range(2)]
                h3 = [next_ps() for _ in range(2)]
                for f in range(2):
                    pt, po, pb = h1[f]
                    for k in range(8):
                        P.op("pe", lambda e: e.matmul(pt[:, po:po + W], lhsT=w1_t[:, k, f * 128:(f + 1) * 128], rhs=hT[:, k, c0:c0 + W],
                                                      start=(k == 0), stop=(k == 7)), reads=[w1_b] + hb, writes=[pb])
                    pt3, po3, pb3 = h3[f]
                    for k in range(8):
                        P.op("pe", lambda e: e.matmul(pt3[:, po3:po3 + W], lhsT=w3_t[:, k, f * 128:(f + 1) * 128], rhs=hT[:, k, c0:c0 + W],
                                                      start=(k == 0), stop=(k == 7)), reads=[w3_b] + hb, writes=[pb3])
                sl_t, sl_b = sil[cnt % 2]
                at_t, at_b = aT[cnt % 2]
                for f in range(2):
                    pt, po, pb = h1[f]
                    P.op("act", lambda e: e.activation(out=sl_t[:, f, :W], in_=pt[:, po:po + W], func=AF.Silu), reads=[pb], writes=[sl_b])
                for f in range(2):
                    pt3, po3, pb3 = h3[f]
                    P.op("dve", lambda e: e.tensor_tensor(out=at_t[:, f, :W], in0=pt3[:, po3:po3 + W], in1=sl_t[:, f, :W], op=ALU.mult),
                         reads=[pb3, sl_b], writes=[at_b])
                return (at_t, at_b, w2_t, w2_b)

            def stage2(it, st):
                e_i, t0, ntl, first = it
                at_t, at_b, w2_t, w2_b = st
                for tl in range(ntl):
                    ti = t0 + tl
                    a_t, a_b = acc[ti]
                    g_t, g_b = gates[ti]
                    for hh in range(2):
                        pt, po, pb = next_ps()
                        for f in range(2):
                            P.op("pe", lambda e: e.matmul(pt[:, po:po + 512], lhsT=at_t[:, f, tl * 128:(tl + 1) * 128],
                                                          rhs=w2_t[:, f, hh * 512:(hh + 1) * 512], start=(f == 0), stop=(f == 1)),
                                 reads=[at_b, w2_b], writes=[pb])
                        P.op("dve", lambda e: e.scalar_tensor_tensor(out=a_t[:, hh * 512:(hh + 1) * 512], in0=pt[:, po:po + 512],
                                                                     scalar=g_t[:, e_i:e_i + 1], in1=a_t[:, hh * 512:(hh + 1) * 512],
                                                                     op0=ALU.mult, op1=ALU.add), reads=[pb, g_b, a_b], writes=[a_b])
            st = stage1(items[0], 0)
            for i in range(len(items)):
                nxt = stage1(items[i + 1], i + 1) if i + 1 < len(items) else None
                stage2(items[i], st)
                st = nxt
            P.barrier()
            if stop == "B":
                P.finish()
                raise _Stop(nc)
            P.es = old_es
        with ExitStack() as esC:
            old_es = P.es
            P.es = esC
            ot = [P.sb("ot%d" % i, [128, 1024], F32) for i in range(2)]
            st6 = [P.sb("cst6_%d" % i, [128, 2, 6], F32) for i in range(2)]
            mv = [P.sb("cmv%d" % i, [128, 2], F32) for i in range(2)]
            rs = [P.sb("crs%d" % i, [128, 2], F32) for i in range(2)]
            for ti in range(NT_TILES):
                s = ti % 2
                a_t, a_b = acc[ti]
                o_t, o_b = ot[s]
                _layer_norm(P, a_t, a_b, o_t, o_b, bct, bct_b, 3, 4, st6[s], mv[s], rs[s])
                P.dma("sp", y[ti * 128:(ti + 1) * 128, :], o_t[:], reads=[o_b])
            P.finish()
            P.es = old_es
    return nc


def _layer_norm(P, x_t, x_b, o_t, o_b, bct, bct_b, gi, bi, st6, mv, rs):
    s_t, s_b = st6
    m_t, m_b = mv
    r_t, r_b = rs
    for hh in range(2):
        P.op("dve", lambda e, hh=hh: e.bn_stats(out=s_t[:, hh, :], in_=x_t[:, hh * 512:(hh + 1) * 512]), reads=[x_b], writes=[s_b])
    P.op("dve", lambda e: e.bn_aggr(out=m_t[:], in_=s_t[:].rearrange("p a b -> p (a b)")), reads=[s_b], writes=[m_b])
    P.op("dve", lambda e: e.tensor_scalar(out=r_t[:, 0:1], in0=m_t[:, 1:2], scalar1=NORM_EPS, scalar2=None, op0=ALU.add), reads=[m_b], writes=[r_b])
    P.op("act", lambda e: e.activation(out=r_t[:, 1:2], in_=r_t[:, 0:1], func=AF.Sqrt), reads=[r_b], writes=[r_b])
    P.op("dve", lambda e: e.reciprocal(out=r_t[:, 0:1], in_=r_t[:, 1:2]), reads=[r_b], writes=[r_b])
    P.op("dve", lambda e: e.tensor_scalar(out=o_t[:], in0=x_t[:], scalar1=m_t[:, 0:1], scalar2=r_t[:, 0:1], op0=ALU.subtract, op1=ALU.mult),
         reads=[x_b, m_b, r_b], writes=[o_b])
    P.op("pool", lambda e: e.tensor_tensor(out=o_t[:], in0=o_t[:], in1=bct[:, gi, :], op=ALU.mult), reads=[o_b, bct_b], writes=[o_b])
    P.op("pool", lambda e: e.tensor_tensor(out=o_t[:], in0=o_t[:], in1=bct[:, bi, :], op=ALU.add), reads=[o_b, bct_b], writes=[o_b])


def _routing(P, pt, po, pb, gate, bct, bct_b, rt, r8, n_exp):
    g_t, g_b = gate
    t_t, t_b = rt
    q_t, q_b = r8
    sc = t_t[:, 0, :]
    bi = t_t[:, 1, :]
    tmp = t_t[:, 2, :]
    msk = t_t[:, 3, :]
    sel = t_t[:, 4, :]
    v3 = lambda ap: ap.rearrange("p (a b) -> p a b", a=8)
    P.op("act", lambda e: e.activation(out=sc, in_=pt[:, po:po + 64], func=AF.Sigmoid), reads=[pb], writes=[t_b])
    P.op("dve", lambda e: e.tensor_tensor(out=bi, in0=sc, in1=bct[:, 5, 0:64], op=ALU.add), reads=[t_b, bct_b], writes=[t_b])
    P.op("dve", lambda e: e.tensor_reduce(out=q_t[:, 0, :], in_=v3(bi), axis=AX.X, op=ALU.max), reads=[t_b], writes=[q_b])
    P.op("dve", lambda e: e.tensor_tensor(out=v3(tmp), in0=v3(bi), in1=q_t[:, 0, :].unsqueeze(2).to_broadcast([128, 8, 8]), op=ALU.is_equal),
         reads=[t_b, q_b], writes=[t_b])
    P.op("dve", lambda e: e.scalar_tensor_tensor(out=tmp, in0=tmp, scalar=-BIG, in1=bi, op0=ALU.mult, op1=ALU.add), reads=[t_b], writes=[t_b])
    P.op("dve", lambda e: e.tensor_reduce(out=q_t[:, 1, :], in_=v3(tmp), axis=AX.X, op=ALU.max), reads=[t_b], writes=[q_b])
    P.op("dve", lambda e: e.tensor_tensor(out=q_t[:, 2, :], in0=q_t[:, 0, :], in1=q_t[:, 1, :], op=ALU.add), reads=[q_b], writes=[q_b])
    P.op("dve", lambda e: e.max(out=q_t[:, 3, :], in_=q_t[:, 2, :]), reads=[q_b], writes=[q_b])
    P.op("dve", lambda e: e.tensor_scalar(out=q_t[:, 4, :], in0=q_t[:, 2, :], scalar1=q_t[:, 3, 3:4], scalar2=None, op0=ALU.is_ge), reads=[q_b], writes=[q_b])
    P.op("dve", lambda e: e.tensor_scalar(out=q_t[:, 4, :], in0=q_t[:, 4, :], scalar1=1.0, scalar2=BIG, op0=ALU.subtract, op1=ALU.mult), reads=[q_b], writes=[q_b])
    P.op("dve", lambda e: e.tensor_tensor(out=v3(msk), in0=v3(bi), in1=q_t[:, 4, :].unsqueeze(2).to_broadcast([128, 8, 8]), op=ALU.add),
         reads=[t_b, q_b], writes=[t_b])
    P.op("dve", lambda e: e.max(out=q_t[:, 5, :], in_=msk), reads=[t_b], writes=[q_b])
    P.op("dve", lambda e: e.tensor_scalar(out=sel, in0=msk, scalar1=q_t[:, 5, 7:8], scalar2=None, op0=ALU.is_ge), reads=[t_b, q_b], writes=[t_b])
    P.op("dve", lambda e: e.tensor_tensor(out=sel, in0=sel, in1=sc, op=ALU.mult), reads=[t_b], writes=[t_b])
    P.op("dve", lambda e: e.tensor_reduce(out=q_t[:, 6, 0:1], in_=sel, axis=AX.X, op=ALU.add), reads=[t_b], writes=[q_b])
    P.op("dve", lambda e: e.reciprocal(out=q_t[:, 6, 1:2], in_=q_t[:, 6, 0:1]), reads=[q_b], writes=[q_b])
    P.op("dve", lambda e: e.tensor_scalar(out=g_t[:, 0:n_exp], in0=sel, scalar1=q_t[:, 6, 1:2], scalar2=ROUTE_SCALE, op0=ALU.mult, op1=ALU.mult),
         reads=[t_b, q_b], writes=[g_b])
    P.op("dve", lambda e: e.memset(g_t[:, n_exp:n_exp + 1], 1.0), reads=[], writes=[g_b])


NEG = -240000.0
COLS_A = [128] * 10 + [64] * 4 + [128] * 3
OFF_A = [int(v) for v in np.cumsum([0] + COLS_A)]


def _nsa_tables(S):
    h = np.arange(1, 17, dtype=np.float64)
    slopes = 2.0 ** (-8.0 * h / 16.0)
    import ml_dtypes
    bf = lambda a: np.asarray(a, np.float32).astype(ml_dtypes.bfloat16).astype(np.float32)
    t = {}
    pos = np.arange(S)
    ka = np.zeros((7, S), np.float32)
    ka[0] = ka[1] = (pos // 128) * 128
    ka[2] = ka[3] = pos % 128
    ka[4:] = 1.0
    t["kaug"] = ka
    nn = np.arange(512)
    ends = nn * 16 + 31
    kc = np.zeros((7, 512), np.float32)
    kc[0] = kc[1] = (ends // 128) * 128
    kc[2] = kc[3] = ends % 128
    kc[4:] = 1.0
    t["kaug_c"] = kc
    qa = np.zeros((4, 7, 4, S), np.float32)
    for g in range(4):
        for j in range(4):
            sl = np.float32(slopes[g * 4 + j] * 8.0)
            sh = bf(sl)
            slo = bf(sl - sh)
            Q = (-(np.float64(sh) + np.float64(slo)) * pos).astype(np.float32)
            qh = bf(Q)
            ql = bf(Q - qh)
            qll = bf(Q - qh - ql)
            qa[g, 0, j] = sh
            qa[g, 1, j] = slo
            qa[g, 2, j] = sh
            qa[g, 3, j] = slo
            qa[g, 4, j] = qh
            qa[g, 5, j] = ql
            qa[g, 6, j] = qll
    t["qaug"] = qa
    i = np.arange(128)[:, None]
    rq = np.arange(128)[None, :]
    t["caus4"] = np.tile(np.where(i <= rq, 0.0, NEG).astype(np.float32), (1, 4))
    t["low4"] = np.tile(np.where(i > rq, 0.0, NEG).astype(np.float32), (1, 4))
    y = np.arange(S)[None, :]
    t["EW"] = (y // 64 == np.arange(128)[:, None]).astype(np.float32)
    yy = np.arange(2432)[None, :]
    t["Zm"] = np.where(yy >= 16 * i + 128, 0.0, NEG).astype(np.float32)
    yf = np.arange(256)[None, :]
    rel = yf - 126
    c = (np.arange(128)[:, None] >= 64).astype(np.int64)
    F = np.zeros((128, 256), np.float32)
    F[(rel == c) | (rel == c - 1)] = 1e30
    F[rel > c] = -1e30
    t["F"] = F
    n = np.arange(512)[:, None] * 16
    m = np.arange(128)[None, :] * 64
    t["ov"] = ((n < m + 64) & (n + 32 > m)).astype(np.float32).reshape(4, 128, 128).transpose(1, 0, 2).copy()
    t["U"] = (i <= rq).astype(np.float32)
    t["ones"] = np.ones((128, 128), np.float32)
    t["ident"] = np.eye(128, dtype=np.float32)
    return t


def build_l1(S, NS, NPG=16, stop=None, nphys=None):
    try:
        return _build_l1(S, NS, NPG, stop, nphys)
    except _Stop as ex:
        return ex.args[0]


def _build_l1(S, NS, NPG, stop, nphys):
    BW = 256
    nc = bass.Bass("TRN2", target_bir_lowering=False)
    dr = lambda name, shape, dt=F32, kind="ExternalInput": nc.dram_tensor(name, list(shape), dt, kind=kind).ap()
    NTL = S // 128
    SS = NPG * 128 + 128
    NTS = SS // 128
    NPHYS = nphys or (128 * NPG * 5 + 3) // 4
    xT = dr("xT", [1024, S])
    wA = dr("wA", [1024, OFF_A[-1]])
    bA = dr("bA", [128, 17])
    wT = dr("wT", [1024, 652])
    bT = dr("bT", [128, 652])
    dtb = dr("dtb", [128, 512])
    alog = dr("alog", [128, 512])
    cw = dr("cw", [128, 6, 4])
    cb = dr("cb", [128, 6])
    dsk = dr("dsk", [128, 4])
    nw = dr("nw", [128, 4])
    w1kv = dr("w1kv", [128, 32, 128])
    pekv = dr("pekv", [128, 32])
    b1 = dr("b1", [128, 2])
    w2kv = dr("w2kv", [128, 128])
    b2k = dr("b2k", [64, 1])
    b2v = dr("b2v", [128, 64])
    kaug = dr("kaug", [7, max(S, SS)])
    kaug_c = dr("kaug_c", [7, 512])
    qaug = dr("qaug", [7, 4, max(S, SS)])
    caus4 = dr("caus4", [128, 512])
    low4 = dr("low4", [128, 512])
    EW = dr("EW", [128, max(S, SS)])
    Zm = dr("Zm", [128, 2432])
    Ftab = dr("F", [128, 256])
    ov = dr("ov", [128, 4, 128])
    Ut = dr("U", [128, 128])
    ones_d = dr("ones", [128, 128])
    ident = dr("ident", [128, 128])
    xTs = dr("xTs", [1024, NS])
    cache = dr("cache", [NPHYS * 128, 256])
    pt_rep = dr("pt_rep", [128, NS * NPG], I32)
    kvwin_s = dr("kvwin_s", [NS, 512, 128])
    ssm_s = dr("ssm_s", [NS, 512, 128])
    convT_s = dr("convT_s", [3, 768, NS])
    colp = dr("colp", [128, 3, 4])
    yT = dr("yT", [768, S], kind="ExternalOutput")
    kvT = dr("kvT", [256, S], kind="ExternalOutput")
    winT = dr("winT", [128, 512], kind="ExternalOutput")
    ssm_p = dr("ssm_p", [512, 128], kind="ExternalOutput")
    convT_p = dr("convT_p", [768, 3], kind="ExternalOutput")
    ysT = dr("ysT", [512, NS], kind="ExternalOutput")
    ynS = dr("ynS", [NS, 256], kind="ExternalOutput")
    kvS = dr("kvS", [256, NS], kind="ExternalOutput")
    winS = dr("winS", [NS, 511, 128], kind="ExternalOutput")
    ssm_o = dr("ssm_o", [NS, 512, 128], kind="ExternalOutput")
    convS = dr("convS", [3, 768, NS], kind="ExternalOutput")
    winnewT = dr("winnewT", [128, NS], kind="ExternalOutput")

    with ExitStack() as es:
        P = Prog(nc, es)
        op = P.op
        pst = [P.ps("ps%d" % i, [128, 1024], F32) for i in range(4)]
        psh = []
        for i in range(4):
            psh.append((pst[i][0], 0, P.buf("psb%da" % i)))
            psh.append((pst[i][0], 512, P.buf("psb%db" % i)))
        rr = [0]

        def next_ps():
            r = psh[6 + rr[0] % 2]
            rr[0] += 1
            return r

        def const(name, src, shape, dt=F32, q="sp"):
            t_, b_ = P.sb(name, shape, dt)
            P.dma("pool" if dt != F32 else q, t_[:], src, writes=[b_])
            return t_, b_

        U_t, U_b = const("U", Ut, [128, 128])
        one_t, one_b = const("ones", ones_d, [128, 128])
        id_t, id_b = const("identf", ident, [128, 128])
        idb_t, idb_b = const("identb", ident, [128, 128], BF16)
        caus_t, caus_b = const("caus4", caus4, [128, 512], BF16)
        low_t, low_b = const("low4", low4, [128, 512], BF16)
        EW_t, EW_b = const("EW", EW, [128, max(S, SS)], BF16)
        Zm_t, Zm_b = const("Zm", Zm, [128, 2432], BF16)
        F_t, F_b = const("Ft", Ftab, [128, 256])
        wA_t, wA_b = P.sb("wA", [128, 8, OFF_A[-1]], BF16)
        for k in range(8):
            P.dma("pool", wA_t[:, k, :], wA[k * 128:(k + 1) * 128, :], writes=[wA_b])
        wT_t, wT_b = P.sb("wT", [128, 8, 652], BF16)
        for k in range(8):
            P.dma("pool", wT_t[:, k, :], wT[k * 128:(k + 1) * 128, :], writes=[wT_b])
        bA_t, bA_b = const("bA", bA, [128, 17])
        bT_t, bT_b = const("bT", bT, [128, 652])
        dtb_t, dtb_b = const("dtb", dtb, [128, 512])
        A_t, A_b = const("Arep", alog, [128, 512])
        cw_t, cw_b = const("cw", cw, [128, 6, 4])
        cb_t, cb_b = const("cb", cb, [128, 6])
        dsk_t, dsk_b = const("dsk", dsk, [128, 4])
        nw_t, nw_b = const("nw", nw, [128, 4])
        w1_t, w1_b = const("w1kv", w1kv, [128, 32, 128], BF16)
        pe_t, pe_b = const("pekv", pekv, [128, 32])
        b1_t, b1_b = const("b1", b1, [128, 2])
        w2_t, w2_b = const("w2kv", w2kv, [128, 128], BF16)
        b2k_t, b2k_b = const("b2k", b2k, [64, 1])
        b2v_t, b2v_b = const("b2v", b2v, [128, 64])
        op("act", lambda e: e.activation(out=A_t[:], in_=A_t[:], func=AF.Exp), reads=[A_b], writes=[A_b])
        op("dve", lambda e: e.tensor_scalar(out=A_t[:], in0=A_t[:], scalar1=-1.0, scalar2=None, op0=ALU.mult), reads=[A_b], writes=[A_b])
        op("dve", lambda e: e.tensor_tensor(out=dtb_t[:], in0=dtb_t[:], in1=bT_t[:, 0:512], op=ALU.add), reads=[dtb_b, bT_b], writes=[dtb_b])
        b1p_t, b1p_b = P.sb("b1p", [128, 2], F32)
        es_tmp = ExitStack()
        P.es = es_tmp
        w1f_t, w1f_b = const("w1kvf", w1kv, [128, 32, 128])
        P.es = es
        for tkv in range(2):
            pt, po, pb = next_ps()
            for l in range(32):
                op("pe", lambda e: e.matmul(pt[:, po:po + 1], lhsT=w1f_t[tkv * 64:(tkv + 1) * 64, l, :], rhs=pe_t[tkv * 64:(tkv + 1) * 64, l:l + 1],
                                            start=(l == 0), stop=(l == 31)), reads=[w1f_b, pe_b], writes=[pb])
            op("dve", lambda e: e.tensor_tensor(out=b1p_t[:, tkv:tkv + 1], in0=pt[:, po:po + 1], in1=b1_t[:, tkv:tkv + 1], op=ALU.add),
               reads=[pb, b1_b], writes=[b1p_b])
        P.barrier()
        es_tmp.close()

        def alloc_kv(pref, Sx, NCx):
            d = {}
            d["Sx"] = Sx
            d["kvc"] = P.sb(pref + "kvc", [128, Sx], BF16)
            d["ksl"] = P.sb(pref + "ksl", [71, Sx], BF16)
            d["kwn"] = P.sb(pref + "kwn", [71, 1024], BF16)
            d["vsl"] = P.sb(pref + "vsl", [128, Sx // 128, 65], BF16)
            d["vwn"] = P.sb(pref + "vwn", [128, 8, 65], BF16)
            d["kc"] = P.sb(pref + "kc", [71, NCx * 128], BF16)
            d["vc"] = P.sb(pref + "vc", [128, NCx, 193], BF16)
            d["qa"] = P.sb(pref + "qa", [71, 4, 128], BF16)
            d["NCx"] = NCx
            P.dma("pool", d["ksl"][0][64:71, :], kaug[:, 0:Sx], writes=[d["ksl"][1]])
            P.dma("pool", d["kc"][0][64:71, :], kaug_c[:, 0:NCx * 128], writes=[d["kc"][1]])
            P.dma("pool", d["vc"][0][:, :, 65:193], ov[:, 0:NCx, :], writes=[d["vc"][1]])
            op("dve", lambda e: e.memset(d["vsl"][0][:, :, 64:65], 1.0), writes=[d["vsl"][1]])
            op("dve", lambda e: e.memset(d["vwn"][0][:, :, 64:65], 1.0), writes=[d["vwn"][1]])
            op("dve", lambda e: e.memset(d["vc"][0][:, :, 64:65], 1.0), writes=[d["vc"][1]])
            return d

        scr = {}
        scr["hid"] = P.sb("hid", [128, 512], BF16)
        scr["pT"] = [P.sb("pT%d" % i, [128, 512], BF16) for i in range(3)]
        scr["imp"] = P.sb("imp", [128, 4, 128], F32)
        scr["sadd"] = P.sb("sadd", [128, 512], F32)
        scr["m8"] = P.sb("m8", [128, 16], F32)
        scr["nst4"] = P.sb("nst4", [128, 4, 128], BF16)
        scr["rd"] = P.sb("rd", [128, 16], F32)
        scr["osb"] = P.sb("osb", [128, 3, 4, 64], F32)
        scr["ny"] = P.sb("ny", [128, 256], F32)
        scr["nyT"] = P.sb("nyT", [128, 2, 128], F32)
        pcnt = [0]

        def compress(kv, n0, nn):
            kvc_t, kvc_b = kv["kvc"]
            kc_t, kc_b = kv["kc"]
            vc_t, vc_b = kv["vc"]
            h_t, h_b = scr["hid"]
            for tkv in range(2):
                pt, po, pb = next_ps()
                lo = tkv * 64
                for l in range(32):
                    rhs = kvc_t[lo:lo + 64, n0 * 16 + l:n0 * 16 + l + (nn - 1) * 16 + 1:16]
                    op("pe", lambda e: e.matmul(pt[:, po:po + nn], lhsT=w1_t[lo:lo + 64, l, :], rhs=rhs, start=(l == 0), stop=(l == 31)),
                       reads=[w1_b, kvc_b], writes=[pb])
                op("act", lambda e: e.activation(out=h_t[:, :nn], in_=pt[:, po:po + nn], func=AF.Silu, bias=b1p_t[:, tkv:tkv + 1], scale=1.0),
                   reads=[pb, b1p_b], writes=[h_b])
                if tkv == 0:
                    p2, o2, b2 = next_ps()
                    op("pe", lambda e: e.matmul(p2[0:64, o2:o2 + nn], lhsT=w2_t[:, 0:64], rhs=h_t[:, :nn], start=True, stop=True),
                       reads=[w2_b, h_b], writes=[b2])
                    op("act", lambda e: e.activation(out=kc_t[0:64, n0:n0 + nn], in_=p2[0:64, o2:o2 + nn], func=AF.Identity, bias=b2k_t[:, 0:1], scale=1.0),
                       reads=[b2, b2k_b], writes=[kc_b])
                else:
                    for c0 in range(0, nn, 128):
                        w = min(128, nn - c0)
                        assert (n0 + c0) % 128 == 0
                        p2, o2, b2 = next_ps()
                        op("pe", lambda e: e.matmul(p2[0:w, o2:o2 + 64], lhsT=h_t[:, c0:c0 + w], rhs=w2_t[:, 64:128], start=True, stop=True),
                           reads=[w2_b, h_b], writes=[b2])
                        op("dve", lambda e: e.tensor_tensor(out=vc_t[0:w, (n0 + c0) // 128, 0:64], in0=p2[0:w, o2:o2 + 64], in1=b2v_t[0:w, :], op=ALU.add),
                           reads=[b2, b2v_b], writes=[vc_b])

        def attn_branch(kv, t, kname, vname, ktiles, mask_fn, acc, ncols, vcols):
            k_t, k_b = kv[kname]
            v_t, v_b = kv[vname]
            qa_t, qa_b = kv["qa"]
            n = len(ktiles)

            def qk(idx):
                kt = ktiles[idx]
                kk = kt % 8 if kname == "kwn" else kt
                pt, po, pb = next_ps()
                extra = mask_fn(kt)
                mm_extra = [x for x in extra if x[0] == "mm"]
                post = [x for x in extra if x[0] == "mul"]
                pre_add = [x for x in extra if x[0] == "add"]
                op("pe", lambda e: e.matmul(pt[:, po:po + 512], lhsT=k_t[0:71, kk * 128:(kk + 1) * 128], rhs=qa_t[0:71, :, :],
                                            start=True, stop=(len(mm_extra) == 0)), reads=[k_b, qa_b], writes=[pb])
                for i_, (_, l_, r_, rd_) in enumerate(mm_extra):
                    op("pe", lambda e: e.matmul(pt[:, po:po + 512], lhsT=l_, rhs=r_, start=False, stop=(i_ == len(mm_extra) - 1)),
                       reads=rd_, writes=[pb])
                p_t, p_b = scr["pT"][pcnt[0] % 3]
                pcnt[0] += 1
                if pre_add:
                    sc_t, sc_b = scr["sadd"]
                    (_, m_ap, rd_) = pre_add[0]
                    op("dve", lambda e: e.tensor_tensor(out=sc_t[:].rearrange("p (h q) -> p h q", h=4), in0=pt[:, po:po + 512].rearrange("p (h q) -> p h q", h=4),
                                                        in1=m_ap, op=ALU.add), reads=[pb] + rd_, writes=[sc_b])
                    op("act", lambda e: e.activation(out=p_t[:], in_=sc_t[:], func=AF.Exp, scale=0.125), reads=[sc_b], writes=[p_b])
                else:
                    op("act", lambda e: e.activation(out=p_t[:], in_=pt[:, po:po + 512], func=AF.Exp, scale=0.125), reads=[pb], writes=[p_b])
                for (_, m_ap, rd_) in post:
                    op("dve", lambda e: e.tensor_tensor(out=p_t[:].rearrange("p (h q) -> p h q", h=4), in0=p_t[:].rearrange("p (h q) -> p h q", h=4),
                                                        in1=m_ap, op=ALU.mult), reads=[p_b] + rd_, writes=[p_b])
                return (kk, p_t, p_b)

            def pv(idx, st):
                kk, p_t, p_b = st
                for h in range(4):
                    at, ao, ab = acc[h // 2]
                    co = ao + (h % 2) * ncols
                    op("pe", lambda e: e.matmul(at[:, co:co + vcols], lhsT=p_t[:, h * 128:(h + 1) * 128], rhs=v_t[:, kk, 0:vcols],
                                                start=(idx == 0 and h % 2 == 0), stop=(idx == n - 1 and h % 2 == 1)), reads=[p_b, v_b], writes=[ab])
            st = qk(0)
            for idx in range(n):
                nxt = qk(idx + 1) if idx + 1 < n else None
                pv(idx, st)
                st = nxt

        def nsa_qtile(kv, t, gate_ap, gate_b, out_fn):
            i_t, i_b = scr["imp"]
            m_t, m_b = scr["m8"]
            n_t, n_b = scr["nst4"]
            r_t, r_b = scr["rd"]
            o_t, o_b = scr["osb"]
            nvalid = min(8 * t + 7, kv["NCx"] * 128)
            cchunks = list(range((nvalid + 127) // 128))
            accc = [psh[0], psh[1]]

            def cmask(c):
                K = 2048 * c - 128 * t + 31
                if 16 * 127 + K <= 0:
                    return []
                o = 128 - K
                assert 0 <= o <= 2432 - 128
                return [("add", Zm_t[:, o:o + 128].unsqueeze(1).to_broadcast([128, 4, 128]), [Zm_b])]
            if cchunks:
                attn_branch(kv, t, "kc", "vc", cchunks, cmask, accc, 193, 193)
            else:
                for (at, ao, ab) in accc:
                    op("dve", lambda e: e.memset(at[:, ao:ao + 386], 0.0), writes=[ab])
            for hp in range(2):
                at, ao, ab = accc[hp]
                v3 = at[:, ao:ao + 386].rearrange("p (h c) -> p h c", c=193)
                op("dve", lambda e: e.tensor_scalar(out=r_t[:, hp * 2:hp * 2 + 2].unsqueeze(2), in0=v3[:, :, 64:65], scalar1=1e-30, scalar2=None, op0=ALU.max),
                   reads=[ab], writes=[r_b])
                op("dve", lambda e: e.tensor_copy(out=o_t[:, 0, hp * 2:hp * 2 + 2, :], in_=v3[:, :, 0:64]), reads=[ab], writes=[o_b])
            op("dve", lambda e: e.reciprocal(out=r_t[:, 0:4], in_=r_t[:, 0:4]), reads=[r_b], writes=[r_b])
            for h in range(4):
                at, ao, ab = accc[h // 2]
                co = ao + (h % 2) * 193
                if h == 0:
                    op("dve", lambda e: e.tensor_scalar(out=i_t[:, 0, :], in0=at[:, co + 65:co + 193], scalar1=r_t[:, 0:1], scalar2=None, op0=ALU.mult),
                       reads=[ab, r_b], writes=[i_b])
                else:
                    op("dve", lambda e: e.scalar_tensor_tensor(out=i_t[:, 0, :], in0=at[:, co + 65:co + 193], scalar=r_t[:, h:h + 1], in1=i_t[:, 0, :],
                                                               op0=ALU.mult, op1=ALU.add), reads=[ab, r_b, i_b], writes=[i_b])
            accw = [psh[4], psh[5]]

            def wmask(kt):
                if kt == t:
                    return [("mm", idb_t[:], caus_t[:], [idb_b, caus_b])]
                if kt == t - 4:
                    return [("mm", idb_t[:], low_t[:], [idb_b, low_b])]
                return []
            attn_branch(kv, t, "kwn", "vwn", [k_ for k_ in range(t - 4, t + 1) if k_ >= 0], wmask, accw, 65, 65)
            o2 = 126 - 2 * t
            op("dve", lambda e: e.tensor_tensor(out=i_t[:, 0, :], in0=i_t[:, 0, :], in1=F_t[:, o2:o2 + 128], op=ALU.add), reads=[i_b, F_b], writes=[i_b])
            op("dve", lambda e: e.memset(i_t[:, 0, 0:1], 1e30), writes=[i_b])
            op("dve", lambda e: e.max(out=m_t[:, 0:8], in_=i_t[:, 0, :]), reads=[i_b], writes=[m_b])
            op("dve", lambda e: e.match_replace(out=i_t[:, 1, :], in_to_replace=m_t[:, 0:8], in_values=i_t[:, 0, :], imm_value=-3.0e38),
               reads=[i_b, m_b], writes=[i_b])
            op("dve", lambda e: e.max(out=m_t[:, 8:16], in_=i_t[:, 1, :]), reads=[i_b], writes=[m_b])
            op("dve", lambda e: e.tensor_scalar(out=m_t[:, 15:16], in0=m_t[:, 15:16], scalar1=-1e29, scalar2=None, op0=ALU.max), reads=[m_b], writes=[m_b])
            op("dve", lambda e: e.tensor_scalar(out=i_t[:, 2, :], in0=i_t[:, 0, :], scalar1=m_t[:, 15:16], scalar2=None, op0=ALU.is_ge),
               reads=[i_b, m_b], writes=[i_b])
            op("dve", lambda e: e.tensor_scalar(out=i_t[:, 2, :], in0=i_t[:, 2, :], scalar1=1.0, scalar2=-NEG, op0=ALU.subtract, op1=ALU.mult),
               reads=[i_b], writes=[i_b])
            pt, po, pb = next_ps()
            op("pe", lambda e: e.transpose(out=pt[:, po:po + 128], in_=i_t[:, 2, :], identity=id_t[:]), reads=[i_b, id_b], writes=[pb])
            op("dve", lambda e: e.tensor_copy(out=n_t[:], in_=pt[:, po:po + 128].unsqueeze(1).to_broadcast([128, 4, 128])), reads=[pb], writes=[n_b])
            accs = [psh[2], psh[3]]

            def smask(kt):
                r = [("mm", EW_t[:, kt * 128:(kt + 1) * 128], n_t[:].rearrange("p h q -> p (h q)"), [EW_b, n_b])]
                if kt == t:
                    r.append(("mm", idb_t[:], caus_t[:], [idb_b, caus_b]))
                return r
            attn_branch(kv, t, "ksl", "vsl", list(range(t + 1)), smask, accs, 65, 65)
            for bi_, acc_ in ((1, accs), (2, accw)):
                for hp in range(2):
                    at, ao, ab = acc_[hp]
                    v3 = at[:, ao:ao + 130].rearrange("p (h c) -> p h c", c=65)
                    op("dve", lambda e: e.tensor_copy(out=r_t[:, bi_ * 4 + hp * 2:bi_ * 4 + hp * 2 + 2].unsqueeze(2), in_=v3[:, :, 64:65]), reads=[ab], writes=[r_b])
                    op("dve", lambda e: e.tensor_copy(out=o_t[:, bi_, hp * 2:hp * 2 + 2, :], in_=v3[:, :, 0:64]), reads=[ab], writes=[o_b])
            op("dve", lambda e: e.reciprocal(out=r_t[:, 4:12], in_=r_t[:, 4:12]), reads=[r_b], writes=[r_b])
            op("dve", lambda e: e.tensor_tensor(out=r_t[:, 0:12].rearrange("p (b h) -> p b h", b=3), in0=r_t[:, 0:12].rearrange("p (b h) -> p b h", b=3),
                                                in1=gate_ap.rearrange("p (h b) -> p b h", b=3), op=ALU.mult), reads=[r_b, gate_b], writes=[r_b])
            y_t, y_b = scr["ny"]
            op("dve", lambda e: e.tensor_tensor(out=o_t[:].rearrange("p b h d -> p (b h) d"), in0=o_t[:].rearrange("p b h d -> p (b h) d"),
                                                in1=r_t[:, 0:12].unsqueeze(2).to_broadcast([128, 12, 64]), op=ALU.mult), reads=[o_b, r_b], writes=[o_b])
            yv = y_t[:].rearrange("p (h d) -> p h d", h=4)
            op("dve", lambda e: e.tensor_tensor(out=yv, in0=o_t[:, 0, :, :], in1=o_t[:, 1, :, :], op=ALU.add), reads=[o_b], writes=[y_b])
            op("dve", lambda e: e.tensor_tensor(out=yv, in0=yv, in1=o_t[:, 2, :, :], op=ALU.add), reads=[o_b, y_b], writes=[y_b])
            out_fn(y_t, y_b)

        ssd = {}
        for nm, shp, dt_ in (("sq", [128, 4, 128], F32), ("rstd", [128, 128], F32), ("yo", [128, 4, 128], F32)):
            ssd[nm] = P.sb("ssd_" + nm, shp, dt_)
        def ssd_chunk(c, blk, col0):
            cs = slice(col0, col0 + 128)
            xTb_t, xTb_b = blk["xTb"]
            T = lambda nm: ssd[nm][0]
            Bf = lambda nm: ssd[nm][1]
            pt, po, pb = psh[0]
            for k in range(8):
                op("pe", lambda e: e.matmul(pt[:, po:po + 512], lhsT=xTb_t[:, k, cs], rhs=wT_t[:, k, 0:512], start=(k == 0), stop=(k == 7)),
                   reads=[xTb_b, wT_b], writes=[pb])
            op("dve", lambda e: e.tensor_tensor(out=T("dt")[:], in0=pt[:, po:po + 512], in1=dtb_t[:], op=ALU.add), reads=[pb, dtb_b], writes=[Bf("dt")])
            op("act", lambda e: e.activation(out=T("dt")[:], in_=T("dt")[:], func=AF.Exp), reads=[Bf("dt")], writes=[Bf("dt")])
            op("act", lambda e: e.activation(out=T("dt")[:], in_=T("dt")[:], func=AF.Ln, bias=1.0, scale=1.0), reads=[Bf("dt")], writes=[Bf("dt")])
            op("dve", lambda e: e.tensor_tensor(out=T("dtA")[:], in0=T("dt")[:], in1=A_t[:], op=ALU.mult), reads=[Bf("dt"), A_b], writes=[Bf("dtA")])
            p1, o1, b1_ = psh[1]
            xs_t, xs_b = blk["xsT"]
            for k in range(4):
                op("pe", lambda e: e.transpose(out=p1[:, o1 + k * 128:o1 + (k + 1) * 128], in_=xs_t[:, k, cs], identity=id_t[:]),
                   reads=[xs_b, id_b], writes=[b1_])
            op("dve", lambda e: e.tensor_copy(out=T("xs")[:], in_=p1[:, o1:o1 + 512]), reads=[b1_], writes=[Bf("xs")])
            p2, o2, b2_ = psh[2]
            BT_t, BT_b = blk["BT"]
            op("pe", lambda e: e.transpose(out=p2[:, o2:o2 + 128], in_=BT_t[:, cs], identity=id_t[:]), reads=[BT_b, id_b], writes=[b2_])
            op("dve", lambda e: e.tensor_copy(out=T("Btb")[:], in_=p2[:, o2:o2 + 128]), reads=[b2_], writes=[Bf("Btb")])
            p3, o3, b3_ = psh[3]
            op("pe", lambda e: e.matmul(p3[:, o3:o3 + 512], lhsT=U_t[:], rhs=T("dtA")[:], start=True, stop=True), reads=[U_b, Bf("dtA")], writes=[b3_])
            p4, o4, b4_ = psh[4]
            op("pe", lambda e: e.matmul(p4[:, o4:o4 + 512], lhsT=one_t[:], rhs=T("dtA")[:], start=True, stop=True), reads=[one_b, Bf("dtA")], writes=[b4_])
            op("dve", lambda e: e.tensor_copy(out=T("acs")[:], in_=p3[:, o3:o3 + 512]), reads=[b3_], writes=[Bf("acs")])
            op("dve", lambda e: e.tensor_tensor(out=T("w")[:], in0=p4[:, o4:o4 + 512], in1=T("acs")[:], op=ALU.subtract), reads=[b4_, Bf("acs")], writes=[Bf("w")])
            op("act", lambda e: e.activation(out=T("w")[:], in_=T("w")[:], func=AF.Exp), reads=[Bf("w")], writes=[Bf("w")])
            op("act", lambda e: e.activation(out=T("dec")[:], in_=p4[:, o4:o4 + 512], func=AF.Exp), reads=[b4_], writes=[Bf("dec")])
            op("dve", lambda e: e.tensor_tensor(out=T("xdt")[:], in0=T("xs")[:], in1=T("dt")[:], op=ALU.mult), reads=[Bf("xs"), Bf("dt")], writes=[Bf("xdt")])
            op("pool", lambda e: e.tensor_copy(out=T("xdtb")[:], in_=T("xdt")[:]), reads=[Bf("xdt")], writes=[Bf("xdtb")])
            op("dve", lambda e: e.tensor_tensor(out=T("xdtw")[:], in0=T("xdt")[:], in1=T("w")[:], op=ALU.mult), reads=[Bf("xdt"), Bf("w")], writes=[Bf("xdtw")])
            p5, o5, b5_ = psh[5]
            op("pe", lambda e: e.matmul(p5[:, o5:o5 + 512], lhsT=T("Btb")[:], rhs=T("xdtw")[:], start=True, stop=True), reads=[Bf("Btb"), Bf("xdtw")], writes=[b5_])
            p6, o6, b6_ = psh[6]
            CTb_t, CTb_b = blk["CTb"]
            BTb_t, BTb_b = blk["BTb"]
            for k in range(4):
                op("pe", lambda e: e.matmul(p6[:, o6 + k * 128:o6 + (k + 1) * 128], lhsT=T("hTb")[:, k * 128:(k + 1) * 128], rhs=CTb_t[:, cs], start=True, stop=True),
                   reads=[Bf("hTb"), CTb_b], writes=[b6_])
            p7, o7, b7_ = psh[7]
            for k in range(4):
                op("pe", lambda e: e.matmul(p7[:, o7 + k * 128:o7 + (k + 1) * 128], lhsT=T("dtA")[:, k * 128:(k + 1) * 128], rhs=U_t[:], start=True, stop=True),
                   reads=[Bf("dtA"), U_b], writes=[b7_])
            op("act", lambda e: e.activation(out=T("ea")[:].rearrange("p k l -> p (k l)"), in_=p7[:, o7:o7 + 512], func=AF.Exp), reads=[b7_], writes=[Bf("ea")])
            op("pe", lambda e: e.matmul(pt[:, po:po + 128], lhsT=BTb_t[:, cs], rhs=CTb_t[:, cs], start=True, stop=True), reads=[BTb_b, CTb_b], writes=[pb])
            op("dve", lambda e: e.tensor_tensor(out=T("cbm")[:], in0=pt[:, po:po + 128], in1=U_t[:], op=ALU.mult), reads=[pb, U_b], writes=[Bf("cbm")])
            dcol = T("dtA")[:].rearrange("p (j q) -> p j q", q=64)[:, :, 0:1]
            op("dve", lambda e: e.tensor_copy(out=T("L1")[:], in_=dcol.to_broadcast([128, 8, 128])), reads=[Bf("dtA")], writes=[Bf("L1")])
            op("dve", lambda e: e.scalar_tensor_tensor(out=T("L2")[:], in0=T("L1")[:], scalar=-1.0, in1=U_t[:].unsqueeze(1).to_broadcast([128, 8, 128]),
                                                       op0=ALU.mult, op1=ALU.mult), reads=[Bf("L1"), U_b], writes=[Bf("L2")])
            for half, (pd, od, bd) in enumerate((psh[1], psh[2])):
                for jj in range(4):
                    j = half * 4 + jj
                    op("pe", lambda e: e.matmul(pd[:, od + jj * 128:od + (jj + 1) * 128], lhsT=T("L1")[:, j, :], rhs=U_t[:], start=True, stop=False),
                       reads=[Bf("L1"), U_b], writes=[bd])
                    op("pe", lambda e: e.matmul(pd[:, od + jj * 128:od + (jj + 1) * 128], lhsT=T("L2")[:, j, :], rhs=one_t[:], start=False, stop=True),
                       reads=[Bf("L2"), one_b], writes=[bd])
                Ef = T("E")[:].rearrange("p k l -> p (k l)")
                op("dve", lambda e: e.tensor_scalar(out=Ef, in0=pd[:, od:od + 512], scalar1=0.0, scalar2=None, op0=ALU.min), reads=[bd], writes=[Bf("E")])
                op("act", lambda e: e.activation(out=Ef, in_=Ef, func=AF.Exp), reads=[Bf("E")], writes=[Bf("E")])
                op("dve", lambda e: e.tensor_tensor(out=T("Mj")[:, half * 4:(half + 1) * 4, :], in0=T("E")[:], in1=T("cbm")[:].unsqueeze(1).to_broadcast([128, 4, 128]),
                                                    op=ALU.mult), reads=[Bf("E"), Bf("cbm")], writes=[Bf("Mj")])
            for j in range(8):
                k, hf = j // 2, j % 2
                op("pe", lambda e: e.matmul(p3[hf * 64:(hf + 1) * 64, o3 + k * 128:o3 + (k + 1) * 128], lhsT=T("xdtb")[:, j * 64:(j + 1) * 64], rhs=T("Mj")[:, j, :],
                                            start=True, stop=True), reads=[Bf("xdtb"), Bf("Mj")], writes=[b3_])
            yf = T("y")[:].rearrange("p k l -> p (k l)")
            op("dve", lambda e: e.tensor_tensor(out=yf, in0=p6[:, o6:o6 + 512], in1=T("ea")[:].rearrange("p k l -> p (k l)"), op=ALU.mult),
               reads=[b6_, Bf("ea")], writes=[Bf("y")])
            op("dve", lambda e: e.tensor_tensor(out=yf, in0=p3[:, o3:o3 + 512], in1=yf, op=ALU.add), reads=[b3_, Bf("y")], writes=[Bf("y")])
            sz_t, sz_b = blk["szT"]
            sqv = ssd["sq"][0]
            op("dve", lambda e: e.tensor_tensor(out=sqv[:], in0=xs_t[:, :, cs], in1=dsk_t[:, :].unsqueeze(2).to_broadcast([128, 4, 128]), op=ALU.mult),
               reads=[xs_b, dsk_b], writes=[ssd["sq"][1]])
            op("dve", lambda e: e.tensor_tensor(out=T("y")[:], in0=T("y")[:], in1=sqv[:], op=ALU.add), reads=[Bf("y"), ssd["sq"][1]], writes=[Bf("y")])
            op("dve", lambda e: e.tensor_tensor(out=T("y")[:], in0=T("y")[:], in1=sz_t[:, :, cs], op=ALU.mult), reads=[Bf("y"), sz_b], writes=[Bf("y")])
            _rms_out(T("y"), Bf("y"), 128, lambda k: yT[k * 128:(k + 1) * 128, c * 128:(c + 1) * 128], p4, o4, b4_)
            op("dve", lambda e: e.tensor_tensor(out=T("tmp")[:], in0=T("hT")[:], in1=T("dec")[:], op=ALU.mult), reads=[Bf("hT"), Bf("dec")], writes=[Bf("tmp")])
            op("dve", lambda e: e.tensor_tensor(out=T("hT")[:], in0=p5[:, o5:o5 + 512], in1=T("tmp")[:], op=ALU.add), reads=[b5_, Bf("tmp")], writes=[Bf("hT")])
            op("pool", lambda e: e.tensor_copy(out=T("hTb")[:], in_=T("hT")[:]), reads=[Bf("hT")], writes=[Bf("hTb")])

        def _rms_out(y_t, y_b, W, dst_fn, pq, oq, bq):
            sq_t, sq_b = ssd["sq"]
            r_t, r_b = ssd["rstd"]
            o_t, o_b = ssd["yo"]
            op("pool", lambda e: e.tensor_tensor(out=sq_t[:, :, :W], in0=y_t[:, :, :W], in1=y_t[:, :, :W], op=ALU.mult), reads=[y_b], writes=[sq_b])
            for k in range(4):
                op("pe", lambda e: e.matmul(pq[:, oq:oq + W], lhsT=one_t[:], rhs=sq_t[:, k, :W], start=(k == 0), stop=(k == 3)), reads=[one_b, sq_b], writes=[bq])
            op("dve", lambda e: e.tensor_scalar(out=r_t[:, :W], in0=pq[:, oq:oq + W], scalar1=1.0 / 512.0, scalar2=NORM_EPS, op0=ALU.mult, op1=ALU.add),
               reads=[bq], writes=[r_b])
            op("act", lambda e: e.activation(out=r_t[:, :W], in_=r_t[:, :W], func=AF.Ln), reads=[r_b], writes=[r_b])
            op("act", lambda e: e.activation(out=r_t[:, :W], in_=r_t[:, :W], func=AF.Exp, scale=-0.5), reads=[r_b], writes=[r_b])
            op("dve", lambda e: e.tensor_tensor(out=o_t[:, :, :W], in0=y_t[:, :, :W], in1=nw_t[:, :].unsqueeze(2).to_broadcast([128, 4, W]), op=ALU.mult),
               reads=[y_b, nw_b], writes=[o_b])
            op("dve", lambda e: e.tensor_tensor(out=o_t[:, :, :W], in0=o_t[:, :, :W], in1=r_t[:, :W].unsqueeze(1).to_broadcast([128, 4, W]), op=ALU.mult),
               reads=[o_b, r_b], writes=[o_b])
            for k in range(4):
                P.dma("sp", dst_fn(k), o_t[:, k, :W], reads=[o_b])

        blkb = {}
        blkb["xTb"] = P.sb("xTb", [128, 8, BW], BF16)
        blkb["pre"] = P.sb("pre", [128, 6, BW + 3], F32)
        blkb["xsT"] = P.sb("xsT", [128, 4, BW], F32)
        blkb["BT"] = P.sb("BT", [128, BW], F32)
        blkb["BTb"] = P.sb("BTb", [128, BW], BF16)
        blkb["CTb"] = P.sb("CTb", [128, BW], BF16)
        blkb["szT"] = P.sb("szT", [128, 4, BW], F32)
        blkb["qb"] = P.sb("qb", [64, 4, BW], BF16)
        blkb["stg"] = [P.sb("stg0", [128, BW], F32)] * 3
        blkb["cacc"] = P.sb("cacc", [128, BW], F32)
        blkb["gate"] = P.sb("gateq", [128, 12], F32)
        blkb["vtok"] = P.sb("vtok", [128, 192], F32)
        op("dve", lambda e: e.memset(blkb["pre"][0][:, :, 0:3], 0.0), writes=[blkb["pre"][1]])

        def inproj_block(xsrc, c0, W, kv, kvcol0, outs, wpos=None):
            xTb_t, xTb_b = blkb["xTb"]
            for k in range(8):
                P.dma("pool", xTb_t[:, k, :W], xsrc[k * 128:(k + 1) * 128, c0:c0 + W], writes=[xTb_b])
            pre_t, pre_b = blkb["pre"]
            sz_t, sz_b = blkb["szT"]
            q_t, q_b = blkb["qb"]
            for ct in range(17):
                M = COLS_A[ct]
                pt, po, pb = next_ps()
                for k in range(8):
                    op("pe", lambda e: e.matmul(pt[0:M, po:po + W], lhsT=wA_t[:, k, OFF_A[ct]:OFF_A[ct] + M], rhs=xTb_t[:, k, :W], start=(k == 0), stop=(k == 7)),
                       reads=[wA_b, xTb_b], writes=[pb])
                bias = bA_t[0:M, ct:ct + 1]
                if ct < 4:
                    op("act", lambda e: e.activation(out=sz_t[:, ct, :W], in_=pt[:, po:po + W], func=AF.Silu, bias=bias, scale=1.0), reads=[pb, bA_b], writes=[sz_b])
                elif ct < 10:
                    op("act", lambda e: e.activation(out=pre_t[:, ct - 4, 3:3 + W], in_=pt[:, po:po + W], func=AF.Identity, bias=bias, scale=1.0),
                       reads=[pb, bA_b], writes=[pre_b])
                elif ct < 14:
                    op("act", lambda e: e.activation(out=q_t[:, ct - 10, :W], in_=pt[0:64, po:po + W], func=AF.Identity, bias=bias, scale=1.0),
                       reads=[pb, bA_b], writes=[q_b])
                else:
                    st_t, st_b = blkb["stg"][ct - 14]
                    op("act", lambda e: e.activation(out=st_t[:, :W], in_=pt[:, po:po + W], func=AF.Identity, bias=bias, scale=1.0), reads=[pb, bA_b], writes=[st_b])
                    if ct == 14:
                        op("pool", lambda e: e.tensor_copy(out=kv["kvc"][0][:, kvcol0:kvcol0 + W], in_=st_t[:, :W]), reads=[st_b], writes=[kv["kvc"][1]])
                    elif ct == 15:
                        op("pool", lambda e: e.tensor_copy(out=kv["ksl"][0][0:64, kvcol0:kvcol0 + W], in_=st_t[0:64, :W]), reads=[st_b], writes=[kv["ksl"][1]])
                    else:
                        rc = kvcol0 % 1024 if wpos is not None else kvcol0
                        op("pool", lambda e: e.tensor_copy(out=kv["kwn"][0][0:64, rc:rc + W], in_=st_t[0:64, :W]), reads=[st_b], writes=[kv["kwn"][1]])
                        if wpos is not None:
                            P.dma("pool", kv["kwn"][0][64:71, rc:rc + W], kaug[:, wpos:wpos + W], writes=[kv["kwn"][1]])
                    outs(ct - 14, st_t, st_b)

        def conv_block(W, hist_fn=None):
            pre_t, pre_b = blkb["pre"]
            a_t, a_b = blkb["cacc"]
            xs_t, xs_b = blkb["xsT"]
            for ci in range(6):
                op("dve", lambda e: e.tensor_scalar(out=a_t[:, :W], in0=pre_t[:, ci, 0:W], scalar1=cw_t[:, ci, 0:1], scalar2=None, op0=ALU.mult),
                   reads=[pre_b, cw_b], writes=[a_b])
                for k in range(1, 4):
                    op("dve", lambda e: e.scalar_tensor_tensor(out=a_t[:, :W], in0=pre_t[:, ci, k:k + W], scalar=cw_t[:, ci, k:k + 1], in1=a_t[:, :W],
                                                               op0=ALU.mult, op1=ALU.add), reads=[pre_b, cw_b, a_b], writes=[a_b])
                if ci < 4:
                    op("act", lambda e: e.activation(out=xs_t[:, ci, :W], in_=a_t[:, :W], func=AF.Silu, bias=cb_t[:, ci:ci + 1], scale=1.0),
                       reads=[a_b, cb_b], writes=[xs_b])
                elif ci == 4:
                    op("act", lambda e: e.activation(out=blkb["BT"][0][:, :W], in_=a_t[:, :W], func=AF.Silu, bias=cb_t[:, ci:ci + 1], scale=1.0),
                       reads=[a_b, cb_b], writes=[blkb["BT"][1]])
                    op("pool", lambda e: e.tensor_copy(out=blkb["BTb"][0][:, :W], in_=blkb["BT"][0][:, :W]), reads=[blkb["BT"][1]], writes=[blkb["BTb"][1]])
                else:
                    op("act", lambda e: e.activation(out=blkb["CTb"][0][:, :W], in_=a_t[:, :W], func=AF.Silu, bias=cb_t[:, ci:ci + 1], scale=1.0),
                       reads=[a_b, cb_b], writes=[blkb["CTb"][1]])

        def tok_proj(c_ap_fn, W, kv, tile_idx, want_dt=False):
            xTb_t, xTb_b = blkb["xTb"]
            pt, po, pb = next_ps()
            for k in range(8):
                op("pe", lambda e: e.matmul(pt[0:W, po:po + 140], lhsT=c_ap_fn(k), rhs=wT_t[:, k, 512:652], start=(k == 0), stop=(k == 7)),
                   reads=[xTb_b, wT_b], writes=[pb])
            g_t, g_b = blkb["gate"]
            v_t, v_b = blkb["vtok"]
            op("dve", lambda e: e.tensor_tensor(out=v_t[0:W, 0:140], in0=pt[0:W, po:po + 140], in1=bT_t[0:W, 512:652], op=ALU.add), reads=[pb, bT_b], writes=[v_b])
            op("act", lambda e: e.activation(out=g_t[0:W, :], in_=v_t[0:W, 0:12], func=AF.Sigmoid), reads=[v_b], writes=[g_b])
            if kv is not None:
                op("pool", lambda e: e.tensor_copy(out=kv["vsl"][0][0:W, tile_idx, 0:64], in_=v_t[0:W, 12:76]), reads=[v_b], writes=[kv["vsl"][1]])
                op("pool", lambda e: e.tensor_copy(out=kv["vwn"][0][0:W, tile_idx % 8, 0:64], in_=v_t[0:W, 76:140]), reads=[v_b], writes=[kv["vwn"][1]])

        es_p = ExitStack()
        P.es = es_p
        ssd["hT"] = P.sb("hT", [128, 512], F32)
        ssd["hTb"] = P.sb("hTb", [128, 512], BF16)
        op("dve", lambda e: e.memset(ssd["hT"][0][:], 0.0), writes=[ssd["hT"][1]])
        op("dve", lambda e: e.memset(ssd["hTb"][0][:], 0.0), writes=[ssd["hTb"][1]])
        for nm, shp, dt_ in (("dt", [128, 512], F32), ("dtA", [128, 512], F32), ("xs", [128, 512], F32), ("Btb", [128, 128], BF16),
                             ("acs", [128, 512], F32), ("w", [128, 512], F32), ("dec", [128, 512], F32), ("xdt", [128, 512], F32),
                             ("xdtb", [128, 512], BF16), ("xdtw", [128, 512], BF16), ("ea", [128, 4, 128], F32), ("cbm", [128, 128], F32),
                             ("L1", [128, 8, 128], F32), ("L2", [128, 8, 128], F32), ("E", [128, 4, 128], F32), ("Mj", [128, 8, 128], BF16),
                             ):
            ssd[nm] = P.sb("ssd_" + nm, shp, dt_)
        ssd["tmp"] = ssd["acs"]
        ssd["y"] = P.sb("ssd_y", [128, 4, 128], F32)

        pkv = alloc_kv("p_", S, 4)
        P.es = es
        for nm in ("kc",):
            op("dve", lambda e: e.memset(pkv[nm][0][0:64, :], 0.0), writes=[pkv[nm][1]])
        op("dve", lambda e: e.memset(pkv["vc"][0][:, :, 0:64], 0.0), writes=[pkv["vc"][1]])
        done_n = 0
        NB = S // BW
        TPB = BW // 128
        for tb in range(NB):
            c0 = tb * BW

            def outs(i, st_t, st_b, c0=c0, tb=tb):
                if i < 2:
                    P.dma("sp", kvT[i * 128:(i + 1) * 128, c0:c0 + BW], st_t[:, :BW], reads=[st_b])
                elif c0 >= S - 512:
                    P.dma("sp", winT[:, c0 - (S - 512):c0 - (S - 512) + BW], st_t[:, :BW], reads=[st_b])
            inproj_block(xT, c0, BW, pkv, c0, outs, wpos=c0)
            if tb == NB - 1:
                P.dma("sp", convT_p.rearrange("(c p) k -> p c k", p=128), blkb["pre"][0][:, :, BW:BW + 3], reads=[blkb["pre"][1]])
            conv_block(BW)
            nmax = min((BW // 16) * (tb + 1) - 1, 511)
            n = done_n
            while n < nmax:
                cch = n // 128
                hi = min(nmax, cch * 128 + 128)
                compress(pkv, cch * 128, hi - cch * 128)
                n = hi
            done_n = nmax
            for tl in range(TPB):
                t = tb * TPB + tl
                cs = slice(tl * 128, (tl + 1) * 128)
                tok_proj(lambda k: blkb["xTb"][0][:, k, cs], 128, pkv, t)
                qa_t, qa_b = pkv["qa"]
                P.dma("pool", qa_t[64:71, :, :], qaug[:, :, t * 128:(t + 1) * 128], writes=[qa_b])
                op("pool", lambda e: e.tensor_copy(out=qa_t[0:64, :, :], in_=blkb["qb"][0][:, :, cs]), reads=[blkb["qb"][1]], writes=[qa_b])
                ssd_chunk(t, blkb, tl * 128)

                def out_fn(y_t, y_b, t=t):
                    n_t, n_b = scr["nyT"]
                    pt, po, pb = next_ps()
                    for k in range(2):
                        op("pe", lambda e: e.transpose(out=pt[:, po + k * 128:po + (k + 1) * 128], in_=y_t[:, k * 128:(k + 1) * 128], identity=id_t[:]),
                           reads=[y_b, id_b], writes=[pb])
                    op("dve", lambda e: e.tensor_copy(out=n_t[:].rearrange("p k q -> p (k q)"), in_=pt[:, po:po + 256]), reads=[pb], writes=[n_b])
                    P.dma("sp", yT[512:768, t * 128:(t + 1) * 128].rearrange("(k p) q -> p k q", p=128), n_t[:], reads=[n_b])
                nsa_qtile(pkv, t, blkb["gate"][0][:, :], blkb["gate"][1], out_fn)
            op("dve", lambda e: e.tensor_copy(out=blkb["pre"][0][:, :, 0:3], in_=blkb["pre"][0][:, :, BW:BW + 3]), reads=[blkb["pre"][1]], writes=[blkb["pre"][1]])
        pt, po, pb = next_ps()
        for k in range(4):
            op("pe", lambda e: e.transpose(out=pt[:, po + k * 128:po + (k + 1) * 128], in_=ssd["hT"][0][:, k * 128:(k + 1) * 128], identity=id_t[:]),
               reads=[ssd["hT"][1], id_b], writes=[pb])
        op("dve", lambda e: e.tensor_copy(out=ssd["tmp"][0][:], in_=pt[:, po:po + 512]), reads=[pb], writes=[ssd["tmp"][1]])
        P.dma("sp", ssm_p.rearrange("(k p) n -> p k n", p=128), ssd["tmp"][0][:].rearrange("p (k n) -> p k n", k=4), reads=[ssd["tmp"][1]])
        if stop == "prompt":
            P.finish()
            raise _Stop(nc)

        P.barrier()
        es_p.close()
        skv = alloc_kv("s_", SS, 2)
        P.dma("pool", skv["kwn"][0][64:71, 512:1024], kaug[:, (NPG - 4) * 128:NPG * 128], writes=[skv["kwn"][1]], key="dl_s_kwn_aug")
        P.dma("pool", skv["kwn"][0][64:71, 0:128], kaug[:, NPG * 128:NPG * 128 + 128], writes=[skv["kwn"][1]], key="dl_s_kwn_aug")
        for nm in ("kvc", "vsl", "vwn", "vc"):
            pass
        for nm, rows in (("kvc", 128), ("ksl", 64), ("kwn", 64), ("kc", 64)):
            op("dve", lambda e: e.memset(skv[nm][0][0:rows, :], 0.0), writes=[skv[nm][1]])
        for nm in ("vsl", "vwn", "vc"):
            op("dve", lambda e: e.memset(skv[nm][0][:, :, 0:64], 0.0), writes=[skv[nm][1]])
        qa_t, qa_b = skv["qa"]
        TQ = NTS - 1
        op("dve", lambda e: e.memset(qa_t[0:64, :, :], 0.0), writes=[qa_b])
        P.dma("pool", qa_t[64:71, :, :], qaug[:, :, TQ * 128:(TQ + 1) * 128], writes=[qa_b])
        newkv = {"kvc": P.sb("n_kvc", [128, NS], BF16), "ksl": P.sb("n_ksl", [64, NS], BF16), "kwn": P.sb("n_kwn", [64, NS], BF16)}
        colp_t, colp_b = const("colp", colp, [128, 3, 4])
        hs_t, hs_b = P.sb("hs", [128, 3, 6, NS], F32)
        for k in range(3):
            P.dma("sp", hs_t[:, k, :, :], convT_s[k].rearrange("(c p) s -> p c s", p=128), writes=[hs_b])

        def outs_s(i, st_t, st_b):
            if i < 2:
                P.dma("sp", kvS[i * 128:(i + 1) * 128, :], st_t[:, :NS], reads=[st_b])
            else:
                P.dma("sp", winnewT[:, :], st_t[:, :NS], reads=[st_b])
        inproj_block(xTs, 0, NS, newkv, 0, outs_s)
        pre_t, pre_b = blkb["pre"]
        P.dma("sp", convS[0].rearrange("(c p) s -> p c s", p=128), hs_t[:, 1, :, :], reads=[hs_b])
        P.dma("sp", convS[1].rearrange("(c p) s -> p c s", p=128), hs_t[:, 2, :, :], reads=[hs_b])
        P.dma("sp", convS[2].rearrange("(c p) s -> p c s", p=128), pre_t[:, :, 3:3 + NS], reads=[pre_b])
        a_t, a_b = blkb["cacc"]
        xs_t, xs_b = blkb["xsT"]
        sBT, sBT_b = blkb["BT"]
        sCT, sCT_b = P.sb("sCT", [128, NS], F32)
        for ci in range(6):
            op("dve", lambda e: e.tensor_scalar(out=a_t[:, :NS], in0=hs_t[:, 0, ci, :], scalar1=cw_t[:, ci, 0:1], scalar2=None, op0=ALU.mult),
               reads=[hs_b, cw_b], writes=[a_b])
            for k in range(1, 4):
                src = hs_t[:, k, ci, :] if k < 3 else pre_t[:, ci, 3:3 + NS]
                op("dve", lambda e: e.scalar_tensor_tensor(out=a_t[:, :NS], in0=src, scalar=cw_t[:, ci, k:k + 1], in1=a_t[:, :NS], op0=ALU.mult, op1=ALU.add),
                   reads=[hs_b, pre_b, cw_b, a_b], writes=[a_b])
            dst, dst_b = (xs_t[:, ci, :NS], xs_b) if ci < 4 else ((sBT[:, :NS], sBT_b) if ci == 4 else (sCT[:, :NS], sCT_b))
            op("act", lambda e: e.activation(out=dst, in_=a_t[:, :NS], func=AF.Silu, bias=cb_t[:, ci:ci + 1], scale=1.0), reads=[a_b, cb_b], writes=[dst_b])
        tok_proj(lambda k: blkb["xTb"][0][:, k, :NS], NS, None, 0)
        gtok_t, gtok_b = P.sb("gtok", [128, 12], F32)
        vtokb_t, vtokb_b = P.sb("vtokb", [128, 128], BF16)
        op("dve", lambda e: e.tensor_copy(out=gtok_t[0:NS, :], in_=blkb["gate"][0][0:NS, :]), reads=[blkb["gate"][1]], writes=[gtok_b])
        op("dve", lambda e: e.tensor_copy(out=vtokb_t[0:NS, :], in_=blkb["vtok"][0][0:NS, 12:140]), reads=[blkb["vtok"][1]], writes=[vtokb_b])
        dtT_t, dtT_b = P.sb("dtT", [128, 4, NS], F32)
        dec_t, dec_b = P.sb("decT", [128, 4, NS], F32)
        xdtT_t, xdtT_b = P.sb("xdtT", [128, 4, NS], F32)
        ysm_t, ysm_b = P.sb("ysm", [128, 4, NS], F32)
        bsum_t, bsum_b = P.sb("bsum", [128, 4], F32)
        Acol_t, Acol_b = P.sb("Acol", [128, 4], F32)
        op("dve", lambda e: e.tensor_tensor(out=bsum_t[:], in0=colp_t[:, 0, :], in1=colp_t[:, 1, :], op=ALU.add), reads=[colp_b], writes=[bsum_b])
        op("act", lambda e: e.activation(out=Acol_t[:], in_=colp_t[:, 2, :], func=AF.Exp), reads=[colp_b], writes=[Acol_b])
        op("dve", lambda e: e.tensor_scalar(out=Acol_t[:], in0=Acol_t[:], scalar1=-1.0, scalar2=None, op0=ALU.mult), reads=[Acol_b], writes=[Acol_b])
        for k4 in range(4):
            pt, po, pb = next_ps()
            for k in range(8):
                op("pe", lambda e: e.matmul(pt[:, po:po + NS], lhsT=wT_t[:, k, k4 * 128:(k4 + 1) * 128], rhs=blkb["xTb"][0][:, k, :NS], start=(k == 0), stop=(k == 7)),
                   reads=[wT_b, blkb["xTb"][1]], writes=[pb])
            op("act", lambda e: e.activation(out=dtT_t[:, k4, :], in_=pt[:, po:po + NS], func=AF.Exp, bias=bsum_t[:, k4:k4 + 1], scale=1.0),
               reads=[pb, bsum_b], writes=[dtT_b])
        op("act", lambda e: e.activation(out=dtT_t[:], in_=dtT_t[:], func=AF.Ln, bias=1.0, scale=1.0), reads=[dtT_b], writes=[dtT_b])
        for k4 in range(4):
            op("act", lambda e: e.activation(out=dec_t[:, k4, :], in_=dtT_t[:, k4, :], func=AF.Exp, scale=Acol_t[:, k4:k4 + 1]), reads=[dtT_b, Acol_b], writes=[dec_b])
        op("dve", lambda e: e.tensor_tensor(out=xdtT_t[:], in0=xs_t[:, :, :NS], in1=dtT_t[:], op=ALU.mult), reads=[xs_b, dtT_b], writes=[xdtT_b])
        hst = [P.sb("hst%d" % i, [128, 4, 128], F32) for i in range(2)]
        dgb, dgb_b = P.sb("dgb", [128, 256], F32)
        st1, st1_b = P.sb("sst1", [128, 4, 128], F32)
        st2, st2_b = P.sb("sst2", [128, 4, 128], F32)
        for s_i in range(NS):
            h_t, h_b = hst[s_i % 2]
            P.dma("sp", h_t[:], ssm_s[s_i].rearrange("(k p) n -> p k n", p=128), writes=[h_b])
            op("dve", lambda e: e.tensor_scalar(out=dgb[:, 0:128], in0=id_t[:], scalar1=sBT[:, s_i:s_i + 1], scalar2=None, op0=ALU.mult),
               reads=[id_b, sBT_b], writes=[dgb_b])
            op("dve", lambda e: e.tensor_scalar(out=dgb[:, 128:256], in0=id_t[:], scalar1=sCT[:, s_i:s_i + 1], scalar2=None, op0=ALU.mult),
               reads=[id_b, sCT_b], writes=[dgb_b])
            pt, po, pb = next_ps()
            op("pe", lambda e: e.matmul(pt[:, po:po + 256], lhsT=one_t[:], rhs=dgb[:], start=True, stop=True), reads=[one_b, dgb_b], writes=[pb])
            op("dve", lambda e: e.tensor_tensor(out=st1[:], in0=h_t[:], in1=dec_t[:, :, s_i:s_i + 1].to_broadcast([128, 4, 128]), op=ALU.mult),
               reads=[h_b, dec_b], writes=[st1_b])
            op("dve", lambda e: e.tensor_tensor(out=st2[:], in0=pt[:, po:po + 128].unsqueeze(1).to_broadcast([128, 4, 128]),
                                                in1=xdtT_t[:, :, s_i:s_i + 1].to_broadcast([128, 4, 128]), op=ALU.mult), reads=[pb, xdtT_b], writes=[st2_b])
            op("dve", lambda e: e.tensor_tensor(out=h_t[:], in0=st1[:], in1=st2[:], op=ALU.add), reads=[st1_b, st2_b], writes=[h_b])
            P.dma("sp", ssm_o[s_i].rearrange("(k p) n -> p k n", p=128), h_t[:], reads=[h_b])
            op("dve", lambda e: e.tensor_tensor(out=st1[:], in0=h_t[:], in1=pt[:, po + 128:po + 256].unsqueeze(1).to_broadcast([128, 4, 128]), op=ALU.mult),
               reads=[h_b, pb], writes=[st1_b])
            op("dve", lambda e: e.tensor_reduce(out=ysm_t[:, :, s_i], in_=st1[:], axis=AX.X, op=ALU.add), reads=[st1_b], writes=[ysm_b])
        for k in range(4):
            op("dve", lambda e: e.scalar_tensor_tensor(out=ysm_t[:, k, :], in0=xs_t[:, k, :NS], scalar=dsk_t[:, k:k + 1], in1=ysm_t[:, k, :], op0=ALU.mult, op1=ALU.add),
               reads=[xs_b, dsk_b, ysm_b], writes=[ysm_b])
        op("dve", lambda e: e.tensor_tensor(out=ysm_t[:], in0=ysm_t[:], in1=blkb["szT"][0][:, :, :NS], op=ALU.mult), reads=[ysm_b, blkb["szT"][1]], writes=[ysm_b])
        pq, oq, bq = next_ps()
        _rms_out(ysm_t, ysm_b, NS, lambda k: ysT[k * 128:(k + 1) * 128, :], pq, oq, bq)
        idx_t, idx_b = P.sb("idx", [128, NS * NPG], I32)
        idxf_t, idxf_b = P.sb("idxf", [128, NS * NPG], F32)
        iot_t, iot_b = P.sb("iot", [128, 1], I32)
        iotf_t, iotf_b = P.sb("iotf", [128, 1], F32)
        P.dma("sp", idx_t[:], pt_rep, writes=[idx_b])
        op("pool", lambda e: e.iota(iot_t[:], pattern=[[0, 1]], base=0, channel_multiplier=1), writes=[iot_b])
        op("dve", lambda e: e.tensor_copy(out=iotf_t[:], in_=iot_t[:]), reads=[iot_b], writes=[iotf_b])
        op("dve", lambda e: e.tensor_copy(out=idxf_t[:], in_=idx_t[:]), reads=[idx_b], writes=[idxf_b])
        op("dve", lambda e: e.tensor_scalar(out=idxf_t[:], in0=idxf_t[:], scalar1=128.0, scalar2=iotf_t[:, 0:1], op0=ALU.mult, op1=ALU.add),
           reads=[idxf_b, iotf_b], writes=[idxf_b])
        op("dve", lambda e: e.tensor_copy(out=idx_t[:], in_=idxf_t[:]), reads=[idxf_b], writes=[idx_b])
        G = [P.sb("G0", [128, NPG, 256], F32)] * 2
        Wt = [P.sb("Wt%d" % i, [128, 4, 128], F32) for i in range(2)]
        gq_t, gq_b = P.sb("gq", [128, 12], F32)
        op("dve", lambda e: e.memset(gq_t[:], 0.0), writes=[gq_b])
        for s_i in range(NS):
            g_t, g_b = G[s_i % 2]
            w_t, w_b = Wt[s_i % 2]
            P._deps("pool", [idx_b], [g_b])
            for j in range(NPG):
                inst = nc.gpsimd.indirect_dma_start(out=g_t[:, j, :], out_offset=None, in_=cache,
                                                    in_offset=bass.IndirectOffsetOnAxis(ap=idx_t[:, s_i * NPG + j:s_i * NPG + j + 1], axis=0))
                key = "dl_" + g_b.name
                P._sem(key)
                P.cnt[key] += 16
                inst.then_inc(P.sems[key], 16)
            g_b.w = ("dl_" + g_b.name, P.cnt["dl_" + g_b.name])
            g_b.r = {}
            idx_b.r["dl_" + g_b.name] = P.cnt["dl_" + g_b.name]
            P.dma("sp", w_t[:], kvwin_s[s_i].rearrange("(c p) f -> p c f", p=128), writes=[w_b])
            P.dma("sp", winS[s_i], kvwin_s[s_i, 1:512, :], key="ds_winS")
            for j0 in range(0, NPG, 4):
                pt, po, pb = next_ps()
                for jj in range(4):
                    op("pe", lambda e: e.transpose(out=pt[:, po + jj * 128:po + (jj + 1) * 128], in_=g_t[:, j0 + jj, 0:128], identity=id_t[:]),
                       reads=[g_b, id_b], writes=[pb])
                op("dve", lambda e: e.tensor_copy(out=skv["kvc"][0][:, j0 * 128:(j0 + 4) * 128], in_=pt[:, po:po + 512]), reads=[pb], writes=[skv["kvc"][1]])
                pt, po, pb = next_ps()
                for jj in range(4):
                    op("pe", lambda e: e.transpose(out=pt[0:64, po + jj * 128:po + (jj + 1) * 128], in_=g_t[:, j0 + jj, 128:192], identity=id_t[:]),
                       reads=[g_b, id_b], writes=[pb])
                op("dve", lambda e: e.tensor_copy(out=skv["ksl"][0][0:64, j0 * 128:(j0 + 4) * 128], in_=pt[0:64, po:po + 512]), reads=[pb], writes=[skv["ksl"][1]])
            op("pool", lambda e: e.tensor_copy(out=skv["vsl"][0][:, 0:NPG, 0:64], in_=g_t[:, :, 192:256]), reads=[g_b], writes=[skv["vsl"][1]])
            pt, po, pb = next_ps()
            for cc in range(4):
                op("pe", lambda e: e.transpose(out=pt[0:64, po + cc * 128:po + (cc + 1) * 128], in_=w_t[:, cc, 0:64], identity=id_t[:]),
                   reads=[w_b, id_b], writes=[pb])
            op("dve", lambda e: e.tensor_copy(out=skv["kwn"][0][0:64, 512:1024], in_=pt[0:64, po:po + 512]), reads=[pb], writes=[skv["kwn"][1]])
            op("pool", lambda e: e.tensor_copy(out=skv["vwn"][0][:, 4:8, 0:64], in_=w_t[:, :, 64:128]), reads=[w_b], writes=[skv["vwn"][1]])
            NP0 = NPG * 128
            op("pool", lambda e: e.tensor_copy(out=skv["ksl"][0][0:64, NP0:NP0 + 1], in_=newkv["ksl"][0][:, s_i:s_i + 1]), reads=[newkv["ksl"][1]], writes=[skv["ksl"][1]])
            op("pool", lambda e: e.tensor_copy(out=skv["kwn"][0][0:64, 0:1], in_=newkv["kwn"][0][:, s_i:s_i + 1]), reads=[newkv["kwn"][1]], writes=[skv["kwn"][1]])
            P.dma("sp", skv["vsl"][0][0:1, NPG, 0:64], vtokb_t[s_i:s_i + 1, 0:64], reads=[vtokb_b], writes=[skv["vsl"][1]], key="dl_svsl")
            P.dma("sp", skv["vwn"][0][0:1, 0, 0:64], vtokb_t[s_i:s_i + 1, 64:128], reads=[vtokb_b], writes=[skv["vwn"][1]], key="dl_svwn")
            P.dma("sp", gq_t[0:1, :], gtok_t[s_i:s_i + 1, :], reads=[gtok_b], writes=[gq_b], key="dl_gq")
            op("pool", lambda e: e.tensor_copy(out=qa_t[0:64, :, 0:1], in_=blkb["qb"][0][:, :, s_i:s_i + 1]), reads=[blkb["qb"][1]], writes=[qa_b])
            compress(skv, 0, 128)

            def out_s(y_t, y_b, s_i=s_i):
                P.dma("sp", ynS[s_i:s_i + 1, :], y_t[0:1, :], reads=[y_b])
            nsa_qtile(skv, TQ, gq_t[:, :], gq_b, out_s)
        P.finish()
    return nc


def _chan(g):
    return np.concatenate([np.arange(g * 512, (g + 1) * 512), 2048 + np.arange(g * 128, (g + 1) * 128), 2560 + np.arange(g * 128, (g + 1) * 128)])


def prep_l1_weights(inp, g, Smax):
    w_in, b_in = inp["w_in"], inp["b_in"]
    colsA = np.concatenate([
        np.arange(g * 512, (g + 1) * 512), 2048 + _chan(g),
        5152 + g * 256 + np.arange(256),
        np.concatenate([6176 + tt * 256 + g * 64 + np.arange(64) for tt in range(6)])])
    wA = np.ascontiguousarray(w_in[:, colsA])
    bfull = b_in[colsA]
    bA = np.zeros((128, 17), np.float32)
    off = 0
    for ct, m in enumerate(COLS_A):
        bA[:m, ct] = bfull[off:off + m]
        off += m
    hd = np.repeat(np.arange(8), 64)
    colsT = np.concatenate([5120 + g * 8 + hd, 7712 + g * 12 + np.arange(12), 6176 + 3 * 256 + g * 64 + np.arange(64), 6176 + 5 * 256 + g * 64 + np.arange(64)])
    wT = np.ascontiguousarray(w_in[:, colsT])
    bT = np.ascontiguousarray(np.broadcast_to(b_in[colsT][None, :], (128, 652)))
    heads = g * 8 + hd
    dtb = np.ascontiguousarray(np.broadcast_to(inp["dt_bias"][heads][None, :], (128, 512)))
    alog = np.ascontiguousarray(np.broadcast_to(inp["a_log"][heads][None, :], (128, 512)))
    ch = _chan(g)
    cw = np.ascontiguousarray(inp["conv_w"][:, ch].reshape(4, 6, 128).transpose(2, 1, 0))
    cb = np.ascontiguousarray(inp["conv_b"][ch].reshape(6, 128).T)
    hp = heads.reshape(4, 128).T
    dsk = np.ascontiguousarray(inp["d_skip"][hp])
    nw = np.ascontiguousarray(inp["ssd_norm_w"][g * 512:(g + 1) * 512].reshape(4, 128).T)
    colp = np.stack([b_in[5120 + hp], inp["dt_bias"][hp], inp["a_log"][hp]], 1).astype(np.float32)
    w1 = inp["cmp_w1"]
    w1kv = np.ascontiguousarray(w1.transpose(0, 2, 1, 3).reshape(128, 32, 128))
    pekv = np.ascontiguousarray(inp["cmp_pe"].transpose(0, 2, 1).reshape(128, 32))
    b1 = np.ascontiguousarray(inp["cmp_b1"].T)
    w2kv = np.ascontiguousarray(np.concatenate([inp["cmp_w2"][0], inp["cmp_w2"][1]], 1))
    b2k = np.ascontiguousarray(inp["cmp_b2"][0][:, None])
    b2v = np.ascontiguousarray(np.broadcast_to(inp["cmp_b2"][1][None, :], (128, 64)))
    t = _nsa_tables(Smax)
    d = dict(wA=wA, bA=bA, wT=wT, bT=bT, dtb=dtb, alog=alog, cw=cw, cb=cb, dsk=dsk, nw=nw, colp=np.ascontiguousarray(colp),
             w1kv=w1kv, pekv=pekv, b1=b1, w2kv=w2kv, b2k=b2k, b2v=b2v,
             kaug=t["kaug"], kaug_c=t["kaug_c"], qaug=np.ascontiguousarray(t["qaug"][g]), caus4=t["caus4"], low4=t["low4"], EW=t["EW"], Zm=t["Zm"],
             F=t["F"], ov=t["ov"], U=t["U"], ones=t["ones"], ident=t["ident"])
    return {k: np.ascontiguousarray(v, dtype=np.float32) for k, v in d.items()}


def prep_l1_data(inp, b, g, S, samples, NPG):
    NS = len(samples)
    ch = _chan(g)
    d = {}
    d["xT"] = np.ascontiguousarray(inp["x_prompt"][b, :S].T)
    d["xTs"] = np.ascontiguousarray(inp["x_sample"][samples, 0].T)
    d["pt_rep"] = np.ascontiguousarray(np.broadcast_to(inp["page_table"][samples].reshape(1, -1), (128, NS * NPG))).astype(np.int32)
    d["kvwin_s"] = np.ascontiguousarray(inp["cache_kv_win"][samples][:, :, :, g, :].reshape(NS, 512, 128))
    d["ssm_s"] = np.ascontiguousarray(inp["state_ssm"][samples, g * 8:(g + 1) * 8].reshape(NS, 512, 128))
    d["convT_s"] = np.ascontiguousarray(inp["state_conv"][samples][:, :, ch].transpose(1, 2, 0))
    return d


def cache_group(inp, g):
    c = inp["cache_kv_paged"]
    return np.ascontiguousarray(c[:, :, :, g, :].reshape(c.shape[0] * 128, 256))


def l2_weights(w):
    bc = np.zeros((128, 6, 1024), np.float32)
    for i, k in enumerate(["b_out", "ln1_g", "ln1_b", "ln2_g", "ln2_b"]):
        bc[:, i, :] = w[k][None, :]
    bc[:, 5, :64] = w["b_router"][None, :]
    return dict(
        w_gm=np.ascontiguousarray(w["w_in"][:, 7760:]), b_gm=np.ascontiguousarray(w["b_in"][7760:].reshape(16, 128).T),
        w_sd=w["w_ssd_down"], w_nd=w["w_nsa_down"], w_out=w["w_out"], bcast=bc, w_r=w["w_router"],
        w_e1=np.concatenate([w["w_e1"], w["w_s1"][None]], 0), w_e3=np.concatenate([w["w_e3"], w["w_s3"][None]], 0),
        w_e2=np.concatenate([w["w_e2"], w["w_s2"][None]], 0), ident=np.eye(128, dtype=np.float32))


def kernel(**inp):
    inp = {k: np.asarray(v) for k, v in inp.items()}
    B, S = inp["x_prompt"].shape[:2]
    DB = inp["x_sample"].shape[0]
    NPG = inp["page_table"].shape[1]
    NS = DB // 2
    SS = NPG * 128 + 128
    nc1 = build_l1(S, NS, NPG, nphys=inp["cache_kv_paged"].shape[0])
    caches = [cache_group(inp, g) for g in range(4)]
    wts = [prep_l1_weights(inp, g, max(S, SS)) for g in range(4)]
    maps = []
    for c in range(8):
        b, g = c // 4, c % 4
        samples = np.arange(b * NS, (b + 1) * NS)
        m = dict(wts[g])
        m.update(prep_l1_data(inp, b, g, S, samples, NPG))
        m["cache"] = caches[g]
        maps.append(m)
    r1 = run_bass_kernel_spmd(nc1, maps, core_ids=list(range(8))).results
    del maps, caches
    f32 = np.float32
    ssd_y = np.zeros((B, S, 2048), f32); nsa_y = np.zeros((B, S, 1024), f32)
    kv_p = np.zeros((B, S, 4, 4, 64), f32); win_p = np.zeros((B, 512, 2, 4, 64), f32)
    ssm_pr = np.zeros((B, 32, 64, 128), f32); conv_p = np.zeros((B, 3, 3072), f32)
    ssd_ys = np.zeros((DB, 2048), f32); nsa_ys = np.zeros((DB, 1024), f32)
    kv_s = np.zeros((DB, 1, 4, 4, 64), f32); win_s = np.zeros((DB, 512, 2, 4, 64), f32)
    ssm_sm = np.zeros((DB, 32, 64, 128), f32); conv_s = np.zeros((DB, 3, 3072), f32)
    for c in range(8):
        b, g = c // 4, c % 4
        r = r1[c]
        sm = slice(b * NS, (b + 1) * NS)
        ch = _chan(g)
        ssd_y[b, :, g * 512:(g + 1) * 512] = r["yT"][0:512].T
        nsa_y[b, :, g * 256:(g + 1) * 256] = r["yT"][512:768].T
        for tt in range(4):
            kv_p[b, :, tt, g, :] = r["kvT"][tt * 64:(tt + 1) * 64].T
            kv_s[sm, 0, tt, g, :] = r["kvS"][tt * 64:(tt + 1) * 64].T
        for t2 in range(2):
            win_p[b, :, t2, g, :] = r["winT"][t2 * 64:(t2 + 1) * 64].T
            win_s[sm, 0:511, t2, g, :] = r["winS"][:, :, t2 * 64:(t2 + 1) * 64]
            win_s[sm, 511, t2, g, :] = r["winnewT"][t2 * 64:(t2 + 1) * 64].T
        ssm_pr[b, g * 8:(g + 1) * 8] = r["ssm_p"].reshape(8, 64, 128)
        conv_p[b][:, ch] = r["convT_p"].T
        ssd_ys[sm, g * 512:(g + 1) * 512] = r["ysT"].T
        nsa_ys[sm, g * 256:(g + 1) * 256] = r["ynS"]
        ssm_sm[sm, g * 8:(g + 1) * 8] = r["ssm_o"].reshape(NS, 8, 64, 128)
        conv_s[sm][:, :, ch] = 0
        cs_ = conv_s[sm]
        cs_[:, :, ch] = r["convS"].transpose(2, 0, 1)
        conv_s[sm] = cs_
    TQ = S // 4
    SQ = DB // 8
    NT_TILES = (TQ + SQ + 127) // 128
    NT = NT_TILES * 128
    nc2 = build_l2(NT_TILES)
    w2 = l2_weights(inp)
    maps = []
    for c in range(8):
        b, k = c // 4, c % 4
        srows = np.arange(b * NS + k * SQ, b * NS + (k + 1) * SQ)
        x = np.zeros((NT, 1024), f32)
        x[:TQ] = inp["x_prompt"][b, k * TQ:(k + 1) * TQ]
        x[TQ:TQ + SQ] = inp["x_sample"][srows, 0]
        ym = np.zeros((NT, 3072), f32)
        ym[:TQ, :2048] = ssd_y[b, k * TQ:(k + 1) * TQ]
        ym[:TQ, 2048:] = nsa_y[b, k * TQ:(k + 1) * TQ]
        ym[TQ:TQ + SQ, :2048] = ssd_ys[srows]
        ym[TQ:TQ + SQ, 2048:] = nsa_ys[srows]
        m = dict(w2)
        m.update(xT=np.ascontiguousarray(x.T), x_tok=x, yT=np.ascontiguousarray(ym.T))
        maps.append(m)
    r2 = run_bass_kernel_spmd(nc2, maps, core_ids=list(range(8))).results
    y_p = np.zeros((B, S, 1024), f32)
    y_s = np.zeros((DB, 1, 1024), f32)
    for c in range(8):
        b, k = c // 4, c % 4
        srows = np.arange(b * NS + k * SQ, b * NS + (k + 1) * SQ)
        y_p[b, k * TQ:(k + 1) * TQ] = r2[c]["y"][:TQ]
        y_s[srows, 0] = r2[c]["y"][TQ:TQ + SQ]
    return (y_p, y_s, kv_p, kv_s, win_p, win_s, ssm_pr, ssm_sm, conv_p, conv_s)
```
